# Optimizing a Trainium2 kernel written in Bass

```python
import math
import jax
import jax.numpy as jnp
from jax import lax
import numpy as np


D_MODEL = 1024
BATCH = 8
SEQ = 4096
DEPTH = 1

D_MIX = D_MODEL
HEAD_DIM = 64
D_HYENA = D_MIX // 2
D_FOURIER = D_MIX - D_HYENA
N_HYENA_HEADS = D_HYENA // HEAD_DIM
N_FOURIER_GROUPS = D_FOURIER // HEAD_DIM
N_MIX_HEADS = N_HYENA_HEADS + N_FOURIER_GROUPS
D_IN_PROJ = 3 * D_HYENA + D_FOURIER
SHORT_CONV = 3
FILTER_EMB = 33
FILTER_BANDS = (FILTER_EMB - 1) // 2
FILTER_ORDER = 64
N_INNER_MLPS = 2
DECAY_FAST_PCT = 0.3
DECAY_SLOW_PCT = 1.5
DECAY_TARGET = 1e-2
N_GROUPS = 4
EXPERTS_PER_GROUP = 8
N_EXPERTS = N_GROUPS * EXPERTS_PER_GROUP
TOP_K = 2
D_EXPERT = D_MODEL // 2
MOE_BLOCK = 128
EPS = 1e-6

kernel_name = "hyena_fnet_hier_moe_block"


def rmsnorm(x, g):
    xf = x.astype(jnp.float32)
    y = xf * lax.rsqrt(jnp.mean(xf * xf, axis=-1, keepdims=True) + EPS)
    return (y * g.astype(jnp.float32)).astype(x.dtype)


def short_conv_centred(u, w, b):
    L = u.shape[1]
    half = SHORT_CONV // 2
    up = jnp.pad(u, ((0, 0), (half, half), (0, 0)))
    y = b
    for k in range(SHORT_CONV):
        y = y + up[:, k:k + L] * w[k]
    return y


def hyena_filters(L, f_w_in, f_b_in, f_w_mid, f_b_mid, f_freq, f_w_out):
    t = jnp.linspace(0.0, 1.0, L, dtype=jnp.float32)[:, None]
    w = (2.0 * math.pi / L) * jnp.arange(L, dtype=jnp.float32)[:, None]
    f = jnp.linspace(1e-4, FILTER_BANDS - 1, FILTER_BANDS, dtype=jnp.float32)[None, :]
    z = jnp.concatenate([t, jnp.cos(f * w), -jnp.sin(f * w)], axis=-1)
    h = jnp.sin(f_freq[0] * (z @ f_w_in + f_b_in))
    for i in range(N_INNER_MLPS):
        h = jnp.sin(f_freq[i + 1] * (h @ f_w_mid[i] + f_b_mid[i]))
    h = (h @ f_w_out).astype(jnp.float32).reshape(L, 2, D_HYENA)
    max_decay = math.log(DECAY_TARGET) / DECAY_FAST_PCT
    min_decay = math.log(DECAY_TARGET) / DECAY_SLOW_PCT
    deltas = jnp.abs(jnp.linspace(min_decay, max_decay, D_HYENA, dtype=jnp.float32))
    decay = jnp.exp(-t * deltas)
    h = h * decay[:, None, :]
    return h[:, 0], h[:, 1]


def bidirectional_long_conv(u, h_fwd, h_bwd):
    L = u.shape[1]
    k = jnp.concatenate([h_fwd, jnp.zeros_like(h_fwd[:1]), h_bwd[:0:-1]], axis=0)
    k = k / jnp.sum(jnp.abs(k), axis=0, keepdims=True)
    k_f = jnp.fft.rfft(k, n=2 * L, axis=0)
    u_f = jnp.fft.rfft(u, n=2 * L, axis=1)
    return jnp.fft.irfft(u_f * k_f[None], n=2 * L, axis=1)[:, :L]


def hyena_mixer(u, conv_w, conv_b, f_w_in, f_b_in, f_w_mid, f_b_mid, f_freq, f_w_out, f_bias):
    uc = short_conv_centred(u, conv_w, conv_b).astype(jnp.float32)
    x0, x1, v = jnp.split(uc, 3, axis=-1)
    h_fwd, h_bwd = hyena_filters(u.shape[1], f_w_in, f_b_in, f_w_mid, f_b_mid, f_freq, f_w_out)
    z = x1 * v
    z = bidirectional_long_conv(z, h_fwd, h_bwd) + z * f_bias.astype(jnp.float32)
    return x0 * z


def fourier_mixer(u):
    B, L, _ = u.shape
    ug = u.astype(jnp.float32).reshape(B, L, N_FOURIER_GROUPS, HEAD_DIM)
    y = jnp.fft.fftn(ug, axes=(1, 3), norm='ortho').real
    return y.reshape(B, L, D_FOURIER)


def head_rmsnorm(y, g):
    B, L, _ = y.shape
    yh = y.reshape(B, L, N_MIX_HEADS, HEAD_DIM)
    yh = yh * lax.rsqrt(jnp.mean(yh * yh, axis=-1, keepdims=True) + EPS)
    return yh.reshape(B, L, D_MIX) * g.astype(jnp.float32)


def mixer_sublayer(x, norm_g, w_in, conv_w, conv_b, f_w_in, f_b_in, f_w_mid, f_b_mid, f_freq, f_w_out,
                   f_bias, mix_g, w_out):
    h = rmsnorm(x, norm_g)
    proj = h @ w_in
    y_hyena = hyena_mixer(proj[..., :3 * D_HYENA], conv_w, conv_b, f_w_in, f_b_in, f_w_mid, f_b_mid,
                          f_freq, f_w_out, f_bias)
    y_fourier = fourier_mixer(proj[..., 3 * D_HYENA:])
    y = head_rmsnorm(jnp.concatenate([y_hyena, y_fourier], axis=-1), mix_g).astype(x.dtype)
    return x + y @ w_out


def hierarchical_moe(h, w_group, b_group, w_router, b_router, w_gate, w_up, w_down):
    B, L, D = h.shape
    T = B * L
    tok = h.reshape(T, D)
    g_logits = (tok @ w_group).astype(jnp.float32) + b_group.astype(jnp.float32)
    g_sel = jnp.argmax(g_logits, axis=-1)
    p_group = jnp.take_along_axis(jax.nn.softmax(g_logits, axis=-1), g_sel[:, None], axis=-1)[:, 0]
    e_logits = jnp.einsum('td,gde->tge', tok, w_router).astype(jnp.float32) + b_router.astype(jnp.float32)
    e_logits = jnp.take_along_axis(e_logits, g_sel[:, None, None], axis=1)[:, 0]
    top_l, top_i = lax.top_k(e_logits, TOP_K)
    weight = p_group[:, None] * jax.nn.softmax(top_l, axis=-1)
    expert = (g_sel[:, None] * EXPERTS_PER_GROUP + top_i).astype(jnp.int32)

    P = T * TOP_K
    flat_e = expert.reshape(P)
    flat_t = jnp.repeat(jnp.arange(T, dtype=jnp.int32), TOP_K)
    flat_w = weight.reshape(P)
    order = jnp.argsort(flat_e)
    se = flat_e[order]
    counts = jnp.bincount(flat_e, length=N_EXPERTS).astype(jnp.int32)
    starts = jnp.cumsum(counts) - counts
    padded = ((counts + MOE_BLOCK - 1) // MOE_BLOCK) * MOE_BLOCK
    pad_ends = jnp.cumsum(padded)
    pad_starts = pad_ends - padded
    dest = pad_starts[se] + (jnp.arange(P, dtype=jnp.int32) - starts[se])
    n_blocks = -(-P // MOE_BLOCK) + N_EXPERTS
    n_rows = n_blocks * MOE_BLOCK
    row_tok = jnp.zeros((n_rows,), jnp.int32).at[dest].set(flat_t[order])
    row_w = jnp.zeros((n_rows,), jnp.float32).at[dest].set(flat_w[order])
    block_start = jnp.arange(n_blocks, dtype=jnp.int32) * MOE_BLOCK
    block_e = jnp.minimum(jnp.searchsorted(pad_ends, block_start, side='right'), N_EXPERTS - 1)

    def expert_block(args):
        e, rows_tok, rows_w = args
        xb = tok[rows_tok]
        a = xb @ w_gate[e]
        u = xb @ w_up[e]
        y = (jax.nn.silu(a) * u) @ w_down[e]
        return (y * rows_w[:, None].astype(y.dtype)).astype(tok.dtype)

    ys = lax.map(expert_block, (block_e, row_tok.reshape(n_blocks, MOE_BLOCK), row_w.reshape(n_blocks, MOE_BLOCK)))
    out = jnp.zeros((T, D), tok.dtype).at[row_tok].add(ys.reshape(n_rows, D))
    return out.reshape(B, L, D)


def setup_inputs(seed: int = 0) -> dict:
    key = jax.random.key(seed)
    ks = jax.random.split(key, 24)
    f32 = jnp.float32
    n = lambda k, shape, s: (jax.random.normal(k, shape, f32) * s)
    return {
        'x': n(ks[0], (BATCH, SEQ, D_MODEL), 1.0),
        'norm1_g': 1.0 + n(ks[1], (DEPTH, D_MODEL), 0.02),
        'w_in': n(ks[2], (DEPTH, D_MODEL, D_IN_PROJ), D_MODEL ** -0.5),
        'conv_w': n(ks[3], (DEPTH, SHORT_CONV, 3 * D_HYENA), SHORT_CONV ** -0.5),
        'conv_b': n(ks[4], (DEPTH, 3 * D_HYENA), 0.02),
        'f_w_in': n(ks[5], (DEPTH, FILTER_EMB, FILTER_ORDER), FILTER_EMB ** -0.5),
        'f_b_in': n(ks[6], (DEPTH, FILTER_ORDER), 0.1),
        'f_w_mid': n(ks[7], (DEPTH, N_INNER_MLPS, FILTER_ORDER, FILTER_ORDER), FILTER_ORDER ** -0.5),
        'f_b_mid': n(ks[8], (DEPTH, N_INNER_MLPS, FILTER_ORDER), 0.1),
        'f_freq': 1.0 + n(ks[9], (DEPTH, N_INNER_MLPS + 1, FILTER_ORDER), 0.1),
        'f_w_out': n(ks[10], (DEPTH, FILTER_ORDER, 2 * D_HYENA), FILTER_ORDER ** -0.5),
        'f_bias': n(ks[11], (DEPTH, D_HYENA), 1.0),
        'mix_g': 1.0 + n(ks[12], (DEPTH, D_MIX), 0.02),
        'w_out': n(ks[13], (DEPTH, D_MIX, D_MODEL), D_MIX ** -0.5),
        'norm2_g': 1.0 + n(ks[14], (DEPTH, D_MODEL), 0.02),
        'w_group': n(ks[15], (DEPTH, D_MODEL, N_GROUPS), D_MODEL ** -0.5),
        'b_group': n(ks[16], (DEPTH, N_GROUPS), 0.01),
        'w_router': n(ks[17], (DEPTH, N_GROUPS, D_MODEL, EXPERTS_PER_GROUP), D_MODEL ** -0.5),
        'b_router': n(ks[18], (DEPTH, N_GROUPS, EXPERTS_PER_GROUP), 0.01),
        'w_gate': n(ks[19], (DEPTH, N_EXPERTS, D_MODEL, D_EXPERT), D_MODEL ** -0.5),
        'w_up': n(ks[20], (DEPTH, N_EXPERTS, D_MODEL, D_EXPERT), D_MODEL ** -0.5),
        'w_down': n(ks[21], (DEPTH, N_EXPERTS, D_EXPERT, D_MODEL), D_EXPERT ** -0.5),
        'final_g': 1.0 + n(ks[22], (D_MODEL,), 0.02),
    }


def reference(x, norm1_g, w_in, conv_w, conv_b, f_w_in, f_b_in, f_w_mid, f_b_mid, f_freq, f_w_out, f_bias,
              mix_g, w_out, norm2_g, w_group, b_group, w_router, b_router, w_gate, w_up, w_down, final_g):
    for i in range(DEPTH):
        x = mixer_sublayer(x, norm1_g[i], w_in[i], conv_w[i], conv_b[i], f_w_in[i], f_b_in[i], f_w_mid[i],
                           f_b_mid[i], f_freq[i], f_w_out[i], f_bias[i], mix_g[i], w_out[i])
        x = x + hierarchical_moe(rmsnorm(x, norm2_g[i]), w_group[i], b_group[i], w_router[i], b_router[i],
                                 w_gate[i], w_up[i], w_down[i])
    return rmsnorm(x, final_g)
```

```python
import numpy as np
import ml_dtypes
from contextlib import ExitStack
import concourse.bass as bass
import concourse.mybir as mybir
from concourse.bass_utils import run_bass_kernel_spmd

F32 = mybir.dt.float32
BF16 = mybir.dt.bfloat16
I32 = mybir.dt.int32
AF = mybir.ActivationFunctionType
ALU = mybir.AluOpType
AX = mybir.AxisListType
bf = ml_dtypes.bfloat16

L = 4096
NF = 8192
D = 1024
CAP = 320
NS = 32 * CAP
NROWS = NS + 128
EPS = 1e-6
TWO_PI = float(2 * np.pi)


class Emit:
    def __init__(self, nc, stack, n_dma_sems=48):
        self.nc = nc
        self.eng = {"pe": nc.tensor, "act": nc.scalar, "dve": nc.vector, "pool": nc.gpsimd, "sp": nc.sync}
        self.sem = {}
        self.cnt = {}
        for k in self.eng:
            self.sem[k] = stack.enter_context(nc.semaphore("s_" + k))
            self.cnt[k] = 0
        self.dma_sems = [stack.enter_context(nc.semaphore("d%d" % i)) for i in range(n_dma_sems)]
        self.dma_cnt = [0] * n_dma_sems
        n_sw = 16
        self.dma_pool = {"hw": list(range(n_sw, n_dma_sems)), "sw": list(range(n_sw))}
        self.dma_rr = {"hw": 0, "sw": 0}
        self.seen = {k: {} for k in self.eng}
        self.lastw = {}
        self.reads = {}
        self.n_wait = 0
        self.n_ins = 0

    def _wait(self, e, ev):
        sem, val, src = ev
        sid = id(sem)
        if self.seen[e].get(sid, 0) >= val:
            return
        self.seen[e][sid] = val
        self.eng[e].wait_ge(sem, val)
        self.n_wait += 1

    def _deps(self, e, reads, writes):
        evs = []
        for r in reads:
            w = self.lastw.get(r)
            if w is not None and not (w[2] == e and e == "pe"):
                evs.append(w)
        for wkey in writes:
            w = self.lastw.get(wkey)
            if w is not None and w[2] != e:
                evs.append(w)
            for ev in self.reads.get(wkey, {}).values():
                if ev[2] != e:
                    evs.append(ev)
        return evs

    def _commit(self, ev, reads, writes):
        for r in reads:
            self.reads.setdefault(r, {})[(ev[2], id(ev[0]))] = ev
        for w in writes:
            self.lastw[w] = ev
            self.reads[w] = {}

    def op(self, e, fn, reads=(), writes=()):
        for ev in self._deps(e, reads, writes):
            self._wait(e, ev)
        ins = fn()
        self.cnt[e] += 1
        ins.then_inc(self.sem[e], 1)
        ev = (self.sem[e], self.cnt[e], e)
        self._commit(ev, reads, writes)
        self.n_ins += 1
        return ev

    def dma(self, q, fn, reads=(), writes=()):
        for ev in self._deps(q, reads, writes):
            self._wait(q, ev)
        kind = "sw" if q == "pool" else "hw"
        lst = self.dma_pool[kind]
        i = lst[self.dma_rr[kind]]
        self.dma_rr[kind] = (self.dma_rr[kind] + 1) % len(lst)
        sem = self.dma_sems[i]
        if self.dma_cnt[i] > 0:
            self._wait(q, (sem, 16 * self.dma_cnt[i], "dma"))
        ins = fn()
        self.dma_cnt[i] += 1
        ins.then_inc(sem, 16)
        ev = (sem, 16 * self.dma_cnt[i], "dma%d" % i)
        self._commit(ev, reads, writes)
        self.n_ins += 1
        return ev

    def barrier(self):
        for e in self.eng:
            for e2 in self.eng:
                if e2 != e and self.cnt[e2] > 0:
                    self._wait(e, (self.sem[e2], self.cnt[e2], e2))
            for i, s in enumerate(self.dma_sems):
                if self.dma_cnt[i] > 0:
                    self._wait(e, (s, 16 * self.dma_cnt[i], "dma"))
        self.lastw = {}
        self.reads = {}


def _kron4(M):
    return np.kron(M, np.eye(4))


def make_consts():
    c = {}
    p = np.arange(256)[:, None].astype(np.float64)
    k1 = np.arange(128)[None, :].astype(np.float64)
    fa = np.zeros((2, 32, 2, 128, 128))
    gt = np.zeros((32, 2, 128, 128))
    for j in range(32):
        th = 2 * np.pi * (p * (k1 + 0.5) / 256.0 + j * (k1 + 0.5) / NF)
        for pt in range(2):
            sgn = 1.0 if pt == 0 else -1.0
            fa[pt, j, 0] = sgn * np.cos(th[pt * 128:(pt + 1) * 128])
            fa[pt, j, 1] = -sgn * np.sin(th[pt * 128:(pt + 1) * 128])
        gt[j, 0] = (2.0 / NF) * np.cos(th[:128]).T
        gt[j, 1] = -(2.0 / NF) * np.sin(th[:128]).T
    c["fa"] = np.ascontiguousarray(fa.transpose(3, 0, 1, 2, 4)).reshape(128, 2 * 32 * 2 * 128).astype(bf)
    c["gt"] = np.ascontiguousarray(gt.transpose(2, 0, 1, 3)).reshape(128, 32 * 2 * 128).astype(bf)
    jj = np.arange(32)[:, None].astype(np.float64)
    kk = np.arange(32)[None, :].astype(np.float64)
    ph = 2 * np.pi * jj * kk / 32.0
    Tc = _kron4(np.cos(ph))
    Ts = _kron4(np.sin(ph))
    c["tct"] = np.stack([Tc, Ts, -Ts], 1).reshape(128, 3 * 128).astype(bf)
    c["tcf"] = np.stack([Tc / 512.0, Ts / 512.0], 1).reshape(128, 2 * 128).astype(bf)
    pa = np.arange(128)[:, None].astype(np.float64)
    ka = np.arange(128)[None, :].astype(np.float64)
    et = np.zeros((32, 3, 128, 128))
    for j in range(32):
        th = 2 * np.pi * (pa * ka / 128.0 + j * ka / 4096.0)
        et[j, 0] = np.cos(th)
        et[j, 1] = np.sin(th)
        et[j, 2] = -np.sin(th)
    c["et"] = np.ascontiguousarray(et.transpose(2, 0, 1, 3)).reshape(128, 32 * 3 * 128).astype(bf)
    cc = np.arange(64)[:, None].astype(np.float64)
    c2 = np.arange(64)[None, :].astype(np.float64)
    C64 = np.cos(2 * np.pi * cc * c2 / 64.0)
    S64 = np.sin(2 * np.pi * cc * c2 / 64.0)
    c["bdt"] = np.stack([np.kron(np.eye(2), C64), np.kron(np.eye(2), -S64)], 1).reshape(128, 256).astype(bf)
    c["ident"] = np.eye(128).astype(bf)
    c["blk"] = (np.kron(np.eye(2), np.ones((64, 64))) / 64.0).astype(bf)
    c["tri"] = np.triu(np.ones((128, 128)), 1).astype(bf)
    c["ones"] = np.ones((128, 128)).astype(bf)
    c["onesf"] = np.ones((128, 128), np.float32)
    c["ecb"] = np.tile((np.arange(32) * CAP).astype(np.float32)[None, :], (128, 1))
    c["trb"] = np.tile((NS + np.arange(128)).astype(np.float32)[:, None], (1, 32))
    n = np.arange(NF)
    m = np.where(n < L, n, NF - n).astype(np.float64)
    m[L] = 0
    t = (m / (L - 1)).astype(np.float32)
    w = (2.0 * np.pi / L) * m
    f = np.linspace(1e-4, 15, 16)[None, :]
    emb = np.concatenate([t[:, None], np.cos(f * w[:, None]), -np.sin(f * w[:, None])], -1).astype(np.float32)
    c["emb2"] = np.concatenate([emb[:L].T, emb[L:].T], 0).astype(np.float32)
    tn = -t.astype(np.float32)
    tn[L] = -1e4
    c["tneg"] = np.ascontiguousarray(tn.reshape(2, 128, 32).transpose(1, 0, 2)).reshape(128, 64)
    max_decay = np.log(1e-2) / 0.3
    min_decay = np.log(1e-2) / 1.5
    deltas = np.abs(np.linspace(min_decay, max_decay, 512)).astype(np.float32)
    c["deltab"] = np.tile(deltas[None, :], (128, 1))
    return c


CONST_SPECS = [
    ("fa", [128, 16384], BF16), ("gt", [128, 8192], BF16), ("tct", [128, 384], BF16), ("tcf", [128, 256], BF16),
    ("et", [128, 12288], BF16), ("bdt", [128, 256], BF16), ("ident", [128, 128], BF16), ("blk", [128, 128], BF16),
    ("tri", [128, 128], BF16), ("ones", [128, 128], BF16), ("onesf", [128, 128], F32), ("ecb", [128, 32], F32),
    ("trb", [128, 32], F32), ("emb2", [66, 4096], F32), ("tneg", [128, 64], F32), ("deltab", [128, 512], F32),
]
IN_SPECS = [
    ("x", [L, D], F32), ("w_in", [D, 2048], F32), ("g1b", [128, D], F32), ("g2b", [128, D], F32), ("gfb", [128, D], F32),
    ("cwp", [128, 36], F32), ("cbp", [128, 12], F32), ("fbp", [128, 4], F32), ("mgp", [128, 8], F32),
    ("w_out", [D, D], F32), ("wr", [D, 36], F32), ("brb", [128, 36], F32),
    ("w_gate", [32, D, 512], F32), ("w_up", [32, D, 512], F32), ("w_down", [32, 512, D], F32),
    ("fwin2", [66, 128], F32), ("fwmid2", [128, 256], F32), ("fq", [128, 3], F32), ("fbb", [128, 3], F32),
    ("fwout2", [128, 1024], F32),
]


def build_nc(stop_after=None, debug=False):
    nc = bass.Bass("TRN2", target_bir_lowering=False)
    T = {}
    for name, shape, dt in IN_SPECS + CONST_SPECS:
        T[name] = nc.dram_tensor(name, shape, dt, kind="ExternalInput").ap()
    out_d = nc.dram_tensor("out", [L, D], F32, kind="ExternalOutput").ap()
    skind = "ExternalOutput" if debug else "Internal"
    Pbuf = nc.dram_tensor("Pbuf", [1536, L], BF16, kind=skind).ap()
    wbuf = [nc.dram_tensor("wbuf%d" % i, [L, 512], BF16, kind=skind).ap() for i in range(2)]
    Hbuf = nc.dram_tensor("Hbuf", [4, 128, 8192], BF16, kind=skind).ap()
    yTbuf = nc.dram_tensor("yTbuf", [D, L], BF16, kind=skind).ap()
    x1buf = nc.dram_tensor("x1buf", [L, D], F32, kind=skind).ap()
    xg = nc.dram_tensor("xg", [NROWS, D], BF16, kind=skind).ap()
    Ybuf = nc.dram_tensor("Ybuf", [NROWS, D], F32, kind=skind).ap()
    dbgL = nc.dram_tensor("dbgL", [128, 32 * 40], F32, kind=skind).ap()

    with ExitStack() as st:
        em = Emit(nc, st)
        V, A, G, PE = nc.vector, nc.scalar, nc.gpsimd, nc.tensor
        ENG = {"dve": V, "act": A, "pool": G}

        def sbuf(stack, name, shape, dt):
            return stack.enter_context(nc.sbuf_tensor("sb_" + name, shape, dt))

        PS = [st.enter_context(nc.psum_tensor("ps%d" % i, [128, 512], F32)) for i in range(8)]
        ps_rr = [0]

        def ps_next():
            i = ps_rr[0]
            ps_rr[0] = (i + 1) % 8
            return PS[i], "ps%d" % i

        def mm(out, lhsT, rhs, start, stop, reads, pk):
            return em.op("pe", lambda: PE.matmul(out, lhsT=lhsT, rhs=rhs, start=start, stop=stop), reads=reads, writes=[pk])

        def tr(out, in_, reads, pk, kdim=128):
            idn = ident[:] if kdim == 128 else ident[0:kdim, 0:kdim]
            return em.op("pe", lambda: PE.transpose(out, in_, idn), reads=list(reads) + ["ident"], writes=[pk])

        def cp(e, out, in_, reads, writes):
            if e == "act":
                return em.op("act", lambda: A.copy(out=out, in_=in_), reads=reads, writes=writes)
            return em.op(e, lambda: ENG[e].tensor_copy(out=out, in_=in_), reads=reads, writes=writes)

        def tt(e, out, in0, in1, op, reads, writes):
            return em.op(e, lambda: ENG[e].tensor_tensor(out=out, in0=in0, in1=in1, op=op), reads=reads, writes=writes)

        def ts(e, out, in0, s1, s2, op0, op1, reads, writes):
            if op1 is None:
                return em.op(e, lambda: ENG[e].tensor_scalar(out=out, in0=in0, scalar1=s1, scalar2=None, op0=op0), reads=reads, writes=writes)
            return em.op(e, lambda: ENG[e].tensor_scalar(out=out, in0=in0, scalar1=s1, scalar2=s2, op0=op0, op1=op1), reads=reads, writes=writes)

        def stt(e, out, in0, scalar, in1, op0, op1, reads, writes):
            return em.op(e, lambda: ENG[e].scalar_tensor_tensor(out=out, in0=in0, scalar=scalar, in1=in1, op0=op0, op1=op1), reads=reads, writes=writes)

        def act(out, in_, func, reads, writes, **kw):
            return em.op("act", lambda: A.activation(out=out, in_=in_, func=func, **kw), reads=reads, writes=writes)

        def dma(q, out, in_, reads, writes):
            e = {"sp": nc.sync, "act": A, "pool": G}[q]
            return em.dma(q, lambda: e.dma_start(out=out, in_=in_), reads=reads, writes=writes)

        final_events = []

        ident = sbuf(st, "ident", [128, 128], BF16)
        blk = sbuf(st, "blk", [128, 128], BF16)
        tct = sbuf(st, "tct", [128, 3, 128], BF16)
        epst = sbuf(st, "epst", [128, 1], F32)
        rinvP = sbuf(st, "rinvP", [128, 4], F32)
        dma("sp", ident[:], T["ident"], [], ["ident"])
        dma("sp", blk[:], T["blk"], [], ["blk"])
        dma("sp", tct[:].rearrange("p a b -> p (a b)"), T["tct"], [], ["tct"])
        em.op("dve", lambda: V.memset(epst[:], EPS), writes=["epst"])

        ztp = sbuf(st, "ztp", [128, 2, D], BF16)
        em.op("pool", lambda: G.memset(ztp[:], 0.0), writes=["ztp"])
        zf_chunks = [(r0, min(256, NROWS - r0)) for r0 in range(0, NROWS, 256)]

        def zero_fill_some(n):
            for _ in range(n):
                if zf_chunks:
                    r0, nr = zf_chunks.pop(0)
                    dma("sp", xg[r0:r0 + nr, :].rearrange("(s p) f -> p s f", p=128), ztp[:, 0:nr // 128, :], ["ztp"], [("xg", r0)])

        def fft_fwd(src, npt, fa_t, bufA, bufB, srck, sink):
            for j0 in range(0, 32, 4):
                for cs in range(2):
                    ps, pk = ps_next()
                    for jj in range(4):
                        j = j0 + jj
                        for pt in range(npt):
                            mm(ps[:, jj * 128:(jj + 1) * 128], fa_t[:, pt, j, cs, :], src[:, pt * 32 + j, :],
                               pt == 0, pt == npt - 1, (srck if isinstance(srck, list) else [srck]) + ["fa"], pk)
                    cp("act" if cs == 0 else "dve",
                       bufA[:].rearrange("p a g (j c) -> p a g j c", c=4)[:, cs, :, j0:j0 + 4, :].rearrange("p g j c -> p j g c"),
                       ps[:].rearrange("p (j g c) -> p j g c", j=4, g=32), [pk], [("bufA", cs, j0)])
            for cs in range(2):
                for g0 in range(0, 32, 8):
                    ps, pk = ps_next()
                    psb = ps[:].bitcast(BF16)
                    for gg in range(8):
                        g = g0 + gg
                        tr(psb[:, gg * 128:(gg + 1) * 128], bufA[:, cs, g, :],
                           [("bufA", cs, j0) for j0 in range(0, 32, 4)], pk)
                    cp("act" if cs == 0 else "dve", bufB[:, cs, g0:g0 + 8, :], psb.rearrange("p (a b) -> p a b", b=128),
                       [pk], [("bufB", cs, g0)])
            for ch in range(8):
                psR, kR = ps_next()
                psI, kI = ps_next()
                bBf = bufB[:].rearrange("p a b c -> p a (b c)")
                br = bBf[:, 0, ch * 512:(ch + 1) * 512]
                bi = bBf[:, 1, ch * 512:(ch + 1) * 512]
                rk = [("bufB", 0, (4 * ch) // 8 * 8), ("bufB", 1, (4 * ch) // 8 * 8), "tct"]
                mm(psR[:], tct[:, 0, :], br, True, False, rk, kR)
                mm(psR[:], tct[:, 1, :], bi, False, True, rk, kR)
                mm(psI[:], tct[:, 0, :], bi, True, False, rk, kI)
                mm(psI[:], tct[:, 2, :], br, False, True, rk, kI)
                sink(ch, psR, kR, psI, kI)

        with ExitStack() as ph:
            fa_t = sbuf(ph, "fa_t", [128, 2, 32, 2, 128], BF16)
            w1t = sbuf(ph, "w1t", [66, 128], F32)
            wmt = sbuf(ph, "wmt", [128, 2, 128], F32)
            fq = sbuf(ph, "fq", [128, 3], F32)
            fbb = sbuf(ph, "fbb", [128, 3], F32)
            fq2 = sbuf(ph, "fq2", [128, 3], F32)
            fqb2 = sbuf(ph, "fqb2", [128, 3], F32)
            fwo = sbuf(ph, "fwo", [128, 1024], BF16)
            onesf = sbuf(ph, "onesf", [128, 128], F32)
            tneg = sbuf(ph, "tneg", [128, 64], F32)
            deltab = sbuf(ph, "deltab", [128, 512], F32)
            hid = sbuf(ph, "hid", [128, L], BF16)
            dma("sp", fa_t[:].rearrange("p a b c d -> p (a b c d)"), T["fa"], [], ["fa"])
            dma("sp", w1t[:], T["fwin2"], [], ["w1t"])
            dma("sp", wmt[:].rearrange("p a b -> p (a b)"), T["fwmid2"], [], ["wmt"])
            dma("sp", fq[:], T["fq"], [], ["fq"])
            dma("sp", fbb[:], T["fbb"], [], ["fbb"])
            dma("pool", fwo[:], T["fwout2"], [], ["fwo"])
            dma("sp", onesf[:], T["onesf"], [], ["onesf"])
            dma("sp", tneg[:], T["tneg"], [], ["tneg"])
            dma("sp", deltab[:], T["deltab"], [], ["deltab"])
            ts("dve", fq2[:], fq[:], float(1.0 / 3.0), None, ALU.mult, None, ["fq"], ["fq2"])
            tt("dve", fqb2[:], fq2[:], fbb[:], ALU.mult, ["fq2", "fbb"], ["fqb2"])
            with ExitStack() as ph2:
                emb = sbuf(ph2, "emb", [66, L], F32)
                hA = sbuf(ph2, "hA", [128, L], F32)
                hB = sbuf(ph2, "hB", [128, L], F32)
                for ch in range(8):
                    dma("sp", emb[:, ch * 512:(ch + 1) * 512], T["emb2"][:, ch * 512:(ch + 1) * 512], [], [("emb", ch)])
                ub = [sbuf(ph2, "ub%d" % i, [128, 512], F32) for i in range(8)]
                rb = [sbuf(ph2, "rb%d" % i, [128, 512], F32) for i in range(8)]
                srcs = [(emb, "emb", w1t[:]), (hA, "hA", wmt[:, 0, :]), (hB, "hB", wmt[:, 1, :])]
                dsts = [(hA, "hA"), (hB, "hB"), (hid, "hid")]
                for l in range(3):
                    s_t, s_k, lhsT = srcs[l]
                    d_t, d_k = dsts[l]
                    pss = {}
                    for ch in range(8):
                        ps, pk = ps_next()
                        pss[ch] = (ps, pk)
                        mm(ps[:], lhsT, s_t[:, ch * 512:(ch + 1) * 512], True, True, [(s_k, ch), "w1t", "wmt"], pk)
                    for ch in range(8):
                        ps, pk = pss[ch]
                        act(ub[ch][:], ps[:], AF.Sin, [pk, "fq2", "fqb2"], [("ub", ch)], scale=fq2[:, l:l + 1], bias=fqb2[:, l:l + 1])
                    for ch in range(8):
                        tt("dve", rb[ch][:], ub[ch][:], ub[ch][:], ALU.mult, [("ub", ch)], [("rb", ch)])
                    for ch in range(8):
                        ts("dve", rb[ch][:], rb[ch][:], -4.0, 3.0, ALU.mult, ALU.add, [("rb", ch)], [("rb", ch)])
                    for ch in range(8):
                        tt("dve", d_t[:, ch * 512:(ch + 1) * 512], rb[ch][:], ub[ch][:], ALU.mult, [("rb", ch), ("ub", ch)], [(d_k, ch)])
                em.barrier()
            with ExitStack() as ph2:
                decs = [sbuf(ph2, "dec%d" % i, [128, 64, 128], BF16) for i in range(2)]
                kbs = [sbuf(ph2, "kb%d" % i, [128, 64, 128], BF16) for i in range(2)]
                acc = sbuf(ph2, "acc", [128, 128], F32)
                bufA = sbuf(ph2, "bufA", [128, 2, 32, 128], BF16)
                bufB = sbuf(ph2, "bufB", [128, 2, 32, 128], BF16)
                Hsb = sbuf(ph2, "Hsb", [128, 2, L], BF16)

                def dec_gen(hc):
                    for col in range(64):
                        act(decs[hc % 2][:, col, :], deltab[:, hc * 128:(hc + 1) * 128], AF.Exp, ["deltab", "tneg"], [("dec", hc % 2, col // 4)],
                            scale=tneg[:, col:col + 1])

                def out_layer(hc):
                    dec = decs[hc % 2]
                    kb = kbs[hc % 2]
                    for c0 in range(0, 64, 4):
                        ps, pk = ps_next()
                        for cc in range(4):
                            col = c0 + cc
                            pt, j = col // 32, col % 32
                            mm(ps[:, cc * 128:(cc + 1) * 128], hid[64 * pt:64 * pt + 64, j::32],
                               fwo[64 * pt:64 * pt + 64, pt * 512 + hc * 128:pt * 512 + (hc + 1) * 128], True, True,
                               [("hid", ch) for ch in range(8)] + ["fwo"], pk)
                        tt("dve", kb[:, c0:c0 + 4, :], ps[:].rearrange("p (a b) -> p a b", b=128), dec[:, c0:c0 + 4, :], ALU.mult,
                           [pk, ("dec", hc % 2, c0 // 4)], [("kb", hc % 2, c0 // 4)])

                def sinkH(ch, psR, kR, psI, kI):
                    cp("act", Hsb[:, 0, ch * 512:(ch + 1) * 512], psR[:], [kR], [("Hsb", ch)])
                    cp("dve", Hsb[:, 1, ch * 512:(ch + 1) * 512], psI[:], [kI], [("Hsb", ch)])

                dec_gen(0)
                out_layer(0)
                for hc in range(4):
                    kb = kbs[hc % 2]
                    kbk = [("kb", hc % 2, i) for i in range(16)]
                    if hc + 1 < 4:
                        dec_gen(hc + 1)
                        out_layer(hc + 1)
                    fft_fwd(kb, 2, fa_t, bufA, bufB, kbk, sinkH)
                    dma("sp", Hbuf[hc], Hsb[:].rearrange("p a b -> p (a b)"), [("Hsb", ch) for ch in range(8)], [("Hbuf", hc)])
                    em.op("dve", lambda: V.tensor_reduce(out=acc[:], in_=kb[:].rearrange("p n c -> p c n"), axis=AX.X, op=ALU.add,
                                                         apply_absolute_value=True), reads=kbk, writes=["acc"])
                    ps, pk = ps_next()
                    mm(ps[:, 0:1], acc[:], onesf[:, 0:1], True, True, ["onesf", "acc"], pk)
                    em.op("dve", lambda: V.reciprocal(out=rinvP[:, hc:hc + 1], in_=ps[:, 0:1]), reads=[pk], writes=[("rinvP", hc)])
                em.barrier()
        if stop_after == "F0":
            em.barrier()
            return nc

        with ExitStack() as ph:
            Wb = sbuf(ph, "Wb", [128, 8, 2048], BF16)
            Wf = sbuf(ph, "Wf", [128, 2, 8, 512], BF16)
            g1b = sbuf(ph, "g1b", [128, D], F32)
            for kc in range(8):
                for h2_ in range(2):
                    dma("pool", Wb[:, kc, h2_ * 1024:(h2_ + 1) * 1024], T["w_in"][kc * 128:(kc + 1) * 128, h2_ * 1024:(h2_ + 1) * 1024],
                        [], [("Wb", kc, h2_)])
            dma("sp", g1b[:], T["g1b"], [], ["g1b"])
            with ExitStack() as ph2:
                bdt = sbuf(ph2, "bdt", [128, 2, 128], BF16)
                WfT = sbuf(ph2, "WfT", [128, 4, D], BF16)
                dma("sp", bdt[:].rearrange("p a b -> p (a b)"), T["bdt"], [], ["bdt"])
                for n4 in range(4):
                    ps, pk = ps_next()
                    psb = ps[:].bitcast(BF16)
                    for kc in range(8):
                        tr(psb[:, kc * 128:(kc + 1) * 128], Wb[:, kc, 1536 + n4 * 128:1536 + (n4 + 1) * 128], [("Wb", kc, 0), ("Wb", kc, 1)], pk)
                    cp("act", WfT[:, n4, :], psb, [pk], [("WfT", n4)])
                for part in range(2):
                    for kc in range(8):
                        ps, pk = ps_next()
                        for n4 in range(4):
                            mm(ps[:, n4 * 128:(n4 + 1) * 128], WfT[:, n4, kc * 128:(kc + 1) * 128], bdt[:, part, :], True, True,
                               [("WfT", n4), "bdt"], pk)
                        cp("dve", Wf[:, part, kc, :], ps[:], [pk], [("Wf", part, kc)])
                em.barrier()
            xt = [sbuf(ph, "xt%d" % i, [128, D], F32) for i in range(2)]
            junk = sbuf(ph, "junk", [128, D], BF16)
            ss = sbuf(ph, "ss", [128, 32], F32)
            rs = sbuf(ph, "rs", [128, 32], F32)
            hb = [sbuf(ph, "hb%d" % i, [128, D], BF16) for i in range(2)]
            hTc = [sbuf(ph, "hTc%d" % i, [128, 8, 512], BF16) for i in range(2)]
            Pst = [sbuf(ph, "Pst%d" % i, [128, 12, 512], BF16) for i in range(2)]
            wst = [sbuf(ph, "wst%d" % i, [128, 2, 512], BF16) for i in range(2)]
            em.op("dve", lambda: V.memset(ss[:], 0.0), writes=["ss"])
            ev_cnt = [0]

            def a_norm(tc, i):
                hb_ = tc % 2
                j2 = 4 * tc + i
                b = j2 % 2
                dma("sp", xt[b][:], T["x"][j2 * 128:(j2 + 1) * 128, :], [], [("xt", b)])
                zero_fill_some(2)
                act(junk[:], xt[b][:], AF.Square, [("xt", b), "ss"], ["junk", ("ss", j2)], accum_out=ss[:, j2:j2 + 1])
                act(rs[:, j2:j2 + 1], ss[:, j2:j2 + 1], AF.Sqrt, [("ss", j2), "epst"], [("rs", j2)], scale=1.0 / D, bias=epst[:])
                em.op("dve", lambda: V.reciprocal(out=rs[:, j2:j2 + 1], in_=rs[:, j2:j2 + 1]), reads=[("rs", j2)], writes=[("rs", j2)])
                stt("dve", hb[b][:], xt[b][:], rs[:, j2:j2 + 1], g1b[:], ALU.mult, ALU.mult, [("xt", b), ("rs", j2), "g1b"], [("hb", b)])
                ps, pk = ps_next()
                psb = ps[:].bitcast(BF16)
                for kc in range(8):
                    tr(psb[:, kc * 128:(kc + 1) * 128], hb[b][:, kc * 128:(kc + 1) * 128], [("hb", b)], pk)
                cp("act", hTc[hb_][:, :, i * 128:(i + 1) * 128], psb.rearrange("p (a b) -> p a b", b=128), [pk], [("hTc", hb_, i)])

            def a_mm(tc, part):
                hb_ = tc % 2
                hk = [("hTc", hb_, i) for i in range(4)]
                for cch in range(3 * part, 3 * part + 3):
                    ps, pk = ps_next()
                    for kc in range(8):
                        mm(ps[:], Wb[:, kc, cch * 128:(cch + 1) * 128], hTc[hb_][:, kc, :], kc == 0, kc == 7, hk + [("Wb", kc, 0), ("Wb", kc, 1)], pk)
                    ev_cnt[0] += 1
                    cp("act" if ev_cnt[0] % 2 else "dve", Pst[hb_][:, cch, :], ps[:], [pk], [("Pst", hb_, cch)])
                if part == 3:
                    dma("sp", Pbuf[:, tc * 512:(tc + 1) * 512].rearrange("(c p) t -> p c t", p=128), Pst[hb_][:],
                        [("Pst", hb_, c_) for c_ in range(12)], [("Pbuf", tc)])
                i = part
                j2 = 4 * tc + i
                wb_ = j2 % 2
                for fpart in range(2):
                    ps, pk = ps_next()
                    for kc in range(8):
                        mm(ps[:], hTc[hb_][:, kc, i * 128:(i + 1) * 128], Wf[:, fpart, kc, :], kc == 0, kc == 7,
                           [("hTc", hb_, i), ("Wf", fpart, kc)], pk)
                    ev_cnt[0] += 1
                    cp("act" if ev_cnt[0] % 2 else "dve", wst[wb_][:, fpart, :], ps[:], [pk], [("wst", wb_, fpart)])
                    dma("sp", wbuf[fpart][j2 * 128:(j2 + 1) * 128, :], wst[wb_][:, fpart, :], [("wst", wb_, fpart)], [("wbuf", fpart, j2)])

            for i in range(4):
                a_norm(0, i)
            for tc in range(8):
                for part in range(4):
                    if tc + 1 < 8:
                        a_norm(tc + 1, part)
                    a_mm(tc, part)
            em.barrier()
        if stop_after == "A":
            return nc

        def head_norm_wave(rs_, mg_ap, row0, rsqs, rstdb):
            for q in range(4):
                act(rsqs[q][:], rs_[q][0], AF.Square, [rs_[q][1]], [("rsq", q)])
            for q in range(4):
                for h_ in range(2):
                    ps, pk = ps_next()
                    mm(ps[:], blk[:], rsqs[q][:, h_ * 512:(h_ + 1) * 512], True, True, [("rsq", q), "blk"], pk)
                    act(rstdb[q][:, h_ * 512:(h_ + 1) * 512], ps[:], AF.Ln, [pk, "epst"], [("rstd_t", q, h_)], bias=epst[:], scale=1.0)
            for q in range(4):
                act(rstdb[q][:], rstdb[q][:], AF.Exp, [("rstd_t", q, 0), ("rstd_t", q, 1)], [("rstd_t", q, 0), ("rstd_t", q, 1)], scale=-0.5)
            for q in range(4):
                stt("dve", rsqs[q][:], rs_[q][0], mg_ap, rstdb[q][:], ALU.mult, ALU.mult,
                    [rs_[q][1], ("rstd_t", q, 0), ("rstd_t", q, 1), "mgp"], [("rsq", q)])
                dma("sp", yTbuf[row0:row0 + 128, q * 1024:(q + 1) * 1024], rsqs[q][:], [("rsq", q)], [("yTbuf", row0, q)])

        with ExitStack() as ph:
            fa_t = sbuf(ph, "fa_tB", [128, 32, 2, 128], BF16)
            gt_t = sbuf(ph, "gt_t", [128, 32, 2, 128], BF16)
            cwp = sbuf(ph, "cwp", [128, 12, 3], F32)
            cbp = sbuf(ph, "cbp", [128, 12], F32)
            fbp = sbuf(ph, "fbp", [128, 4], F32)
            mgp = sbuf(ph, "mgp", [128, 8], F32)
            dma("sp", fa_t[:].rearrange("p b c d -> p (b c d)"), T["fa"][:, 0:8192], [], ["fa"])
            dma("sp", gt_t[:].rearrange("p b c d -> p (b c d)"), T["gt"], [], ["gt"])
            dma("sp", cwp[:].rearrange("p a b -> p (a b)"), T["cwp"], [], ["cwp"])
            dma("sp", cbp[:], T["cbp"], [], ["cbp"])
            dma("sp", fbp[:], T["fbp"], [], ["fbp"])
            dma("sp", mgp[:], T["mgp"], [], ["mgp"])
            Pt = [sbuf(ph, "Pt%d" % s, [128, L + 2], BF16) for s in range(3)]
            Hsb = sbuf(ph, "HsbB", [128, 2, L], BF16)
            ux0s = [sbuf(ph, "ux0_%d" % i, [128, L], BF16) for i in range(2)]
            zTs = [sbuf(ph, "zT_%d" % i, [128, L], BF16) for i in range(2)]
            tA = sbuf(ph, "tA", [128, 1024], F32)
            tB = sbuf(ph, "tB", [128, 1024], F32)
            tC = sbuf(ph, "tC", [128, 1024], F32)
            zP1 = sbuf(ph, "zP1", [128, 32, 128], BF16)
            bufA = sbuf(ph, "bufAB", [128, 2, 32, 128], BF16)
            bufB = sbuf(ph, "bufBB", [128, 2, 32, 128], BF16)
            yc = sbuf(ph, "yc", [128, L], BF16)
            rsqs = [sbuf(ph, "rsq%d" % i, [128, 1024], BF16) for i in range(4)]
            rstdb = [sbuf(ph, "rstdb%d" % i, [128, 1024], BF16) for i in range(4)]
            tD = sbuf(ph, "tD", [128, 1024], F32)
            mts = [[sbuf(ph, "mt%d_%d" % (i, k), [128, 512], F32) for k in range(4)] for i in range(2)]
            for s in range(3):
                em.op("dve", lambda s=s: V.memset(Pt[s][:, 0:1], 0.0), writes=[("Ppad", s)])
                em.op("dve", lambda s=s: V.memset(Pt[s][:, L + 1:L + 2], 0.0), writes=[("Ppad", s)])
            fa5 = fa_t[:].rearrange("p (a b) c d -> p a b c d", a=1)
            def load_P(hc):
                for s in range(3):
                    dma("sp", Pt[s][:, 1:L + 1], Pbuf[(s * 4 + hc) * 128:(s * 4 + hc + 1) * 128, :], [], [("Pt", s)])

            def load_H(hc):
                dma("sp", Hsb[:].rearrange("p a b -> p (a b)"), Hbuf[hc], [], ["HsbB"])

            def conv(hc):
                ux0 = ux0s[hc % 2]
                zT = zTs[hc % 2]
                par = hc % 2
                for q in range(4):
                    t0 = q * 1024
                    tmps = [(tA, "tA"), (tB, "tB"), (tC, "tC")]
                    cc = hc
                    pk_ = [("Pt", 0), ("Ppad", 0), "cwp", "cbp"]
                    act(tA[:], Pt[0][:, 1 + t0:1 + t0 + 1024], AF.Identity, pk_, ["tA"], scale=cwp[:, cc, 1:2], bias=cbp[:, cc:cc + 1])
                    act(tD[:], Pt[0][:, t0:t0 + 1024], AF.Identity, pk_, ["tD"], scale=cwp[:, cc, 0:1])
                    tt("pool", tA[:], tA[:], tD[:], ALU.add, ["tA", "tD"], ["tA"])
                    act(tD[:], Pt[0][:, 2 + t0:2 + t0 + 1024], AF.Identity, pk_, ["tD"], scale=cwp[:, cc, 2:3])
                    tt("pool", ux0[:, t0:t0 + 1024], tA[:], tD[:], ALU.add, ["tA", "tD"], [("ux0", par, q)])
                    for s in (1, 2):
                        cc = s * 4 + hc
                        pk_ = [("Pt", s), ("Ppad", s), "cwp", "cbp"]
                        tmp, tk = tmps[s]
                        ts("dve", tmp[:], Pt[s][:, 1 + t0:1 + t0 + 1024], cwp[:, cc, 1:2], cbp[:, cc:cc + 1], ALU.mult, ALU.add, pk_, [tk])
                        stt("dve", tmp[:], Pt[s][:, t0:t0 + 1024], cwp[:, cc, 0:1], tmp[:], ALU.mult, ALU.add, pk_ + [tk], [tk])
                        stt("dve", tmp[:], Pt[s][:, 2 + t0:2 + t0 + 1024], cwp[:, cc, 2:3], tmp[:], ALU.mult, ALU.add, pk_ + [tk], [tk])
                    tt("dve", zT[:, t0:t0 + 1024], tB[:], tC[:], ALU.mult, ["tB", "tC"], [("zT", par, q)])

            def fwd(hc):
                zT = zTs[hc % 2]
                par = hc % 2
                zk = [("zT", par, q) for q in range(4)]
                for j0 in range(0, 32, 8):
                    ps, pk = ps_next()
                    psb = ps[:].bitcast(BF16)
                    for jj in range(8):
                        tr(psb[:, jj * 128:(jj + 1) * 128], zT[:, j0 + jj::32], zk, pk)
                    cp("act", zP1[:, j0:j0 + 8, :], psb.rearrange("p (a b) -> p a b", b=128), [pk], ["zP1"])

                fft_fwd_keys_A = [("bufA", cs, j0) for cs in range(2) for j0 in range(0, 32, 4)]

                def sinkY_guard(ch, psR, kR, psI, kI):
                    sl = slice(ch * 512, (ch + 1) * 512)
                    ya = bufA[:].rearrange("p a b c -> p a (b c)")
                    m = mts[ch % 2]
                    mk = [("mt", ch % 2, k) for k in range(4)]
                    tt("dve", m[0][:], psR[:], Hsb[:, 0, sl], ALU.mult, [kR, "HsbB"], [mk[0]])
                    tt("dve", m[1][:], psI[:], Hsb[:, 1, sl], ALU.mult, [kI, "HsbB"], [mk[1]])
                    tt("dve", m[2][:], psR[:], Hsb[:, 1, sl], ALU.mult, [kR, "HsbB"], [mk[2]])
                    tt("dve", m[3][:], psI[:], Hsb[:, 0, sl], ALU.mult, [kI, "HsbB"], [mk[3]])
                    tt("pool", ya[:, 0, sl], m[0][:], m[1][:], ALU.subtract, [mk[0], mk[1]], [("Y", ch)] + fft_fwd_keys_A)
                    tt("pool", ya[:, 1, sl], m[2][:], m[3][:], ALU.add, [mk[2], mk[3]], [("Y", ch)])

                fft_fwd(zP1, 1, fa5, bufA, bufB, "zP1", sinkY_guard)

            def inv(hc):
                ux0 = ux0s[hc % 2]
                zT = zTs[hc % 2]
                par = hc % 2
                zk = [("zT", par, q) for q in range(4)]
                ya = bufA[:].rearrange("p a b c -> p a (b c)")
                bufB_keys = [("bufB", cs, g0) for cs in range(2) for g0 in range(0, 32, 8)]
                for ch in range(8):
                    sl = slice(ch * 512, (ch + 1) * 512)
                    psR, kR = ps_next()
                    psI, kI = ps_next()
                    rk = [("Y", ch), "tct"]
                    mm(psR[:], tct[:, 0, :], ya[:, 0, sl], True, False, rk, kR)
                    mm(psR[:], tct[:, 2, :], ya[:, 1, sl], False, True, rk, kR)
                    mm(psI[:], tct[:, 1, :], ya[:, 0, sl], True, False, rk, kI)
                    mm(psI[:], tct[:, 0, :], ya[:, 1, sl], False, True, rk, kI)
                    wk = [("Cb", ch)] + (bufB_keys if ch == 0 else [])
                    cp("act", bufB[:, 0, 4 * ch:4 * ch + 4, :], psR[:].rearrange("p (a b) -> p a b", b=128), [kR], wk)
                    cp("dve", bufB[:, 1, 4 * ch:4 * ch + 4, :], psI[:].rearrange("p (a b) -> p a b", b=128), [kI], [("Cb", ch)])
                first = True
                for cs in range(2):
                    for g0 in range(0, 32, 8):
                        ps, pk = ps_next()
                        psb = ps[:].bitcast(BF16)
                        for gg in range(8):
                            g = g0 + gg
                            tr(psb[:, gg * 128:(gg + 1) * 128], bufB[:, cs, g, :], [("Cb", g // 4)], pk)
                        wk = [("Ct", cs, g0)] + ([("Y", ch) for ch in range(8)] if first else [])
                        first = False
                        cp("act" if cs == 0 else "dve", bufA[:, cs, :, 4 * g0:4 * g0 + 32].rearrange("p j (g c) -> p g j c", c=4),
                           psb.rearrange("p (g j c) -> p g j c", g=8, j=32), [pk], wk)
                ctk = [("Ct", cs, g0) for cs in range(2) for g0 in range(0, 32, 8)]
                yc3 = yc[:].rearrange("c (p j) -> c p j", j=32)
                for j0 in range(0, 32, 4):
                    ps, pk = ps_next()
                    for jj in range(4):
                        j = j0 + jj
                        mm(ps[:, jj * 128:(jj + 1) * 128], bufA[:, 0, j, :], gt_t[:, j, 0, :], True, False, ctk + ["gt"], pk)
                        mm(ps[:, jj * 128:(jj + 1) * 128], bufA[:, 1, j, :], gt_t[:, j, 1, :], False, True, ctk + ["gt"], pk)
                    act(yc3[:, :, j0:j0 + 4].rearrange("c p j -> c j p"), ps[:].rearrange("c (j p) -> c j p", p=128), AF.Identity,
                        [pk, ("rinvP", hc)], [("yc", j0)], scale=rinvP[:, hc:hc + 1])
                yck = [("yc", j0) for j0 in range(0, 32, 4)]
                tq = [(tA, "tA"), (tB, "tB"), (tC, "tC"), (tD, "tD")]
                for q in range(4):
                    t0 = q * 1024
                    stt("dve", tq[q][0][:], zT[:, t0:t0 + 1024], fbp[:, hc:hc + 1], yc[:, t0:t0 + 1024], ALU.mult, ALU.add,
                        zk + yck + ["fbp"], [tq[q][1]])
                for q in range(4):
                    t0 = q * 1024
                    tt("pool", tq[q][0][:], tq[q][0][:], ux0[:, t0:t0 + 1024], ALU.mult, [tq[q][1], ("ux0", par, q)], [tq[q][1]])
                head_norm_wave([(tq[q][0][:], tq[q][1]) for q in range(4)], mgp[:, hc:hc + 1], hc * 128, rsqs, rstdb)

            load_P(0)
            load_H(0)
            conv(0)
            load_P(1)
            for hc in range(4):
                fwd(hc)
                if hc + 1 < 4:
                    load_H(hc + 1)
                    conv(hc + 1)
                    if hc + 2 < 4:
                        load_P(hc + 2)
                inv(hc)
            em.barrier()
        if stop_after == "B":
            return nc

        with ExitStack() as ph:
            et_t = sbuf(ph, "et_t", [128, 32, 3, 128], BF16)
            tcf = sbuf(ph, "tcf", [128, 2, 128], BF16)
            mgp = sbuf(ph, "mgpC", [128, 8], F32)
            dma("sp", et_t[:].rearrange("p b c d -> p (b c d)"), T["et"], [], ["et"])
            dma("sp", tcf[:].rearrange("p a b -> p (a b)"), T["tcf"], [], ["tcf"])
            dma("sp", mgp[:], T["mgp"], [], ["mgp"])
            wris = [sbuf(ph, "wri%d" % i, [128, 2, 32, 128], BF16) for i in range(2)]
            bufA = sbuf(ph, "bufAC", [128, 2, 32, 128], BF16)
            bufB = sbuf(ph, "bufBC", [128, 2, 32, 128], BF16)
            yf = sbuf(ph, "yf", [128, 32, 128], BF16)
            rTs = [sbuf(ph, "rT%d" % i, [128, 1024], F32) for i in range(4)]
            rsqs = [sbuf(ph, "rsqC%d" % i, [128, 1024], BF16) for i in range(4)]
            rstdb = [sbuf(ph, "rstdbC%d" % i, [128, 1024], BF16) for i in range(4)]
            def c_load(fc):
                for part in range(2):
                    dma("sp", wris[fc % 2][:, part, :, :], wbuf[part][:, fc * 128:(fc + 1) * 128].rearrange("(p j) c -> p j c", j=32), [],
                        [("wri", fc % 2, part)])

            c_load(0)
            for fc in range(4):
                wri = wris[fc % 2]
                if fc + 1 < 4:
                    c_load(fc + 1)
                for j0 in range(0, 32, 4):
                    for cs in range(2):
                        ps, pk = ps_next()
                        for jj in range(4):
                            j = j0 + jj
                            if cs == 0:
                                mm(ps[:, jj * 128:(jj + 1) * 128], et_t[:, j, 0, :], wri[:, 0, j, :], True, False, [("wri", fc % 2, 0), ("wri", fc % 2, 1), "et"], pk)
                                mm(ps[:, jj * 128:(jj + 1) * 128], et_t[:, j, 1, :], wri[:, 1, j, :], False, True, [("wri", fc % 2, 0), ("wri", fc % 2, 1), "et"], pk)
                            else:
                                mm(ps[:, jj * 128:(jj + 1) * 128], et_t[:, j, 0, :], wri[:, 1, j, :], True, False, [("wri", fc % 2, 0), ("wri", fc % 2, 1), "et"], pk)
                                mm(ps[:, jj * 128:(jj + 1) * 128], et_t[:, j, 2, :], wri[:, 0, j, :], False, True, [("wri", fc % 2, 0), ("wri", fc % 2, 1), "et"], pk)
                        cp("act" if cs == 0 else "dve",
                           bufA[:].rearrange("p a g (j c) -> p a g j c", c=4)[:, cs, :, j0:j0 + 4, :].rearrange("p g j c -> p j g c"),
                           ps[:].rearrange("p (j g c) -> p j g c", j=4, g=32), [pk], [("bufA", cs, j0)])
                ak = [("bufA", cs, j0) for cs in range(2) for j0 in range(0, 32, 4)]
                for cs in range(2):
                    for g0 in range(0, 32, 8):
                        ps, pk = ps_next()
                        psb = ps[:].bitcast(BF16)
                        for gg in range(8):
                            g = g0 + gg
                            tr(psb[:, gg * 128:(gg + 1) * 128], bufA[:, cs, g, :], ak, pk)
                        cp("act" if cs == 0 else "dve", bufB[:, cs, g0:g0 + 8, :], psb.rearrange("p (a b) -> p a b", b=128), [pk], [("bufB", cs, g0)])
                for g0 in range(0, 32, 4):
                    ps, pk = ps_next()
                    for gg in range(4):
                        g = g0 + gg
                        rk = [("bufB", 0, g // 8 * 8), ("bufB", 1, g // 8 * 8), "tcf"]
                        mm(ps[:, gg * 128:(gg + 1) * 128], bufB[:, 0, g, :], tcf[:, 0, :], True, False, rk, pk)
                        mm(ps[:, gg * 128:(gg + 1) * 128], bufB[:, 1, g, :], tcf[:, 1, :], False, True, rk, pk)
                    cp("act" if (g0 // 4) % 2 else "dve", yf[:, :, 4 * g0:4 * g0 + 16].rearrange("p k (g c) -> p g k c", c=4),
                       ps[:].rearrange("p (g k c) -> p g k c", g=4, k=32), [pk], [("yf", g0)])
                yfk = [("yf", g0) for g0 in range(0, 32, 4)]
                for q in range(4):
                    ps, pk = ps_next()
                    psb = ps[:].bitcast(BF16)
                    for kk_ in range(8):
                        kb_ = q * 8 + kk_
                        tr(psb[:, kk_ * 128:(kk_ + 1) * 128], yf[:, kb_, :], yfk, pk)
                    cp("dve" if q % 2 else "act", rTs[q][:], psb, [pk], [("rT", q)])
                head_norm_wave([(rTs[q][:], ("rT", q)) for q in range(4)], mgp[:, 4 + fc:5 + fc], 512 + fc * 128, rsqs, rstdb)
            em.barrier()
        if stop_after == "C":
            return nc

        ph_moe = st.enter_context(ExitStack())
        w1 = sbuf(ph_moe, "w1", [128, 32], F32)
        w2 = sbuf(ph_moe, "w2", [128, 32], F32)
        ds_i = sbuf(ph_moe, "ds_i", [128, 2, 32], I32)
        dg_i = sbuf(ph_moe, "dg_i", [128, 2, 32], I32)
        ph_tok = ExitStack()
        h2tok = sbuf(ph_tok, "h2tok", [128, 32, D], BF16)
        Lg = sbuf(ph_tok, "Lg", [128, 32, 36], F32)
        with ExitStack() as ph:
            Wo = sbuf(ph, "Wo", [128, 8, D], BF16)
            Wr = sbuf(ph, "Wr", [128, 8, 36], BF16)
            g2b = sbuf(ph, "g2b", [128, D], F32)
            brb = sbuf(ph, "brb", [128, 36], F32)
            for kc in range(8):
                dma("pool", Wo[:, kc, :], T["w_out"][kc * 128:(kc + 1) * 128, :], [], [("Wo", kc)])
            dma("pool", Wr[:], T["wr"].rearrange("(k p) n -> p k n", p=128), [], ["Wr"])
            dma("sp", g2b[:], T["g2b"], [], ["g2b"])
            dma("sp", brb[:], T["brb"], [], ["brb"])
            yTb = [sbuf(ph, "yTb%d" % i, [128, 8, 512], BF16) for i in range(2)]
            xt = [sbuf(ph, "xtD%d" % i, [128, D], F32) for i in range(3)]
            x1t = [sbuf(ph, "x1t%d" % i, [128, D], F32) for i in range(3)]
            junk = sbuf(ph, "junkD", [128, D], BF16)
            ss = sbuf(ph, "ssD", [128, 32], F32)
            rs = sbuf(ph, "rsD", [128, 32], F32)
            h2T = [sbuf(ph, "h2T%d" % i, [128, 8, 128], BF16) for i in range(2)]
            em.op("dve", lambda: V.memset(ss[:], 0.0), writes=["ssD"])
            wo_ps = {}

            def d_front(j2):
                tc, i = j2 // 4, j2 % 4
                yb = tc % 2
                b = j2 % 3
                if i == 0:
                    dma("sp", yTb[yb][:], yTbuf[:, tc * 512:(tc + 1) * 512].rearrange("(c p) t -> p c t", p=128), [], [("yTb", yb)])
                dma("sp", xt[b][:], T["x"][j2 * 128:(j2 + 1) * 128, :], [], [("xtD", b)])
                for half in range(2):
                    ps, pk = ps_next()
                    for cc in range(8):
                        mm(ps[:], yTb[yb][:, cc, i * 128:(i + 1) * 128], Wo[:, cc, half * 512:(half + 1) * 512], cc == 0, cc == 7,
                           [("yTb", yb), ("Wo", cc)], pk)
                    wo_ps[(j2, half)] = (ps, pk)

            def d_back(j2):
                b = j2 % 3
                for half in range(2):
                    ps, pk = wo_ps.pop((j2, half))
                    tt("dve", x1t[b][:, half * 512:(half + 1) * 512], xt[b][:, half * 512:(half + 1) * 512], ps[:], ALU.add,
                       [("xtD", b), pk], [("x1t", b, half)])
                xk = [("x1t", b, 0), ("x1t", b, 1)]
                dma("sp", x1buf[j2 * 128:(j2 + 1) * 128, :], x1t[b][:], xk, [("x1buf", j2)])
                act(junk[:], x1t[b][:], AF.Square, xk + ["ssD"], ["junkD", ("ssD", j2)], accum_out=ss[:, j2:j2 + 1])
                act(rs[:, j2:j2 + 1], ss[:, j2:j2 + 1], AF.Sqrt, [("ssD", j2), "epst"], [("rsD", j2)], scale=1.0 / D, bias=epst[:])
                em.op("dve", lambda: V.reciprocal(out=rs[:, j2:j2 + 1], in_=rs[:, j2:j2 + 1]), reads=[("rsD", j2)], writes=[("rsD", j2)])
                stt("dve", h2tok[:, j2, :], x1t[b][:], rs[:, j2:j2 + 1], g2b[:], ALU.mult, ALU.mult, xk + [("rsD", j2), "g2b"], [("h2tok", j2)])
                ps, pk = ps_next()
                psb = ps[:].bitcast(BF16)
                for kc in range(8):
                    tr(psb[:, kc * 128:(kc + 1) * 128], h2tok[:, j2, kc * 128:(kc + 1) * 128], [("h2tok", j2)], pk)
                cp("act", h2T[j2 % 2][:], psb.rearrange("p (a b) -> p a b", b=128), [pk], [("h2T", j2 % 2)])
                ps, pk = ps_next()
                for kc in range(8):
                    mm(ps[:, 0:36], h2T[j2 % 2][:, kc, :], Wr[:, kc, :], kc == 0, kc == 7, [("h2T", j2 % 2), "Wr"], pk)
                tt("dve", Lg[:, j2, :], ps[:, 0:36], brb[:], ALU.add, [pk, "brb"], [("Lg", j2)])

            d_front(0)
            for j2 in range(32):
                if j2 + 1 < 32:
                    d_front(j2 + 1)
                d_back(j2)
            em.barrier()
        if debug:
            dma("sp", dbgL[:, 0:32 * 36], Lg[:].rearrange("p a b -> p (a b)"), [("Lg", j2) for j2 in range(32)], ["dbgL"])
        if stop_after == "D":
            em.barrier()
            return nc

        with ExitStack() as ph:
            tri = sbuf(ph, "tri", [128, 128], BF16)
            ones = sbuf(ph, "ones", [128, 128], BF16)
            ecb = sbuf(ph, "ecb", [128, 32], F32)
            trb = sbuf(ph, "trb", [128, 32], F32)
            dma("sp", tri[:], T["tri"], [], ["tri"])
            dma("sp", ones[:], T["ones"], [], ["ones"])
            dma("sp", ecb[:], T["ecb"], [], ["ecb"])
            dma("sp", trb[:], T["trb"], [], ["trb"])
            S = lambda name, shape, dt=F32: sbuf(ph, name, shape, dt)
            gmax = S("gmax", [128, 32]); goh = S("goh", [128, 32, 4]); gd = S("gd", [128, 32, 4]); gsum = S("gsum", [128, 32])
            pg = S("pg", [128, 32]); sel4 = S("sel4", [128, 32, 4, 8]); esel = S("esel", [128, 32, 8]); m1 = S("m1", [128, 32])
            oh1 = S("oh1", [128, 32, 8]); e2 = S("e2", [128, 32, 8]); m2 = S("m2", [128, 32]); oh2 = S("oh2", [128, 32, 8])
            dd = S("dd", [128, 32]); A1 = S("A1", [128, 32, 4, 8]); A2 = S("A2", [128, 32, 4, 8]); Mb = S("Mb", [128, 1024], BF16)
            pin = S("pin", [128, 32, 32]); cnt = S("cnt", [128, 32, 32]); base = S("base", [128, 32, 32]); slot = S("slot", [128, 32, 32])
            tmp3 = S("tmp3", [128, 32, 32]); dk = S("dk", [128, 2, 32]); pk_ = S("pk_", [128, 2, 32]); ok = S("ok", [128, 2, 32])
            dsf = S("dsf", [128, 2, 32]); dgf = S("dgf", [128, 2, 32])
            allL = ["LgAll"]
            K = "route"
            lg_keys = [("Lg", j2) for j2 in range(32)]
            gl = Lg[:, :, 0:4]
            em.op("dve", lambda: V.tensor_reduce(out=gmax[:], in_=gl, axis=AX.X, op=ALU.max), reads=lg_keys, writes=["gmax"])
            tt("dve", goh[:], gl, gmax[:].unsqueeze(2).to_broadcast([128, 32, 4]), ALU.is_equal, lg_keys + ["gmax"], ["goh"])
            tt("dve", gd[:], gl, gmax[:].unsqueeze(2).to_broadcast([128, 32, 4]), ALU.subtract, lg_keys + ["gmax"], ["gd"])
            act(gd[:], gd[:], AF.Exp, ["gd"], ["gd"])
            em.op("dve", lambda: V.tensor_reduce(out=gsum[:], in_=gd[:], axis=AX.X, op=ALU.add), reads=["gd"], writes=["gsum"])
            em.op("dve", lambda: V.reciprocal(out=pg[:], in_=gsum[:]), reads=["gsum"], writes=["pg"])
            el4 = Lg[:, :, 4:36].rearrange("p a (g i) -> p a g i", i=8)
            tt("dve", sel4[:], el4, goh[:].unsqueeze(3).to_broadcast([128, 32, 4, 8]), ALU.mult, lg_keys + ["goh"], ["sel4"])
            tt("dve", esel[:], sel4[:, :, 0, :], sel4[:, :, 1, :], ALU.add, ["sel4"], ["esel"])
            tt("dve", esel[:], esel[:], sel4[:, :, 2, :], ALU.add, ["sel4", "esel"], ["esel"])
            tt("dve", esel[:], esel[:], sel4[:, :, 3, :], ALU.add, ["sel4", "esel"], ["esel"])
            em.op("dve", lambda: V.tensor_reduce(out=m1[:], in_=esel[:], axis=AX.X, op=ALU.max), reads=["esel"], writes=["m1"])
            tt("dve", oh1[:], esel[:], m1[:].unsqueeze(2).to_broadcast([128, 32, 8]), ALU.is_equal, ["esel", "m1"], ["oh1"])
            stt("dve", e2[:], oh1[:], -1e30, esel[:], ALU.mult, ALU.add, ["oh1", "esel"], ["e2"])
            em.op("dve", lambda: V.tensor_reduce(out=m2[:], in_=e2[:], axis=AX.X, op=ALU.max), reads=["e2"], writes=["m2"])
            tt("dve", oh2[:], e2[:], m2[:].unsqueeze(2).to_broadcast([128, 32, 8]), ALU.is_equal, ["e2", "m2"], ["oh2"])
            tt("dve", dd[:], m2[:], m1[:], ALU.subtract, ["m1", "m2"], ["dd"])
            act(dd[:], dd[:], AF.Exp, ["dd"], ["dd"])
            ts("dve", w1[:], dd[:], 1.0, None, ALU.add, None, ["dd"], ["w1"])
            em.op("dve", lambda: V.reciprocal(out=w1[:], in_=w1[:]), reads=["w1"], writes=["w1"])
            tt("dve", w2[:], dd[:], w1[:], ALU.mult, ["dd", "w1"], ["w2"])
            tt("dve", w1[:], w1[:], pg[:], ALU.mult, ["w1", "pg"], ["w1"])
            tt("dve", w2[:], w2[:], pg[:], ALU.mult, ["w2", "pg"], ["w2"])
            gb = goh[:].unsqueeze(3).to_broadcast([128, 32, 4, 8])
            tt("dve", A1[:], gb, oh1[:].unsqueeze(2).to_broadcast([128, 32, 4, 8]), ALU.mult, ["goh", "oh1"], ["A1"])
            tt("dve", A2[:], gb, oh2[:].unsqueeze(2).to_broadcast([128, 32, 4, 8]), ALU.mult, ["goh", "oh2"], ["A2"])
            A1f = A1[:].rearrange("p a g i -> p a (g i)")
            A2f = A2[:].rearrange("p a g i -> p a (g i)")
            tt("dve", Mb[:].rearrange("p (a e) -> p a e", e=32), A1f, A2f, ALU.add, ["A1", "A2"], ["Mb"])
            for h_ in range(2):
                ps, pk = ps_next()
                mm(ps[:], tri[:], Mb[:, h_ * 512:(h_ + 1) * 512], True, True, ["tri", "Mb"], pk)
                cp("act", pin[:, h_ * 16:(h_ + 1) * 16, :], ps[:].rearrange("p (a e) -> p a e", e=32), [pk], ["pin"])
                ps, pk = ps_next()
                mm(ps[:], ones[:], Mb[:, h_ * 512:(h_ + 1) * 512], True, True, ["ones", "Mb"], pk)
                cp("act", cnt[:, h_ * 16:(h_ + 1) * 16, :], ps[:].rearrange("p (a e) -> p a e", e=32), [pk], ["cnt"])
            em.op("dve", lambda: V.memset(base[:, 0, :], 0.0), writes=["base"])
            for j2 in range(1, 32):
                tt("dve", base[:, j2, :], base[:, j2 - 1, :], cnt[:, j2 - 1, :], ALU.add, ["base", "cnt"], ["base"])
            tt("dve", slot[:], pin[:], base[:], ALU.add, ["pin", "base"], ["slot"])
            for k, Af in enumerate([A1f, A2f]):
                tt("dve", tmp3[:], Af, slot[:], ALU.mult, ["A1", "A2", "slot"], ["tmp3"])
                em.op("dve", lambda k=k: V.tensor_reduce(out=pk_[:, k, :], in_=tmp3[:], axis=AX.X, op=ALU.add), reads=["tmp3"], writes=["pk_"])
                tt("dve", tmp3[:], Af, ecb[:].unsqueeze(1).to_broadcast([128, 32, 32]), ALU.mult, ["A1", "A2", "ecb"], ["tmp3"])
                em.op("dve", lambda k=k: V.tensor_reduce(out=dk[:, k, :], in_=tmp3[:], axis=AX.X, op=ALU.add), reads=["tmp3"], writes=["dk"])
            tt("dve", dk[:], dk[:], pk_[:], ALU.add, ["dk", "pk_"], ["dk"])
            ts("dve", ok[:], pk_[:], float(CAP), None, ALU.is_lt, None, ["pk_"], ["ok"])
            tt("dve", dgf[:], dk[:], ok[:], ALU.mult, ["dk", "ok"], ["dgf"])
            tt("dve", dsf[:], dk[:], trb[:].unsqueeze(1).to_broadcast([128, 2, 32]), ALU.subtract, ["dk", "trb"], ["dsf"])
            tt("dve", dsf[:], dsf[:], ok[:], ALU.mult, ["dsf", "ok"], ["dsf"])
            tt("dve", dsf[:], dsf[:], trb[:].unsqueeze(1).to_broadcast([128, 2, 32]), ALU.add, ["dsf", "trb"], ["dsf"])
            tt("dve", w1[:], w1[:], ok[:, 0, :], ALU.mult, ["w1", "ok"], ["w1"])
            tt("dve", w2[:], w2[:], ok[:, 1, :], ALU.mult, ["w2", "ok"], ["w2"])
            cp("dve", ds_i[:], dsf[:], ["dsf"], ["ds_i"])
            cp("dve", dg_i[:], dgf[:], ["dgf"], ["dg_i"])
            if debug:
                dma("sp", dbgL[:, 32 * 36:32 * 36 + 64], dsf[:].rearrange("p a b -> p (a b)"), ["dsf"], ["dbgL2"])
                dma("sp", dbgL[:, 32 * 36 + 64:32 * 36 + 96], w1[:], ["w1"], ["dbgL3"])
                dma("sp", dbgL[:, 32 * 36 + 96:32 * 36 + 128], w2[:], ["w2"], ["dbgL4"])
            for j2 in range(32):
                for k in range(2):
                    em.dma("pool", lambda j2=j2, k=k: G.indirect_dma_start(
                        out=xg, out_offset=bass.IndirectOffsetOnAxis(ap=ds_i[:, k, j2:j2 + 1], axis=0),
                        in_=h2tok[:, j2, :], in_offset=None), reads=["ds_i", ("h2tok", j2)],
                        writes=[("xgs", j2, k)])
            em.barrier()
        ph_tok.close()
        if stop_after == "E":
            return nc

        with ExitStack() as ph:
            NB = 2
            Wg = [sbuf(ph, "Wg%d" % i, [128, 8, 512], BF16) for i in range(NB)]
            Wu = [sbuf(ph, "Wu%d" % i, [128, 8, 512], BF16) for i in range(NB)]
            Wd = [sbuf(ph, "Wd%d" % i, [128, 4, D], BF16) for i in range(NB)]
            xgt = [sbuf(ph, "xgt%d" % i, [128, 3, D], BF16) for i in range(2)]
            xgT = [sbuf(ph, "xgT%d" % i, [128, 8, CAP], BF16) for i in range(2)]
            sg = [sbuf(ph, "sg%d" % i, [128, CAP], F32) for i in range(2)]
            hT = [sbuf(ph, "hT%d" % i, [128, 4, CAP], BF16) for i in range(2)]
            yt = [sbuf(ph, "yt%d" % i, [128, D], F32) for i in range(4)]
            yt_rr = 0
            NST = (CAP + 127) // 128
            NSTG = 5
            stg = [sbuf(ph, "stg%d" % i, [128, 4096], F32) for i in range(NSTG)]
            stg_rr = [0]

            def load_expert(e):
                b = e % NB
                b2 = e % 2
                items = [
                    (T["w_gate"][e].rearrange("(p k) n -> p k n", k=8), Wg[b], ("Wg", b), 8, 512, "act"),
                    (T["w_up"][e].rearrange("(p k) n -> p k n", k=8), Wu[b], ("Wu", b), 8, 512, "dve"),
                    (T["w_down"][e].rearrange("(k p) n -> p k n", p=128), Wd[b], ("Wd", b), 4, 1024, "pool"),
                ]
                for src, dst, key, nk, nn, ce in items:
                    i = stg_rr[0] % NSTG
                    stg_rr[0] += 1
                    sv = stg[i][:].rearrange("p (k n) -> p k n", k=nk)
                    dma("sp", sv, src, [], [("stg", i)])
                    hk_ = nk // 2
                    for h_ in range(2):
                        wk = [(key[0], key[1], 0), (key[0], key[1], 4 if key[0] != "Wd" else 2)] if h_ == 0 else []
                        ce_ = ce if ce != "pool" else ("dve" if h_ == 0 else "act")
                        cp(ce_, dst[:, h_ * hk_:(h_ + 1) * hk_, :], sv[:, h_ * hk_:(h_ + 1) * hk_, :], [("stg", i)],
                           [(key[0], key[1], "h%d" % h_)] + wk)
                for s_ in range((CAP + 127) // 128):
                    w_ = min(128, CAP - s_ * 128)
                    dma("pool", xgt[b2][0:w_, s_, :], xg[e * CAP + s_ * 128:e * CAP + s_ * 128 + w_, :], [], [("xgt", b2, s_)])

            def f_transposes(e):
                b2 = e % 2
                for s in range(3):
                    if s * 128 >= CAP:
                        break
                    w_ = min(128, CAP - s * 128)
                    ps, pk = ps_next()
                    psb = ps[:].bitcast(BF16)
                    for kc in range(8):
                        tr(psb[:, kc * 128:kc * 128 + w_], xgt[b2][0:w_, s, kc::8], [("xgt", b2, s)], pk, kdim=w_)
                    cp("act" if s % 2 else "dve", xgT[b2][:, :, s * 128:s * 128 + w_],
                       psb.rearrange("p (a b) -> p a b", b=128)[:, :, 0:w_], [pk], [("xgT", b2, s)])

            load_expert(0)
            f_transposes(0)
            for e in range(32):
                b = e % NB
                b2 = e % 2
                if e + 1 < 32:
                    load_expert(e + 1)
                xk = [("xgT", b2, s) for s in range(NST)]
                for mc in range(4):
                    psG, kG = ps_next()
                    psU, kU = ps_next()
                    for kc in range(8):
                        mm(psG[:, 0:CAP], Wg[b][:, kc, mc * 128:(mc + 1) * 128], xgT[b2][:, kc, :], kc == 0, kc == 7, xk + [("Wg", b, "h0"), ("Wg", b, "h1"), ("Wg", b, 0), ("Wg", b, 4)], kG)
                    for kc in range(8):
                        mm(psU[:, 0:CAP], Wu[b][:, kc, mc * 128:(mc + 1) * 128], xgT[b2][:, kc, :], kc == 0, kc == 7, xk + [("Wu", b, "h0"), ("Wu", b, "h1"), ("Wu", b, 0), ("Wu", b, 4)], kU)
                    sb_ = mc % 2
                    act(sg[sb_][:], psG[:, 0:CAP], AF.Silu, [kG], [("sg", sb_)])
                    tt("dve", hT[b2][:, mc, :], sg[sb_][:], psU[:, 0:CAP], ALU.mult, [("sg", sb_), kU], [("hT", b2, mc)])
                if e + 1 < 32:
                    f_transposes(e + 1)
                hk = [("hT", b2, mc) for mc in range(4)]
                for s in range(NST):
                    w_ = min(128, CAP - s * 128)
                    yb = yt_rr % 4
                    yt_rr += 1
                    for half in range(2):
                        ps, pk = ps_next()
                        for mc in range(4):
                            mm(ps[0:w_, :], hT[b2][:, mc, s * 128:s * 128 + w_], Wd[b][:, mc, half * 512:(half + 1) * 512], mc == 0, mc == 3,
                               hk + [("Wd", b, "h0"), ("Wd", b, "h1"), ("Wd", b, 0), ("Wd", b, 2)], pk)
                        cp("act" if half else "dve", yt[yb][0:w_, half * 512:(half + 1) * 512], ps[0:w_, :], [pk], [("yt", yb, half)])
                    dma("pool", Ybuf[e * CAP + s * 128:e * CAP + s * 128 + w_, :], yt[yb][0:w_, :], [("yt", yb, 0), ("yt", yb, 1)], [("Ybuf", e, s)])
            em.barrier()
        if stop_after == "F":
            return nc

        with ExitStack() as ph:
            gfb = sbuf(ph, "gfb", [128, D], F32)
            dma("sp", gfb[:], T["gfb"], [], ["gfb"])
            NG = 4
            Y1 = [sbuf(ph, "Y1_%d" % i, [128, D], F32) for i in range(NG)]
            Y2 = [sbuf(ph, "Y2_%d" % i, [128, D], F32) for i in range(NG)]
            xt = [sbuf(ph, "xtG%d" % i, [128, D], F32) for i in range(NG)]
            junk = sbuf(ph, "junkG", [128, D], BF16)
            ss = sbuf(ph, "ssG", [128, 32], F32)
            rs = sbuf(ph, "rsG", [128, 32], F32)
            em.op("dve", lambda: V.memset(ss[:], 0.0), writes=["ssG"])

            def g_load(j2):
                b = j2 % NG
                em.dma("pool", lambda: G.indirect_dma_start(out=Y1[b][:], out_offset=None, in_=Ybuf,
                                                            in_offset=bass.IndirectOffsetOnAxis(ap=dg_i[:, 0, j2:j2 + 1], axis=0)),
                       reads=["dg_i"], writes=[("Y1", b)])
                em.dma("pool", lambda: G.indirect_dma_start(out=Y2[b][:], out_offset=None, in_=Ybuf,
                                                            in_offset=bass.IndirectOffsetOnAxis(ap=dg_i[:, 1, j2:j2 + 1], axis=0)),
                       reads=["dg_i"], writes=[("Y2", b)])
                dma("sp", xt[b][:], x1buf[j2 * 128:(j2 + 1) * 128, :], [], [("xtG", b)])

            for j2 in range(min(3, 32)):
                g_load(j2)
            for j2 in range(32):
                b = j2 % NG
                if j2 + 3 < 32:
                    g_load(j2 + 3)
                stt("dve", xt[b][:], Y1[b][:], w1[:, j2:j2 + 1], xt[b][:], ALU.mult, ALU.add, [("Y1", b), ("xtG", b), "w1"], [("xtG", b)])
                stt("dve", xt[b][:], Y2[b][:], w2[:, j2:j2 + 1], xt[b][:], ALU.mult, ALU.add, [("Y2", b), ("xtG", b), "w2"], [("xtG", b)])
                act(junk[:], xt[b][:], AF.Square, [("xtG", b), "ssG"], ["junkG", ("ssG", j2)], accum_out=ss[:, j2:j2 + 1])
                act(rs[:, j2:j2 + 1], ss[:, j2:j2 + 1], AF.Sqrt, [("ssG", j2), "epst"], [("rsG", j2)], scale=1.0 / D, bias=epst[:])
                em.op("dve", lambda: V.reciprocal(out=rs[:, j2:j2 + 1], in_=rs[:, j2:j2 + 1]), reads=[("rsG", j2)], writes=[("rsG", j2)])
                stt("dve", Y1[b][:], xt[b][:], rs[:, j2:j2 + 1], gfb[:], ALU.mult, ALU.mult, [("xtG", b), ("rsG", j2), "gfb"], [("Y1", b)])
                final_events.append(dma("sp", out_d[j2 * 128:(j2 + 1) * 128, :], Y1[b][:], [("Y1", b)], [("out", j2)]))
            em.barrier()
    return nc


def prep_inputs(inp):
    f32 = np.float32
    g = lambda k: np.asarray(inp[k], dtype=f32)
    rep = lambda v: np.ascontiguousarray(np.tile(v.reshape(1, -1), (128, 1)))
    sh = {}
    sh["w_in"] = np.ascontiguousarray(g("w_in")[0])
    sh["g1b"] = rep(g("norm1_g")[0])
    sh["g2b"] = rep(g("norm2_g")[0])
    sh["gfb"] = rep(g("final_g"))
    cw = g("conv_w")[0]
    sh["cwp"] = np.ascontiguousarray(cw.reshape(3, 12, 128).transpose(2, 1, 0)).reshape(128, 36)
    sh["cbp"] = np.ascontiguousarray(g("conv_b")[0].reshape(12, 128).T)
    sh["fbp"] = np.ascontiguousarray(g("f_bias")[0].reshape(4, 128).T)
    sh["mgp"] = np.ascontiguousarray(g("mix_g")[0].reshape(8, 128).T)
    sh["w_out"] = np.ascontiguousarray(g("w_out")[0])
    wr = np.concatenate([g("w_group")[0], g("w_router")[0].transpose(1, 0, 2).reshape(D, 32)], axis=1)
    sh["wr"] = np.ascontiguousarray(wr)
    sh["brb"] = rep(np.concatenate([g("b_group")[0], g("b_router")[0].reshape(32)]))
    sh["w_gate"] = np.ascontiguousarray(g("w_gate")[0])
    sh["w_up"] = np.ascontiguousarray(g("w_up")[0])
    sh["w_down"] = np.ascontiguousarray(g("w_down")[0])
    fwin = g("f_w_in")[0]
    w1 = np.zeros((66, 128), f32)
    w1[0:33, 0:64] = fwin
    w1[33:66, 64:128] = fwin
    sh["fwin2"] = w1
    fm = g("f_w_mid")[0]
    wm = np.zeros((128, 2, 128), f32)
    for l in range(2):
        wm[0:64, l, 0:64] = fm[l]
        wm[64:128, l, 64:128] = fm[l]
    sh["fwmid2"] = wm.reshape(128, 256)
    fq = g("f_freq")[0].T
    sh["fq"] = np.ascontiguousarray(np.concatenate([fq, fq], 0))
    fb = np.stack([g("f_b_in")[0], g("f_b_mid")[0][0], g("f_b_mid")[0][1]], 1)
    sh["fbb"] = np.ascontiguousarray(np.concatenate([fb, fb], 0))
    fo = g("f_w_out")[0]
    sh["fwout2"] = np.ascontiguousarray(np.concatenate([fo, fo], 0))
    return sh


_CACHE = {}


def kernel(**inputs):
    if "nc" not in _CACHE:
        _CACHE["nc"] = build_nc()
        _CACHE["consts"] = make_consts()
    nc = _CACHE["nc"]
    shared = prep_inputs(inputs)
    shared.update(_CACHE["consts"])
    x = np.asarray(inputs["x"], dtype=np.float32)
    in_maps = []
    for c in range(8):
        m = dict(shared)
        m["x"] = np.ascontiguousarray(x[c])
        in_maps.append(m)
    res = run_bass_kernel_spmd(nc, in_maps, core_ids=list(range(8)))
    out = np.stack([np.asarray(res.results[c]["out"], dtype=np.float32) for c in range(8)], axis=0)
    return out
```

```python
import numpy as np
import ml_dtypes
from contextlib import ExitStack
import concourse.bass as bass
import concourse.mybir as mybir
from concourse.bass_utils import run_bass_kernel_spmd

F32 = mybir.dt.float32
BF16 = mybir.dt.bfloat16
I32 = mybir.dt.int32
AF = mybir.ActivationFunctionType
ALU = mybir.AluOpType
AX = mybir.AxisListType
bf = ml_dtypes.bfloat16

L = 4096
NF = 8192
D = 1024
CAP = 320
NS = 32 * CAP
NROWS = NS + 128
EPS = 1e-6
TWO_PI = float(2 * np.pi)


class Emit:
    def __init__(self, nc, stack, n_dma_sems=48):
        self.nc = nc
        self.eng = {"pe": nc.tensor, "act": nc.scalar, "dve": nc.vector, "pool": nc.gpsimd, "sp": nc.sync}
        self.sem = {}
        self.cnt = {}
        for k in self.eng:
            self.sem[k] = stack.enter_context(nc.semaphore("s_" + k))
            self.cnt[k] = 0
        self.dma_sems = [stack.enter_context(nc.semaphore("d%d" % i)) for i in range(n_dma_sems)]
        self.dma_cnt = [0] * n_dma_sems
        n_sw = 16
        self.dma_pool = {"hw": list(range(n_sw, n_dma_sems)), "sw": list(range(n_sw))}
        self.dma_rr = {"hw": 0, "sw": 0}
        self.seen = {k: {} for k in self.eng}
        self.lastw = {}
        self.reads = {}
        self.n_wait = 0
        self.n_ins = 0

    def _wait(self, e, ev):
        sem, val, src = ev
        sid = id(sem)
        if self.seen[e].get(sid, 0) >= val:
            return
        self.seen[e][sid] = val
        self.eng[e].wait_ge(sem, val)
        self.n_wait += 1

    def _deps(self, e, reads, writes):
        evs = []
        for r in reads:
            w = self.lastw.get(r)
            if w is not None and not (w[2] == e and e == "pe"):
                evs.append(w)
        same_ok = e in ("pe",)
        for wkey in writes:
            w = self.lastw.get(wkey)
            if w is not None and (w[2] != e or not same_ok):
                evs.append(w)
            for ev in self.reads.get(wkey, {}).values():
                if ev[2] != e or not same_ok:
                    evs.append(ev)
        return evs

    def _commit(self, ev, reads, writes):
        for r in reads:
            self.reads.setdefault(r, {})[(ev[2], id(ev[0]))] = ev
        for w in writes:
            self.lastw[w] = ev
            self.reads[w] = {}

    def op(self, e, fn, reads=(), writes=()):
        for ev in self._deps(e, reads, writes):
            self._wait(e, ev)
        ins = fn()
        self.cnt[e] += 1
        ins.then_inc(self.sem[e], 1)
        ev = (self.sem[e], self.cnt[e], e)
        self._commit(ev, reads, writes)
        self.n_ins += 1
        return ev

    def dma(self, q, fn, reads=(), writes=()):
        for ev in self._deps(q, reads, writes):
            self._wait(q, ev)
        kind = "sw" if q == "pool" else "hw"
        lst = self.dma_pool[kind]
        i = lst[self.dma_rr[kind]]
        self.dma_rr[kind] = (self.dma_rr[kind] + 1) % len(lst)
        sem = self.dma_sems[i]
        if self.dma_cnt[i] > 0:
            self._wait(q, (sem, 16 * self.dma_cnt[i], "dma"))
        ins = fn()
        self.dma_cnt[i] += 1
        ins.then_inc(sem, 16)
        ev = (sem, 16 * self.dma_cnt[i], "dma%d" % i)
        self._commit(ev, reads, writes)
        self.n_ins += 1
        return ev

    def barrier(self):
        for e in self.eng:
            for e2 in self.eng:
                if e2 != e and self.cnt[e2] > 0:
                    self._wait(e, (self.sem[e2], self.cnt[e2], e2))
            for i, s in enumerate(self.dma_sems):
                if self.dma_cnt[i] > 0:
                    self._wait(e, (s, 16 * self.dma_cnt[i], "dma"))
        self.lastw = {}
        self.reads = {}


def _kron4(M):
    return np.kron(M, np.eye(4))


def make_consts():
    c = {}
    p = np.arange(256)[:, None].astype(np.float64)
    k1 = np.arange(128)[None, :].astype(np.float64)
    fa = np.zeros((2, 32, 2, 128, 128))
    gt = np.zeros((32, 2, 128, 128))
    for j in range(32):
        th = 2 * np.pi * (p * (k1 + 0.5) / 256.0 + j * (k1 + 0.5) / NF)
        for pt in range(2):
            sgn = 1.0 if pt == 0 else -1.0
            fa[pt, j, 0] = sgn * np.cos(th[pt * 128:(pt + 1) * 128])
            fa[pt, j, 1] = -sgn * np.sin(th[pt * 128:(pt + 1) * 128])
        gt[j, 0] = (2.0 / NF) * np.cos(th[:128]).T
        gt[j, 1] = -(2.0 / NF) * np.sin(th[:128]).T
    c["fa"] = np.ascontiguousarray(fa.transpose(3, 0, 1, 2, 4)).reshape(128, 2 * 32 * 2 * 128).astype(bf)
    c["gt"] = np.ascontiguousarray(gt.transpose(2, 0, 1, 3)).reshape(128, 32 * 2 * 128).astype(bf)
    jj = np.arange(32)[:, None].astype(np.float64)
    kk = np.arange(32)[None, :].astype(np.float64)
    ph = 2 * np.pi * jj * kk / 32.0
    Tc = _kron4(np.cos(ph))
    Ts = _kron4(np.sin(ph))
    c["tct"] = np.stack([Tc, Ts, -Ts], 1).reshape(128, 3 * 128).astype(bf)
    c["tcf"] = np.stack([Tc / 512.0, Ts / 512.0], 1).reshape(128, 2 * 128).astype(bf)
    pa = np.arange(128)[:, None].astype(np.float64)
    ka = np.arange(128)[None, :].astype(np.float64)
    et = np.zeros((32, 3, 128, 128))
    for j in range(32):
        th = 2 * np.pi * (pa * ka / 128.0 + j * ka / 4096.0)
        et[j, 0] = np.cos(th)
        et[j, 1] = np.sin(th)
        et[j, 2] = -np.sin(th)
    c["et"] = np.ascontiguousarray(et.transpose(2, 0, 1, 3)).reshape(128, 32 * 3 * 128).astype(bf)
    cc = np.arange(64)[:, None].astype(np.float64)
    c2 = np.arange(64)[None, :].astype(np.float64)
    C64 = np.cos(2 * np.pi * cc * c2 / 64.0)
    S64 = np.sin(2 * np.pi * cc * c2 / 64.0)
    c["bdt"] = np.stack([np.kron(np.eye(2), C64), np.kron(np.eye(2), -S64)], 1).reshape(128, 256).astype(bf)
    c["ident"] = np.eye(128).astype(bf)
    c["blk"] = (np.kron(np.eye(2), np.ones((64, 64))) / 64.0).astype(bf)
    c["tri"] = np.triu(np.ones((128, 128)), 1).astype(bf)
    c["ones"] = np.ones((128, 128)).astype(bf)
    c["onesf"] = np.ones((128, 128), np.float32)
    c["ecb"] = np.tile((np.arange(32) * CAP).astype(np.float32)[None, :], (128, 1))
    c["trb"] = np.tile((NS + np.arange(128)).astype(np.float32)[:, None], (1, 32))
    n = np.arange(NF)
    m = np.where(n < L, n, NF - n).astype(np.float64)
    m[L] = 0
    t = (m / (L - 1)).astype(np.float32)
    w = (2.0 * np.pi / L) * m
    f = np.linspace(1e-4, 15, 16)[None, :]
    emb = np.concatenate([t[:, None], np.cos(f * w[:, None]), -np.sin(f * w[:, None])], -1).astype(np.float32)
    c["emb2"] = np.concatenate([emb[:L].T, emb[L:].T], 0).astype(np.float32)
    tn = -t.astype(np.float32)
    tn[L] = -1e4
    c["tneg"] = np.ascontiguousarray(tn.reshape(2, 128, 32).transpose(1, 0, 2)).reshape(128, 64)
    max_decay = np.log(1e-2) / 0.3
    min_decay = np.log(1e-2) / 1.5
    deltas = np.abs(np.linspace(min_decay, max_decay, 512)).astype(np.float32)
    c["deltab"] = np.tile(deltas[None, :], (128, 1))
    return c


CONST_SPECS = [
    ("fa", [128, 16384], BF16), ("gt", [128, 8192], BF16), ("tct", [128, 384], BF16), ("tcf", [128, 256], BF16),
    ("et", [128, 12288], BF16), ("bdt", [128, 256], BF16), ("ident", [128, 128], BF16), ("blk", [128, 128], BF16),
    ("tri", [128, 128], BF16), ("ones", [128, 128], BF16), ("onesf", [128, 128], F32), ("ecb", [128, 32], F32),
    ("trb", [128, 32], F32), ("emb2", [66, 4096], F32), ("tneg", [128, 64], F32), ("deltab", [128, 512], F32),
]
IN_SPECS = [
    ("x", [L, D], F32), ("w_in", [D, 2048], F32), ("g1b", [128, D], F32), ("g2b", [128, D], F32), ("gfb", [128, D], F32),
    ("cwp", [128, 36], F32), ("cbp", [128, 12], F32), ("fbp", [128, 4], F32), ("mgp", [128, 8], F32),
    ("w_out", [D, D], F32), ("wr", [D, 36], F32), ("brb", [128, 36], F32),
    ("w_gate", [32, D, 512], F32), ("w_up", [32, D, 512], F32), ("w_down", [32, 512, D], F32),
    ("fwin2", [66, 128], F32), ("fwmid2", [128, 256], F32), ("fq", [128, 3], F32), ("fbb", [128, 3], F32),
    ("fwout2", [128, 1024], F32),
]


def build_nc(stop_after=None, debug=False):
    nc = bass.Bass("TRN2", target_bir_lowering=False)
    T = {}
    for name, shape, dt in IN_SPECS + CONST_SPECS:
        T[name] = nc.dram_tensor(name, shape, dt, kind="ExternalInput").ap()
    out_d = nc.dram_tensor("out", [L, D], F32, kind="ExternalOutput").ap()
    skind = "ExternalOutput" if debug else "Internal"
    Pbuf = nc.dram_tensor("Pbuf", [1536, L], BF16, kind=skind).ap()
    wbuf = [nc.dram_tensor("wbuf%d" % i, [L, 512], BF16, kind=skind).ap() for i in range(2)]
    Hbuf = nc.dram_tensor("Hbuf", [4, 128, 8192], BF16, kind=skind).ap()
    yTbuf = nc.dram_tensor("yTbuf", [D, L], BF16, kind=skind).ap()
    x1buf = nc.dram_tensor("x1buf", [L, D], F32, kind=skind).ap()
    xg = nc.dram_tensor("xg", [NROWS, D], BF16, kind=skind).ap()
    Ybuf = nc.dram_tensor("Ybuf", [NROWS, D], F32, kind=skind).ap()
    dbgL = nc.dram_tensor("dbgL", [128, 32 * 40], F32, kind=skind).ap()

    with ExitStack() as st:
        em = Emit(nc, st)
        V, A, G, PE = nc.vector, nc.scalar, nc.gpsimd, nc.tensor
        ENG = {"dve": V, "act": A, "pool": G}

        def sbuf(stack, name, shape, dt):
            return stack.enter_context(nc.sbuf_tensor("sb_" + name, shape, dt))

        PS = [st.enter_context(nc.psum_tensor("ps%d" % i, [128, 512], F32)) for i in range(8)]
        ps_rr = [0]

        def ps_next():
            i = ps_rr[0]
            ps_rr[0] = (i + 1) % 8
            return PS[i], "ps%d" % i

        def mm(out, lhsT, rhs, start, stop, reads, pk):
            return em.op("pe", lambda: PE.matmul(out, lhsT=lhsT, rhs=rhs, start=start, stop=stop), reads=reads, writes=[pk])

        def tr(out, in_, reads, pk, kdim=128):
            idn = ident[:] if kdim == 128 else ident[0:kdim, 0:kdim]
            return em.op("pe", lambda: PE.transpose(out, in_, idn), reads=list(reads) + ["ident"], writes=[pk])

        def cp(e, out, in_, reads, writes):
            if e == "act":
                return em.op("act", lambda: A.copy(out=out, in_=in_), reads=reads, writes=writes)
            return em.op(e, lambda: ENG[e].tensor_copy(out=out, in_=in_), reads=reads, writes=writes)

        def tt(e, out, in0, in1, op, reads, writes):
            return em.op(e, lambda: ENG[e].tensor_tensor(out=out, in0=in0, in1=in1, op=op), reads=reads, writes=writes)

        def ts(e, out, in0, s1, s2, op0, op1, reads, writes):
            if op1 is None:
                return em.op(e, lambda: ENG[e].tensor_scalar(out=out, in0=in0, scalar1=s1, scalar2=None, op0=op0), reads=reads, writes=writes)
            return em.op(e, lambda: ENG[e].tensor_scalar(out=out, in0=in0, scalar1=s1, scalar2=s2, op0=op0, op1=op1), reads=reads, writes=writes)

        def stt(e, out, in0, scalar, in1, op0, op1, reads, writes):
            return em.op(e, lambda: ENG[e].scalar_tensor_tensor(out=out, in0=in0, scalar=scalar, in1=in1, op0=op0, op1=op1), reads=reads, writes=writes)

        def act(out, in_, func, reads, writes, **kw):
            return em.op("act", lambda: A.activation(out=out, in_=in_, func=func, **kw), reads=reads, writes=writes)

        def dma(q, out, in_, reads, writes):
            e = {"sp": nc.sync, "act": A, "pool": G}[q]
            return em.dma(q, lambda: e.dma_start(out=out, in_=in_), reads=reads, writes=writes)

        final_events = []

        ident = sbuf(st, "ident", [128, 128], BF16)
        blk = sbuf(st, "blk", [128, 128], BF16)
        tct = sbuf(st, "tct", [128, 3, 128], BF16)
        epst = sbuf(st, "epst", [128, 1], F32)
        rinvP = sbuf(st, "rinvP", [128, 4], F32)
        dma("sp", ident[:], T["ident"], [], ["ident"])
        dma("sp", blk[:], T["blk"], [], ["blk"])
        dma("sp", tct[:].rearrange("p a b -> p (a b)"), T["tct"], [], ["tct"])
        em.op("dve", lambda: V.memset(epst[:], EPS), writes=["epst"])

        ztp = sbuf(st, "ztp", [128, 2, D], BF16)
        em.op("pool", lambda: G.memset(ztp[:], 0.0), writes=["ztp"])
        zf_chunks = [(r0, min(256, NROWS - r0)) for r0 in range(0, NROWS, 256)]

        def zero_fill_some(n):
            for _ in range(n):
                if zf_chunks:
                    r0, nr = zf_chunks.pop(0)
                    dma("sp", xg[r0:r0 + nr, :].rearrange("(s p) f -> p s f", p=128), ztp[:, 0:nr // 128, :], ["ztp"], [("xg", r0)])

        def fft_fwd(src, npt, fa_t, bufA, bufB, srck, sink):
            for j0 in range(0, 32, 4):
                for cs in range(2):
                    ps, pk = ps_next()
                    for jj in range(4):
                        j = j0 + jj
                        for pt in range(npt):
                            mm(ps[:, jj * 128:(jj + 1) * 128], fa_t[:, pt, j, cs, :], src[:, pt * 32 + j, :],
                               pt == 0, pt == npt - 1, (srck if isinstance(srck, list) else [srck]) + ["fa"], pk)
                    cp("act" if cs == 0 else "dve",
                       bufA[:].rearrange("p a g (j c) -> p a g j c", c=4)[:, cs, :, j0:j0 + 4, :].rearrange("p g j c -> p j g c"),
                       ps[:].rearrange("p (j g c) -> p j g c", j=4, g=32), [pk], [("bufA", cs, j0)])
            for cs in range(2):
                for g0 in range(0, 32, 8):
                    ps, pk = ps_next()
                    psb = ps[:].bitcast(BF16)
                    for gg in range(8):
                        g = g0 + gg
                        tr(psb[:, gg * 128:(gg + 1) * 128], bufA[:, cs, g, :],
                           [("bufA", cs, j0) for j0 in range(0, 32, 4)], pk)
                    cp("act" if cs == 0 else "dve", bufB[:, cs, g0:g0 + 8, :], psb.rearrange("p (a b) -> p a b", b=128),
                       [pk], [("bufB", cs, g0)])
            for ch in range(8):
                psR, kR = ps_next()
                psI, kI = ps_next()
                bBf = bufB[:].rearrange("p a b c -> p a (b c)")
                br = bBf[:, 0, ch * 512:(ch + 1) * 512]
                bi = bBf[:, 1, ch * 512:(ch + 1) * 512]
                rk = [("bufB", 0, (4 * ch) // 8 * 8), ("bufB", 1, (4 * ch) // 8 * 8), "tct"]
                mm(psR[:], tct[:, 0, :], br, True, False, rk, kR)
                mm(psR[:], tct[:, 1, :], bi, False, True, rk, kR)
                mm(psI[:], tct[:, 0, :], bi, True, False, rk, kI)
                mm(psI[:], tct[:, 2, :], br, False, True, rk, kI)
                sink(ch, psR, kR, psI, kI)

        with ExitStack() as ph:
            fa_t = sbuf(ph, "fa_t", [128, 2, 32, 2, 128], BF16)
            w1t = sbuf(ph, "w1t", [66, 128], F32)
            wmt = sbuf(ph, "wmt", [128, 2, 128], F32)
            fq = sbuf(ph, "fq", [128, 3], F32)
            fbb = sbuf(ph, "fbb", [128, 3], F32)
            fq2 = sbuf(ph, "fq2", [128, 3], F32)
            fqb2 = sbuf(ph, "fqb2", [128, 3], F32)
            fwo = sbuf(ph, "fwo", [128, 1024], BF16)
            onesf = sbuf(ph, "onesf", [128, 128], F32)
            tneg = sbuf(ph, "tneg", [128, 64], F32)
            deltab = sbuf(ph, "deltab", [128, 512], F32)
            hid = sbuf(ph, "hid", [128, L], BF16)
            dma("sp", fa_t[:].rearrange("p a b c d -> p (a b c d)"), T["fa"], [], ["fa"])
            dma("sp", w1t[:], T["fwin2"], [], ["w1t"])
            dma("sp", wmt[:].rearrange("p a b -> p (a b)"), T["fwmid2"], [], ["wmt"])
            dma("sp", fq[:], T["fq"], [], ["fq"])
            dma("sp", fbb[:], T["fbb"], [], ["fbb"])
            dma("pool", fwo[:], T["fwout2"], [], ["fwo"])
            dma("sp", onesf[:], T["onesf"], [], ["onesf"])
            dma("sp", tneg[:], T["tneg"], [], ["tneg"])
            dma("sp", deltab[:], T["deltab"], [], ["deltab"])
            ts("dve", fq2[:], fq[:], float(1.0 / 3.0), None, ALU.mult, None, ["fq"], ["fq2"])
            tt("dve", fqb2[:], fq2[:], fbb[:], ALU.mult, ["fq2", "fbb"], ["fqb2"])
            with ExitStack() as ph2:
                emb = sbuf(ph2, "emb", [66, L], F32)
                hA = sbuf(ph2, "hA", [128, L], F32)
                hB = sbuf(ph2, "hB", [128, L], F32)
                for ch in range(8):
                    dma("sp", emb[:, ch * 512:(ch + 1) * 512], T["emb2"][:, ch * 512:(ch + 1) * 512], [], [("emb", ch)])
                ub = [sbuf(ph2, "ub%d" % i, [128, 512], F32) for i in range(8)]
                rb = [sbuf(ph2, "rb%d" % i, [128, 512], F32) for i in range(8)]
                srcs = [(emb, "emb", w1t[:]), (hA, "hA", wmt[:, 0, :]), (hB, "hB", wmt[:, 1, :])]
                dsts = [(hA, "hA"), (hB, "hB"), (hid, "hid")]
                for l in range(3):
                    s_t, s_k, lhsT = srcs[l]
                    d_t, d_k = dsts[l]
                    pss = {}
                    for ch in range(8):
                        ps, pk = ps_next()
                        pss[ch] = (ps, pk)
                        mm(ps[:], lhsT, s_t[:, ch * 512:(ch + 1) * 512], True, True, [(s_k, ch), "w1t", "wmt"], pk)
                    for ch in range(8):
                        ps, pk = pss[ch]
                        act(ub[ch][:], ps[:], AF.Sin, [pk, "fq2", "fqb2"], [("ub", ch)], scale=fq2[:, l:l + 1], bias=fqb2[:, l:l + 1])
                    for ch in range(8):
                        tt("dve", rb[ch][:], ub[ch][:], ub[ch][:], ALU.mult, [("ub", ch)], [("rb", ch)])
                    for ch in range(8):
                        ts("dve", rb[ch][:], rb[ch][:], -4.0, 3.0, ALU.mult, ALU.add, [("rb", ch)], [("rb", ch)])
                    for ch in range(8):
                        tt("dve", d_t[:, ch * 512:(ch + 1) * 512], rb[ch][:], ub[ch][:], ALU.mult, [("rb", ch), ("ub", ch)], [(d_k, ch)])
                em.barrier()
            with ExitStack() as ph2:
                decs = [sbuf(ph2, "dec%d" % i, [128, 64, 128], BF16) for i in range(2)]
                kbs = [sbuf(ph2, "kb%d" % i, [128, 64, 128], BF16) for i in range(2)]
                acc = sbuf(ph2, "acc", [128, 128], F32)
                bufA = sbuf(ph2, "bufA", [128, 2, 32, 128], BF16)
                bufB = sbuf(ph2, "bufB", [128, 2, 32, 128], BF16)
                Hsb = sbuf(ph2, "Hsb", [128, 2, L], BF16)

                def dec_gen(hc):
                    for col in range(64):
                        act(decs[hc % 2][:, col, :], deltab[:, hc * 128:(hc + 1) * 128], AF.Exp, ["deltab", "tneg"], [("dec", hc % 2, col // 4)],
                            scale=tneg[:, col:col + 1])

                def out_layer(hc):
                    dec = decs[hc % 2]
                    kb = kbs[hc % 2]
                    for c0 in range(0, 64, 4):
                        ps, pk = ps_next()
                        for cc in range(4):
                            col = c0 + cc
                            pt, j = col // 32, col % 32
                            mm(ps[:, cc * 128:(cc + 1) * 128], hid[64 * pt:64 * pt + 64, j::32],
                               fwo[64 * pt:64 * pt + 64, pt * 512 + hc * 128:pt * 512 + (hc + 1) * 128], True, True,
                               [("hid", ch) for ch in range(8)] + ["fwo"], pk)
                        tt("dve", kb[:, c0:c0 + 4, :], ps[:].rearrange("p (a b) -> p a b", b=128), dec[:, c0:c0 + 4, :], ALU.mult,
                           [pk, ("dec", hc % 2, c0 // 4)], [("kb", hc % 2, c0 // 4)])

                def sinkH(ch, psR, kR, psI, kI):
                    cp("act", Hsb[:, 0, ch * 512:(ch + 1) * 512], psR[:], [kR], [("Hsb", ch)])
                    cp("dve", Hsb[:, 1, ch * 512:(ch + 1) * 512], psI[:], [kI], [("Hsb", ch)])

                dec_gen(0)
                out_layer(0)
                for hc in range(4):
                    kb = kbs[hc % 2]
                    kbk = [("kb", hc % 2, i) for i in range(16)]
                    if hc + 1 < 4:
                        dec_gen(hc + 1)
                        out_layer(hc + 1)
                    fft_fwd(kb, 2, fa_t, bufA, bufB, kbk, sinkH)
                    dma("sp", Hbuf[hc], Hsb[:].rearrange("p a b -> p (a b)"), [("Hsb", ch) for ch in range(8)], [("Hbuf", hc)])
                    em.op("dve", lambda: V.tensor_reduce(out=acc[:], in_=kb[:].rearrange("p n c -> p c n"), axis=AX.X, op=ALU.add,
                                                         apply_absolute_value=True), reads=kbk, writes=["acc"])
                    ps, pk = ps_next()
                    mm(ps[:, 0:1], acc[:], onesf[:, 0:1], True, True, ["onesf", "acc"], pk)
                    em.op("dve", lambda: V.reciprocal(out=rinvP[:, hc:hc + 1], in_=ps[:, 0:1]), reads=[pk], writes=[("rinvP", hc)])
                em.barrier()
        if stop_after == "F0":
            em.barrier()
            return nc

        with ExitStack() as ph:
            Wb = sbuf(ph, "Wb", [128, 8, 2048], BF16)
            Wf = sbuf(ph, "Wf", [128, 2, 8, 512], BF16)
            g1b = sbuf(ph, "g1b", [128, D], F32)
            for kc in range(8):
                for h2_ in range(2):
                    dma("pool", Wb[:, kc, h2_ * 1024:(h2_ + 1) * 1024], T["w_in"][kc * 128:(kc + 1) * 128, h2_ * 1024:(h2_ + 1) * 1024],
                        [], [("Wb", kc, h2_)])
            dma("sp", g1b[:], T["g1b"], [], ["g1b"])
            with ExitStack() as ph2:
                bdt = sbuf(ph2, "bdt", [128, 2, 128], BF16)
                WfT = sbuf(ph2, "WfT", [128, 4, D], BF16)
                dma("sp", bdt[:].rearrange("p a b -> p (a b)"), T["bdt"], [], ["bdt"])
                for n4 in range(4):
                    ps, pk = ps_next()
                    psb = ps[:].bitcast(BF16)
                    for kc in range(8):
                        tr(psb[:, kc * 128:(kc + 1) * 128], Wb[:, kc, 1536 + n4 * 128:1536 + (n4 + 1) * 128], [("Wb", kc, 0), ("Wb", kc, 1)], pk)
                    cp("act", WfT[:, n4, :], psb, [pk], [("WfT", n4)])
                for part in range(2):
                    for kc in range(8):
                        ps, pk = ps_next()
                        for n4 in range(4):
                            mm(ps[:, n4 * 128:(n4 + 1) * 128], WfT[:, n4, kc * 128:(kc + 1) * 128], bdt[:, part, :], True, True,
                               [("WfT", n4), "bdt"], pk)
                        cp("dve", Wf[:, part, kc, :], ps[:], [pk], [("Wf", part, kc)])
                em.barrier()
            xt = [sbuf(ph, "xt%d" % i, [128, D], F32) for i in range(4)]
            junk = sbuf(ph, "junk", [128, D], BF16)
            ss = sbuf(ph, "ss", [128, 32], F32)
            rs = sbuf(ph, "rs", [128, 32], F32)
            hb = [sbuf(ph, "hb%d" % i, [128, D], BF16) for i in range(2)]
            hTc = [sbuf(ph, "hTc%d" % i, [128, 8, 512], BF16) for i in range(2)]
            Pst = [sbuf(ph, "Pst%d" % i, [128, 12, 512], BF16) for i in range(2)]
            wst = [sbuf(ph, "wst%d" % i, [128, 2, 512], BF16) for i in range(2)]
            em.op("dve", lambda: V.memset(ss[:], 0.0), writes=["ss"])
            ev_cnt = [0]

            def a_load(j2):
                dma("sp", xt[j2 % 4][:], T["x"][j2 * 128:(j2 + 1) * 128, :], [], [("xt", j2 % 4)])
                zero_fill_some(2)

            def a_norm(tc, i):
                hb_ = tc % 2
                j2 = 4 * tc + i
                b = j2 % 2
                if j2 + 3 < 32:
                    a_load(j2 + 3)
                xb = j2 % 4
                act(junk[:], xt[xb][:], AF.Square, [("xt", xb), "ss"], ["junk", ("ss", j2)], accum_out=ss[:, j2:j2 + 1])
                act(rs[:, j2:j2 + 1], ss[:, j2:j2 + 1], AF.Sqrt, [("ss", j2), "epst"], [("rs", j2)], scale=1.0 / D, bias=epst[:])
                em.op("dve", lambda: V.reciprocal(out=rs[:, j2:j2 + 1], in_=rs[:, j2:j2 + 1]), reads=[("rs", j2)], writes=[("rs", j2)])
                stt("dve", hb[b][:], xt[xb][:], rs[:, j2:j2 + 1], g1b[:], ALU.mult, ALU.mult, [("xt", xb), ("rs", j2), "g1b"], [("hb", b)])
                ps, pk = ps_next()
                psb = ps[:].bitcast(BF16)
                for kc in range(8):
                    tr(psb[:, kc * 128:(kc + 1) * 128], hb[b][:, kc * 128:(kc + 1) * 128], [("hb", b)], pk)
                cp("act", hTc[hb_][:, :, i * 128:(i + 1) * 128], psb.rearrange("p (a b) -> p a b", b=128), [pk], [("hTc", hb_, i)])

            def a_mm(tc, part):
                hb_ = tc % 2
                hk = [("hTc", hb_, i) for i in range(4)]
                for cch in range(3 * part, 3 * part + 3):
                    ps, pk = ps_next()
                    for kc in range(8):
                        mm(ps[:], Wb[:, kc, cch * 128:(cch + 1) * 128], hTc[hb_][:, kc, :], kc == 0, kc == 7, hk + [("Wb", kc, 0), ("Wb", kc, 1)], pk)
                    ev_cnt[0] += 1
                    cp("act" if ev_cnt[0] % 2 else "dve", Pst[hb_][:, cch, :], ps[:], [pk], [("Pst", hb_, cch)])
                if part == 3:
                    dma("pool", Pbuf[:, tc * 512:(tc + 1) * 512].rearrange("(c p) t -> p c t", p=128), Pst[hb_][:],
                        [("Pst", hb_, c_) for c_ in range(12)], [("Pbuf", tc)])
                i = part
                j2 = 4 * tc + i
                wb_ = j2 % 2
                for fpart in range(2):
                    ps, pk = ps_next()
                    for kc in range(8):
                        mm(ps[:], hTc[hb_][:, kc, i * 128:(i + 1) * 128], Wf[:, fpart, kc, :], kc == 0, kc == 7,
                           [("hTc", hb_, i), ("Wf", fpart, kc)], pk)
                    ev_cnt[0] += 1
                    cp("act" if ev_cnt[0] % 2 else "dve", wst[wb_][:, fpart, :], ps[:], [pk], [("wst", wb_, fpart)])
                    dma("pool", wbuf[fpart][j2 * 128:(j2 + 1) * 128, :], wst[wb_][:, fpart, :], [("wst", wb_, fpart)], [("wbuf", fpart, j2)])

            for j2_ in range(3):
                a_load(j2_)
            for i in range(4):
                a_norm(0, i)
            for tc in range(8):
                for part in range(4):
                    if tc + 1 < 8:
                        a_norm(tc + 1, part)
                    a_mm(tc, part)
            em.barrier()
        if stop_after == "A":
            return nc

        def head_norm_wave(rs_, mg_ap, row0, rsqs, rstdb):
            for q in range(4):
                act(rsqs[q][:], rs_[q][0], AF.Square, [rs_[q][1]], [("rsq", q)])
            for q in range(4):
                for h_ in range(2):
                    ps, pk = ps_next()
                    mm(ps[:], blk[:], rsqs[q][:, h_ * 512:(h_ + 1) * 512], True, True, [("rsq", q), "blk"], pk)
                    act(rstdb[q][:, h_ * 512:(h_ + 1) * 512], ps[:], AF.Ln, [pk, "epst"], [("rstd_t", q, h_)], bias=epst[:], scale=1.0)
            for q in range(4):
                act(rstdb[q][:], rstdb[q][:], AF.Exp, [("rstd_t", q, 0), ("rstd_t", q, 1)], [("rstd_t", q, 0), ("rstd_t", q, 1)], scale=-0.5)
            for q in range(4):
                stt("dve", rsqs[q][:], rs_[q][0], mg_ap, rstdb[q][:], ALU.mult, ALU.mult,
                    [rs_[q][1], ("rstd_t", q, 0), ("rstd_t", q, 1), "mgp"], [("rsq", q)])
                dma("sp", yTbuf[row0:row0 + 128, q * 1024:(q + 1) * 1024], rsqs[q][:], [("rsq", q)], [("yTbuf", row0, q)])

        with ExitStack() as ph:
            fa_t = sbuf(ph, "fa_tB", [128, 32, 2, 128], BF16)
            gt_t = sbuf(ph, "gt_t", [128, 32, 2, 128], BF16)
            cwp = sbuf(ph, "cwp", [128, 12, 3], F32)
            cbp = sbuf(ph, "cbp", [128, 12], F32)
            fbp = sbuf(ph, "fbp", [128, 4], F32)
            mgp = sbuf(ph, "mgp", [128, 8], F32)
            dma("sp", fa_t[:].rearrange("p b c d -> p (b c d)"), T["fa"][:, 0:8192], [], ["fa"])
            dma("sp", gt_t[:].rearrange("p b c d -> p (b c d)"), T["gt"], [], ["gt"])
            dma("sp", cwp[:].rearrange("p a b -> p (a b)"), T["cwp"], [], ["cwp"])
            dma("sp", cbp[:], T["cbp"], [], ["cbp"])
            dma("sp", fbp[:], T["fbp"], [], ["fbp"])
            dma("sp", mgp[:], T["mgp"], [], ["mgp"])
            Pt = [sbuf(ph, "Pt%d" % s, [128, L + 2], BF16) for s in range(3)]
            Hsb = sbuf(ph, "HsbB", [128, 2, L], BF16)
            ux0s = [sbuf(ph, "ux0_%d" % i, [128, L], BF16) for i in range(2)]
            zTs = [sbuf(ph, "zT_%d" % i, [128, L], BF16) for i in range(2)]
            tA = sbuf(ph, "tA", [128, 1024], F32)
            tB = sbuf(ph, "tB", [128, 1024], F32)
            tC = sbuf(ph, "tC", [128, 1024], F32)
            zP1 = sbuf(ph, "zP1", [128, 32, 128], BF16)
            bufA = sbuf(ph, "bufAB", [128, 2, 32, 128], BF16)
            bufB = sbuf(ph, "bufBB", [128, 2, 32, 128], BF16)
            yc = sbuf(ph, "yc", [128, L], BF16)
            rsqs = [sbuf(ph, "rsq%d" % i, [128, 1024], BF16) for i in range(4)]
            rstdb = [sbuf(ph, "rstdb%d" % i, [128, 1024], BF16) for i in range(4)]
            tD = sbuf(ph, "tD", [128, 1024], F32)
            mts = [[sbuf(ph, "mt%d_%d" % (i, k), [128, 512], F32) for k in range(4)] for i in range(2)]
            for s in range(3):
                em.op("dve", lambda s=s: V.memset(Pt[s][:, 0:1], 0.0), writes=[("Ppad", s)])
                em.op("dve", lambda s=s: V.memset(Pt[s][:, L + 1:L + 2], 0.0), writes=[("Ppad", s)])
            fa5 = fa_t[:].rearrange("p (a b) c d -> p a b c d", a=1)
            def load_P(hc):
                for s in range(3):
                    dma("sp", Pt[s][:, 1:L + 1], Pbuf[(s * 4 + hc) * 128:(s * 4 + hc + 1) * 128, :], [], [("Pt", s)])

            def load_H(hc):
                dma("sp", Hsb[:].rearrange("p a b -> p (a b)"), Hbuf[hc], [], ["HsbB"])

            def conv(hc):
                ux0 = ux0s[hc % 2]
                zT = zTs[hc % 2]
                par = hc % 2
                for q in range(4):
                    t0 = q * 1024
                    tmps = [(tA, "tA"), (tB, "tB"), (tC, "tC")]
                    cc = hc
                    pk_ = [("Pt", 0), ("Ppad", 0), "cwp", "cbp"]
                    act(tA[:], Pt[0][:, 1 + t0:1 + t0 + 1024], AF.Identity, pk_, ["tA"], scale=cwp[:, cc, 1:2], bias=cbp[:, cc:cc + 1])
                    act(tD[:], Pt[0][:, t0:t0 + 1024], AF.Identity, pk_, ["tD"], scale=cwp[:, cc, 0:1])
                    tt("pool", tA[:], tA[:], tD[:], ALU.add, ["tA", "tD"], ["tA"])
                    act(tD[:], Pt[0][:, 2 + t0:2 + t0 + 1024], AF.Identity, pk_, ["tD"], scale=cwp[:, cc, 2:3])
                    tt("pool", ux0[:, t0:t0 + 1024], tA[:], tD[:], ALU.add, ["tA", "tD"], [("ux0", par, q)])
                    for s in (1, 2):
                        cc = s * 4 + hc
                        pk_ = [("Pt", s), ("Ppad", s), "cwp", "cbp"]
                        tmp, tk = tmps[s]
                        ts("dve", tmp[:], Pt[s][:, 1 + t0:1 + t0 + 1024], cwp[:, cc, 1:2], cbp[:, cc:cc + 1], ALU.mult, ALU.add, pk_, [tk])
                        stt("dve", tmp[:], Pt[s][:, t0:t0 + 1024], cwp[:, cc, 0:1], tmp[:], ALU.mult, ALU.add, pk_ + [tk], [tk])
                        stt("dve", tmp[:], Pt[s][:, 2 + t0:2 + t0 + 1024], cwp[:, cc, 2:3], tmp[:], ALU.mult, ALU.add, pk_ + [tk], [tk])
                    tt("dve", zT[:, t0:t0 + 1024], tB[:], tC[:], ALU.mult, ["tB", "tC"], [("zT", par, q)])

            def fwd(hc):
                zT = zTs[hc % 2]
                par = hc % 2
                zk = [("zT", par, q) for q in range(4)]
                for j0 in range(0, 32, 8):
                    ps, pk = ps_next()
                    psb = ps[:].bitcast(BF16)
                    for jj in range(8):
                        tr(psb[:, jj * 128:(jj + 1) * 128], zT[:, j0 + jj::32], zk, pk)
                    cp("act", zP1[:, j0:j0 + 8, :], psb.rearrange("p (a b) -> p a b", b=128), [pk], ["zP1"])

                fft_fwd_keys_A = [("bufA", cs, j0) for cs in range(2) for j0 in range(0, 32, 4)]

                def sinkY_guard(ch, psR, kR, psI, kI):
                    sl = slice(ch * 512, (ch + 1) * 512)
                    ya = bufA[:].rearrange("p a b c -> p a (b c)")
                    m = mts[ch % 2]
                    mk = [("mt", ch % 2, k) for k in range(4)]
                    tt("dve", m[0][:], psR[:], Hsb[:, 0, sl], ALU.mult, [kR, "HsbB"], [mk[0]])
                    tt("dve", m[1][:], psI[:], Hsb[:, 1, sl], ALU.mult, [kI, "HsbB"], [mk[1]])
                    tt("dve", m[2][:], psR[:], Hsb[:, 1, sl], ALU.mult, [kR, "HsbB"], [mk[2]])
                    tt("dve", m[3][:], psI[:], Hsb[:, 0, sl], ALU.mult, [kI, "HsbB"], [mk[3]])
                    tt("pool", ya[:, 0, sl], m[0][:], m[1][:], ALU.subtract, [mk[0], mk[1]], [("Y", ch)] + fft_fwd_keys_A)
                    tt("pool", ya[:, 1, sl], m[2][:], m[3][:], ALU.add, [mk[2], mk[3]], [("Y", ch)])

                fft_fwd(zP1, 1, fa5, bufA, bufB, "zP1", sinkY_guard)

            def inv(hc):
                ux0 = ux0s[hc % 2]
                zT = zTs[hc % 2]
                par = hc % 2
                zk = [("zT", par, q) for q in range(4)]
                ya = bufA[:].rearrange("p a b c -> p a (b c)")
                bufB_keys = [("bufB", cs, g0) for cs in range(2) for g0 in range(0, 32, 8)]
                for ch in range(8):
                    sl = slice(ch * 512, (ch + 1) * 512)
                    psR, kR = ps_next()
                    psI, kI = ps_next()
                    rk = [("Y", ch), "tct"]
                    mm(psR[:], tct[:, 0, :], ya[:, 0, sl], True, False, rk, kR)
                    mm(psR[:], tct[:, 2, :], ya[:, 1, sl], False, True, rk, kR)
                    mm(psI[:], tct[:, 1, :], ya[:, 0, sl], True, False, rk, kI)
                    mm(psI[:], tct[:, 0, :], ya[:, 1, sl], False, True, rk, kI)
                    wk = [("Cb", ch)] + (bufB_keys if ch == 0 else [])
                    cp("act", bufB[:, 0, 4 * ch:4 * ch + 4, :], psR[:].rearrange("p (a b) -> p a b", b=128), [kR], wk)
                    cp("dve", bufB[:, 1, 4 * ch:4 * ch + 4, :], psI[:].rearrange("p (a b) -> p a b", b=128), [kI], [("Cb", ch)])
                first = True
                for cs in range(2):
                    for g0 in range(0, 32, 8):
                        ps, pk = ps_next()
                        psb = ps[:].bitcast(BF16)
                        for gg in range(8):
                            g = g0 + gg
                            tr(psb[:, gg * 128:(gg + 1) * 128], bufB[:, cs, g, :], [("Cb", g // 4)], pk)
                        wk = [("Ct", cs, g0)] + ([("Y", ch) for ch in range(8)] if first else [])
                        first = False
                        cp("act" if cs == 0 else "dve", bufA[:, cs, :, 4 * g0:4 * g0 + 32].rearrange("p j (g c) -> p g j c", c=4),
                           psb.rearrange("p (g j c) -> p g j c", g=8, j=32), [pk], wk)
                ctk = [("Ct", cs, g0) for cs in range(2) for g0 in range(0, 32, 8)]
                yc3 = yc[:].rearrange("c (p j) -> c p j", j=32)
                for j0 in range(0, 32, 4):
                    ps, pk = ps_next()
                    for jj in range(4):
                        j = j0 + jj
                        mm(ps[:, jj * 128:(jj + 1) * 128], bufA[:, 0, j, :], gt_t[:, j, 0, :], True, False, ctk + ["gt"], pk)
                        mm(ps[:, jj * 128:(jj + 1) * 128], bufA[:, 1, j, :], gt_t[:, j, 1, :], False, True, ctk + ["gt"], pk)
                    act(yc3[:, :, j0:j0 + 4].rearrange("c p j -> c j p"), ps[:].rearrange("c (j p) -> c j p", p=128), AF.Identity,
                        [pk, ("rinvP", hc)], [("yc", j0)], scale=rinvP[:, hc:hc + 1])
                yck = [("yc", j0) for j0 in range(0, 32, 4)]
                tq = [(tA, "tA"), (tB, "tB"), (tC, "tC"), (tD, "tD")]
                for q in range(4):
                    t0 = q * 1024
                    stt("dve", tq[q][0][:], zT[:, t0:t0 + 1024], fbp[:, hc:hc + 1], yc[:, t0:t0 + 1024], ALU.mult, ALU.add,
                        zk + yck + ["fbp"], [tq[q][1]])
                for q in range(4):
                    t0 = q * 1024
                    tt("pool", tq[q][0][:], tq[q][0][:], ux0[:, t0:t0 + 1024], ALU.mult, [tq[q][1], ("ux0", par, q)], [tq[q][1]])
                head_norm_wave([(tq[q][0][:], tq[q][1]) for q in range(4)], mgp[:, hc:hc + 1], hc * 128, rsqs, rstdb)

            load_P(0)
            load_H(0)
            conv(0)
            load_P(1)
            for hc in range(4):
                fwd(hc)
                if hc + 1 < 4:
                    load_H(hc + 1)
                    conv(hc + 1)
                    if hc + 2 < 4:
                        load_P(hc + 2)
                inv(hc)
            em.barrier()
        if stop_after == "B":
            return nc

        with ExitStack() as ph:
            et_t = sbuf(ph, "et_t", [128, 32, 3, 128], BF16)
            tcf = sbuf(ph, "tcf", [128, 2, 128], BF16)
            mgp = sbuf(ph, "mgpC", [128, 8], F32)
            dma("sp", et_t[:].rearrange("p b c d -> p (b c d)"), T["et"], [], ["et"])
            dma("sp", tcf[:].rearrange("p a b -> p (a b)"), T["tcf"], [], ["tcf"])
            dma("sp", mgp[:], T["mgp"], [], ["mgp"])
            wris = [sbuf(ph, "wri%d" % i, [128, 2, 32, 128], BF16) for i in range(2)]
            bufA = sbuf(ph, "bufAC", [128, 2, 32, 128], BF16)
            bufB = sbuf(ph, "bufBC", [128, 2, 32, 128], BF16)
            yf = sbuf(ph, "yf", [128, 32, 128], BF16)
            rTs = [sbuf(ph, "rT%d" % i, [128, 1024], F32) for i in range(4)]
            rsqs = [sbuf(ph, "rsqC%d" % i, [128, 1024], BF16) for i in range(4)]
            rstdb = [sbuf(ph, "rstdbC%d" % i, [128, 1024], BF16) for i in range(4)]
            def c_load(fc):
                for part in range(2):
                    dma("sp", wris[fc % 2][:, part, :, :], wbuf[part][:, fc * 128:(fc + 1) * 128].rearrange("(p j) c -> p j c", j=32), [],
                        [("wri", fc % 2, part)])

            c_load(0)
            for fc in range(4):
                wri = wris[fc % 2]
                if fc + 1 < 4:
                    c_load(fc + 1)
                for j0 in range(0, 32, 4):
                    for cs in range(2):
                        ps, pk = ps_next()
                        for jj in range(4):
                            j = j0 + jj
                            if cs == 0:
                                mm(ps[:, jj * 128:(jj + 1) * 128], et_t[:, j, 0, :], wri[:, 0, j, :], True, False, [("wri", fc % 2, 0), ("wri", fc % 2, 1), "et"], pk)
                                mm(ps[:, jj * 128:(jj + 1) * 128], et_t[:, j, 1, :], wri[:, 1, j, :], False, True, [("wri", fc % 2, 0), ("wri", fc % 2, 1), "et"], pk)
                            else:
                                mm(ps[:, jj * 128:(jj + 1) * 128], et_t[:, j, 0, :], wri[:, 1, j, :], True, False, [("wri", fc % 2, 0), ("wri", fc % 2, 1), "et"], pk)
                                mm(ps[:, jj * 128:(jj + 1) * 128], et_t[:, j, 2, :], wri[:, 0, j, :], False, True, [("wri", fc % 2, 0), ("wri", fc % 2, 1), "et"], pk)
                        cp("act" if cs == 0 else "dve",
                           bufA[:].rearrange("p a g (j c) -> p a g j c", c=4)[:, cs, :, j0:j0 + 4, :].rearrange("p g j c -> p j g c"),
                           ps[:].rearrange("p (j g c) -> p j g c", j=4, g=32), [pk], [("bufA", cs, j0)])
                ak = [("bufA", cs, j0) for cs in range(2) for j0 in range(0, 32, 4)]
                for cs in range(2):
                    for g0 in range(0, 32, 8):
                        ps, pk = ps_next()
                        psb = ps[:].bitcast(BF16)
                        for gg in range(8):
                            g = g0 + gg
                            tr(psb[:, gg * 128:(gg + 1) * 128], bufA[:, cs, g, :], ak, pk)
                        cp("act" if cs == 0 else "dve", bufB[:, cs, g0:g0 + 8, :], psb.rearrange("p (a b) -> p a b", b=128), [pk], [("bufB", cs, g0)])
                for g0 in range(0, 32, 4):
                    ps, pk = ps_next()
                    for gg in range(4):
                        g = g0 + gg
                        rk = [("bufB", 0, g // 8 * 8), ("bufB", 1, g // 8 * 8), "tcf"]
                        mm(ps[:, gg * 128:(gg + 1) * 128], bufB[:, 0, g, :], tcf[:, 0, :], True, False, rk, pk)
                        mm(ps[:, gg * 128:(gg + 1) * 128], bufB[:, 1, g, :], tcf[:, 1, :], False, True, rk, pk)
                    cp("act" if (g0 // 4) % 2 else "dve", yf[:, :, 4 * g0:4 * g0 + 16].rearrange("p k (g c) -> p g k c", c=4),
                       ps[:].rearrange("p (g k c) -> p g k c", g=4, k=32), [pk], [("yf", g0)])
                yfk = [("yf", g0) for g0 in range(0, 32, 4)]
                for q in range(4):
                    ps, pk = ps_next()
                    psb = ps[:].bitcast(BF16)
                    for kk_ in range(8):
                        kb_ = q * 8 + kk_
                        tr(psb[:, kk_ * 128:(kk_ + 1) * 128], yf[:, kb_, :], yfk, pk)
                    cp("dve" if q % 2 else "act", rTs[q][:], psb, [pk], [("rT", q)])
                head_norm_wave([(rTs[q][:], ("rT", q)) for q in range(4)], mgp[:, 4 + fc:5 + fc], 512 + fc * 128, rsqs, rstdb)
            em.barrier()
        if stop_after == "C":
            return nc

        ph_moe = st.enter_context(ExitStack())
        w1 = sbuf(ph_moe, "w1", [128, 32], F32)
        w2 = sbuf(ph_moe, "w2", [128, 32], F32)
        ds_i = sbuf(ph_moe, "ds_i", [128, 2, 32], I32)
        dg_i = sbuf(ph_moe, "dg_i", [128, 2, 32], I32)
        ph_tok = ExitStack()
        h2tok = sbuf(ph_tok, "h2tok", [128, 32, D], BF16)
        Lg = sbuf(ph_tok, "Lg", [128, 32, 36], F32)
        with ExitStack() as ph:
            Wo = sbuf(ph, "Wo", [128, 8, D], BF16)
            Wr = sbuf(ph, "Wr", [128, 8, 36], BF16)
            g2b = sbuf(ph, "g2b", [128, D], F32)
            brb = sbuf(ph, "brb", [128, 36], F32)
            for kc in range(8):
                dma("pool", Wo[:, kc, :], T["w_out"][kc * 128:(kc + 1) * 128, :], [], [("Wo", kc)])
            dma("pool", Wr[:], T["wr"].rearrange("(k p) n -> p k n", p=128), [], ["Wr"])
            dma("sp", g2b[:], T["g2b"], [], ["g2b"])
            dma("sp", brb[:], T["brb"], [], ["brb"])
            yTb = [sbuf(ph, "yTb%d" % i, [128, 8, 512], BF16) for i in range(2)]
            xt = [sbuf(ph, "xtD%d" % i, [128, D], F32) for i in range(3)]
            x1t = [sbuf(ph, "x1t%d" % i, [128, D], F32) for i in range(3)]
            junk = sbuf(ph, "junkD", [128, D], BF16)
            ss = sbuf(ph, "ssD", [128, 32], F32)
            rs = sbuf(ph, "rsD", [128, 32], F32)
            h2T = [sbuf(ph, "h2T%d" % i, [128, 8, 128], BF16) for i in range(2)]
            em.op("dve", lambda: V.memset(ss[:], 0.0), writes=["ssD"])
            wo_ps = {}

            def d_front(j2):
                tc, i = j2 // 4, j2 % 4
                yb = tc % 2
                b = j2 % 3
                if i == 0:
                    dma("sp", yTb[yb][:], yTbuf[:, tc * 512:(tc + 1) * 512].rearrange("(c p) t -> p c t", p=128), [], [("yTb", yb)])
                dma("sp", xt[b][:], T["x"][j2 * 128:(j2 + 1) * 128, :], [], [("xtD", b)])
                for half in range(2):
                    ps, pk = ps_next()
                    for cc in range(8):
                        mm(ps[:], yTb[yb][:, cc, i * 128:(i + 1) * 128], Wo[:, cc, half * 512:(half + 1) * 512], cc == 0, cc == 7,
                           [("yTb", yb), ("Wo", cc)], pk)
                    wo_ps[(j2, half)] = (ps, pk)

            def d_back(j2):
                b = j2 % 3
                for half in range(2):
                    ps, pk = wo_ps.pop((j2, half))
                    tt("dve", x1t[b][:, half * 512:(half + 1) * 512], xt[b][:, half * 512:(half + 1) * 512], ps[:], ALU.add,
                       [("xtD", b), pk], [("x1t", b, half)])
                xk = [("x1t", b, 0), ("x1t", b, 1)]
                dma("sp", x1buf[j2 * 128:(j2 + 1) * 128, :], x1t[b][:], xk, [("x1buf", j2)])
                act(junk[:], x1t[b][:], AF.Square, xk + ["ssD"], ["junkD", ("ssD", j2)], accum_out=ss[:, j2:j2 + 1])
                act(rs[:, j2:j2 + 1], ss[:, j2:j2 + 1], AF.Sqrt, [("ssD", j2), "epst"], [("rsD", j2)], scale=1.0 / D, bias=epst[:])
                em.op("dve", lambda: V.reciprocal(out=rs[:, j2:j2 + 1], in_=rs[:, j2:j2 + 1]), reads=[("rsD", j2)], writes=[("rsD", j2)])
                stt("dve", h2tok[:, j2, :], x1t[b][:], rs[:, j2:j2 + 1], g2b[:], ALU.mult, ALU.mult, xk + [("rsD", j2), "g2b"], [("h2tok", j2)])
                ps, pk = ps_next()
                psb = ps[:].bitcast(BF16)
                for kc in range(8):
                    tr(psb[:, kc * 128:(kc + 1) * 128], h2tok[:, j2, kc * 128:(kc + 1) * 128], [("h2tok", j2)], pk)
                cp("act", h2T[j2 % 2][:], psb.rearrange("p (a b) -> p a b", b=128), [pk], [("h2T", j2 % 2)])
                ps, pk = ps_next()
                for kc in range(8):
                    mm(ps[:, 0:36], h2T[j2 % 2][:, kc, :], Wr[:, kc, :], kc == 0, kc == 7, [("h2T", j2 % 2), "Wr"], pk)
                tt("dve", Lg[:, j2, :], ps[:, 0:36], brb[:], ALU.add, [pk, "brb"], [("Lg", j2)])

            d_front(0)
            for j2 in range(32):
                if j2 + 1 < 32:
                    d_front(j2 + 1)
                d_back(j2)
            em.barrier()
        if debug:
            dma("sp", dbgL[:, 0:32 * 36], Lg[:].rearrange("p a b -> p (a b)"), [("Lg", j2) for j2 in range(32)], ["dbgL"])
        if stop_after == "D":
            em.barrier()
            return nc

        with ExitStack() as ph:
            tri = sbuf(ph, "tri", [128, 128], BF16)
            ones = sbuf(ph, "ones", [128, 128], BF16)
            ecb = sbuf(ph, "ecb", [128, 32], F32)
            trb = sbuf(ph, "trb", [128, 32], F32)
            dma("sp", tri[:], T["tri"], [], ["tri"])
            dma("sp", ones[:], T["ones"], [], ["ones"])
            dma("sp", ecb[:], T["ecb"], [], ["ecb"])
            dma("sp", trb[:], T["trb"], [], ["trb"])
            S = lambda name, shape, dt=F32: sbuf(ph, name, shape, dt)
            gmax = S("gmax", [128, 32]); goh = S("goh", [128, 32, 4]); gd = S("gd", [128, 32, 4]); gsum = S("gsum", [128, 32])
            pg = S("pg", [128, 32]); sel4 = S("sel4", [128, 32, 4, 8]); esel = S("esel", [128, 32, 8]); m1 = S("m1", [128, 32])
            oh1 = S("oh1", [128, 32, 8]); e2 = S("e2", [128, 32, 8]); m2 = S("m2", [128, 32]); oh2 = S("oh2", [128, 32, 8])
            dd = S("dd", [128, 32]); A1 = S("A1", [128, 32, 4, 8]); A2 = S("A2", [128, 32, 4, 8]); Mb = S("Mb", [128, 1024], BF16)
            pin = S("pin", [128, 32, 32]); cnt = S("cnt", [128, 32, 32]); base = S("base", [128, 32, 32]); slot = S("slot", [128, 32, 32])
            tmp3 = S("tmp3", [128, 32, 32]); dk = S("dk", [128, 2, 32]); pk_ = S("pk_", [128, 2, 32]); ok = S("ok", [128, 2, 32])
            dsf = S("dsf", [128, 2, 32]); dgf = S("dgf", [128, 2, 32])
            allL = ["LgAll"]
            K = "route"
            lg_keys = [("Lg", j2) for j2 in range(32)]
            gl = Lg[:, :, 0:4]
            em.op("dve", lambda: V.tensor_reduce(out=gmax[:], in_=gl, axis=AX.X, op=ALU.max), reads=lg_keys, writes=["gmax"])
            tt("dve", goh[:], gl, gmax[:].unsqueeze(2).to_broadcast([128, 32, 4]), ALU.is_equal, lg_keys + ["gmax"], ["goh"])
            tt("dve", gd[:], gl, gmax[:].unsqueeze(2).to_broadcast([128, 32, 4]), ALU.subtract, lg_keys + ["gmax"], ["gd"])
            act(gd[:], gd[:], AF.Exp, ["gd"], ["gd"])
            em.op("dve", lambda: V.tensor_reduce(out=gsum[:], in_=gd[:], axis=AX.X, op=ALU.add), reads=["gd"], writes=["gsum"])
            em.op("dve", lambda: V.reciprocal(out=pg[:], in_=gsum[:]), reads=["gsum"], writes=["pg"])
            el4 = Lg[:, :, 4:36].rearrange("p a (g i) -> p a g i", i=8)
            tt("dve", sel4[:], el4, goh[:].unsqueeze(3).to_broadcast([128, 32, 4, 8]), ALU.mult, lg_keys + ["goh"], ["sel4"])
            tt("dve", esel[:], sel4[:, :, 0, :], sel4[:, :, 1, :], ALU.add, ["sel4"], ["esel"])
            tt("dve", esel[:], esel[:], sel4[:, :, 2, :], ALU.add, ["sel4", "esel"], ["esel"])
            tt("dve", esel[:], esel[:], sel4[:, :, 3, :], ALU.add, ["sel4", "esel"], ["esel"])
            em.op("dve", lambda: V.tensor_reduce(out=m1[:], in_=esel[:], axis=AX.X, op=ALU.max), reads=["esel"], writes=["m1"])
            tt("dve", oh1[:], esel[:], m1[:].unsqueeze(2).to_broadcast([128, 32, 8]), ALU.is_equal, ["esel", "m1"], ["oh1"])
            stt("dve", e2[:], oh1[:], -1e30, esel[:], ALU.mult, ALU.add, ["oh1", "esel"], ["e2"])
            em.op("dve", lambda: V.tensor_reduce(out=m2[:], in_=e2[:], axis=AX.X, op=ALU.max), reads=["e2"], writes=["m2"])
            tt("dve", oh2[:], e2[:], m2[:].unsqueeze(2).to_broadcast([128, 32, 8]), ALU.is_equal, ["e2", "m2"], ["oh2"])
            tt("dve", dd[:], m2[:], m1[:], ALU.subtract, ["m1", "m2"], ["dd"])
            act(dd[:], dd[:], AF.Exp, ["dd"], ["dd"])
            ts("dve", w1[:], dd[:], 1.0, None, ALU.add, None, ["dd"], ["w1"])
            em.op("dve", lambda: V.reciprocal(out=w1[:], in_=w1[:]), reads=["w1"], writes=["w1"])
            tt("dve", w2[:], dd[:], w1[:], ALU.mult, ["dd", "w1"], ["w2"])
            tt("dve", w1[:], w1[:], pg[:], ALU.mult, ["w1", "pg"], ["w1"])
            tt("dve", w2[:], w2[:], pg[:], ALU.mult, ["w2", "pg"], ["w2"])
            gb = goh[:].unsqueeze(3).to_broadcast([128, 32, 4, 8])
            tt("dve", A1[:], gb, oh1[:].unsqueeze(2).to_broadcast([128, 32, 4, 8]), ALU.mult, ["goh", "oh1"], ["A1"])
            tt("dve", A2[:], gb, oh2[:].unsqueeze(2).to_broadcast([128, 32, 4, 8]), ALU.mult, ["goh", "oh2"], ["A2"])
            A1f = A1[:].rearrange("p a g i -> p a (g i)")
            A2f = A2[:].rearrange("p a g i -> p a (g i)")
            tt("dve", Mb[:].rearrange("p (a e) -> p a e", e=32), A1f, A2f, ALU.add, ["A1", "A2"], ["Mb"])
            for h_ in range(2):
                ps, pk = ps_next()
                mm(ps[:], tri[:], Mb[:, h_ * 512:(h_ + 1) * 512], True, True, ["tri", "Mb"], pk)
                cp("act", pin[:, h_ * 16:(h_ + 1) * 16, :], ps[:].rearrange("p (a e) -> p a e", e=32), [pk], ["pin"])
                ps, pk = ps_next()
                mm(ps[:], ones[:], Mb[:, h_ * 512:(h_ + 1) * 512], True, True, ["ones", "Mb"], pk)
                cp("act", cnt[:, h_ * 16:(h_ + 1) * 16, :], ps[:].rearrange("p (a e) -> p a e", e=32), [pk], ["cnt"])
            em.op("dve", lambda: V.memset(base[:, 0, :], 0.0), writes=["base"])
            for j2 in range(1, 32):
                tt("dve", base[:, j2, :], base[:, j2 - 1, :], cnt[:, j2 - 1, :], ALU.add, ["base", "cnt"], ["base"])
            tt("dve", slot[:], pin[:], base[:], ALU.add, ["pin", "base"], ["slot"])
            for k, Af in enumerate([A1f, A2f]):
                tt("dve", tmp3[:], Af, slot[:], ALU.mult, ["A1", "A2", "slot"], ["tmp3"])
                em.op("dve", lambda k=k: V.tensor_reduce(out=pk_[:, k, :], in_=tmp3[:], axis=AX.X, op=ALU.add), reads=["tmp3"], writes=["pk_"])
                tt("dve", tmp3[:], Af, ecb[:].unsqueeze(1).to_broadcast([128, 32, 32]), ALU.mult, ["A1", "A2", "ecb"], ["tmp3"])
                em.op("dve", lambda k=k: V.tensor_reduce(out=dk[:, k, :], in_=tmp3[:], axis=AX.X, op=ALU.add), reads=["tmp3"], writes=["dk"])
            tt("dve", dk[:], dk[:], pk_[:], ALU.add, ["dk", "pk_"], ["dk"])
            ts("dve", ok[:], pk_[:], float(CAP), None, ALU.is_lt, None, ["pk_"], ["ok"])
            tt("dve", dgf[:], dk[:], ok[:], ALU.mult, ["dk", "ok"], ["dgf"])
            tt("dve", dsf[:], dk[:], trb[:].unsqueeze(1).to_broadcast([128, 2, 32]), ALU.subtract, ["dk", "trb"], ["dsf"])
            tt("dve", dsf[:], dsf[:], ok[:], ALU.mult, ["dsf", "ok"], ["dsf"])
            tt("dve", dsf[:], dsf[:], trb[:].unsqueeze(1).to_broadcast([128, 2, 32]), ALU.add, ["dsf", "trb"], ["dsf"])
            tt("dve", w1[:], w1[:], ok[:, 0, :], ALU.mult, ["w1", "ok"], ["w1"])
            tt("dve", w2[:], w2[:], ok[:, 1, :], ALU.mult, ["w2", "ok"], ["w2"])
            cp("dve", ds_i[:], dsf[:], ["dsf"], ["ds_i"])
            cp("dve", dg_i[:], dgf[:], ["dgf"], ["dg_i"])
            if debug:
                dma("sp", dbgL[:, 32 * 36:32 * 36 + 64], dsf[:].rearrange("p a b -> p (a b)"), ["dsf"], ["dbgL2"])
                dma("sp", dbgL[:, 32 * 36 + 64:32 * 36 + 96], w1[:], ["w1"], ["dbgL3"])
                dma("sp", dbgL[:, 32 * 36 + 96:32 * 36 + 128], w2[:], ["w2"], ["dbgL4"])
            for j2 in range(32):
                for k in range(2):
                    em.dma("pool", lambda j2=j2, k=k: G.indirect_dma_start(
                        out=xg, out_offset=bass.IndirectOffsetOnAxis(ap=ds_i[:, k, j2:j2 + 1], axis=0),
                        in_=h2tok[:, j2, :], in_offset=None), reads=["ds_i", ("h2tok", j2)],
                        writes=[("xgs", j2, k)])
            em.barrier()
        ph_tok.close()
        if stop_after == "E":
            return nc

        with ExitStack() as ph:
            NB = 2
            Wg = [sbuf(ph, "Wg%d" % i, [128, 8, 512], BF16) for i in range(NB)]
            Wu = [sbuf(ph, "Wu%d" % i, [128, 8, 512], BF16) for i in range(NB)]
            Wd = [sbuf(ph, "Wd%d" % i, [128, 4, D], BF16) for i in range(NB)]
            xgt = [sbuf(ph, "xgt%d" % i, [128, 3, D], BF16) for i in range(2)]
            xgT = [sbuf(ph, "xgT%d" % i, [128, 8, CAP], BF16) for i in range(2)]
            sg = [sbuf(ph, "sg%d" % i, [128, CAP], F32) for i in range(2)]
            hT = [sbuf(ph, "hT%d" % i, [128, 4, CAP], BF16) for i in range(2)]
            yt = [sbuf(ph, "yt%d" % i, [128, D], F32) for i in range(4)]
            yt_rr = 0
            NST = (CAP + 127) // 128
            NSTG = 5
            stg = [sbuf(ph, "stg%d" % i, [128, 4096], F32) for i in range(NSTG)]
            stg_rr = [0]

            def load_expert(e):
                b = e % NB
                b2 = e % 2
                items = [
                    (T["w_gate"][e].rearrange("(p k) n -> p k n", k=8), Wg[b], ("Wg", b), 8, 512, "act"),
                    (T["w_up"][e].rearrange("(p k) n -> p k n", k=8), Wu[b], ("Wu", b), 8, 512, "dve"),
                    (T["w_down"][e].rearrange("(k p) n -> p k n", p=128), Wd[b], ("Wd", b), 4, 1024, "pool"),
                ]
                for src, dst, key, nk, nn, ce in items:
                    i = stg_rr[0] % NSTG
                    stg_rr[0] += 1
                    sv = stg[i][:].rearrange("p (k n) -> p k n", k=nk)
                    dma("sp", sv, src, [], [("stg", i)])
                    hk_ = nk // 2
                    for h_ in range(2):
                        wk = [(key[0], key[1], 0), (key[0], key[1], 4 if key[0] != "Wd" else 2)] if h_ == 0 else []
                        ce_ = ce if ce != "pool" else ("dve" if h_ == 0 else "act")
                        cp(ce_, dst[:, h_ * hk_:(h_ + 1) * hk_, :], sv[:, h_ * hk_:(h_ + 1) * hk_, :], [("stg", i)],
                           [(key[0], key[1], "h%d" % h_)] + wk)
                for s_ in range((CAP + 127) // 128):
                    w_ = min(128, CAP - s_ * 128)
                    dma("pool", xgt[b2][0:w_, s_, :], xg[e * CAP + s_ * 128:e * CAP + s_ * 128 + w_, :], [], [("xgt", b2, s_)])

            def f_transposes(e):
                b2 = e % 2
                for s in range(3):
                    if s * 128 >= CAP:
                        break
                    w_ = min(128, CAP - s * 128)
                    ps, pk = ps_next()
                    psb = ps[:].bitcast(BF16)
                    for kc in range(8):
                        tr(psb[:, kc * 128:kc * 128 + w_], xgt[b2][0:w_, s, kc::8], [("xgt", b2, s)], pk, kdim=w_)
                    cp("act" if s % 2 else "dve", xgT[b2][:, :, s * 128:s * 128 + w_],
                       psb.rearrange("p (a b) -> p a b", b=128)[:, :, 0:w_], [pk], [("xgT", b2, s)])

            load_expert(0)
            f_transposes(0)
            for e in range(32):
                b = e % NB
                b2 = e % 2
                if e + 1 < 32:
                    load_expert(e + 1)
                xk = [("xgT", b2, s) for s in range(NST)]
                for mc in range(4):
                    psG, kG = ps_next()
                    psU, kU = ps_next()
                    for kc in range(8):
                        mm(psG[:, 0:CAP], Wg[b][:, kc, mc * 128:(mc + 1) * 128], xgT[b2][:, kc, :], kc == 0, kc == 7, xk + [("Wg", b, "h0"), ("Wg", b, "h1"), ("Wg", b, 0), ("Wg", b, 4)], kG)
                    for kc in range(8):
                        mm(psU[:, 0:CAP], Wu[b][:, kc, mc * 128:(mc + 1) * 128], xgT[b2][:, kc, :], kc == 0, kc == 7, xk + [("Wu", b, "h0"), ("Wu", b, "h1"), ("Wu", b, 0), ("Wu", b, 4)], kU)
                    sb_ = mc % 2
                    act(sg[sb_][:], psG[:, 0:CAP], AF.Silu, [kG], [("sg", sb_)])
                    tt("dve", hT[b2][:, mc, :], sg[sb_][:], psU[:, 0:CAP], ALU.mult, [("sg", sb_), kU], [("hT", b2, mc)])
                if e + 1 < 32:
                    f_transposes(e + 1)
                hk = [("hT", b2, mc) for mc in range(4)]
                for s in range(NST):
                    w_ = min(128, CAP - s * 128)
                    yb = yt_rr % 4
                    yt_rr += 1
                    for half in range(2):
                        ps, pk = ps_next()
                        for mc in range(4):
                            mm(ps[0:w_, :], hT[b2][:, mc, s * 128:s * 128 + w_], Wd[b][:, mc, half * 512:(half + 1) * 512], mc == 0, mc == 3,
                               hk + [("Wd", b, "h0"), ("Wd", b, "h1"), ("Wd", b, 0), ("Wd", b, 2)], pk)
                        cp("act" if half else "dve", yt[yb][0:w_, half * 512:(half + 1) * 512], ps[0:w_, :], [pk], [("yt", yb, half)])
                    dma("pool", Ybuf[e * CAP + s * 128:e * CAP + s * 128 + w_, :], yt[yb][0:w_, :], [("yt", yb, 0), ("yt", yb, 1)], [("Ybuf", e, s)])
            em.barrier()
        if stop_after == "F":
            return nc

        with ExitStack() as ph:
            gfb = sbuf(ph, "gfb", [128, D], F32)
            dma("sp", gfb[:], T["gfb"], [], ["gfb"])
            NG = 4
            Y1 = [sbuf(ph, "Y1_%d" % i, [128, D], F32) for i in range(NG)]
            Y2 = [sbuf(ph, "Y2_%d" % i, [128, D], F32) for i in range(NG)]
            xt = [sbuf(ph, "xtG%d" % i, [128, D], F32) for i in range(NG)]
            junk = sbuf(ph, "junkG", [128, D], BF16)
            ss = sbuf(ph, "ssG", [128, 32], F32)
            rs = sbuf(ph, "rsG", [128, 32], F32)
            em.op("dve", lambda: V.memset(ss[:], 0.0), writes=["ssG"])

            def g_load(j2):
                b = j2 % NG
                em.dma("pool", lambda: G.indirect_dma_start(out=Y1[b][:], out_offset=None, in_=Ybuf,
                                                            in_offset=bass.IndirectOffsetOnAxis(ap=dg_i[:, 0, j2:j2 + 1], axis=0)),
                       reads=["dg_i"], writes=[("Y1", b)])
                em.dma("pool", lambda: G.indirect_dma_start(out=Y2[b][:], out_offset=None, in_=Ybuf,
                                                            in_offset=bass.IndirectOffsetOnAxis(ap=dg_i[:, 1, j2:j2 + 1], axis=0)),
                       reads=["dg_i"], writes=[("Y2", b)])
                dma("sp", xt[b][:], x1buf[j2 * 128:(j2 + 1) * 128, :], [], [("xtG", b)])

            for j2 in range(min(3, 32)):
                g_load(j2)
            for j2 in range(32):
                b = j2 % NG
                if j2 + 3 < 32:
                    g_load(j2 + 3)
                stt("dve", xt[b][:], Y1[b][:], w1[:, j2:j2 + 1], xt[b][:], ALU.mult, ALU.add, [("Y1", b), ("xtG", b), "w1"], [("xtG", b)])
                stt("dve", xt[b][:], Y2[b][:], w2[:, j2:j2 + 1], xt[b][:], ALU.mult, ALU.add, [("Y2", b), ("xtG", b), "w2"], [("xtG", b)])
                act(junk[:], xt[b][:], AF.Square, [("xtG", b), "ssG"], ["junkG", ("ssG", j2)], accum_out=ss[:, j2:j2 + 1])
                act(rs[:, j2:j2 + 1], ss[:, j2:j2 + 1], AF.Sqrt, [("ssG", j2), "epst"], [("rsG", j2)], scale=1.0 / D, bias=epst[:])
                em.op("dve", lambda: V.reciprocal(out=rs[:, j2:j2 + 1], in_=rs[:, j2:j2 + 1]), reads=[("rsG", j2)], writes=[("rsG", j2)])
                stt("dve", Y1[b][:], xt[b][:], rs[:, j2:j2 + 1], gfb[:], ALU.mult, ALU.mult, [("xtG", b), ("rsG", j2), "gfb"], [("Y1", b)])
                final_events.append(dma("sp", out_d[j2 * 128:(j2 + 1) * 128, :], Y1[b][:], [("Y1", b)], [("out", j2)]))
            em.barrier()
    return nc


def prep_inputs(inp):
    f32 = np.float32
    g = lambda k: np.asarray(inp[k], dtype=f32)
    rep = lambda v: np.ascontiguousarray(np.tile(v.reshape(1, -1), (128, 1)))
    sh = {}
    sh["w_in"] = np.ascontiguousarray(g("w_in")[0])
    sh["g1b"] = rep(g("norm1_g")[0])
    sh["g2b"] = rep(g("norm2_g")[0])
    sh["gfb"] = rep(g("final_g"))
    cw = g("conv_w")[0]
    sh["cwp"] = np.ascontiguousarray(cw.reshape(3, 12, 128).transpose(2, 1, 0)).reshape(128, 36)
    sh["cbp"] = np.ascontiguousarray(g("conv_b")[0].reshape(12, 128).T)
    sh["fbp"] = np.ascontiguousarray(g("f_bias")[0].reshape(4, 128).T)
    sh["mgp"] = np.ascontiguousarray(g("mix_g")[0].reshape(8, 128).T)
    sh["w_out"] = np.ascontiguousarray(g("w_out")[0])
    wr = np.concatenate([g("w_group")[0], g("w_router")[0].transpose(1, 0, 2).reshape(D, 32)], axis=1)
    sh["wr"] = np.ascontiguousarray(wr)
    sh["brb"] = rep(np.concatenate([g("b_group")[0], g("b_router")[0].reshape(32)]))
    sh["w_gate"] = np.ascontiguousarray(g("w_gate")[0])
    sh["w_up"] = np.ascontiguousarray(g("w_up")[0])
    sh["w_down"] = np.ascontiguousarray(g("w_down")[0])
    fwin = g("f_w_in")[0]
    w1 = np.zeros((66, 128), f32)
    w1[0:33, 0:64] = fwin
    w1[33:66, 64:128] = fwin
    sh["fwin2"] = w1
    fm = g("f_w_mid")[0]
    wm = np.zeros((128, 2, 128), f32)
    for l in range(2):
        wm[0:64, l, 0:64] = fm[l]
        wm[64:128, l, 64:128] = fm[l]
    sh["fwmid2"] = wm.reshape(128, 256)
    fq = g("f_freq")[0].T
    sh["fq"] = np.ascontiguousarray(np.concatenate([fq, fq], 0))
    fb = np.stack([g("f_b_in")[0], g("f_b_mid")[0][0], g("f_b_mid")[0][1]], 1)
    sh["fbb"] = np.ascontiguousarray(np.concatenate([fb, fb], 0))
    fo = g("f_w_out")[0]
    sh["fwout2"] = np.ascontiguousarray(np.concatenate([fo, fo], 0))
    return sh


_CACHE = {}


def kernel(**inputs):
    if "nc" not in _CACHE:
        _CACHE["nc"] = build_nc()
        _CACHE["consts"] = make_consts()
    nc = _CACHE["nc"]
    shared = prep_inputs(inputs)
    shared.update(_CACHE["consts"])
    x = np.asarray(inputs["x"], dtype=np.float32)
    in_maps = []
    for c in range(8):
        m = dict(shared)
        m["x"] = np.ascontiguousarray(x[c])
        in_maps.append(m)
    res = run_bass_kernel_spmd(nc, in_maps, core_ids=list(range(8)))
    out = np.stack([np.asarray(res.results[c]["out"], dtype=np.float32) for c in range(8)], axis=0)
    return out
```

```python
import numpy as np
import ml_dtypes
from contextlib import ExitStack
import concourse.bass as bass
import concourse.mybir as mybir
from concourse.bass_utils import run_bass_kernel_spmd

F32 = mybir.dt.float32
BF16 = mybir.dt.bfloat16
I32 = mybir.dt.int32
AF = mybir.ActivationFunctionType
ALU = mybir.AluOpType
AX = mybir.AxisListType
bf = ml_dtypes.bfloat16

L = 4096
NF = 8192
D = 1024
CAP = 320
NS = 32 * CAP
NROWS = NS + 128
EPS = 1e-6
TWO_PI = float(2 * np.pi)


class Emit:
    def __init__(self, nc, stack, n_dma_sems=48):
        self.nc = nc
        self.eng = {"pe": nc.tensor, "act": nc.scalar, "dve": nc.vector, "pool": nc.gpsimd, "sp": nc.sync}
        self.sem = {}
        self.cnt = {}
        for k in self.eng:
            self.sem[k] = stack.enter_context(nc.semaphore("s_" + k))
            self.cnt[k] = 0
        self.dma_sems = [stack.enter_context(nc.semaphore("d%d" % i)) for i in range(n_dma_sems)]
        self.dma_cnt = [0] * n_dma_sems
        n_sw = 16
        self.dma_pool = {"hw": list(range(n_sw, n_dma_sems)), "sw": list(range(n_sw))}
        self.dma_rr = {"hw": 0, "sw": 0}
        self.seen = {k: {} for k in self.eng}
        self.lastw = {}
        self.reads = {}
        self.n_wait = 0
        self.n_ins = 0

    def _wait(self, e, ev):
        sem, val, src = ev
        sid = id(sem)
        if self.seen[e].get(sid, 0) >= val:
            return
        self.seen[e][sid] = val
        self.eng[e].wait_ge(sem, val)
        self.n_wait += 1

    def _deps(self, e, reads, writes):
        evs = []
        for r in reads:
            w = self.lastw.get(r)
            if w is not None and not (w[2] == e and e == "pe"):
                evs.append(w)
        same_ok = e in ("pe",)
        for wkey in writes:
            w = self.lastw.get(wkey)
            if w is not None and (w[2] != e or not same_ok):
                evs.append(w)
            for ev in self.reads.get(wkey, {}).values():
                if ev[2] != e or not same_ok:
                    evs.append(ev)
        return evs

    def _commit(self, ev, reads, writes):
        for r in reads:
            self.reads.setdefault(r, {})[(ev[2], id(ev[0]))] = ev
        for w in writes:
            self.lastw[w] = ev
            self.reads[w] = {}

    def op(self, e, fn, reads=(), writes=()):
        for ev in self._deps(e, reads, writes):
            self._wait(e, ev)
        ins = fn()
        self.cnt[e] += 1
        ins.then_inc(self.sem[e], 1)
        ev = (self.sem[e], self.cnt[e], e)
        self._commit(ev, reads, writes)
        self.n_ins += 1
        return ev

    def dma(self, q, fn, reads=(), writes=()):
        for ev in self._deps(q, reads, writes):
            self._wait(q, ev)
        kind = "sw" if q == "pool" else "hw"
        lst = self.dma_pool[kind]
        i = lst[self.dma_rr[kind]]
        self.dma_rr[kind] = (self.dma_rr[kind] + 1) % len(lst)
        sem = self.dma_sems[i]
        if self.dma_cnt[i] > 0:
            self._wait(q, (sem, 16 * self.dma_cnt[i], "dma"))
        ins = fn()
        self.dma_cnt[i] += 1
        ins.then_inc(sem, 16)
        ev = (sem, 16 * self.dma_cnt[i], "dma%d" % i)
        self._commit(ev, reads, writes)
        self.n_ins += 1
        return ev

    def barrier(self):
        for e in self.eng:
            for e2 in self.eng:
                if e2 != e and self.cnt[e2] > 0:
                    self._wait(e, (self.sem[e2], self.cnt[e2], e2))
            for i, s in enumerate(self.dma_sems):
                if self.dma_cnt[i] > 0:
                    self._wait(e, (s, 16 * self.dma_cnt[i], "dma"))
        self.lastw = {}
        self.reads = {}


def _kron4(M):
    return np.kron(M, np.eye(4))


def make_consts():
    c = {}
    p = np.arange(256)[:, None].astype(np.float64)
    k1 = np.arange(128)[None, :].astype(np.float64)
    fa = np.zeros((2, 32, 2, 128, 128))
    gt = np.zeros((32, 2, 128, 128))
    for j in range(32):
        th = 2 * np.pi * (p * (k1 + 0.5) / 256.0 + j * (k1 + 0.5) / NF)
        for pt in range(2):
            sgn = 1.0 if pt == 0 else -1.0
            fa[pt, j, 0] = sgn * np.cos(th[pt * 128:(pt + 1) * 128])
            fa[pt, j, 1] = -sgn * np.sin(th[pt * 128:(pt + 1) * 128])
        gt[j, 0] = (2.0 / NF) * np.cos(th[:128]).T
        gt[j, 1] = -(2.0 / NF) * np.sin(th[:128]).T
    c["fa"] = np.ascontiguousarray(fa.transpose(3, 0, 1, 2, 4)).reshape(128, 2 * 32 * 2 * 128).astype(bf)
    c["gt"] = np.ascontiguousarray(gt.transpose(2, 0, 1, 3)).reshape(128, 32 * 2 * 128).astype(bf)
    jj = np.arange(32)[:, None].astype(np.float64)
    kk = np.arange(32)[None, :].astype(np.float64)
    ph = 2 * np.pi * jj * kk / 32.0
    Tc = _kron4(np.cos(ph))
    Ts = _kron4(np.sin(ph))
    c["tct"] = np.stack([Tc, Ts, -Ts], 1).reshape(128, 3 * 128).astype(bf)
    c["tcf"] = np.stack([Tc / 512.0, Ts / 512.0], 1).reshape(128, 2 * 128).astype(bf)
    pa = np.arange(128)[:, None].astype(np.float64)
    ka = np.arange(128)[None, :].astype(np.float64)
    et = np.zeros((32, 3, 128, 128))
    for j in range(32):
        th = 2 * np.pi * (pa * ka / 128.0 + j * ka / 4096.0)
        et[j, 0] = np.cos(th)
        et[j, 1] = np.sin(th)
        et[j, 2] = -np.sin(th)
    c["et"] = np.ascontiguousarray(et.transpose(2, 0, 1, 3)).reshape(128, 32 * 3 * 128).astype(bf)
    cc = np.arange(64)[:, None].astype(np.float64)
    c2 = np.arange(64)[None, :].astype(np.float64)
    C64 = np.cos(2 * np.pi * cc * c2 / 64.0)
    S64 = np.sin(2 * np.pi * cc * c2 / 64.0)
    c["bdt"] = np.stack([np.kron(np.eye(2), C64), np.kron(np.eye(2), -S64)], 1).reshape(128, 256).astype(bf)
    c["ident"] = np.eye(128).astype(bf)
    c["blk"] = (np.kron(np.eye(2), np.ones((64, 64))) / 64.0).astype(bf)
    c["tri"] = np.triu(np.ones((128, 128)), 1).astype(bf)
    c["ones"] = np.ones((128, 128)).astype(bf)
    c["onesf"] = np.ones((128, 128), np.float32)
    c["ecb"] = np.tile((np.arange(32) * CAP).astype(np.float32)[None, :], (128, 1))
    c["trb"] = np.tile((NS + np.arange(128)).astype(np.float32)[:, None], (1, 32))
    n = np.arange(NF)
    m = np.where(n < L, n, NF - n).astype(np.float64)
    m[L] = 0
    t = (m / (L - 1)).astype(np.float32)
    w = (2.0 * np.pi / L) * m
    f = np.linspace(1e-4, 15, 16)[None, :]
    emb = np.concatenate([t[:, None], np.cos(f * w[:, None]), -np.sin(f * w[:, None])], -1).astype(np.float32)
    c["emb2"] = np.concatenate([emb[:L].T, emb[L:].T], 0).astype(np.float32)
    tn = -t.astype(np.float32)
    tn[L] = -1e4
    c["tneg"] = np.ascontiguousarray(tn.reshape(2, 128, 32).transpose(1, 0, 2)).reshape(128, 64)
    max_decay = np.log(1e-2) / 0.3
    min_decay = np.log(1e-2) / 1.5
    deltas = np.abs(np.linspace(min_decay, max_decay, 512)).astype(np.float32)
    c["deltab"] = np.tile(deltas[None, :], (128, 1))
    return c


CONST_SPECS = [
    ("fa", [128, 16384], BF16), ("gt", [128, 8192], BF16), ("tct", [128, 384], BF16), ("tcf", [128, 256], BF16),
    ("et", [128, 12288], BF16), ("bdt", [128, 256], BF16), ("ident", [128, 128], BF16), ("blk", [128, 128], BF16),
    ("tri", [128, 128], BF16), ("ones", [128, 128], BF16), ("onesf", [128, 128], F32), ("ecb", [128, 32], F32),
    ("trb", [128, 32], F32), ("emb2", [66, 4096], F32), ("tneg", [128, 64], F32), ("deltab", [128, 512], F32),
]
IN_SPECS = [
    ("x", [L, D], F32), ("w_in", [D, 2048], F32), ("g1b", [128, D], F32), ("g2b", [128, D], F32), ("gfb", [128, D], F32),
    ("cwp", [128, 36], F32), ("cbp", [128, 12], F32), ("fbp", [128, 4], F32), ("mgp", [128, 8], F32),
    ("w_out", [D, D], F32), ("wr", [D, 36], F32), ("brb", [128, 36], F32),
    ("w_gate", [32, D, 512], F32), ("w_up", [32, D, 512], F32), ("w_down", [32, 512, D], F32),
    ("fwin2", [66, 128], F32), ("fwmid2", [128, 256], F32), ("fq", [128, 3], F32), ("fbb", [128, 3], F32),
    ("fwout2", [128, 1024], F32),
]


def build_nc(stop_after=None, debug=False):
    nc = bass.Bass("TRN2", target_bir_lowering=False)
    T = {}
    for name, shape, dt in IN_SPECS + CONST_SPECS:
        T[name] = nc.dram_tensor(name, shape, dt, kind="ExternalInput").ap()
    out_d = nc.dram_tensor("out", [L, D], F32, kind="ExternalOutput").ap()
    skind = "ExternalOutput" if debug else "Internal"
    Pbuf = nc.dram_tensor("Pbuf", [1536, L], BF16, kind=skind).ap()
    wbuf = [nc.dram_tensor("wbuf%d" % i, [L, 512], BF16, kind=skind).ap() for i in range(2)]
    Hbuf = nc.dram_tensor("Hbuf", [4, 128, 8192], BF16, kind=skind).ap()
    yTbuf = nc.dram_tensor("yTbuf", [D, L], BF16, kind=skind).ap()
    x1buf = nc.dram_tensor("x1buf", [L, D], F32, kind=skind).ap()
    xg = nc.dram_tensor("xg", [NROWS, D], BF16, kind=skind).ap()
    Ybuf = nc.dram_tensor("Ybuf", [NROWS, D], F32, kind=skind).ap()
    dbgL = nc.dram_tensor("dbgL", [128, 32 * 40], F32, kind=skind).ap()

    with ExitStack() as st:
        em = Emit(nc, st)
        V, A, G, PE = nc.vector, nc.scalar, nc.gpsimd, nc.tensor
        ENG = {"dve": V, "act": A, "pool": G}

        def sbuf(stack, name, shape, dt):
            return stack.enter_context(nc.sbuf_tensor("sb_" + name, shape, dt))

        PS = [st.enter_context(nc.psum_tensor("ps%d" % i, [128, 512], F32)) for i in range(8)]
        ps_rr = [0]

        def ps_next():
            i = ps_rr[0]
            ps_rr[0] = (i + 1) % 8
            return PS[i], "ps%d" % i

        def mm(out, lhsT, rhs, start, stop, reads, pk):
            return em.op("pe", lambda: PE.matmul(out, lhsT=lhsT, rhs=rhs, start=start, stop=stop), reads=reads, writes=[pk])

        def tr(out, in_, reads, pk, kdim=128):
            idn = ident[:] if kdim == 128 else ident[0:kdim, 0:kdim]
            return em.op("pe", lambda: PE.transpose(out, in_, idn), reads=list(reads) + ["ident"], writes=[pk])

        def cp(e, out, in_, reads, writes):
            if e == "act":
                return em.op("act", lambda: A.copy(out=out, in_=in_), reads=reads, writes=writes)
            return em.op(e, lambda: ENG[e].tensor_copy(out=out, in_=in_), reads=reads, writes=writes)

        def tt(e, out, in0, in1, op, reads, writes):
            return em.op(e, lambda: ENG[e].tensor_tensor(out=out, in0=in0, in1=in1, op=op), reads=reads, writes=writes)

        def ts(e, out, in0, s1, s2, op0, op1, reads, writes):
            if op1 is None:
                return em.op(e, lambda: ENG[e].tensor_scalar(out=out, in0=in0, scalar1=s1, scalar2=None, op0=op0), reads=reads, writes=writes)
            return em.op(e, lambda: ENG[e].tensor_scalar(out=out, in0=in0, scalar1=s1, scalar2=s2, op0=op0, op1=op1), reads=reads, writes=writes)

        def stt(e, out, in0, scalar, in1, op0, op1, reads, writes):
            return em.op(e, lambda: ENG[e].scalar_tensor_tensor(out=out, in0=in0, scalar=scalar, in1=in1, op0=op0, op1=op1), reads=reads, writes=writes)

        def act(out, in_, func, reads, writes, **kw):
            return em.op("act", lambda: A.activation(out=out, in_=in_, func=func, **kw), reads=reads, writes=writes)

        def dma(q, out, in_, reads, writes):
            e = {"sp": nc.sync, "act": A, "pool": G}[q]
            return em.dma(q, lambda: e.dma_start(out=out, in_=in_), reads=reads, writes=writes)

        final_events = []

        ident = sbuf(st, "ident", [128, 128], BF16)
        blk = sbuf(st, "blk", [128, 128], BF16)
        tct = sbuf(st, "tct", [128, 3, 128], BF16)
        epst = sbuf(st, "epst", [128, 1], F32)
        rinvP = sbuf(st, "rinvP", [128, 4], F32)
        dma("sp", ident[:], T["ident"], [], ["ident"])
        dma("sp", blk[:], T["blk"], [], ["blk"])
        dma("sp", tct[:].rearrange("p a b -> p (a b)"), T["tct"], [], ["tct"])
        em.op("dve", lambda: V.memset(epst[:], EPS), writes=["epst"])

        ztp = sbuf(st, "ztp", [128, 2, D], BF16)
        em.op("pool", lambda: G.memset(ztp[:], 0.0), writes=["ztp"])
        zf_chunks = [(r0, min(256, NROWS - r0)) for r0 in range(0, NROWS, 256)]

        def zero_fill_some(n):
            for _ in range(n):
                if zf_chunks:
                    r0, nr = zf_chunks.pop(0)
                    dma("sp", xg[r0:r0 + nr, :].rearrange("(s p) f -> p s f", p=128), ztp[:, 0:nr // 128, :], ["ztp"], [("xg", r0)])

        def fft_fwd(src, npt, fa_t, bufA, bufB, srck, sink):
            for j0 in range(0, 32, 4):
                for cs in range(2):
                    ps, pk = ps_next()
                    for jj in range(4):
                        j = j0 + jj
                        for pt in range(npt):
                            mm(ps[:, jj * 128:(jj + 1) * 128], fa_t[:, pt, j, cs, :], src[:, pt * 32 + j, :],
                               pt == 0, pt == npt - 1, (srck if isinstance(srck, list) else [srck]) + ["fa"], pk)
                    cp("act" if cs == 0 else "dve",
                       bufA[:].rearrange("p a g (j c) -> p a g j c", c=4)[:, cs, :, j0:j0 + 4, :].rearrange("p g j c -> p j g c"),
                       ps[:].rearrange("p (j g c) -> p j g c", j=4, g=32), [pk], [("bufA", cs, j0)])
            for cs in range(2):
                for g0 in range(0, 32, 8):
                    ps, pk = ps_next()
                    psb = ps[:].bitcast(BF16)
                    for gg in range(8):
                        g = g0 + gg
                        tr(psb[:, gg * 128:(gg + 1) * 128], bufA[:, cs, g, :],
                           [("bufA", cs, j0) for j0 in range(0, 32, 4)], pk)
                    cp("act" if cs == 0 else "dve", bufB[:, cs, g0:g0 + 8, :], psb.rearrange("p (a b) -> p a b", b=128),
                       [pk], [("bufB", cs, g0)])
            for ch in range(8):
                psR, kR = ps_next()
                psI, kI = ps_next()
                bBf = bufB[:].rearrange("p a b c -> p a (b c)")
                br = bBf[:, 0, ch * 512:(ch + 1) * 512]
                bi = bBf[:, 1, ch * 512:(ch + 1) * 512]
                rk = [("bufB", 0, (4 * ch) // 8 * 8), ("bufB", 1, (4 * ch) // 8 * 8), "tct"]
                mm(psR[:], tct[:, 0, :], br, True, False, rk, kR)
                mm(psR[:], tct[:, 1, :], bi, False, True, rk, kR)
                mm(psI[:], tct[:, 0, :], bi, True, False, rk, kI)
                mm(psI[:], tct[:, 2, :], br, False, True, rk, kI)
                sink(ch, psR, kR, psI, kI)

        with ExitStack() as ph:
            fa_t = sbuf(ph, "fa_t", [128, 2, 32, 2, 128], BF16)
            w1t = sbuf(ph, "w1t", [66, 128], F32)
            wmt = sbuf(ph, "wmt", [128, 2, 128], F32)
            fq = sbuf(ph, "fq", [128, 3], F32)
            fbb = sbuf(ph, "fbb", [128, 3], F32)
            fq2 = sbuf(ph, "fq2", [128, 3], F32)
            fqb2 = sbuf(ph, "fqb2", [128, 3], F32)
            fwo = sbuf(ph, "fwo", [128, 1024], BF16)
            onesf = sbuf(ph, "onesf", [128, 128], F32)
            tneg = sbuf(ph, "tneg", [128, 64], F32)
            deltab = sbuf(ph, "deltab", [128, 512], F32)
            hid = sbuf(ph, "hid", [128, L], BF16)
            dma("sp", fa_t[:].rearrange("p a b c d -> p (a b c d)"), T["fa"], [], ["fa"])
            dma("sp", w1t[:], T["fwin2"], [], ["w1t"])
            dma("sp", wmt[:].rearrange("p a b -> p (a b)"), T["fwmid2"], [], ["wmt"])
            dma("sp", fq[:], T["fq"], [], ["fq"])
            dma("sp", fbb[:], T["fbb"], [], ["fbb"])
            dma("pool", fwo[:], T["fwout2"], [], ["fwo"])
            dma("sp", onesf[:], T["onesf"], [], ["onesf"])
            dma("sp", tneg[:], T["tneg"], [], ["tneg"])
            dma("sp", deltab[:], T["deltab"], [], ["deltab"])
            ts("dve", fq2[:], fq[:], float(1.0 / 3.0), None, ALU.mult, None, ["fq"], ["fq2"])
            tt("dve", fqb2[:], fq2[:], fbb[:], ALU.mult, ["fq2", "fbb"], ["fqb2"])
            with ExitStack() as ph2:
                emb = sbuf(ph2, "emb", [66, L], F32)
                hA = sbuf(ph2, "hA", [128, L], F32)
                hB = sbuf(ph2, "hB", [128, L], F32)
                for ch in range(8):
                    dma("sp", emb[:, ch * 512:(ch + 1) * 512], T["emb2"][:, ch * 512:(ch + 1) * 512], [], [("emb", ch)])
                ub = [sbuf(ph2, "ub%d" % i, [128, 512], F32) for i in range(8)]
                rb = [sbuf(ph2, "rb%d" % i, [128, 512], F32) for i in range(8)]
                srcs = [(emb, "emb", w1t[:]), (hA, "hA", wmt[:, 0, :]), (hB, "hB", wmt[:, 1, :])]
                dsts = [(hA, "hA"), (hB, "hB"), (hid, "hid")]
                for l in range(3):
                    s_t, s_k, lhsT = srcs[l]
                    d_t, d_k = dsts[l]
                    pss = {}
                    for ch in range(8):
                        ps, pk = ps_next()
                        pss[ch] = (ps, pk)
                        mm(ps[:], lhsT, s_t[:, ch * 512:(ch + 1) * 512], True, True, [(s_k, ch), "w1t", "wmt"], pk)
                    for ch in range(8):
                        ps, pk = pss[ch]
                        act(ub[ch][:], ps[:], AF.Sin, [pk, "fq2", "fqb2"], [("ub", ch)], scale=fq2[:, l:l + 1], bias=fqb2[:, l:l + 1])
                    for ch in range(8):
                        tt("dve", rb[ch][:], ub[ch][:], ub[ch][:], ALU.mult, [("ub", ch)], [("rb", ch)])
                    for ch in range(8):
                        ts("dve", rb[ch][:], rb[ch][:], -4.0, 3.0, ALU.mult, ALU.add, [("rb", ch)], [("rb", ch)])
                    for ch in range(8):
                        tt("dve", d_t[:, ch * 512:(ch + 1) * 512], rb[ch][:], ub[ch][:], ALU.mult, [("rb", ch), ("ub", ch)], [(d_k, ch)])
                em.barrier()
            with ExitStack() as ph2:
                decs = [sbuf(ph2, "dec%d" % i, [128, 64, 128], BF16) for i in range(2)]
                kbs = [sbuf(ph2, "kb%d" % i, [128, 64, 128], BF16) for i in range(2)]
                acc = sbuf(ph2, "acc", [128, 128], F32)
                bufA = sbuf(ph2, "bufA", [128, 2, 32, 128], BF16)
                bufB = sbuf(ph2, "bufB", [128, 2, 32, 128], BF16)
                Hsb = sbuf(ph2, "Hsb", [128, 2, L], BF16)

                def dec_gen(hc):
                    for col in range(64):
                        act(decs[hc % 2][:, col, :], deltab[:, hc * 128:(hc + 1) * 128], AF.Exp, ["deltab", "tneg"], [("dec", hc % 2, col // 4)],
                            scale=tneg[:, col:col + 1])

                def out_layer(hc):
                    dec = decs[hc % 2]
                    kb = kbs[hc % 2]
                    for c0 in range(0, 64, 4):
                        ps, pk = ps_next()
                        for cc in range(4):
                            col = c0 + cc
                            pt, j = col // 32, col % 32
                            mm(ps[:, cc * 128:(cc + 1) * 128], hid[64 * pt:64 * pt + 64, j::32],
                               fwo[64 * pt:64 * pt + 64, pt * 512 + hc * 128:pt * 512 + (hc + 1) * 128], True, True,
                               [("hid", ch) for ch in range(8)] + ["fwo"], pk)
                        tt("dve", kb[:, c0:c0 + 4, :], ps[:].rearrange("p (a b) -> p a b", b=128), dec[:, c0:c0 + 4, :], ALU.mult,
                           [pk, ("dec", hc % 2, c0 // 4)], [("kb", hc % 2, c0 // 4)])

                def sinkH(ch, psR, kR, psI, kI):
                    cp("act", Hsb[:, 0, ch * 512:(ch + 1) * 512], psR[:], [kR], [("Hsb", ch)])
                    cp("dve", Hsb[:, 1, ch * 512:(ch + 1) * 512], psI[:], [kI], [("Hsb", ch)])

                dec_gen(0)
                out_layer(0)
                for hc in range(4):
                    kb = kbs[hc % 2]
                    kbk = [("kb", hc % 2, i) for i in range(16)]
                    if hc + 1 < 4:
                        dec_gen(hc + 1)
                        out_layer(hc + 1)
                    fft_fwd(kb, 2, fa_t, bufA, bufB, kbk, sinkH)
                    dma("sp", Hbuf[hc], Hsb[:].rearrange("p a b -> p (a b)"), [("Hsb", ch) for ch in range(8)], [("Hbuf", hc)])
                    em.op("dve", lambda: V.tensor_reduce(out=acc[:], in_=kb[:].rearrange("p n c -> p c n"), axis=AX.X, op=ALU.add,
                                                         apply_absolute_value=True), reads=kbk, writes=["acc"])
                    ps, pk = ps_next()
                    mm(ps[:, 0:1], acc[:], onesf[:, 0:1], True, True, ["onesf", "acc"], pk)
                    em.op("dve", lambda: V.reciprocal(out=rinvP[:, hc:hc + 1], in_=ps[:, 0:1]), reads=[pk], writes=[("rinvP", hc)])
                em.barrier()
        if stop_after == "F0":
            em.barrier()
            return nc

        with ExitStack() as ph:
            Wb = sbuf(ph, "Wb", [128, 8, 2048], BF16)
            Wf = sbuf(ph, "Wf", [128, 2, 8, 512], BF16)
            g1b = sbuf(ph, "g1b", [128, D], F32)
            for kc in range(8):
                for h2_ in range(2):
                    dma("pool", Wb[:, kc, h2_ * 1024:(h2_ + 1) * 1024], T["w_in"][kc * 128:(kc + 1) * 128, h2_ * 1024:(h2_ + 1) * 1024],
                        [], [("Wb", kc, h2_)])
            dma("sp", g1b[:], T["g1b"], [], ["g1b"])
            if True:
                bdt = sbuf(ph, "bdt", [128, 2, 128], BF16)
                WfT = sbuf(ph, "WfT", [128, 4, D], BF16)
                dma("sp", bdt[:].rearrange("p a b -> p (a b)"), T["bdt"], [], ["bdt"])
                for n4 in range(4):
                    ps, pk = ps_next()
                    psb = ps[:].bitcast(BF16)
                    for kc in range(8):
                        tr(psb[:, kc * 128:(kc + 1) * 128], Wb[:, kc, 1536 + n4 * 128:1536 + (n4 + 1) * 128], [("Wb", kc, 0), ("Wb", kc, 1)], pk)
                    cp("act", WfT[:, n4, :], psb, [pk], [("WfT", n4)])
                for part in range(2):
                    for kc in range(8):
                        ps, pk = ps_next()
                        for n4 in range(4):
                            mm(ps[:, n4 * 128:(n4 + 1) * 128], WfT[:, n4, kc * 128:(kc + 1) * 128], bdt[:, part, :], True, True,
                               [("WfT", n4), "bdt"], pk)
                        cp("dve", Wf[:, part, kc, :], ps[:], [pk], [("Wf", part, kc)])
            xt = [sbuf(ph, "xt%d" % i, [128, D], F32) for i in range(4)]
            junk = sbuf(ph, "junk", [128, D], BF16)
            ss = sbuf(ph, "ss", [128, 32], F32)
            rs = sbuf(ph, "rs", [128, 32], F32)
            hb = [sbuf(ph, "hb%d" % i, [128, D], BF16) for i in range(2)]
            hTc = [sbuf(ph, "hTc%d" % i, [128, 8, 512], BF16) for i in range(2)]
            Pst = [sbuf(ph, "Pst%d" % i, [128, 12, 512], BF16) for i in range(2)]
            wst = [sbuf(ph, "wst%d" % i, [128, 2, 512], BF16) for i in range(2)]
            em.op("dve", lambda: V.memset(ss[:], 0.0), writes=["ss"])
            ev_cnt = [0]

            def a_load(j2):
                dma("sp", xt[j2 % 4][:], T["x"][j2 * 128:(j2 + 1) * 128, :], [], [("xt", j2 % 4)])
                zero_fill_some(2)

            def a_norm(tc, i):
                hb_ = tc % 2
                j2 = 4 * tc + i
                b = j2 % 2
                if j2 + 3 < 32:
                    a_load(j2 + 3)
                xb = j2 % 4
                act(junk[:], xt[xb][:], AF.Square, [("xt", xb), "ss"], ["junk", ("ss", j2)], accum_out=ss[:, j2:j2 + 1])
                act(rs[:, j2:j2 + 1], ss[:, j2:j2 + 1], AF.Sqrt, [("ss", j2), "epst"], [("rs", j2)], scale=1.0 / D, bias=epst[:])
                em.op("dve", lambda: V.reciprocal(out=rs[:, j2:j2 + 1], in_=rs[:, j2:j2 + 1]), reads=[("rs", j2)], writes=[("rs", j2)])
                stt("dve", hb[b][:], xt[xb][:], rs[:, j2:j2 + 1], g1b[:], ALU.mult, ALU.mult, [("xt", xb), ("rs", j2), "g1b"], [("hb", b)])
                ps, pk = ps_next()
                psb = ps[:].bitcast(BF16)
                for kc in range(8):
                    tr(psb[:, kc * 128:(kc + 1) * 128], hb[b][:, kc * 128:(kc + 1) * 128], [("hb", b)], pk)
                cp("act", hTc[hb_][:, :, i * 128:(i + 1) * 128], psb.rearrange("p (a b) -> p a b", b=128), [pk], [("hTc", hb_, i)])

            def a_mm(tc, part):
                hb_ = tc % 2
                hk = [("hTc", hb_, i) for i in range(4)]
                for cch in range(3 * part, 3 * part + 3):
                    ps, pk = ps_next()
                    for kc in range(8):
                        mm(ps[:], Wb[:, kc, cch * 128:(cch + 1) * 128], hTc[hb_][:, kc, :], kc == 0, kc == 7, hk + [("Wb", kc, 0), ("Wb", kc, 1)], pk)
                    ev_cnt[0] += 1
                    cp("act" if ev_cnt[0] % 2 else "dve", Pst[hb_][:, cch, :], ps[:], [pk], [("Pst", hb_, cch)])
                if part == 3:
                    dma("pool", Pbuf[:, tc * 512:(tc + 1) * 512].rearrange("(c p) t -> p c t", p=128), Pst[hb_][:],
                        [("Pst", hb_, c_) for c_ in range(12)], [("Pbuf", tc)])
                i = part
                j2 = 4 * tc + i
                wb_ = j2 % 2
                for fpart in range(2):
                    ps, pk = ps_next()
                    for kc in range(8):
                        mm(ps[:], hTc[hb_][:, kc, i * 128:(i + 1) * 128], Wf[:, fpart, kc, :], kc == 0, kc == 7,
                           [("hTc", hb_, i), ("Wf", fpart, kc)], pk)
                    ev_cnt[0] += 1
                    cp("act" if ev_cnt[0] % 2 else "dve", wst[wb_][:, fpart, :], ps[:], [pk], [("wst", wb_, fpart)])
                    dma("pool", wbuf[fpart][j2 * 128:(j2 + 1) * 128, :], wst[wb_][:, fpart, :], [("wst", wb_, fpart)], [("wbuf", fpart, j2)])

            for j2_ in range(3):
                a_load(j2_)
            for i in range(4):
                a_norm(0, i)
            for tc in range(8):
                for part in range(4):
                    if tc + 1 < 8:
                        a_norm(tc + 1, part)
                    a_mm(tc, part)
            em.barrier()
        if stop_after == "A":
            return nc

        def head_norm_wave(rs_, mg_ap, row0, rsqs, rstdb):
            for q in range(4):
                act(rsqs[q][:], rs_[q][0], AF.Square, [rs_[q][1]], [("rsq", q)])
            for q in range(4):
                for h_ in range(2):
                    ps, pk = ps_next()
                    mm(ps[:], blk[:], rsqs[q][:, h_ * 512:(h_ + 1) * 512], True, True, [("rsq", q), "blk"], pk)
                    act(rstdb[q][:, h_ * 512:(h_ + 1) * 512], ps[:], AF.Ln, [pk, "epst"], [("rstd_t", q, h_)], bias=epst[:], scale=1.0)
            for q in range(4):
                act(rstdb[q][:], rstdb[q][:], AF.Exp, [("rstd_t", q, 0), ("rstd_t", q, 1)], [("rstd_t", q, 0), ("rstd_t", q, 1)], scale=-0.5)
            for q in range(4):
                stt("dve", rsqs[q][:], rs_[q][0], mg_ap, rstdb[q][:], ALU.mult, ALU.mult,
                    [rs_[q][1], ("rstd_t", q, 0), ("rstd_t", q, 1), "mgp"], [("rsq", q)])
                dma("sp", yTbuf[row0:row0 + 128, q * 1024:(q + 1) * 1024], rsqs[q][:], [("rsq", q)], [("yTbuf", row0, q)])

        with ExitStack() as ph:
            fa_t = sbuf(ph, "fa_tB", [128, 32, 2, 128], BF16)
            gt_t = sbuf(ph, "gt_t", [128, 32, 2, 128], BF16)
            cwp = sbuf(ph, "cwp", [128, 12, 3], F32)
            cbp = sbuf(ph, "cbp", [128, 12], F32)
            fbp = sbuf(ph, "fbp", [128, 4], F32)
            mgp = sbuf(ph, "mgp", [128, 8], F32)
            dma("sp", fa_t[:].rearrange("p b c d -> p (b c d)"), T["fa"][:, 0:8192], [], ["fa"])
            dma("sp", gt_t[:].rearrange("p b c d -> p (b c d)"), T["gt"], [], ["gt"])
            dma("sp", cwp[:].rearrange("p a b -> p (a b)"), T["cwp"], [], ["cwp"])
            dma("sp", cbp[:], T["cbp"], [], ["cbp"])
            dma("sp", fbp[:], T["fbp"], [], ["fbp"])
            dma("sp", mgp[:], T["mgp"], [], ["mgp"])
            Pt = [sbuf(ph, "Pt%d" % s, [128, L + 2], BF16) for s in range(3)]
            Hsb = sbuf(ph, "HsbB", [128, 2, L], BF16)
            ux0s = [sbuf(ph, "ux0_%d" % i, [128, L], BF16) for i in range(2)]
            zTs = [sbuf(ph, "zT_%d" % i, [128, L], BF16) for i in range(2)]
            tA = sbuf(ph, "tA", [128, 1024], F32)
            tB = sbuf(ph, "tB", [128, 1024], F32)
            tC = sbuf(ph, "tC", [128, 1024], F32)
            zP1 = sbuf(ph, "zP1", [128, 32, 128], BF16)
            bufA = sbuf(ph, "bufAB", [128, 2, 32, 128], BF16)
            bufB = sbuf(ph, "bufBB", [128, 2, 32, 128], BF16)
            yc = sbuf(ph, "yc", [128, L], BF16)
            rsqs = [sbuf(ph, "rsq%d" % i, [128, 1024], BF16) for i in range(4)]
            rstdb = [sbuf(ph, "rstdb%d" % i, [128, 1024], BF16) for i in range(4)]
            tD = sbuf(ph, "tD", [128, 1024], F32)
            mts = [[sbuf(ph, "mt%d_%d" % (i, k), [128, 512], F32) for k in range(4)] for i in range(2)]
            for s in range(3):
                em.op("dve", lambda s=s: V.memset(Pt[s][:, 0:1], 0.0), writes=[("Ppad", s)])
                em.op("dve", lambda s=s: V.memset(Pt[s][:, L + 1:L + 2], 0.0), writes=[("Ppad", s)])
            fa5 = fa_t[:].rearrange("p (a b) c d -> p a b c d", a=1)
            def load_P(hc):
                for s in range(3):
                    dma("sp", Pt[s][:, 1:L + 1], Pbuf[(s * 4 + hc) * 128:(s * 4 + hc + 1) * 128, :], [], [("Pt", s)])

            def load_H(hc):
                dma("sp", Hsb[:].rearrange("p a b -> p (a b)"), Hbuf[hc], [], ["HsbB"])

            def conv(hc):
                ux0 = ux0s[hc % 2]
                zT = zTs[hc % 2]
                par = hc % 2
                for q in range(4):
                    t0 = q * 1024
                    tmps = [(tA, "tA"), (tB, "tB"), (tC, "tC")]
                    cc = hc
                    pk_ = [("Pt", 0), ("Ppad", 0), "cwp", "cbp"]
                    act(tA[:], Pt[0][:, 1 + t0:1 + t0 + 1024], AF.Identity, pk_, ["tA"], scale=cwp[:, cc, 1:2], bias=cbp[:, cc:cc + 1])
                    act(tD[:], Pt[0][:, t0:t0 + 1024], AF.Identity, pk_, ["tD"], scale=cwp[:, cc, 0:1])
                    tt("pool", tA[:], tA[:], tD[:], ALU.add, ["tA", "tD"], ["tA"])
                    act(tD[:], Pt[0][:, 2 + t0:2 + t0 + 1024], AF.Identity, pk_, ["tD"], scale=cwp[:, cc, 2:3])
                    tt("pool", ux0[:, t0:t0 + 1024], tA[:], tD[:], ALU.add, ["tA", "tD"], [("ux0", par, q)])
                    for s in (1, 2):
                        cc = s * 4 + hc
                        pk_ = [("Pt", s), ("Ppad", s), "cwp", "cbp"]
                        tmp, tk = tmps[s]
                        if s == 2:
                            act(tmp[:], Pt[s][:, 1 + t0:1 + t0 + 1024], AF.Identity, pk_, [tk], scale=cwp[:, cc, 1:2], bias=cbp[:, cc:cc + 1])
                        else:
                            ts("dve", tmp[:], Pt[s][:, 1 + t0:1 + t0 + 1024], cwp[:, cc, 1:2], cbp[:, cc:cc + 1], ALU.mult, ALU.add, pk_, [tk])
                        stt("dve", tmp[:], Pt[s][:, t0:t0 + 1024], cwp[:, cc, 0:1], tmp[:], ALU.mult, ALU.add, pk_ + [tk], [tk])
                        stt("dve", tmp[:], Pt[s][:, 2 + t0:2 + t0 + 1024], cwp[:, cc, 2:3], tmp[:], ALU.mult, ALU.add, pk_ + [tk], [tk])
                    tt("dve", zT[:, t0:t0 + 1024], tB[:], tC[:], ALU.mult, ["tB", "tC"], [("zT", par, q)])

            def fwd(hc):
                zT = zTs[hc % 2]
                par = hc % 2
                zk = [("zT", par, q) for q in range(4)]
                for j0 in range(0, 32, 8):
                    ps, pk = ps_next()
                    psb = ps[:].bitcast(BF16)
                    for jj in range(8):
                        tr(psb[:, jj * 128:(jj + 1) * 128], zT[:, j0 + jj::32], zk, pk)
                    cp("act", zP1[:, j0:j0 + 8, :], psb.rearrange("p (a b) -> p a b", b=128), [pk], ["zP1"])

                fft_fwd_keys_A = [("bufA", cs, j0) for cs in range(2) for j0 in range(0, 32, 4)]

                def sinkY_guard(ch, psR, kR, psI, kI):
                    sl = slice(ch * 512, (ch + 1) * 512)
                    ya = bufA[:].rearrange("p a b c -> p a (b c)")
                    m = mts[ch % 2]
                    mk = [("mt", ch % 2, k) for k in range(4)]
                    tt("dve", m[0][:], psR[:], Hsb[:, 0, sl], ALU.mult, [kR, "HsbB"], [mk[0]])
                    tt("dve", m[1][:], psI[:], Hsb[:, 1, sl], ALU.mult, [kI, "HsbB"], [mk[1]])
                    tt("dve", m[2][:], psR[:], Hsb[:, 1, sl], ALU.mult, [kR, "HsbB"], [mk[2]])
                    tt("dve", m[3][:], psI[:], Hsb[:, 0, sl], ALU.mult, [kI, "HsbB"], [mk[3]])
                    tt("pool", ya[:, 0, sl], m[0][:], m[1][:], ALU.subtract, [mk[0], mk[1]], [("Y", ch)] + fft_fwd_keys_A)
                    tt("pool", ya[:, 1, sl], m[2][:], m[3][:], ALU.add, [mk[2], mk[3]], [("Y", ch)])

                fft_fwd(zP1, 1, fa5, bufA, bufB, "zP1", sinkY_guard)

            def inv(hc):
                ux0 = ux0s[hc % 2]
                zT = zTs[hc % 2]
                par = hc % 2
                zk = [("zT", par, q) for q in range(4)]
                ya = bufA[:].rearrange("p a b c -> p a (b c)")
                bufB_keys = [("bufB", cs, g0) for cs in range(2) for g0 in range(0, 32, 8)]
                for ch in range(8):
                    sl = slice(ch * 512, (ch + 1) * 512)
                    psR, kR = ps_next()
                    psI, kI = ps_next()
                    rk = [("Y", ch), "tct"]
                    mm(psR[:], tct[:, 0, :], ya[:, 0, sl], True, False, rk, kR)
                    mm(psR[:], tct[:, 2, :], ya[:, 1, sl], False, True, rk, kR)
                    mm(psI[:], tct[:, 1, :], ya[:, 0, sl], True, False, rk, kI)
                    mm(psI[:], tct[:, 0, :], ya[:, 1, sl], False, True, rk, kI)
                    wk = [("Cb", ch)] + (bufB_keys if ch == 0 else [])
                    cp("act", bufB[:, 0, 4 * ch:4 * ch + 4, :], psR[:].rearrange("p (a b) -> p a b", b=128), [kR], wk)
                    cp("dve", bufB[:, 1, 4 * ch:4 * ch + 4, :], psI[:].rearrange("p (a b) -> p a b", b=128), [kI], [("Cb", ch)])
                first = True
                for cs in range(2):
                    for g0 in range(0, 32, 8):
                        ps, pk = ps_next()
                        psb = ps[:].bitcast(BF16)
                        for gg in range(8):
                            g = g0 + gg
                            tr(psb[:, gg * 128:(gg + 1) * 128], bufB[:, cs, g, :], [("Cb", g // 4)], pk)
                        wk = [("Ct", cs, g0)] + ([("Y", ch) for ch in range(8)] if first else [])
                        first = False
                        cp("act" if cs == 0 else "dve", bufA[:, cs, :, 4 * g0:4 * g0 + 32].rearrange("p j (g c) -> p g j c", c=4),
                           psb.rearrange("p (g j c) -> p g j c", g=8, j=32), [pk], wk)
                ctk = [("Ct", cs, g0) for cs in range(2) for g0 in range(0, 32, 8)]
                yc3 = yc[:].rearrange("c (p j) -> c p j", j=32)
                for j0 in range(0, 32, 4):
                    ps, pk = ps_next()
                    for jj in range(4):
                        j = j0 + jj
                        mm(ps[:, jj * 128:(jj + 1) * 128], bufA[:, 0, j, :], gt_t[:, j, 0, :], True, False, ctk + ["gt"], pk)
                        mm(ps[:, jj * 128:(jj + 1) * 128], bufA[:, 1, j, :], gt_t[:, j, 1, :], False, True, ctk + ["gt"], pk)
                    act(yc3[:, :, j0:j0 + 4].rearrange("c p j -> c j p"), ps[:].rearrange("c (j p) -> c j p", p=128), AF.Identity,
                        [pk, ("rinvP", hc)], [("yc", j0)], scale=rinvP[:, hc:hc + 1])
                yck = [("yc", j0) for j0 in range(0, 32, 4)]
                tq = [(tA, "tA"), (tB, "tB"), (tC, "tC"), (tD, "tD")]
                for q in range(4):
                    t0 = q * 1024
                    stt("dve", tq[q][0][:], zT[:, t0:t0 + 1024], fbp[:, hc:hc + 1], yc[:, t0:t0 + 1024], ALU.mult, ALU.add,
                        zk + yck + ["fbp"], [tq[q][1]])
                for q in range(4):
                    t0 = q * 1024
                    tt("pool", tq[q][0][:], tq[q][0][:], ux0[:, t0:t0 + 1024], ALU.mult, [tq[q][1], ("ux0", par, q)], [tq[q][1]])
                head_norm_wave([(tq[q][0][:], tq[q][1]) for q in range(4)], mgp[:, hc:hc + 1], hc * 128, rsqs, rstdb)

            load_P(0)
            load_H(0)
            conv(0)
            load_P(1)
            for hc in range(4):
                fwd(hc)
                if hc + 1 < 4:
                    load_H(hc + 1)
                    conv(hc + 1)
                    if hc + 2 < 4:
                        load_P(hc + 2)
                inv(hc)
            em.barrier()
        if stop_after == "B":
            return nc

        with ExitStack() as ph:
            et_t = sbuf(ph, "et_t", [128, 32, 3, 128], BF16)
            tcf = sbuf(ph, "tcf", [128, 2, 128], BF16)
            mgp = sbuf(ph, "mgpC", [128, 8], F32)
            dma("sp", et_t[:].rearrange("p b c d -> p (b c d)"), T["et"], [], ["et"])
            dma("sp", tcf[:].rearrange("p a b -> p (a b)"), T["tcf"], [], ["tcf"])
            dma("sp", mgp[:], T["mgp"], [], ["mgp"])
            wris = [sbuf(ph, "wri%d" % i, [128, 2, 32, 128], BF16) for i in range(2)]
            bufA = sbuf(ph, "bufAC", [128, 2, 32, 128], BF16)
            bufB = sbuf(ph, "bufBC", [128, 2, 32, 128], BF16)
            yf = sbuf(ph, "yf", [128, 32, 128], BF16)
            rTs = [sbuf(ph, "rT%d" % i, [128, 1024], F32) for i in range(4)]
            rsqs = [sbuf(ph, "rsqC%d" % i, [128, 1024], BF16) for i in range(4)]
            rstdb = [sbuf(ph, "rstdbC%d" % i, [128, 1024], BF16) for i in range(4)]
            def c_load(fc):
                for part in range(2):
                    dma("sp", wris[fc % 2][:, part, :, :], wbuf[part][:, fc * 128:(fc + 1) * 128].rearrange("(p j) c -> p j c", j=32), [],
                        [("wri", fc % 2, part)])

            c_load(0)
            for fc in range(4):
                wri = wris[fc % 2]
                if fc + 1 < 4:
                    c_load(fc + 1)
                for j0 in range(0, 32, 4):
                    for cs in range(2):
                        ps, pk = ps_next()
                        for jj in range(4):
                            j = j0 + jj
                            if cs == 0:
                                mm(ps[:, jj * 128:(jj + 1) * 128], et_t[:, j, 0, :], wri[:, 0, j, :], True, False, [("wri", fc % 2, 0), ("wri", fc % 2, 1), "et"], pk)
                                mm(ps[:, jj * 128:(jj + 1) * 128], et_t[:, j, 1, :], wri[:, 1, j, :], False, True, [("wri", fc % 2, 0), ("wri", fc % 2, 1), "et"], pk)
                            else:
                                mm(ps[:, jj * 128:(jj + 1) * 128], et_t[:, j, 0, :], wri[:, 1, j, :], True, False, [("wri", fc % 2, 0), ("wri", fc % 2, 1), "et"], pk)
                                mm(ps[:, jj * 128:(jj + 1) * 128], et_t[:, j, 2, :], wri[:, 0, j, :], False, True, [("wri", fc % 2, 0), ("wri", fc % 2, 1), "et"], pk)
                        cp("act" if cs == 0 else "dve",
                           bufA[:].rearrange("p a g (j c) -> p a g j c", c=4)[:, cs, :, j0:j0 + 4, :].rearrange("p g j c -> p j g c"),
                           ps[:].rearrange("p (j g c) -> p j g c", j=4, g=32), [pk], [("bufA", cs, j0)])
                ak = [("bufA", cs, j0) for cs in range(2) for j0 in range(0, 32, 4)]
                for cs in range(2):
                    for g0 in range(0, 32, 8):
                        ps, pk = ps_next()
                        psb = ps[:].bitcast(BF16)
                        for gg in range(8):
                            g = g0 + gg
                            tr(psb[:, gg * 128:(gg + 1) * 128], bufA[:, cs, g, :], ak, pk)
                        cp("act" if cs == 0 else "dve", bufB[:, cs, g0:g0 + 8, :], psb.rearrange("p (a b) -> p a b", b=128), [pk], [("bufB", cs, g0)])
                for g0 in range(0, 32, 4):
                    ps, pk = ps_next()
                    for gg in range(4):
                        g = g0 + gg
                        rk = [("bufB", 0, g // 8 * 8), ("bufB", 1, g // 8 * 8), "tcf"]
                        mm(ps[:, gg * 128:(gg + 1) * 128], bufB[:, 0, g, :], tcf[:, 0, :], True, False, rk, pk)
                        mm(ps[:, gg * 128:(gg + 1) * 128], bufB[:, 1, g, :], tcf[:, 1, :], False, True, rk, pk)
                    cp("act" if (g0 // 4) % 2 else "dve", yf[:, :, 4 * g0:4 * g0 + 16].rearrange("p k (g c) -> p g k c", c=4),
                       ps[:].rearrange("p (g k c) -> p g k c", g=4, k=32), [pk], [("yf", g0)])
                yfk = [("yf", g0) for g0 in range(0, 32, 4)]
                for q in range(4):
                    ps, pk = ps_next()
                    psb = ps[:].bitcast(BF16)
                    for kk_ in range(8):
                        kb_ = q * 8 + kk_
                        tr(psb[:, kk_ * 128:(kk_ + 1) * 128], yf[:, kb_, :], yfk, pk)
                    cp("dve" if q % 2 else "act", rTs[q][:], psb, [pk], [("rT", q)])
                head_norm_wave([(rTs[q][:], ("rT", q)) for q in range(4)], mgp[:, 4 + fc:5 + fc], 512 + fc * 128, rsqs, rstdb)
            em.barrier()
        if stop_after == "C":
            return nc

        ph_moe = st.enter_context(ExitStack())
        w1 = sbuf(ph_moe, "w1", [128, 32], F32)
        w2 = sbuf(ph_moe, "w2", [128, 32], F32)
        ds_i = sbuf(ph_moe, "ds_i", [128, 2, 32], I32)
        dg_i = sbuf(ph_moe, "dg_i", [128, 2, 32], I32)
        ph_tok = ExitStack()
        h2tok = sbuf(ph_tok, "h2tok", [128, 32, D], BF16)
        Lg = sbuf(ph_tok, "Lg", [128, 32, 36], F32)
        with ExitStack() as ph:
            Wo = sbuf(ph, "Wo", [128, 8, D], BF16)
            Wr = sbuf(ph, "Wr", [128, 8, 36], BF16)
            g2b = sbuf(ph, "g2b", [128, D], F32)
            brb = sbuf(ph, "brb", [128, 36], F32)
            for kc in range(8):
                dma("pool", Wo[:, kc, :], T["w_out"][kc * 128:(kc + 1) * 128, :], [], [("Wo", kc)])
            dma("pool", Wr[:], T["wr"].rearrange("(k p) n -> p k n", p=128), [], ["Wr"])
            dma("sp", g2b[:], T["g2b"], [], ["g2b"])
            dma("sp", brb[:], T["brb"], [], ["brb"])
            yTb = [sbuf(ph, "yTb%d" % i, [128, 8, 512], BF16) for i in range(2)]
            xt = [sbuf(ph, "xtD%d" % i, [128, D], F32) for i in range(3)]
            x1t = [sbuf(ph, "x1t%d" % i, [128, D], F32) for i in range(3)]
            junk = sbuf(ph, "junkD", [128, D], BF16)
            ss = sbuf(ph, "ssD", [128, 32], F32)
            rs = sbuf(ph, "rsD", [128, 32], F32)
            h2T = [sbuf(ph, "h2T%d" % i, [128, 8, 128], BF16) for i in range(2)]
            em.op("dve", lambda: V.memset(ss[:], 0.0), writes=["ssD"])
            wo_ps = {}

            def d_front(j2):
                tc, i = j2 // 4, j2 % 4
                yb = tc % 2
                b = j2 % 3
                if i == 0:
                    dma("sp", yTb[yb][:], yTbuf[:, tc * 512:(tc + 1) * 512].rearrange("(c p) t -> p c t", p=128), [], [("yTb", yb)])
                dma("sp", xt[b][:], T["x"][j2 * 128:(j2 + 1) * 128, :], [], [("xtD", b)])
                for half in range(2):
                    ps, pk = ps_next()
                    for cc in range(8):
                        mm(ps[:], yTb[yb][:, cc, i * 128:(i + 1) * 128], Wo[:, cc, half * 512:(half + 1) * 512], cc == 0, cc == 7,
                           [("yTb", yb), ("Wo", cc)], pk)
                    wo_ps[(j2, half)] = (ps, pk)

            def d_back(j2):
                b = j2 % 3
                for half in range(2):
                    ps, pk = wo_ps.pop((j2, half))
                    tt("dve", x1t[b][:, half * 512:(half + 1) * 512], xt[b][:, half * 512:(half + 1) * 512], ps[:], ALU.add,
                       [("xtD", b), pk], [("x1t", b, half)])
                xk = [("x1t", b, 0), ("x1t", b, 1)]
                dma("sp", x1buf[j2 * 128:(j2 + 1) * 128, :], x1t[b][:], xk, [("x1buf", j2)])
                act(junk[:], x1t[b][:], AF.Square, xk + ["ssD"], ["junkD", ("ssD", j2)], accum_out=ss[:, j2:j2 + 1])
                act(rs[:, j2:j2 + 1], ss[:, j2:j2 + 1], AF.Sqrt, [("ssD", j2), "epst"], [("rsD", j2)], scale=1.0 / D, bias=epst[:])
                em.op("dve", lambda: V.reciprocal(out=rs[:, j2:j2 + 1], in_=rs[:, j2:j2 + 1]), reads=[("rsD", j2)], writes=[("rsD", j2)])
                stt("dve", h2tok[:, j2, :], x1t[b][:], rs[:, j2:j2 + 1], g2b[:], ALU.mult, ALU.mult, xk + [("rsD", j2), "g2b"], [("h2tok", j2)])
                ps, pk = ps_next()
                psb = ps[:].bitcast(BF16)
                for kc in range(8):
                    tr(psb[:, kc * 128:(kc + 1) * 128], h2tok[:, j2, kc * 128:(kc + 1) * 128], [("h2tok", j2)], pk)
                cp("act", h2T[j2 % 2][:], psb.rearrange("p (a b) -> p a b", b=128), [pk], [("h2T", j2 % 2)])
                ps, pk = ps_next()
                for kc in range(8):
                    mm(ps[:, 0:36], h2T[j2 % 2][:, kc, :], Wr[:, kc, :], kc == 0, kc == 7, [("h2T", j2 % 2), "Wr"], pk)
                tt("dve", Lg[:, j2, :], ps[:, 0:36], brb[:], ALU.add, [pk, "brb"], [("Lg", j2)])

            d_front(0)
            for j2 in range(32):
                if j2 + 1 < 32:
                    d_front(j2 + 1)
                d_back(j2)
            em.barrier()
        if debug:
            dma("sp", dbgL[:, 0:32 * 36], Lg[:].rearrange("p a b -> p (a b)"), [("Lg", j2) for j2 in range(32)], ["dbgL"])
        if stop_after == "D":
            em.barrier()
            return nc

        with ExitStack() as ph:
            tri = sbuf(ph, "tri", [128, 128], BF16)
            ones = sbuf(ph, "ones", [128, 128], BF16)
            ecb = sbuf(ph, "ecb", [128, 32], F32)
            trb = sbuf(ph, "trb", [128, 32], F32)
            dma("sp", tri[:], T["tri"], [], ["tri"])
            dma("sp", ones[:], T["ones"], [], ["ones"])
            dma("sp", ecb[:], T["ecb"], [], ["ecb"])
            dma("sp", trb[:], T["trb"], [], ["trb"])
            S = lambda name, shape, dt=F32: sbuf(ph, name, shape, dt)
            gmax = S("gmax", [128, 32]); goh = S("goh", [128, 32, 4]); gd = S("gd", [128, 32, 4]); gsum = S("gsum", [128, 32])
            pg = S("pg", [128, 32]); sel4 = S("sel4", [128, 32, 4, 8]); esel = S("esel", [128, 32, 8]); m1 = S("m1", [128, 32])
            oh1 = S("oh1", [128, 32, 8]); e2 = S("e2", [128, 32, 8]); m2 = S("m2", [128, 32]); oh2 = S("oh2", [128, 32, 8])
            dd = S("dd", [128, 32]); A1 = S("A1", [128, 32, 4, 8]); A2 = S("A2", [128, 32, 4, 8]); Mb = S("Mb", [128, 1024], BF16)
            pin = S("pin", [128, 32, 32]); cnt = S("cnt", [128, 32, 32]); base = S("base", [128, 32, 32]); slot = S("slot", [128, 32, 32])
            tmp3 = S("tmp3", [128, 32, 32]); dk = S("dk", [128, 2, 32]); pk_ = S("pk_", [128, 2, 32]); ok = S("ok", [128, 2, 32])
            dsf = S("dsf", [128, 2, 32]); dgf = S("dgf", [128, 2, 32])
            allL = ["LgAll"]
            K = "route"
            lg_keys = [("Lg", j2) for j2 in range(32)]
            gl = Lg[:, :, 0:4]
            em.op("dve", lambda: V.tensor_reduce(out=gmax[:], in_=gl, axis=AX.X, op=ALU.max), reads=lg_keys, writes=["gmax"])
            tt("dve", goh[:], gl, gmax[:].unsqueeze(2).to_broadcast([128, 32, 4]), ALU.is_equal, lg_keys + ["gmax"], ["goh"])
            tt("dve", gd[:], gl, gmax[:].unsqueeze(2).to_broadcast([128, 32, 4]), ALU.subtract, lg_keys + ["gmax"], ["gd"])
            act(gd[:], gd[:], AF.Exp, ["gd"], ["gd"])
            em.op("dve", lambda: V.tensor_reduce(out=gsum[:], in_=gd[:], axis=AX.X, op=ALU.add), reads=["gd"], writes=["gsum"])
            em.op("dve", lambda: V.reciprocal(out=pg[:], in_=gsum[:]), reads=["gsum"], writes=["pg"])
            el4 = Lg[:, :, 4:36].rearrange("p a (g i) -> p a g i", i=8)
            tt("dve", sel4[:], el4, goh[:].unsqueeze(3).to_broadcast([128, 32, 4, 8]), ALU.mult, lg_keys + ["goh"], ["sel4"])
            tt("dve", esel[:], sel4[:, :, 0, :], sel4[:, :, 1, :], ALU.add, ["sel4"], ["esel"])
            tt("dve", esel[:], esel[:], sel4[:, :, 2, :], ALU.add, ["sel4", "esel"], ["esel"])
            tt("dve", esel[:], esel[:], sel4[:, :, 3, :], ALU.add, ["sel4", "esel"], ["esel"])
            em.op("dve", lambda: V.tensor_reduce(out=m1[:], in_=esel[:], axis=AX.X, op=ALU.max), reads=["esel"], writes=["m1"])
            tt("dve", oh1[:], esel[:], m1[:].unsqueeze(2).to_broadcast([128, 32, 8]), ALU.is_equal, ["esel", "m1"], ["oh1"])
            stt("dve", e2[:], oh1[:], -1e30, esel[:], ALU.mult, ALU.add, ["oh1", "esel"], ["e2"])
            em.op("dve", lambda: V.tensor_reduce(out=m2[:], in_=e2[:], axis=AX.X, op=ALU.max), reads=["e2"], writes=["m2"])
            tt("dve", oh2[:], e2[:], m2[:].unsqueeze(2).to_broadcast([128, 32, 8]), ALU.is_equal, ["e2", "m2"], ["oh2"])
            tt("dve", dd[:], m2[:], m1[:], ALU.subtract, ["m1", "m2"], ["dd"])
            act(dd[:], dd[:], AF.Exp, ["dd"], ["dd"])
            ts("dve", w1[:], dd[:], 1.0, None, ALU.add, None, ["dd"], ["w1"])
            em.op("dve", lambda: V.reciprocal(out=w1[:], in_=w1[:]), reads=["w1"], writes=["w1"])
            tt("dve", w2[:], dd[:], w1[:], ALU.mult, ["dd", "w1"], ["w2"])
            tt("dve", w1[:], w1[:], pg[:], ALU.mult, ["w1", "pg"], ["w1"])
            tt("dve", w2[:], w2[:], pg[:], ALU.mult, ["w2", "pg"], ["w2"])
            gb = goh[:].unsqueeze(3).to_broadcast([128, 32, 4, 8])
            tt("dve", A1[:], gb, oh1[:].unsqueeze(2).to_broadcast([128, 32, 4, 8]), ALU.mult, ["goh", "oh1"], ["A1"])
            tt("dve", A2[:], gb, oh2[:].unsqueeze(2).to_broadcast([128, 32, 4, 8]), ALU.mult, ["goh", "oh2"], ["A2"])
            A1f = A1[:].rearrange("p a g i -> p a (g i)")
            A2f = A2[:].rearrange("p a g i -> p a (g i)")
            tt("dve", Mb[:].rearrange("p (a e) -> p a e", e=32), A1f, A2f, ALU.add, ["A1", "A2"], ["Mb"])
            for h_ in range(2):
                ps, pk = ps_next()
                mm(ps[:], tri[:], Mb[:, h_ * 512:(h_ + 1) * 512], True, True, ["tri", "Mb"], pk)
                cp("act", pin[:, h_ * 16:(h_ + 1) * 16, :], ps[:].rearrange("p (a e) -> p a e", e=32), [pk], ["pin"])
                ps, pk = ps_next()
                mm(ps[:], ones[:], Mb[:, h_ * 512:(h_ + 1) * 512], True, True, ["ones", "Mb"], pk)
                cp("act", cnt[:, h_ * 16:(h_ + 1) * 16, :], ps[:].rearrange("p (a e) -> p a e", e=32), [pk], ["cnt"])
            em.op("dve", lambda: V.memset(base[:, 0, :], 0.0), writes=["base"])
            for j2 in range(1, 32):
                tt("dve", base[:, j2, :], base[:, j2 - 1, :], cnt[:, j2 - 1, :], ALU.add, ["base", "cnt"], ["base"])
            tt("dve", slot[:], pin[:], base[:], ALU.add, ["pin", "base"], ["slot"])
            for k, Af in enumerate([A1f, A2f]):
                tt("dve", tmp3[:], Af, slot[:], ALU.mult, ["A1", "A2", "slot"], ["tmp3"])
                em.op("dve", lambda k=k: V.tensor_reduce(out=pk_[:, k, :], in_=tmp3[:], axis=AX.X, op=ALU.add), reads=["tmp3"], writes=["pk_"])
                tt("dve", tmp3[:], Af, ecb[:].unsqueeze(1).to_broadcast([128, 32, 32]), ALU.mult, ["A1", "A2", "ecb"], ["tmp3"])
                em.op("dve", lambda k=k: V.tensor_reduce(out=dk[:, k, :], in_=tmp3[:], axis=AX.X, op=ALU.add), reads=["tmp3"], writes=["dk"])
            tt("dve", dk[:], dk[:], pk_[:], ALU.add, ["dk", "pk_"], ["dk"])
            ts("dve", ok[:], pk_[:], float(CAP), None, ALU.is_lt, None, ["pk_"], ["ok"])
            tt("dve", dgf[:], dk[:], ok[:], ALU.mult, ["dk", "ok"], ["dgf"])
            tt("dve", dsf[:], dk[:], trb[:].unsqueeze(1).to_broadcast([128, 2, 32]), ALU.subtract, ["dk", "trb"], ["dsf"])
            tt("dve", dsf[:], dsf[:], ok[:], ALU.mult, ["dsf", "ok"], ["dsf"])
            tt("dve", dsf[:], dsf[:], trb[:].unsqueeze(1).to_broadcast([128, 2, 32]), ALU.add, ["dsf", "trb"], ["dsf"])
            tt("dve", w1[:], w1[:], ok[:, 0, :], ALU.mult, ["w1", "ok"], ["w1"])
            tt("dve", w2[:], w2[:], ok[:, 1, :], ALU.mult, ["w2", "ok"], ["w2"])
            cp("dve", ds_i[:], dsf[:], ["dsf"], ["ds_i"])
            cp("dve", dg_i[:], dgf[:], ["dgf"], ["dg_i"])
            if debug:
                dma("sp", dbgL[:, 32 * 36:32 * 36 + 64], dsf[:].rearrange("p a b -> p (a b)"), ["dsf"], ["dbgL2"])
                dma("sp", dbgL[:, 32 * 36 + 64:32 * 36 + 96], w1[:], ["w1"], ["dbgL3"])
                dma("sp", dbgL[:, 32 * 36 + 96:32 * 36 + 128], w2[:], ["w2"], ["dbgL4"])
            for j2 in range(32):
                for k in range(2):
                    em.dma("pool", lambda j2=j2, k=k: G.indirect_dma_start(
                        out=xg, out_offset=bass.IndirectOffsetOnAxis(ap=ds_i[:, k, j2:j2 + 1], axis=0),
                        in_=h2tok[:, j2, :], in_offset=None), reads=["ds_i", ("h2tok", j2)],
                        writes=[("xgs", j2, k)])
            em.barrier()
        ph_tok.close()
        if stop_after == "E":
            return nc

        with ExitStack() as ph:
            NB = 2
            Wg = [sbuf(ph, "Wg%d" % i, [128, 8, 512], BF16) for i in range(NB)]
            Wu = [sbuf(ph, "Wu%d" % i, [128, 8, 512], BF16) for i in range(NB)]
            Wd = [sbuf(ph, "Wd%d" % i, [128, 4, D], BF16) for i in range(NB)]
            xgt = [sbuf(ph, "xgt%d" % i, [128, 3, D], BF16) for i in range(2)]
            xgT = [sbuf(ph, "xgT%d" % i, [128, 8, CAP], BF16) for i in range(2)]
            sg = [sbuf(ph, "sg%d" % i, [128, CAP], F32) for i in range(2)]
            hT = [sbuf(ph, "hT%d" % i, [128, 4, CAP], BF16) for i in range(2)]
            yt = [sbuf(ph, "yt%d" % i, [128, D], F32) for i in range(4)]
            yt_rr = 0
            NST = (CAP + 127) // 128
            NSTG = 5
            stg = [sbuf(ph, "stg%d" % i, [128, 4096], F32) for i in range(NSTG)]
            stg_rr = [0]

            def load_expert(e):
                b = e % NB
                b2 = e % 2
                items = [
                    (T["w_gate"][e].rearrange("(p k) n -> p k n", k=8), Wg[b], ("Wg", b), 8, 512, "act"),
                    (T["w_up"][e].rearrange("(p k) n -> p k n", k=8), Wu[b], ("Wu", b), 8, 512, "dve"),
                    (T["w_down"][e].rearrange("(k p) n -> p k n", p=128), Wd[b], ("Wd", b), 4, 1024, "pool"),
                ]
                for src, dst, key, nk, nn, ce in items:
                    i = stg_rr[0] % NSTG
                    stg_rr[0] += 1
                    sv = stg[i][:].rearrange("p (k n) -> p k n", k=nk)
                    dma("sp", sv, src, [], [("stg", i)])
                    hk_ = nk // 2
                    for h_ in range(2):
                        wk = [(key[0], key[1], 0), (key[0], key[1], 4 if key[0] != "Wd" else 2)] if h_ == 0 else []
                        ce_ = ce if ce != "pool" else ("dve" if h_ == 0 else "act")
                        cp(ce_, dst[:, h_ * hk_:(h_ + 1) * hk_, :], sv[:, h_ * hk_:(h_ + 1) * hk_, :], [("stg", i)],
                           [(key[0], key[1], "h%d" % h_)] + wk)
                for s_ in range((CAP + 127) // 128):
                    w_ = min(128, CAP - s_ * 128)
                    dma("pool", xgt[b2][0:w_, s_, :], xg[e * CAP + s_ * 128:e * CAP + s_ * 128 + w_, :], [], [("xgt", b2, s_)])

            def f_transposes(e):
                b2 = e % 2
                for s in range(3):
                    if s * 128 >= CAP:
                        break
                    w_ = min(128, CAP - s * 128)
                    ps, pk = ps_next()
                    psb = ps[:].bitcast(BF16)
                    for kc in range(8):
                        tr(psb[:, kc * 128:kc * 128 + w_], xgt[b2][0:w_, s, kc::8], [("xgt", b2, s)], pk, kdim=w_)
                    cp("act" if s % 2 else "dve", xgT[b2][:, :, s * 128:s * 128 + w_],
                       psb.rearrange("p (a b) -> p a b", b=128)[:, :, 0:w_], [pk], [("xgT", b2, s)])

            load_expert(0)
            f_transposes(0)
            for e in range(32):
                b = e % NB
                b2 = e % 2
                if e + 1 < 32:
                    load_expert(e + 1)
                xk = [("xgT", b2, s) for s in range(NST)]
                for mc in range(4):
                    psG, kG = ps_next()
                    psU, kU = ps_next()
                    for kc in range(8):
                        mm(psG[:, 0:CAP], Wg[b][:, kc, mc * 128:(mc + 1) * 128], xgT[b2][:, kc, :], kc == 0, kc == 7, xk + [("Wg", b, "h0"), ("Wg", b, "h1"), ("Wg", b, 0), ("Wg", b, 4)], kG)
                    for kc in range(8):
                        mm(psU[:, 0:CAP], Wu[b][:, kc, mc * 128:(mc + 1) * 128], xgT[b2][:, kc, :], kc == 0, kc == 7, xk + [("Wu", b, "h0"), ("Wu", b, "h1"), ("Wu", b, 0), ("Wu", b, 4)], kU)
                    sb_ = mc % 2
                    act(sg[sb_][:], psG[:, 0:CAP], AF.Silu, [kG], [("sg", sb_)])
                    tt("dve", hT[b2][:, mc, :], sg[sb_][:], psU[:, 0:CAP], ALU.mult, [("sg", sb_), kU], [("hT", b2, mc)])
                if e + 1 < 32:
                    f_transposes(e + 1)
                hk = [("hT", b2, mc) for mc in range(4)]
                for s in range(NST):
                    w_ = min(128, CAP - s * 128)
                    yb = yt_rr % 4
                    yt_rr += 1
                    for half in range(2):
                        ps, pk = ps_next()
                        for mc in range(4):
                            mm(ps[0:w_, :], hT[b2][:, mc, s * 128:s * 128 + w_], Wd[b][:, mc, half * 512:(half + 1) * 512], mc == 0, mc == 3,
                               hk + [("Wd", b, "h0"), ("Wd", b, "h1"), ("Wd", b, 0), ("Wd", b, 2)], pk)
                        cp("act" if half else "dve", yt[yb][0:w_, half * 512:(half + 1) * 512], ps[0:w_, :], [pk], [("yt", yb, half)])
                    dma("pool", Ybuf[e * CAP + s * 128:e * CAP + s * 128 + w_, :], yt[yb][0:w_, :], [("yt", yb, 0), ("yt", yb, 1)], [("Ybuf", e, s)])
            em.barrier()
        if stop_after == "F":
            return nc

        with ExitStack() as ph:
            gfb = sbuf(ph, "gfb", [128, D], F32)
            dma("sp", gfb[:], T["gfb"], [], ["gfb"])
            NG = 4
            Y1 = [sbuf(ph, "Y1_%d" % i, [128, D], F32) for i in range(NG)]
            Y2 = [sbuf(ph, "Y2_%d" % i, [128, D], F32) for i in range(NG)]
            xt = [sbuf(ph, "xtG%d" % i, [128, D], F32) for i in range(NG)]
            junk = sbuf(ph, "junkG", [128, D], BF16)
            ss = sbuf(ph, "ssG", [128, 32], F32)
            rs = sbuf(ph, "rsG", [128, 32], F32)
            em.op("dve", lambda: V.memset(ss[:], 0.0), writes=["ssG"])

            def g_load(j2):
                b = j2 % NG
                em.dma("pool", lambda: G.indirect_dma_start(out=Y1[b][:], out_offset=None, in_=Ybuf,
                                                            in_offset=bass.IndirectOffsetOnAxis(ap=dg_i[:, 0, j2:j2 + 1], axis=0)),
                       reads=["dg_i"], writes=[("Y1", b)])
                em.dma("pool", lambda: G.indirect_dma_start(out=Y2[b][:], out_offset=None, in_=Ybuf,
                                                            in_offset=bass.IndirectOffsetOnAxis(ap=dg_i[:, 1, j2:j2 + 1], axis=0)),
                       reads=["dg_i"], writes=[("Y2", b)])
                dma("sp", xt[b][:], x1buf[j2 * 128:(j2 + 1) * 128, :], [], [("xtG", b)])

            for j2 in range(min(3, 32)):
                g_load(j2)
            for j2 in range(32):
                b = j2 % NG
                if j2 + 3 < 32:
                    g_load(j2 + 3)
                stt("dve", xt[b][:], Y1[b][:], w1[:, j2:j2 + 1], xt[b][:], ALU.mult, ALU.add, [("Y1", b), ("xtG", b), "w1"], [("xtG", b)])
                stt("dve", xt[b][:], Y2[b][:], w2[:, j2:j2 + 1], xt[b][:], ALU.mult, ALU.add, [("Y2", b), ("xtG", b), "w2"], [("xtG", b)])
                act(junk[:], xt[b][:], AF.Square, [("xtG", b), "ssG"], ["junkG", ("ssG", j2)], accum_out=ss[:, j2:j2 + 1])
                act(rs[:, j2:j2 + 1], ss[:, j2:j2 + 1], AF.Sqrt, [("ssG", j2), "epst"], [("rsG", j2)], scale=1.0 / D, bias=epst[:])
                em.op("dve", lambda: V.reciprocal(out=rs[:, j2:j2 + 1], in_=rs[:, j2:j2 + 1]), reads=[("rsG", j2)], writes=[("rsG", j2)])
                stt("dve", Y1[b][:], xt[b][:], rs[:, j2:j2 + 1], gfb[:], ALU.mult, ALU.mult, [("xtG", b), ("rsG", j2), "gfb"], [("Y1", b)])
                final_events.append(dma("sp", out_d[j2 * 128:(j2 + 1) * 128, :], Y1[b][:], [("Y1", b)], [("out", j2)]))
            em.barrier()
    return nc


def prep_inputs(inp):
    f32 = np.float32
    g = lambda k: np.asarray(inp[k], dtype=f32)
    rep = lambda v: np.ascontiguousarray(np.tile(v.reshape(1, -1), (128, 1)))
    sh = {}
    sh["w_in"] = np.ascontiguousarray(g("w_in")[0])
    sh["g1b"] = rep(g("norm1_g")[0])
    sh["g2b"] = rep(g("norm2_g")[0])
    sh["gfb"] = rep(g("final_g"))
    cw = g("conv_w")[0]
    sh["cwp"] = np.ascontiguousarray(cw.reshape(3, 12, 128).transpose(2, 1, 0)).reshape(128, 36)
    sh["cbp"] = np.ascontiguousarray(g("conv_b")[0].reshape(12, 128).T)
    sh["fbp"] = np.ascontiguousarray(g("f_bias")[0].reshape(4, 128).T)
    sh["mgp"] = np.ascontiguousarray(g("mix_g")[0].reshape(8, 128).T)
    sh["w_out"] = np.ascontiguousarray(g("w_out")[0])
    wr = np.concatenate([g("w_group")[0], g("w_router")[0].transpose(1, 0, 2).reshape(D, 32)], axis=1)
    sh["wr"] = np.ascontiguousarray(wr)
    sh["brb"] = rep(np.concatenate([g("b_group")[0], g("b_router")[0].reshape(32)]))
    sh["w_gate"] = np.ascontiguousarray(g("w_gate")[0])
    sh["w_up"] = np.ascontiguousarray(g("w_up")[0])
    sh["w_down"] = np.ascontiguousarray(g("w_down")[0])
    fwin = g("f_w_in")[0]
    w1 = np.zeros((66, 128), f32)
    w1[0:33, 0:64] = fwin
    w1[33:66, 64:128] = fwin
    sh["fwin2"] = w1
    fm = g("f_w_mid")[0]
    wm = np.zeros((128, 2, 128), f32)
    for l in range(2):
        wm[0:64, l, 0:64] = fm[l]
        wm[64:128, l, 64:128] = fm[l]
    sh["fwmid2"] = wm.reshape(128, 256)
    fq = g("f_freq")[0].T
    sh["fq"] = np.ascontiguousarray(np.concatenate([fq, fq], 0))
    fb = np.stack([g("f_b_in")[0], g("f_b_mid")[0][0], g("f_b_mid")[0][1]], 1)
    sh["fbb"] = np.ascontiguousarray(np.concatenate([fb, fb], 0))
    fo = g("f_w_out")[0]
    sh["fwout2"] = np.ascontiguousarray(np.concatenate([fo, fo], 0))
    return sh


_CACHE = {}


def kernel(**inputs):
    if "nc" not in _CACHE:
        _CACHE["nc"] = build_nc()
        _CACHE["consts"] = make_consts()
    nc = _CACHE["nc"]
    shared = prep_inputs(inputs)
    shared.update(_CACHE["consts"])
    x = np.asarray(inputs["x"], dtype=np.float32)
    in_maps = []
    for c in range(8):
        m = dict(shared)
        m["x"] = np.ascontiguousarray(x[c])
        in_maps.append(m)
    res = run_bass_kernel_spmd(nc, in_maps, core_ids=list(range(8)))
    out = np.stack([np.asarray(res.results[c]["out"], dtype=np.float32) for c in range(8)], axis=0)
    return out
```

```python
import numpy as np
import ml_dtypes
from contextlib import ExitStack
import concourse.bass as bass
import concourse.mybir as mybir
from concourse.bass_utils import run_bass_kernel_spmd

F32 = mybir.dt.float32
BF16 = mybir.dt.bfloat16
I32 = mybir.dt.int32
AF = mybir.ActivationFunctionType
ALU = mybir.AluOpType
AX = mybir.AxisListType
bf = ml_dtypes.bfloat16

L = 4096
NF = 8192
D = 1024
CAP = 320
NS = 32 * CAP
NROWS = NS + 128
EPS = 1e-6
TWO_PI = float(2 * np.pi)


class Emit:
    def __init__(self, nc, stack, n_dma_sems=48):
        self.nc = nc
        self.eng = {"pe": nc.tensor, "act": nc.scalar, "dve": nc.vector, "pool": nc.gpsimd, "sp": nc.sync}
        self.sem = {}
        self.cnt = {}
        for k in self.eng:
            self.sem[k] = stack.enter_context(nc.semaphore("s_" + k))
            self.cnt[k] = 0
        self.dma_sems = [stack.enter_context(nc.semaphore("d%d" % i)) for i in range(n_dma_sems)]
        self.dma_cnt = [0] * n_dma_sems
        n_sw = 16
        self.dma_pool = {"hw": list(range(n_sw, n_dma_sems)), "sw": list(range(n_sw))}
        self.dma_rr = {"hw": 0, "sw": 0}
        self.seen = {k: {} for k in self.eng}
        self.lastw = {}
        self.reads = {}
        self.n_wait = 0
        self.n_ins = 0

    def _wait(self, e, ev):
        sem, val, src = ev
        sid = id(sem)
        if self.seen[e].get(sid, 0) >= val:
            return
        self.seen[e][sid] = val
        self.eng[e].wait_ge(sem, val)
        self.n_wait += 1

    def _deps(self, e, reads, writes):
        evs = []
        for r in reads:
            w = self.lastw.get(r)
            if w is not None and not (w[2] == e and e == "pe"):
                evs.append(w)
        same_ok = e in ("pe",)
        for wkey in writes:
            w = self.lastw.get(wkey)
            if w is not None and (w[2] != e or not same_ok):
                evs.append(w)
            for ev in self.reads.get(wkey, {}).values():
                if ev[2] != e or not same_ok:
                    evs.append(ev)
        return evs

    def _commit(self, ev, reads, writes):
        for r in reads:
            self.reads.setdefault(r, {})[(ev[2], id(ev[0]))] = ev
        for w in writes:
            self.lastw[w] = ev
            self.reads[w] = {}

    def op(self, e, fn, reads=(), writes=()):
        for ev in self._deps(e, reads, writes):
            self._wait(e, ev)
        ins = fn()
        self.cnt[e] += 1
        ins.then_inc(self.sem[e], 1)
        ev = (self.sem[e], self.cnt[e], e)
        self._commit(ev, reads, writes)
        self.n_ins += 1
        return ev

    def dma(self, q, fn, reads=(), writes=()):
        for ev in self._deps(q, reads, writes):
            self._wait(q, ev)
        kind = "sw" if q == "pool" else "hw"
        lst = self.dma_pool[kind]
        i = lst[self.dma_rr[kind]]
        self.dma_rr[kind] = (self.dma_rr[kind] + 1) % len(lst)
        sem = self.dma_sems[i]
        if self.dma_cnt[i] > 0:
            self._wait(q, (sem, 16 * self.dma_cnt[i], "dma"))
        ins = fn()
        self.dma_cnt[i] += 1
        ins.then_inc(sem, 16)
        ev = (sem, 16 * self.dma_cnt[i], "dma%d" % i)
        self._commit(ev, reads, writes)
        self.n_ins += 1
        return ev

    def barrier(self):
        for e in self.eng:
            for e2 in self.eng:
                if e2 != e and self.cnt[e2] > 0:
                    self._wait(e, (self.sem[e2], self.cnt[e2], e2))
            for i, s in enumerate(self.dma_sems):
                if self.dma_cnt[i] > 0:
                    self._wait(e, (s, 16 * self.dma_cnt[i], "dma"))
        self.lastw = {}
        self.reads = {}


def _kron4(M):
    return np.kron(M, np.eye(4))


def make_consts():
    c = {}
    p = np.arange(256)[:, None].astype(np.float64)
    k1 = np.arange(128)[None, :].astype(np.float64)
    fa = np.zeros((2, 32, 2, 128, 128))
    gt = np.zeros((32, 2, 128, 128))
    for j in range(32):
        th = 2 * np.pi * (p * (k1 + 0.5) / 256.0 + j * (k1 + 0.5) / NF)
        for pt in range(2):
            sgn = 1.0 if pt == 0 else -1.0
            fa[pt, j, 0] = sgn * np.cos(th[pt * 128:(pt + 1) * 128])
            fa[pt, j, 1] = -sgn * np.sin(th[pt * 128:(pt + 1) * 128])
        gt[j, 0] = (2.0 / NF) * np.cos(th[:128]).T
        gt[j, 1] = -(2.0 / NF) * np.sin(th[:128]).T
    c["fa"] = np.ascontiguousarray(fa.transpose(3, 0, 1, 2, 4)).reshape(128, 2 * 32 * 2 * 128).astype(bf)
    c["gt"] = np.ascontiguousarray(gt.transpose(2, 0, 1, 3)).reshape(128, 32 * 2 * 128).astype(bf)
    jj = np.arange(32)[:, None].astype(np.float64)
    kk = np.arange(32)[None, :].astype(np.float64)
    ph = 2 * np.pi * jj * kk / 32.0
    Tc = _kron4(np.cos(ph))
    Ts = _kron4(np.sin(ph))
    c["tct"] = np.stack([Tc, Ts, -Ts], 1).reshape(128, 3 * 128).astype(bf)
    c["tcf"] = np.stack([Tc / 512.0, Ts / 512.0], 1).reshape(128, 2 * 128).astype(bf)
    pa = np.arange(128)[:, None].astype(np.float64)
    ka = np.arange(128)[None, :].astype(np.float64)
    et = np.zeros((32, 3, 128, 128))
    for j in range(32):
        th = 2 * np.pi * (pa * ka / 128.0 + j * ka / 4096.0)
        et[j, 0] = np.cos(th)
        et[j, 1] = np.sin(th)
        et[j, 2] = -np.sin(th)
    c["et"] = np.ascontiguousarray(et.transpose(2, 0, 1, 3)).reshape(128, 32 * 3 * 128).astype(bf)
    cc = np.arange(64)[:, None].astype(np.float64)
    c2 = np.arange(64)[None, :].astype(np.float64)
    C64 = np.cos(2 * np.pi * cc * c2 / 64.0)
    S64 = np.sin(2 * np.pi * cc * c2 / 64.0)
    c["bdt"] = np.stack([np.kron(np.eye(2), C64), np.kron(np.eye(2), -S64)], 1).reshape(128, 256).astype(bf)
    c["ident"] = np.eye(128).astype(bf)
    c["blk"] = (np.kron(np.eye(2), np.ones((64, 64))) / 64.0).astype(bf)
    c["tri"] = np.triu(np.ones((128, 128)), 1).astype(bf)
    c["ones"] = np.ones((128, 128)).astype(bf)
    c["onesf"] = np.ones((128, 128), np.float32)
    c["ecb"] = np.tile((np.arange(32) * CAP).astype(np.float32)[None, :], (128, 1))
    c["trb"] = np.tile((NS + np.arange(128)).astype(np.float32)[:, None], (1, 32))
    n = np.arange(NF)
    m = np.where(n < L, n, NF - n).astype(np.float64)
    m[L] = 0
    t = (m / (L - 1)).astype(np.float32)
    w = (2.0 * np.pi / L) * m
    f = np.linspace(1e-4, 15, 16)[None, :]
    emb = np.concatenate([t[:, None], np.cos(f * w[:, None]), -np.sin(f * w[:, None])], -1).astype(np.float32)
    c["emb2"] = np.concatenate([emb[:L].T, emb[L:].T], 0).astype(np.float32)
    tn = -t.astype(np.float32)
    tn[L] = -1e4
    c["tneg"] = np.ascontiguousarray(tn.reshape(2, 128, 32).transpose(1, 0, 2)).reshape(128, 64)
    max_decay = np.log(1e-2) / 0.3
    min_decay = np.log(1e-2) / 1.5
    deltas = np.abs(np.linspace(min_decay, max_decay, 512)).astype(np.float32)
    c["deltab"] = np.tile(deltas[None, :], (128, 1))
    return c


CONST_SPECS = [
    ("fa", [128, 16384], BF16), ("gt", [128, 8192], BF16), ("tct", [128, 384], BF16), ("tcf", [128, 256], BF16),
    ("et", [128, 12288], BF16), ("bdt", [128, 256], BF16), ("ident", [128, 128], BF16), ("blk", [128, 128], BF16),
    ("tri", [128, 128], BF16), ("ones", [128, 128], BF16), ("onesf", [128, 128], F32), ("ecb", [128, 32], F32),
    ("trb", [128, 32], F32), ("emb2", [66, 4096], F32), ("tneg", [128, 64], F32), ("deltab", [128, 512], F32),
]
IN_SPECS = [
    ("x", [L, D], F32), ("w_in", [D, 2048], F32), ("g1b", [128, D], F32), ("g2b", [128, D], F32), ("gfb", [128, D], F32),
    ("cwp", [128, 36], F32), ("cbp", [128, 12], F32), ("fbp", [128, 4], F32), ("mgp", [128, 8], F32),
    ("w_out", [D, D], F32), ("wr", [D, 36], F32), ("brb", [128, 36], F32),
    ("w_gate", [32, D, 512], F32), ("w_up", [32, D, 512], F32), ("w_down", [32, 512, D], F32),
    ("fwin2", [66, 128], F32), ("fwmid2", [128, 256], F32), ("fq", [128, 3], F32), ("fbb", [128, 3], F32),
    ("fwout2", [128, 1024], F32),
]


def build_nc(stop_after=None, debug=False):
    nc = bass.Bass("TRN2", target_bir_lowering=False)
    T = {}
    for name, shape, dt in IN_SPECS + CONST_SPECS:
        T[name] = nc.dram_tensor(name, shape, dt, kind="ExternalInput").ap()
    out_d = nc.dram_tensor("out", [L, D], F32, kind="ExternalOutput").ap()
    skind = "ExternalOutput" if debug else "Internal"
    Pbuf = nc.dram_tensor("Pbuf", [1536, L], BF16, kind=skind).ap()
    wbuf = [nc.dram_tensor("wbuf%d" % i, [L, 512], BF16, kind=skind).ap() for i in range(2)]
    Hbuf = nc.dram_tensor("Hbuf", [4, 128, 8192], BF16, kind=skind).ap()
    yTbuf = nc.dram_tensor("yTbuf", [D, L], BF16, kind=skind).ap()
    x1buf = nc.dram_tensor("x1buf", [L, D], F32, kind=skind).ap()
    xg = nc.dram_tensor("xg", [NROWS, D], BF16, kind=skind).ap()
    Ybuf = nc.dram_tensor("Ybuf", [NROWS, D], F32, kind=skind).ap()
    dbgL = nc.dram_tensor("dbgL", [128, 32 * 40], F32, kind=skind).ap()

    with ExitStack() as st:
        em = Emit(nc, st)
        V, A, G, PE = nc.vector, nc.scalar, nc.gpsimd, nc.tensor
        ENG = {"dve": V, "act": A, "pool": G}

        def sbuf(stack, name, shape, dt):
            return stack.enter_context(nc.sbuf_tensor("sb_" + name, shape, dt))

        PS = [st.enter_context(nc.psum_tensor("ps%d" % i, [128, 512], F32)) for i in range(8)]
        ps_rr = [0]

        def ps_next():
            i = ps_rr[0]
            ps_rr[0] = (i + 1) % 8
            return PS[i], "ps%d" % i

        def mm(out, lhsT, rhs, start, stop, reads, pk):
            return em.op("pe", lambda: PE.matmul(out, lhsT=lhsT, rhs=rhs, start=start, stop=stop), reads=reads, writes=[pk])

        def tr(out, in_, reads, pk, kdim=128):
            idn = ident[:] if kdim == 128 else ident[0:kdim, 0:kdim]
            return em.op("pe", lambda: PE.transpose(out, in_, idn), reads=list(reads) + ["ident"], writes=[pk])

        def cp(e, out, in_, reads, writes):
            if e == "act":
                return em.op("act", lambda: A.copy(out=out, in_=in_), reads=reads, writes=writes)
            return em.op(e, lambda: ENG[e].tensor_copy(out=out, in_=in_), reads=reads, writes=writes)

        def tt(e, out, in0, in1, op, reads, writes):
            return em.op(e, lambda: ENG[e].tensor_tensor(out=out, in0=in0, in1=in1, op=op), reads=reads, writes=writes)

        def ts(e, out, in0, s1, s2, op0, op1, reads, writes):
            if op1 is None:
                return em.op(e, lambda: ENG[e].tensor_scalar(out=out, in0=in0, scalar1=s1, scalar2=None, op0=op0), reads=reads, writes=writes)
            return em.op(e, lambda: ENG[e].tensor_scalar(out=out, in0=in0, scalar1=s1, scalar2=s2, op0=op0, op1=op1), reads=reads, writes=writes)

        def stt(e, out, in0, scalar, in1, op0, op1, reads, writes):
            return em.op(e, lambda: ENG[e].scalar_tensor_tensor(out=out, in0=in0, scalar=scalar, in1=in1, op0=op0, op1=op1), reads=reads, writes=writes)

        def act(out, in_, func, reads, writes, **kw):
            return em.op("act", lambda: A.activation(out=out, in_=in_, func=func, **kw), reads=reads, writes=writes)

        def dma(q, out, in_, reads, writes):
            e = {"sp": nc.sync, "act": A, "pool": G}[q]
            return em.dma(q, lambda: e.dma_start(out=out, in_=in_), reads=reads, writes=writes)

        final_events = []

        ident = sbuf(st, "ident", [128, 128], BF16)
        blk = sbuf(st, "blk", [128, 128], BF16)
        tct = sbuf(st, "tct", [128, 3, 128], BF16)
        epst = sbuf(st, "epst", [128, 1], F32)
        rinvP = sbuf(st, "rinvP", [128, 4], F32)
        dma("sp", ident[:], T["ident"], [], ["ident"])
        dma("sp", blk[:], T["blk"], [], ["blk"])
        dma("sp", tct[:].rearrange("p a b -> p (a b)"), T["tct"], [], ["tct"])
        em.op("dve", lambda: V.memset(epst[:], EPS), writes=["epst"])

        ztp = sbuf(st, "ztp", [128, 2, D], BF16)
        em.op("pool", lambda: G.memset(ztp[:], 0.0), writes=["ztp"])
        zf_chunks = [(r0, min(256, NROWS - r0)) for r0 in range(0, NROWS, 256)]

        def zero_fill_some(n):
            for _ in range(n):
                if zf_chunks:
                    r0, nr = zf_chunks.pop(0)
                    dma("sp", xg[r0:r0 + nr, :].rearrange("(s p) f -> p s f", p=128), ztp[:, 0:nr // 128, :], ["ztp"], [("xg", r0)])

        def fft_fwd(src, npt, fa_t, bufA, bufB, srck, sink):
            for j0 in range(0, 32, 4):
                for cs in range(2):
                    ps, pk = ps_next()
                    for jj in range(4):
                        j = j0 + jj
                        for pt in range(npt):
                            mm(ps[:, jj * 128:(jj + 1) * 128], fa_t[:, pt, j, cs, :], src[:, pt * 32 + j, :],
                               pt == 0, pt == npt - 1, (srck if isinstance(srck, list) else [srck]) + ["fa"], pk)
                    cp("act" if cs == 0 else "dve",
                       bufA[:].rearrange("p a g (j c) -> p a g j c", c=4)[:, cs, :, j0:j0 + 4, :].rearrange("p g j c -> p j g c"),
                       ps[:].rearrange("p (j g c) -> p j g c", j=4, g=32), [pk], [("bufA", cs, j0)])
            for cs in range(2):
                for g0 in range(0, 32, 8):
                    ps, pk = ps_next()
                    psb = ps[:].bitcast(BF16)
                    for gg in range(8):
                        g = g0 + gg
                        tr(psb[:, gg * 128:(gg + 1) * 128], bufA[:, cs, g, :],
                           [("bufA", cs, j0) for j0 in range(0, 32, 4)], pk)
                    cp("act" if cs == 0 else "dve", bufB[:, cs, g0:g0 + 8, :], psb.rearrange("p (a b) -> p a b", b=128),
                       [pk], [("bufB", cs, g0)])
            for ch in range(8):
                psR, kR = ps_next()
                psI, kI = ps_next()
                bBf = bufB[:].rearrange("p a b c -> p a (b c)")
                br = bBf[:, 0, ch * 512:(ch + 1) * 512]
                bi = bBf[:, 1, ch * 512:(ch + 1) * 512]
                rk = [("bufB", 0, (4 * ch) // 8 * 8), ("bufB", 1, (4 * ch) // 8 * 8), "tct"]
                mm(psR[:], tct[:, 0, :], br, True, False, rk, kR)
                mm(psR[:], tct[:, 1, :], bi, False, True, rk, kR)
                mm(psI[:], tct[:, 0, :], bi, True, False, rk, kI)
                mm(psI[:], tct[:, 2, :], br, False, True, rk, kI)
                sink(ch, psR, kR, psI, kI)

        with ExitStack() as ph:
            fa_t = sbuf(ph, "fa_t", [128, 2, 32, 2, 128], BF16)
            w1t = sbuf(ph, "w1t", [66, 128], F32)
            wmt = sbuf(ph, "wmt", [128, 2, 128], F32)
            fq = sbuf(ph, "fq", [128, 3], F32)
            fbb = sbuf(ph, "fbb", [128, 3], F32)
            fq2 = sbuf(ph, "fq2", [128, 3], F32)
            fqb2 = sbuf(ph, "fqb2", [128, 3], F32)
            fwo = sbuf(ph, "fwo", [128, 1024], BF16)
            onesf = sbuf(ph, "onesf", [128, 128], F32)
            tneg = sbuf(ph, "tneg", [128, 64], F32)
            deltab = sbuf(ph, "deltab", [128, 512], F32)
            hid = sbuf(ph, "hid", [128, L], BF16)
            dma("sp", fa_t[:].rearrange("p a b c d -> p (a b c d)"), T["fa"], [], ["fa"])
            dma("sp", w1t[:], T["fwin2"], [], ["w1t"])
            dma("sp", wmt[:].rearrange("p a b -> p (a b)"), T["fwmid2"], [], ["wmt"])
            dma("sp", fq[:], T["fq"], [], ["fq"])
            dma("sp", fbb[:], T["fbb"], [], ["fbb"])
            dma("pool", fwo[:], T["fwout2"], [], ["fwo"])
            dma("sp", onesf[:], T["onesf"], [], ["onesf"])
            dma("sp", tneg[:], T["tneg"], [], ["tneg"])
            dma("sp", deltab[:], T["deltab"], [], ["deltab"])
            ts("dve", fq2[:], fq[:], float(1.0 / 3.0), None, ALU.mult, None, ["fq"], ["fq2"])
            tt("dve", fqb2[:], fq2[:], fbb[:], ALU.mult, ["fq2", "fbb"], ["fqb2"])
            with ExitStack() as ph2:
                emb = sbuf(ph2, "emb", [66, L], F32)
                hA = sbuf(ph2, "hA", [128, L], F32)
                hB = sbuf(ph2, "hB", [128, L], F32)
                for ch in range(8):
                    dma("sp", emb[:, ch * 512:(ch + 1) * 512], T["emb2"][:, ch * 512:(ch + 1) * 512], [], [("emb", ch)])
                ub = [sbuf(ph2, "ub%d" % i, [128, 512], F32) for i in range(8)]
                rb = [sbuf(ph2, "rb%d" % i, [128, 512], F32) for i in range(8)]
                srcs = [(emb, "emb", w1t[:]), (hA, "hA", wmt[:, 0, :]), (hB, "hB", wmt[:, 1, :])]
                dsts = [(hA, "hA"), (hB, "hB"), (hid, "hid")]
                for l in range(3):
                    s_t, s_k, lhsT = srcs[l]
                    d_t, d_k = dsts[l]
                    pss = {}
                    for ch in range(8):
                        ps, pk = ps_next()
                        pss[ch] = (ps, pk)
                        mm(ps[:], lhsT, s_t[:, ch * 512:(ch + 1) * 512], True, True, [(s_k, ch), "w1t", "wmt"], pk)
                    for ch in range(8):
                        ps, pk = pss[ch]
                        act(ub[ch][:], ps[:], AF.Sin, [pk, "fq2", "fqb2"], [("ub", ch)], scale=fq2[:, l:l + 1], bias=fqb2[:, l:l + 1])
                    for ch in range(8):
                        tt("dve", rb[ch][:], ub[ch][:], ub[ch][:], ALU.mult, [("ub", ch)], [("rb", ch)])
                    for ch in range(8):
                        ts("dve", rb[ch][:], rb[ch][:], -4.0, 3.0, ALU.mult, ALU.add, [("rb", ch)], [("rb", ch)])
                    for ch in range(8):
                        tt("dve", d_t[:, ch * 512:(ch + 1) * 512], rb[ch][:], ub[ch][:], ALU.mult, [("rb", ch), ("ub", ch)], [(d_k, ch)])
                em.barrier()
            with ExitStack() as ph2:
                decs = [sbuf(ph2, "dec%d" % i, [128, 64, 128], BF16) for i in range(2)]
                kbs = [sbuf(ph2, "kb%d" % i, [128, 64, 128], BF16) for i in range(2)]
                acc = sbuf(ph2, "acc", [128, 128], F32)
                bufA = sbuf(ph2, "bufA", [128, 2, 32, 128], BF16)
                bufB = sbuf(ph2, "bufB", [128, 2, 32, 128], BF16)
                Hsb = sbuf(ph2, "Hsb", [128, 2, L], BF16)

                def dec_gen(hc):
                    for col in range(64):
                        act(decs[hc % 2][:, col, :], deltab[:, hc * 128:(hc + 1) * 128], AF.Exp, ["deltab", "tneg"], [("dec", hc % 2, col // 4)],
                            scale=tneg[:, col:col + 1])

                def out_layer(hc):
                    dec = decs[hc % 2]
                    kb = kbs[hc % 2]
                    for c0 in range(0, 64, 4):
                        ps, pk = ps_next()
                        for cc in range(4):
                            col = c0 + cc
                            pt, j = col // 32, col % 32
                            mm(ps[:, cc * 128:(cc + 1) * 128], hid[64 * pt:64 * pt + 64, j::32],
                               fwo[64 * pt:64 * pt + 64, pt * 512 + hc * 128:pt * 512 + (hc + 1) * 128], True, True,
                               [("hid", ch) for ch in range(8)] + ["fwo"], pk)
                        tt("dve", kb[:, c0:c0 + 4, :], ps[:].rearrange("p (a b) -> p a b", b=128), dec[:, c0:c0 + 4, :], ALU.mult,
                           [pk, ("dec", hc % 2, c0 // 4)], [("kb", hc % 2, c0 // 4)])

                def sinkH(ch, psR, kR, psI, kI):
                    cp("act", Hsb[:, 0, ch * 512:(ch + 1) * 512], psR[:], [kR], [("Hsb", ch)])
                    cp("dve", Hsb[:, 1, ch * 512:(ch + 1) * 512], psI[:], [kI], [("Hsb", ch)])

                dec_gen(0)
                out_layer(0)
                for hc in range(4):
                    kb = kbs[hc % 2]
                    kbk = [("kb", hc % 2, i) for i in range(16)]
                    if hc + 1 < 4:
                        dec_gen(hc + 1)
                        out_layer(hc + 1)
                    fft_fwd(kb, 2, fa_t, bufA, bufB, kbk, sinkH)
                    dma("sp", Hbuf[hc], Hsb[:].rearrange("p a b -> p (a b)"), [("Hsb", ch) for ch in range(8)], [("Hbuf", hc)])
                    em.op("dve", lambda: V.tensor_reduce(out=acc[:], in_=kb[:].rearrange("p n c -> p c n"), axis=AX.X, op=ALU.add,
                                                         apply_absolute_value=True), reads=kbk, writes=["acc"])
                    ps, pk = ps_next()
                    mm(ps[:, 0:1], acc[:], onesf[:, 0:1], True, True, ["onesf", "acc"], pk)
                    em.op("dve", lambda: V.reciprocal(out=rinvP[:, hc:hc + 1], in_=ps[:, 0:1]), reads=[pk], writes=[("rinvP", hc)])
                em.barrier()
        if stop_after == "F0":
            em.barrier()
            return nc

        with ExitStack() as ph:
            Wb = sbuf(ph, "Wb", [128, 8, 2048], BF16)
            Wf = sbuf(ph, "Wf", [128, 2, 8, 512], BF16)
            g1b = sbuf(ph, "g1b", [128, D], F32)
            for kc in range(8):
                for h2_ in range(2):
                    dma("pool", Wb[:, kc, h2_ * 1024:(h2_ + 1) * 1024], T["w_in"][kc * 128:(kc + 1) * 128, h2_ * 1024:(h2_ + 1) * 1024],
                        [], [("Wb", kc, h2_)])
            dma("sp", g1b[:], T["g1b"], [], ["g1b"])
            if True:
                bdt = sbuf(ph, "bdt", [128, 2, 128], BF16)
                WfT = sbuf(ph, "WfT", [128, 4, D], BF16)
                dma("sp", bdt[:].rearrange("p a b -> p (a b)"), T["bdt"], [], ["bdt"])
                for n4 in range(4):
                    ps, pk = ps_next()
                    psb = ps[:].bitcast(BF16)
                    for kc in range(8):
                        tr(psb[:, kc * 128:(kc + 1) * 128], Wb[:, kc, 1536 + n4 * 128:1536 + (n4 + 1) * 128], [("Wb", kc, 0), ("Wb", kc, 1)], pk)
                    cp("act", WfT[:, n4, :], psb, [pk], [("WfT", n4)])
                for part in range(2):
                    for kc in range(8):
                        ps, pk = ps_next()
                        for n4 in range(4):
                            mm(ps[:, n4 * 128:(n4 + 1) * 128], WfT[:, n4, kc * 128:(kc + 1) * 128], bdt[:, part, :], True, True,
                               [("WfT", n4), "bdt"], pk)
                        cp("dve", Wf[:, part, kc, :], ps[:], [pk], [("Wf", part, kc)])
            xt = [sbuf(ph, "xt%d" % i, [128, D], F32) for i in range(4)]
            junk = sbuf(ph, "junk", [128, D], BF16)
            ss = sbuf(ph, "ss", [128, 32], F32)
            rs = sbuf(ph, "rs", [128, 32], F32)
            hb = [sbuf(ph, "hb%d" % i, [128, D], BF16) for i in range(2)]
            hTc = [sbuf(ph, "hTc%d" % i, [128, 8, 512], BF16) for i in range(2)]
            Pst = [sbuf(ph, "Pst%d" % i, [128, 12, 512], BF16) for i in range(2)]
            wst = [sbuf(ph, "wst%d" % i, [128, 2, 512], BF16) for i in range(2)]
            em.op("dve", lambda: V.memset(ss[:], 0.0), writes=["ss"])
            ev_cnt = [0]

            def a_load(j2):
                dma("sp", xt[j2 % 4][:], T["x"][j2 * 128:(j2 + 1) * 128, :], [], [("xt", j2 % 4)])
                zero_fill_some(2)

            def a_norm(tc, i):
                hb_ = tc % 2
                j2 = 4 * tc + i
                b = j2 % 2
                if j2 + 3 < 32:
                    a_load(j2 + 3)
                xb = j2 % 4
                act(junk[:], xt[xb][:], AF.Square, [("xt", xb), "ss"], ["junk", ("ss", j2)], accum_out=ss[:, j2:j2 + 1])
                act(rs[:, j2:j2 + 1], ss[:, j2:j2 + 1], AF.Sqrt, [("ss", j2), "epst"], [("rs", j2)], scale=1.0 / D, bias=epst[:])
                em.op("dve", lambda: V.reciprocal(out=rs[:, j2:j2 + 1], in_=rs[:, j2:j2 + 1]), reads=[("rs", j2)], writes=[("rs", j2)])
                stt("dve", hb[b][:], xt[xb][:], rs[:, j2:j2 + 1], g1b[:], ALU.mult, ALU.mult, [("xt", xb), ("rs", j2), "g1b"], [("hb", b)])
                ps, pk = ps_next()
                psb = ps[:].bitcast(BF16)
                for kc in range(8):
                    tr(psb[:, kc * 128:(kc + 1) * 128], hb[b][:, kc * 128:(kc + 1) * 128], [("hb", b)], pk)
                cp("act", hTc[hb_][:, :, i * 128:(i + 1) * 128], psb.rearrange("p (a b) -> p a b", b=128), [pk], [("hTc", hb_, i)])

            def a_mm(tc, part):
                hb_ = tc % 2
                hk = [("hTc", hb_, i) for i in range(4)]
                for cch in range(3 * part, 3 * part + 3):
                    ps, pk = ps_next()
                    for kc in range(8):
                        mm(ps[:], Wb[:, kc, cch * 128:(cch + 1) * 128], hTc[hb_][:, kc, :], kc == 0, kc == 7, hk + [("Wb", kc, 0), ("Wb", kc, 1)], pk)
                    ev_cnt[0] += 1
                    cp("act" if ev_cnt[0] % 2 else "dve", Pst[hb_][:, cch, :], ps[:], [pk], [("Pst", hb_, cch)])
                if part == 3:
                    dma("pool", Pbuf[:, tc * 512:(tc + 1) * 512].rearrange("(c p) t -> p c t", p=128), Pst[hb_][:],
                        [("Pst", hb_, c_) for c_ in range(12)], [("Pbuf", tc)])
                i = part
                j2 = 4 * tc + i
                wb_ = j2 % 2
                for fpart in range(2):
                    ps, pk = ps_next()
                    for kc in range(8):
                        mm(ps[:], hTc[hb_][:, kc, i * 128:(i + 1) * 128], Wf[:, fpart, kc, :], kc == 0, kc == 7,
                           [("hTc", hb_, i), ("Wf", fpart, kc)], pk)
                    ev_cnt[0] += 1
                    cp("act" if ev_cnt[0] % 2 else "dve", wst[wb_][:, fpart, :], ps[:], [pk], [("wst", wb_, fpart)])
                    dma("pool", wbuf[fpart][j2 * 128:(j2 + 1) * 128, :], wst[wb_][:, fpart, :], [("wst", wb_, fpart)], [("wbuf", fpart, j2)])

            for j2_ in range(3):
                a_load(j2_)
            for i in range(4):
                a_norm(0, i)
            for tc in range(8):
                for part in range(4):
                    if tc + 1 < 8:
                        a_norm(tc + 1, part)
                    a_mm(tc, part)
            em.barrier()
        if stop_after == "A":
            return nc

        def head_norm_wave(rs_, mg_ap, row0, rsqs, rstdb):
            for q in range(4):
                act(rsqs[q][:], rs_[q][0], AF.Square, [rs_[q][1]], [("rsq", q)])
            for q in range(4):
                for h_ in range(2):
                    ps, pk = ps_next()
                    mm(ps[:], blk[:], rsqs[q][:, h_ * 512:(h_ + 1) * 512], True, True, [("rsq", q), "blk"], pk)
                    act(rstdb[q][:, h_ * 512:(h_ + 1) * 512], ps[:], AF.Ln, [pk, "epst"], [("rstd_t", q, h_)], bias=epst[:], scale=1.0)
            for q in range(4):
                act(rstdb[q][:], rstdb[q][:], AF.Exp, [("rstd_t", q, 0), ("rstd_t", q, 1)], [("rstd_t", q, 0), ("rstd_t", q, 1)], scale=-0.5)
            for q in range(4):
                stt("dve", rsqs[q][:], rs_[q][0], mg_ap, rstdb[q][:], ALU.mult, ALU.mult,
                    [rs_[q][1], ("rstd_t", q, 0), ("rstd_t", q, 1), "mgp"], [("rsq", q)])
                dma("sp", yTbuf[row0:row0 + 128, q * 1024:(q + 1) * 1024], rsqs[q][:], [("rsq", q)], [("yTbuf", row0, q)])

        with ExitStack() as ph:
            fa_t = sbuf(ph, "fa_tB", [128, 32, 2, 128], BF16)
            gt_t = sbuf(ph, "gt_t", [128, 32, 2, 128], BF16)
            cwp = sbuf(ph, "cwp", [128, 12, 3], F32)
            cbp = sbuf(ph, "cbp", [128, 12], F32)
            fbp = sbuf(ph, "fbp", [128, 4], F32)
            mgp = sbuf(ph, "mgp", [128, 8], F32)
            dma("sp", fa_t[:].rearrange("p b c d -> p (b c d)"), T["fa"][:, 0:8192], [], ["fa"])
            dma("sp", gt_t[:].rearrange("p b c d -> p (b c d)"), T["gt"], [], ["gt"])
            dma("sp", cwp[:].rearrange("p a b -> p (a b)"), T["cwp"], [], ["cwp"])
            dma("sp", cbp[:], T["cbp"], [], ["cbp"])
            dma("sp", fbp[:], T["fbp"], [], ["fbp"])
            dma("sp", mgp[:], T["mgp"], [], ["mgp"])
            Pt = [sbuf(ph, "Pt%d" % s, [128, L + 2], BF16) for s in range(3)]
            Hsb = sbuf(ph, "HsbB", [128, 2, L], BF16)
            ux0s = [sbuf(ph, "ux0_%d" % i, [128, L], BF16) for i in range(2)]
            zTs = [sbuf(ph, "zT_%d" % i, [128, L], BF16) for i in range(2)]
            tA = sbuf(ph, "tA", [128, 1024], F32)
            tB = sbuf(ph, "tB", [128, 1024], F32)
            tC = sbuf(ph, "tC", [128, 1024], F32)
            zP1 = sbuf(ph, "zP1", [128, 32, 128], BF16)
            bufA = sbuf(ph, "bufAB", [128, 2, 32, 128], BF16)
            bufB = sbuf(ph, "bufBB", [128, 2, 32, 128], BF16)
            yc = sbuf(ph, "yc", [128, L], BF16)
            rsqs = [sbuf(ph, "rsq%d" % i, [128, 1024], BF16) for i in range(4)]
            rstdb = [sbuf(ph, "rstdb%d" % i, [128, 1024], BF16) for i in range(4)]
            tD = sbuf(ph, "tD", [128, 1024], F32)
            mts = [[sbuf(ph, "mt%d_%d" % (i, k), [128, 512], F32) for k in range(4)] for i in range(2)]
            for s in range(3):
                em.op("dve", lambda s=s: V.memset(Pt[s][:, 0:1], 0.0), writes=[("Ppad", s)])
                em.op("dve", lambda s=s: V.memset(Pt[s][:, L + 1:L + 2], 0.0), writes=[("Ppad", s)])
            fa5 = fa_t[:].rearrange("p (a b) c d -> p a b c d", a=1)
            def load_P(hc):
                for s in range(3):
                    dma("sp", Pt[s][:, 1:L + 1], Pbuf[(s * 4 + hc) * 128:(s * 4 + hc + 1) * 128, :], [], [("Pt", s)])

            def load_H(hc):
                dma("sp", Hsb[:].rearrange("p a b -> p (a b)"), Hbuf[hc], [], ["HsbB"])

            def conv(hc, qs=(0, 1, 2, 3)):
                ux0 = ux0s[hc % 2]
                zT = zTs[hc % 2]
                par = hc % 2
                for q in qs:
                    t0 = q * 1024
                    tmps = [(tA, "tA"), (tB, "tB"), (tC, "tC")]
                    cc = hc
                    pk_ = [("Pt", 0), ("Ppad", 0), "cwp", "cbp"]
                    act(tA[:], Pt[0][:, 1 + t0:1 + t0 + 1024], AF.Identity, pk_, ["tA"], scale=cwp[:, cc, 1:2], bias=cbp[:, cc:cc + 1])
                    act(tD[:], Pt[0][:, t0:t0 + 1024], AF.Identity, pk_, ["tD"], scale=cwp[:, cc, 0:1])
                    tt("pool", tA[:], tA[:], tD[:], ALU.add, ["tA", "tD"], ["tA"])
                    act(tD[:], Pt[0][:, 2 + t0:2 + t0 + 1024], AF.Identity, pk_, ["tD"], scale=cwp[:, cc, 2:3])
                    tt("pool", ux0[:, t0:t0 + 1024], tA[:], tD[:], ALU.add, ["tA", "tD"], [("ux0", par, q)])
                    for s in (1, 2):
                        cc = s * 4 + hc
                        pk_ = [("Pt", s), ("Ppad", s), "cwp", "cbp"]
                        tmp, tk = tmps[s]
                        if s == 2:
                            act(tmp[:], Pt[s][:, 1 + t0:1 + t0 + 1024], AF.Identity, pk_, [tk], scale=cwp[:, cc, 1:2], bias=cbp[:, cc:cc + 1])
                        else:
                            ts("dve", tmp[:], Pt[s][:, 1 + t0:1 + t0 + 1024], cwp[:, cc, 1:2], cbp[:, cc:cc + 1], ALU.mult, ALU.add, pk_, [tk])
                        stt("dve", tmp[:], Pt[s][:, t0:t0 + 1024], cwp[:, cc, 0:1], tmp[:], ALU.mult, ALU.add, pk_ + [tk], [tk])
                        stt("dve", tmp[:], Pt[s][:, 2 + t0:2 + t0 + 1024], cwp[:, cc, 2:3], tmp[:], ALU.mult, ALU.add, pk_ + [tk], [tk])
                    tt("dve", zT[:, t0:t0 + 1024], tB[:], tC[:], ALU.mult, ["tB", "tC"], [("zT", par, q)])

            def fwd(hc):
                zT = zTs[hc % 2]
                par = hc % 2
                zk = [("zT", par, q) for q in range(4)]
                for j0 in range(0, 32, 8):
                    ps, pk = ps_next()
                    psb = ps[:].bitcast(BF16)
                    for jj in range(8):
                        tr(psb[:, jj * 128:(jj + 1) * 128], zT[:, j0 + jj::32], zk, pk)
                    cp("act", zP1[:, j0:j0 + 8, :], psb.rearrange("p (a b) -> p a b", b=128), [pk], ["zP1"])

                fft_fwd_keys_A = [("bufA", cs, j0) for cs in range(2) for j0 in range(0, 32, 4)]

                def sinkY_guard(ch, psR, kR, psI, kI):
                    sl = slice(ch * 512, (ch + 1) * 512)
                    ya = bufA[:].rearrange("p a b c -> p a (b c)")
                    m = mts[ch % 2]
                    mk = [("mt", ch % 2, k) for k in range(4)]
                    tt("dve", m[0][:], psR[:], Hsb[:, 0, sl], ALU.mult, [kR, "HsbB"], [mk[0]])
                    tt("dve", m[1][:], psI[:], Hsb[:, 1, sl], ALU.mult, [kI, "HsbB"], [mk[1]])
                    tt("dve", m[2][:], psR[:], Hsb[:, 1, sl], ALU.mult, [kR, "HsbB"], [mk[2]])
                    tt("dve", m[3][:], psI[:], Hsb[:, 0, sl], ALU.mult, [kI, "HsbB"], [mk[3]])
                    tt("pool", ya[:, 0, sl], m[0][:], m[1][:], ALU.subtract, [mk[0], mk[1]], [("Y", ch)] + fft_fwd_keys_A)
                    tt("pool", ya[:, 1, sl], m[2][:], m[3][:], ALU.add, [mk[2], mk[3]], [("Y", ch)])

                fft_fwd(zP1, 1, fa5, bufA, bufB, "zP1", sinkY_guard)

            def inv(hc, hooks):
                ux0 = ux0s[hc % 2]
                zT = zTs[hc % 2]
                par = hc % 2
                zk = [("zT", par, q) for q in range(4)]
                ya = bufA[:].rearrange("p a b c -> p a (b c)")
                bufB_keys = [("bufB", cs, g0) for cs in range(2) for g0 in range(0, 32, 8)]
                for ch in range(8):
                    sl = slice(ch * 512, (ch + 1) * 512)
                    psR, kR = ps_next()
                    psI, kI = ps_next()
                    rk = [("Y", ch), "tct"]
                    mm(psR[:], tct[:, 0, :], ya[:, 0, sl], True, False, rk, kR)
                    mm(psR[:], tct[:, 2, :], ya[:, 1, sl], False, True, rk, kR)
                    mm(psI[:], tct[:, 1, :], ya[:, 0, sl], True, False, rk, kI)
                    mm(psI[:], tct[:, 0, :], ya[:, 1, sl], False, True, rk, kI)
                    wk = [("Cb", ch)] + (bufB_keys if ch == 0 else [])
                    cp("act", bufB[:, 0, 4 * ch:4 * ch + 4, :], psR[:].rearrange("p (a b) -> p a b", b=128), [kR], wk)
                    cp("dve", bufB[:, 1, 4 * ch:4 * ch + 4, :], psI[:].rearrange("p (a b) -> p a b", b=128), [kI], [("Cb", ch)])
                hooks[0]()
                first = True
                for cs in range(2):
                    for g0 in range(0, 32, 8):
                        ps, pk = ps_next()
                        psb = ps[:].bitcast(BF16)
                        for gg in range(8):
                            g = g0 + gg
                            tr(psb[:, gg * 128:(gg + 1) * 128], bufB[:, cs, g, :], [("Cb", g // 4)], pk)
                        wk = [("Ct", cs, g0)] + ([("Y", ch) for ch in range(8)] if first else [])
                        first = False
                        cp("act" if cs == 0 else "dve", bufA[:, cs, :, 4 * g0:4 * g0 + 32].rearrange("p j (g c) -> p g j c", c=4),
                           psb.rearrange("p (g j c) -> p g j c", g=8, j=32), [pk], wk)
                ctk = [("Ct", cs, g0) for cs in range(2) for g0 in range(0, 32, 8)]
                hooks[1]()
                yc3 = yc[:].rearrange("c (p j) -> c p j", j=32)
                for j0 in range(0, 32, 4):
                    ps, pk = ps_next()
                    for jj in range(4):
                        j = j0 + jj
                        mm(ps[:, jj * 128:(jj + 1) * 128], bufA[:, 0, j, :], gt_t[:, j, 0, :], True, False, ctk + ["gt"], pk)
                        mm(ps[:, jj * 128:(jj + 1) * 128], bufA[:, 1, j, :], gt_t[:, j, 1, :], False, True, ctk + ["gt"], pk)
                    act(yc3[:, :, j0:j0 + 4].rearrange("c p j -> c j p"), ps[:].rearrange("c (j p) -> c j p", p=128), AF.Identity,
                        [pk, ("rinvP", hc)], [("yc", j0)], scale=rinvP[:, hc:hc + 1])
                yck = [("yc", j0) for j0 in range(0, 32, 4)]
                hooks[2]()
                tq = [(tA, "tA"), (tB, "tB"), (tC, "tC"), (tD, "tD")]
                for q in range(4):
                    t0 = q * 1024
                    stt("dve", tq[q][0][:], zT[:, t0:t0 + 1024], fbp[:, hc:hc + 1], yc[:, t0:t0 + 1024], ALU.mult, ALU.add,
                        zk + yck + ["fbp"], [tq[q][1]])
                for q in range(4):
                    t0 = q * 1024
                    tt("pool", tq[q][0][:], tq[q][0][:], ux0[:, t0:t0 + 1024], ALU.mult, [tq[q][1], ("ux0", par, q)], [tq[q][1]])
                head_norm_wave([(tq[q][0][:], tq[q][1]) for q in range(4)], mgp[:, hc:hc + 1], hc * 128, rsqs, rstdb)

            load_P(0)
            load_H(0)
            conv(0)
            load_P(1)
            nop = lambda: None
            for hc in range(4):
                fwd(hc)
                if hc + 1 < 4:
                    load_H(hc + 1)

                    def h0(hc=hc):
                        conv(hc + 1, (0,))

                    def h1(hc=hc):
                        conv(hc + 1, (1,))

                    def h2(hc=hc):
                        conv(hc + 1, (2, 3))
                        if hc + 2 < 4:
                            load_P(hc + 2)

                    inv(hc, [h0, h1, h2])
                else:
                    inv(hc, [nop, nop, nop])
            em.barrier()
        if stop_after == "B":
            return nc

        with ExitStack() as ph:
            et_t = sbuf(ph, "et_t", [128, 32, 3, 128], BF16)
            tcf = sbuf(ph, "tcf", [128, 2, 128], BF16)
            mgp = sbuf(ph, "mgpC", [128, 8], F32)
            dma("sp", et_t[:].rearrange("p b c d -> p (b c d)"), T["et"], [], ["et"])
            dma("sp", tcf[:].rearrange("p a b -> p (a b)"), T["tcf"], [], ["tcf"])
            dma("sp", mgp[:], T["mgp"], [], ["mgp"])
            wris = [sbuf(ph, "wri%d" % i, [128, 2, 32, 128], BF16) for i in range(2)]
            bufA = sbuf(ph, "bufAC", [128, 2, 32, 128], BF16)
            bufB = sbuf(ph, "bufBC", [128, 2, 32, 128], BF16)
            yf = sbuf(ph, "yf", [128, 32, 128], BF16)
            rTs = [sbuf(ph, "rT%d" % i, [128, 1024], F32) for i in range(4)]
            rsqs = [sbuf(ph, "rsqC%d" % i, [128, 1024], BF16) for i in range(4)]
            rstdb = [sbuf(ph, "rstdbC%d" % i, [128, 1024], BF16) for i in range(4)]
            def c_load(fc):
                for part in range(2):
                    dma("sp", wris[fc % 2][:, part, :, :], wbuf[part][:, fc * 128:(fc + 1) * 128].rearrange("(p j) c -> p j c", j=32), [],
                        [("wri", fc % 2, part)])

            c_load(0)
            for fc in range(4):
                wri = wris[fc % 2]
                if fc + 1 < 4:
                    c_load(fc + 1)
                for j0 in range(0, 32, 4):
                    for cs in range(2):
                        ps, pk = ps_next()
                        for jj in range(4):
                            j = j0 + jj
                            if cs == 0:
                                mm(ps[:, jj * 128:(jj + 1) * 128], et_t[:, j, 0, :], wri[:, 0, j, :], True, False, [("wri", fc % 2, 0), ("wri", fc % 2, 1), "et"], pk)
                                mm(ps[:, jj * 128:(jj + 1) * 128], et_t[:, j, 1, :], wri[:, 1, j, :], False, True, [("wri", fc % 2, 0), ("wri", fc % 2, 1), "et"], pk)
                            else:
                                mm(ps[:, jj * 128:(jj + 1) * 128], et_t[:, j, 0, :], wri[:, 1, j, :], True, False, [("wri", fc % 2, 0), ("wri", fc % 2, 1), "et"], pk)
                                mm(ps[:, jj * 128:(jj + 1) * 128], et_t[:, j, 2, :], wri[:, 0, j, :], False, True, [("wri", fc % 2, 0), ("wri", fc % 2, 1), "et"], pk)
                        cp("act" if cs == 0 else "dve",
                           bufA[:].rearrange("p a g (j c) -> p a g j c", c=4)[:, cs, :, j0:j0 + 4, :].rearrange("p g j c -> p j g c"),
                           ps[:].rearrange("p (j g c) -> p j g c", j=4, g=32), [pk], [("bufA", cs, j0)])
                ak = [("bufA", cs, j0) for cs in range(2) for j0 in range(0, 32, 4)]
                for cs in range(2):
                    for g0 in range(0, 32, 8):
                        ps, pk = ps_next()
                        psb = ps[:].bitcast(BF16)
                        for gg in range(8):
                            g = g0 + gg
                            tr(psb[:, gg * 128:(gg + 1) * 128], bufA[:, cs, g, :], ak, pk)
                        cp("act" if cs == 0 else "dve", bufB[:, cs, g0:g0 + 8, :], psb.rearrange("p (a b) -> p a b", b=128), [pk], [("bufB", cs, g0)])
                for g0 in range(0, 32, 4):
                    ps, pk = ps_next()
                    for gg in range(4):
                        g = g0 + gg
                        rk = [("bufB", 0, g // 8 * 8), ("bufB", 1, g // 8 * 8), "tcf"]
                        mm(ps[:, gg * 128:(gg + 1) * 128], bufB[:, 0, g, :], tcf[:, 0, :], True, False, rk, pk)
                        mm(ps[:, gg * 128:(gg + 1) * 128], bufB[:, 1, g, :], tcf[:, 1, :], False, True, rk, pk)
                    cp("act" if (g0 // 4) % 2 else "dve", yf[:, :, 4 * g0:4 * g0 + 16].rearrange("p k (g c) -> p g k c", c=4),
                       ps[:].rearrange("p (g k c) -> p g k c", g=4, k=32), [pk], [("yf", g0)])
                yfk = [("yf", g0) for g0 in range(0, 32, 4)]
                for q in range(4):
                    ps, pk = ps_next()
                    psb = ps[:].bitcast(BF16)
                    for kk_ in range(8):
                        kb_ = q * 8 + kk_
                        tr(psb[:, kk_ * 128:(kk_ + 1) * 128], yf[:, kb_, :], yfk, pk)
                    cp("dve" if q % 2 else "act", rTs[q][:], psb, [pk], [("rT", q)])
                head_norm_wave([(rTs[q][:], ("rT", q)) for q in range(4)], mgp[:, 4 + fc:5 + fc], 512 + fc * 128, rsqs, rstdb)
            em.barrier()
        if stop_after == "C":
            return nc

        ph_moe = st.enter_context(ExitStack())
        w1 = sbuf(ph_moe, "w1", [128, 32], F32)
        w2 = sbuf(ph_moe, "w2", [128, 32], F32)
        ds_i = sbuf(ph_moe, "ds_i", [128, 2, 32], I32)
        dg_i = sbuf(ph_moe, "dg_i", [128, 2, 32], I32)
        ph_tok = ExitStack()
        h2tok = sbuf(ph_tok, "h2tok", [128, 32, D], BF16)
        Lg = sbuf(ph_tok, "Lg", [128, 32, 36], F32)
        with ExitStack() as ph:
            Wo = sbuf(ph, "Wo", [128, 8, D], BF16)
            Wr = sbuf(ph, "Wr", [128, 8, 36], BF16)
            g2b = sbuf(ph, "g2b", [128, D], F32)
            brb = sbuf(ph, "brb", [128, 36], F32)
            for kc in range(8):
                dma("pool", Wo[:, kc, :], T["w_out"][kc * 128:(kc + 1) * 128, :], [], [("Wo", kc)])
            dma("pool", Wr[:], T["wr"].rearrange("(k p) n -> p k n", p=128), [], ["Wr"])
            dma("sp", g2b[:], T["g2b"], [], ["g2b"])
            dma("sp", brb[:], T["brb"], [], ["brb"])
            yTb = [sbuf(ph, "yTb%d" % i, [128, 8, 512], BF16) for i in range(2)]
            xt = [sbuf(ph, "xtD%d" % i, [128, D], F32) for i in range(3)]
            x1t = [sbuf(ph, "x1t%d" % i, [128, D], F32) for i in range(3)]
            junk = sbuf(ph, "junkD", [128, D], BF16)
            ss = sbuf(ph, "ssD", [128, 32], F32)
            rs = sbuf(ph, "rsD", [128, 32], F32)
            h2T = [sbuf(ph, "h2T%d" % i, [128, 8, 128], BF16) for i in range(2)]
            em.op("dve", lambda: V.memset(ss[:], 0.0), writes=["ssD"])
            wo_ps = {}

            def d_front(j2):
                tc, i = j2 // 4, j2 % 4
                yb = tc % 2
                b = j2 % 3
                if i == 0:
                    dma("sp", yTb[yb][:], yTbuf[:, tc * 512:(tc + 1) * 512].rearrange("(c p) t -> p c t", p=128), [], [("yTb", yb)])
                dma("sp", xt[b][:], T["x"][j2 * 128:(j2 + 1) * 128, :], [], [("xtD", b)])
                for half in range(2):
                    ps, pk = ps_next()
                    for cc in range(8):
                        mm(ps[:], yTb[yb][:, cc, i * 128:(i + 1) * 128], Wo[:, cc, half * 512:(half + 1) * 512], cc == 0, cc == 7,
                           [("yTb", yb), ("Wo", cc)], pk)
                    wo_ps[(j2, half)] = (ps, pk)

            def d_back(j2):
                b = j2 % 3
                for half in range(2):
                    ps, pk = wo_ps.pop((j2, half))
                    tt("dve", x1t[b][:, half * 512:(half + 1) * 512], xt[b][:, half * 512:(half + 1) * 512], ps[:], ALU.add,
                       [("xtD", b), pk], [("x1t", b, half)])
                xk = [("x1t", b, 0), ("x1t", b, 1)]
                dma("sp", x1buf[j2 * 128:(j2 + 1) * 128, :], x1t[b][:], xk, [("x1buf", j2)])
                act(junk[:], x1t[b][:], AF.Square, xk + ["ssD"], ["junkD", ("ssD", j2)], accum_out=ss[:, j2:j2 + 1])
                act(rs[:, j2:j2 + 1], ss[:, j2:j2 + 1], AF.Sqrt, [("ssD", j2), "epst"], [("rsD", j2)], scale=1.0 / D, bias=epst[:])
                em.op("dve", lambda: V.reciprocal(out=rs[:, j2:j2 + 1], in_=rs[:, j2:j2 + 1]), reads=[("rsD", j2)], writes=[("rsD", j2)])
                stt("dve", h2tok[:, j2, :], x1t[b][:], rs[:, j2:j2 + 1], g2b[:], ALU.mult, ALU.mult, xk + [("rsD", j2), "g2b"], [("h2tok", j2)])
                ps, pk = ps_next()
                psb = ps[:].bitcast(BF16)
                for kc in range(8):
                    tr(psb[:, kc * 128:(kc + 1) * 128], h2tok[:, j2, kc * 128:(kc + 1) * 128], [("h2tok", j2)], pk)
                cp("act", h2T[j2 % 2][:], psb.rearrange("p (a b) -> p a b", b=128), [pk], [("h2T", j2 % 2)])
                ps, pk = ps_next()
                for kc in range(8):
                    mm(ps[:, 0:36], h2T[j2 % 2][:, kc, :], Wr[:, kc, :], kc == 0, kc == 7, [("h2T", j2 % 2), "Wr"], pk)
                tt("dve", Lg[:, j2, :], ps[:, 0:36], brb[:], ALU.add, [pk, "brb"], [("Lg", j2)])

            d_front(0)
            for j2 in range(32):
                if j2 + 1 < 32:
                    d_front(j2 + 1)
                d_back(j2)
            em.barrier()
        if debug:
            dma("sp", dbgL[:, 0:32 * 36], Lg[:].rearrange("p a b -> p (a b)"), [("Lg", j2) for j2 in range(32)], ["dbgL"])
        if stop_after == "D":
            em.barrier()
            return nc

        with ExitStack() as ph:
            tri = sbuf(ph, "tri", [128, 128], BF16)
            ones = sbuf(ph, "ones", [128, 128], BF16)
            ecb = sbuf(ph, "ecb", [128, 32], F32)
            trb = sbuf(ph, "trb", [128, 32], F32)
            dma("sp", tri[:], T["tri"], [], ["tri"])
            dma("sp", ones[:], T["ones"], [], ["ones"])
            dma("sp", ecb[:], T["ecb"], [], ["ecb"])
            dma("sp", trb[:], T["trb"], [], ["trb"])
            S = lambda name, shape, dt=F32: sbuf(ph, name, shape, dt)
            gmax = S("gmax", [128, 32]); goh = S("goh", [128, 32, 4]); gd = S("gd", [128, 32, 4]); gsum = S("gsum", [128, 32])
            pg = S("pg", [128, 32]); sel4 = S("sel4", [128, 32, 4, 8]); esel = S("esel", [128, 32, 8]); m1 = S("m1", [128, 32])
            oh1 = S("oh1", [128, 32, 8]); e2 = S("e2", [128, 32, 8]); m2 = S("m2", [128, 32]); oh2 = S("oh2", [128, 32, 8])
            dd = S("dd", [128, 32]); A1 = S("A1", [128, 32, 4, 8]); A2 = S("A2", [128, 32, 4, 8]); Mb = S("Mb", [128, 1024], BF16)
            pin = S("pin", [128, 32, 32]); cnt = S("cnt", [128, 32, 32]); base = S("base", [128, 32, 32]); slot = S("slot", [128, 32, 32])
            tmp3 = S("tmp3", [128, 32, 32]); dk = S("dk", [128, 2, 32]); pk_ = S("pk_", [128, 2, 32]); ok = S("ok", [128, 2, 32])
            dsf = S("dsf", [128, 2, 32]); dgf = S("dgf", [128, 2, 32])
            allL = ["LgAll"]
            K = "route"
            lg_keys = [("Lg", j2) for j2 in range(32)]
            gl = Lg[:, :, 0:4]
            em.op("dve", lambda: V.tensor_reduce(out=gmax[:], in_=gl, axis=AX.X, op=ALU.max), reads=lg_keys, writes=["gmax"])
            tt("dve", goh[:], gl, gmax[:].unsqueeze(2).to_broadcast([128, 32, 4]), ALU.is_equal, lg_keys + ["gmax"], ["goh"])
            tt("dve", gd[:], gl, gmax[:].unsqueeze(2).to_broadcast([128, 32, 4]), ALU.subtract, lg_keys + ["gmax"], ["gd"])
            act(gd[:], gd[:], AF.Exp, ["gd"], ["gd"])
            em.op("dve", lambda: V.tensor_reduce(out=gsum[:], in_=gd[:], axis=AX.X, op=ALU.add), reads=["gd"], writes=["gsum"])
            em.op("dve", lambda: V.reciprocal(out=pg[:], in_=gsum[:]), reads=["gsum"], writes=["pg"])
            el4 = Lg[:, :, 4:36].rearrange("p a (g i) -> p a g i", i=8)
            tt("dve", sel4[:], el4, goh[:].unsqueeze(3).to_broadcast([128, 32, 4, 8]), ALU.mult, lg_keys + ["goh"], ["sel4"])
            tt("dve", esel[:], sel4[:, :, 0, :], sel4[:, :, 1, :], ALU.add, ["sel4"], ["esel"])
            tt("dve", esel[:], esel[:], sel4[:, :, 2, :], ALU.add, ["sel4", "esel"], ["esel"])
            tt("dve", esel[:], esel[:], sel4[:, :, 3, :], ALU.add, ["sel4", "esel"], ["esel"])
            em.op("dve", lambda: V.tensor_reduce(out=m1[:], in_=esel[:], axis=AX.X, op=ALU.max), reads=["esel"], writes=["m1"])
            tt("dve", oh1[:], esel[:], m1[:].unsqueeze(2).to_broadcast([128, 32, 8]), ALU.is_equal, ["esel", "m1"], ["oh1"])
            stt("dve", e2[:], oh1[:], -1e30, esel[:], ALU.mult, ALU.add, ["oh1", "esel"], ["e2"])
            em.op("dve", lambda: V.tensor_reduce(out=m2[:], in_=e2[:], axis=AX.X, op=ALU.max), reads=["e2"], writes=["m2"])
            tt("dve", oh2[:], e2[:], m2[:].unsqueeze(2).to_broadcast([128, 32, 8]), ALU.is_equal, ["e2", "m2"], ["oh2"])
            tt("dve", dd[:], m2[:], m1[:], ALU.subtract, ["m1", "m2"], ["dd"])
            act(dd[:], dd[:], AF.Exp, ["dd"], ["dd"])
            ts("dve", w1[:], dd[:], 1.0, None, ALU.add, None, ["dd"], ["w1"])
            em.op("dve", lambda: V.reciprocal(out=w1[:], in_=w1[:]), reads=["w1"], writes=["w1"])
            tt("dve", w2[:], dd[:], w1[:], ALU.mult, ["dd", "w1"], ["w2"])
            tt("dve", w1[:], w1[:], pg[:], ALU.mult, ["w1", "pg"], ["w1"])
            tt("dve", w2[:], w2[:], pg[:], ALU.mult, ["w2", "pg"], ["w2"])
            gb = goh[:].unsqueeze(3).to_broadcast([128, 32, 4, 8])
            tt("dve", A1[:], gb, oh1[:].unsqueeze(2).to_broadcast([128, 32, 4, 8]), ALU.mult, ["goh", "oh1"], ["A1"])
            tt("dve", A2[:], gb, oh2[:].unsqueeze(2).to_broadcast([128, 32, 4, 8]), ALU.mult, ["goh", "oh2"], ["A2"])
            A1f = A1[:].rearrange("p a g i -> p a (g i)")
            A2f = A2[:].rearrange("p a g i -> p a (g i)")
            tt("dve", Mb[:].rearrange("p (a e) -> p a e", e=32), A1f, A2f, ALU.add, ["A1", "A2"], ["Mb"])
            for h_ in range(2):
                ps, pk = ps_next()
                mm(ps[:], tri[:], Mb[:, h_ * 512:(h_ + 1) * 512], True, True, ["tri", "Mb"], pk)
                cp("act", pin[:, h_ * 16:(h_ + 1) * 16, :], ps[:].rearrange("p (a e) -> p a e", e=32), [pk], ["pin"])
                ps, pk = ps_next()
                mm(ps[:], ones[:], Mb[:, h_ * 512:(h_ + 1) * 512], True, True, ["ones", "Mb"], pk)
                cp("act", cnt[:, h_ * 16:(h_ + 1) * 16, :], ps[:].rearrange("p (a e) -> p a e", e=32), [pk], ["cnt"])
            em.op("dve", lambda: V.memset(base[:, 0, :], 0.0), writes=["base"])
            for j2 in range(1, 32):
                tt("dve", base[:, j2, :], base[:, j2 - 1, :], cnt[:, j2 - 1, :], ALU.add, ["base", "cnt"], ["base"])
            tt("dve", slot[:], pin[:], base[:], ALU.add, ["pin", "base"], ["slot"])
            for k, Af in enumerate([A1f, A2f]):
                tt("dve", tmp3[:], Af, slot[:], ALU.mult, ["A1", "A2", "slot"], ["tmp3"])
                em.op("dve", lambda k=k: V.tensor_reduce(out=pk_[:, k, :], in_=tmp3[:], axis=AX.X, op=ALU.add), reads=["tmp3"], writes=["pk_"])
                tt("dve", tmp3[:], Af, ecb[:].unsqueeze(1).to_broadcast([128, 32, 32]), ALU.mult, ["A1", "A2", "ecb"], ["tmp3"])
                em.op("dve", lambda k=k: V.tensor_reduce(out=dk[:, k, :], in_=tmp3[:], axis=AX.X, op=ALU.add), reads=["tmp3"], writes=["dk"])
            tt("dve", dk[:], dk[:], pk_[:], ALU.add, ["dk", "pk_"], ["dk"])
            ts("dve", ok[:], pk_[:], float(CAP), None, ALU.is_lt, None, ["pk_"], ["ok"])
            tt("dve", dgf[:], dk[:], ok[:], ALU.mult, ["dk", "ok"], ["dgf"])
            tt("dve", dsf[:], dk[:], trb[:].unsqueeze(1).to_broadcast([128, 2, 32]), ALU.subtract, ["dk", "trb"], ["dsf"])
            tt("dve", dsf[:], dsf[:], ok[:], ALU.mult, ["dsf", "ok"], ["dsf"])
            tt("dve", dsf[:], dsf[:], trb[:].unsqueeze(1).to_broadcast([128, 2, 32]), ALU.add, ["dsf", "trb"], ["dsf"])
            tt("dve", w1[:], w1[:], ok[:, 0, :], ALU.mult, ["w1", "ok"], ["w1"])
            tt("dve", w2[:], w2[:], ok[:, 1, :], ALU.mult, ["w2", "ok"], ["w2"])
            cp("dve", ds_i[:], dsf[:], ["dsf"], ["ds_i"])
            cp("dve", dg_i[:], dgf[:], ["dgf"], ["dg_i"])
            if debug:
                dma("sp", dbgL[:, 32 * 36:32 * 36 + 64], dsf[:].rearrange("p a b -> p (a b)"), ["dsf"], ["dbgL2"])
                dma("sp", dbgL[:, 32 * 36 + 64:32 * 36 + 96], w1[:], ["w1"], ["dbgL3"])
                dma("sp", dbgL[:, 32 * 36 + 96:32 * 36 + 128], w2[:], ["w2"], ["dbgL4"])
            for j2 in range(32):
                for k in range(2):
                    em.dma("pool", lambda j2=j2, k=k: G.indirect_dma_start(
                        out=xg, out_offset=bass.IndirectOffsetOnAxis(ap=ds_i[:, k, j2:j2 + 1], axis=0),
                        in_=h2tok[:, j2, :], in_offset=None), reads=["ds_i", ("h2tok", j2)],
                        writes=[("xgs", j2, k)])
            em.barrier()
        ph_tok.close()
        if stop_after == "E":
            return nc

        with ExitStack() as ph:
            NB = 2
            Wg = [sbuf(ph, "Wg%d" % i, [128, 8, 512], BF16) for i in range(NB)]
            Wu = [sbuf(ph, "Wu%d" % i, [128, 8, 512], BF16) for i in range(NB)]
            Wd = [sbuf(ph, "Wd%d" % i, [128, 4, D], BF16) for i in range(NB)]
            xgt = [sbuf(ph, "xgt%d" % i, [128, 3, D], BF16) for i in range(2)]
            xgT = [sbuf(ph, "xgT%d" % i, [128, 8, CAP], BF16) for i in range(2)]
            sg = [sbuf(ph, "sg%d" % i, [128, CAP], F32) for i in range(2)]
            hT = [sbuf(ph, "hT%d" % i, [128, 4, CAP], BF16) for i in range(2)]
            yt = [sbuf(ph, "yt%d" % i, [128, D], F32) for i in range(4)]
            yt_rr = 0
            NST = (CAP + 127) // 128
            NSTG = 5
            stg = [sbuf(ph, "stg%d" % i, [128, 4096], F32) for i in range(NSTG)]
            stg_rr = [0]

            def load_expert(e):
                b = e % NB
                b2 = e % 2
                items = [
                    (T["w_gate"][e].rearrange("(p k) n -> p k n", k=8), Wg[b], ("Wg", b), 8, 512, "act"),
                    (T["w_up"][e].rearrange("(p k) n -> p k n", k=8), Wu[b], ("Wu", b), 8, 512, "dve"),
                    (T["w_down"][e].rearrange("(k p) n -> p k n", p=128), Wd[b], ("Wd", b), 4, 1024, "pool"),
                ]
                for src, dst, key, nk, nn, ce in items:
                    i = stg_rr[0] % NSTG
                    stg_rr[0] += 1
                    sv = stg[i][:].rearrange("p (k n) -> p k n", k=nk)
                    dma("sp", sv, src, [], [("stg", i)])
                    hk_ = nk // 2
                    for h_ in range(2):
                        wk = [(key[0], key[1], 0), (key[0], key[1], 4 if key[0] != "Wd" else 2)] if h_ == 0 else []
                        ce_ = ce if ce != "pool" else ("dve" if h_ == 0 else "act")
                        cp(ce_, dst[:, h_ * hk_:(h_ + 1) * hk_, :], sv[:, h_ * hk_:(h_ + 1) * hk_, :], [("stg", i)],
                           [(key[0], key[1], "h%d" % h_)] + wk)
                for s_ in range((CAP + 127) // 128):
                    w_ = min(128, CAP - s_ * 128)
                    dma("pool", xgt[b2][0:w_, s_, :], xg[e * CAP + s_ * 128:e * CAP + s_ * 128 + w_, :], [], [("xgt", b2, s_)])

            def f_transposes(e):
                b2 = e % 2
                for s in range(3):
                    if s * 128 >= CAP:
                        break
                    w_ = min(128, CAP - s * 128)
                    ps, pk = ps_next()
                    psb = ps[:].bitcast(BF16)
                    for kc in range(8):
                        tr(psb[:, kc * 128:kc * 128 + w_], xgt[b2][0:w_, s, kc::8], [("xgt", b2, s)], pk, kdim=w_)
                    cp("act" if s % 2 else "dve", xgT[b2][:, :, s * 128:s * 128 + w_],
                       psb.rearrange("p (a b) -> p a b", b=128)[:, :, 0:w_], [pk], [("xgT", b2, s)])

            load_expert(0)
            f_transposes(0)
            for e in range(32):
                b = e % NB
                b2 = e % 2
                if e + 1 < 32:
                    load_expert(e + 1)
                xk = [("xgT", b2, s) for s in range(NST)]
                for mc in range(4):
                    psG, kG = ps_next()
                    psU, kU = ps_next()
                    for kc in range(8):
                        mm(psG[:, 0:CAP], Wg[b][:, kc, mc * 128:(mc + 1) * 128], xgT[b2][:, kc, :], kc == 0, kc == 7, xk + [("Wg", b, "h0"), ("Wg", b, "h1"), ("Wg", b, 0), ("Wg", b, 4)], kG)
                    for kc in range(8):
                        mm(psU[:, 0:CAP], Wu[b][:, kc, mc * 128:(mc + 1) * 128], xgT[b2][:, kc, :], kc == 0, kc == 7, xk + [("Wu", b, "h0"), ("Wu", b, "h1"), ("Wu", b, 0), ("Wu", b, 4)], kU)
                    sb_ = mc % 2
                    act(sg[sb_][:], psG[:, 0:CAP], AF.Silu, [kG], [("sg", sb_)])
                    tt("dve", hT[b2][:, mc, :], sg[sb_][:], psU[:, 0:CAP], ALU.mult, [("sg", sb_), kU], [("hT", b2, mc)])
                if e + 1 < 32:
                    f_transposes(e + 1)
                hk = [("hT", b2, mc) for mc in range(4)]
                for s in range(NST):
                    w_ = min(128, CAP - s * 128)
                    yb = yt_rr % 4
                    yt_rr += 1
                    for half in range(2):
                        ps, pk = ps_next()
                        for mc in range(4):
                            mm(ps[0:w_, :], hT[b2][:, mc, s * 128:s * 128 + w_], Wd[b][:, mc, half * 512:(half + 1) * 512], mc == 0, mc == 3,
                               hk + [("Wd", b, "h0"), ("Wd", b, "h1"), ("Wd", b, 0), ("Wd", b, 2)], pk)
                        cp("act" if half else "dve", yt[yb][0:w_, half * 512:(half + 1) * 512], ps[0:w_, :], [pk], [("yt", yb, half)])
                    dma("pool", Ybuf[e * CAP + s * 128:e * CAP + s * 128 + w_, :], yt[yb][0:w_, :], [("yt", yb, 0), ("yt", yb, 1)], [("Ybuf", e, s)])
            em.barrier()
        if stop_after == "F":
            return nc

        with ExitStack() as ph:
            gfb = sbuf(ph, "gfb", [128, D], F32)
            dma("sp", gfb[:], T["gfb"], [], ["gfb"])
            NG = 4
            Y1 = [sbuf(ph, "Y1_%d" % i, [128, D], F32) for i in range(NG)]
            Y2 = [sbuf(ph, "Y2_%d" % i, [128, D], F32) for i in range(NG)]
            xt = [sbuf(ph, "xtG%d" % i, [128, D], F32) for i in range(NG)]
            junk = sbuf(ph, "junkG", [128, D], BF16)
            ss = sbuf(ph, "ssG", [128, 32], F32)
            rs = sbuf(ph, "rsG", [128, 32], F32)
            em.op("dve", lambda: V.memset(ss[:], 0.0), writes=["ssG"])

            def g_load(j2):
                b = j2 % NG
                em.dma("pool", lambda: G.indirect_dma_start(out=Y1[b][:], out_offset=None, in_=Ybuf,
                                                            in_offset=bass.IndirectOffsetOnAxis(ap=dg_i[:, 0, j2:j2 + 1], axis=0)),
                       reads=["dg_i"], writes=[("Y1", b)])
                em.dma("pool", lambda: G.indirect_dma_start(out=Y2[b][:], out_offset=None, in_=Ybuf,
                                                            in_offset=bass.IndirectOffsetOnAxis(ap=dg_i[:, 1, j2:j2 + 1], axis=0)),
                       reads=["dg_i"], writes=[("Y2", b)])
                dma("sp", xt[b][:], x1buf[j2 * 128:(j2 + 1) * 128, :], [], [("xtG", b)])

            for j2 in range(min(3, 32)):
                g_load(j2)
            for j2 in range(32):
                b = j2 % NG
                if j2 + 3 < 32:
                    g_load(j2 + 3)
                stt("dve", xt[b][:], Y1[b][:], w1[:, j2:j2 + 1], xt[b][:], ALU.mult, ALU.add, [("Y1", b), ("xtG", b), "w1"], [("xtG", b)])
                stt("dve", xt[b][:], Y2[b][:], w2[:, j2:j2 + 1], xt[b][:], ALU.mult, ALU.add, [("Y2", b), ("xtG", b), "w2"], [("xtG", b)])
                act(junk[:], xt[b][:], AF.Square, [("xtG", b), "ssG"], ["junkG", ("ssG", j2)], accum_out=ss[:, j2:j2 + 1])
                act(rs[:, j2:j2 + 1], ss[:, j2:j2 + 1], AF.Sqrt, [("ssG", j2), "epst"], [("rsG", j2)], scale=1.0 / D, bias=epst[:])
                em.op("dve", lambda: V.reciprocal(out=rs[:, j2:j2 + 1], in_=rs[:, j2:j2 + 1]), reads=[("rsG", j2)], writes=[("rsG", j2)])
                stt("dve", Y1[b][:], xt[b][:], rs[:, j2:j2 + 1], gfb[:], ALU.mult, ALU.mult, [("xtG", b), ("rsG", j2), "gfb"], [("Y1", b)])
                final_events.append(dma("sp", out_d[j2 * 128:(j2 + 1) * 128, :], Y1[b][:], [("Y1", b)], [("out", j2)]))
            em.barrier()
    return nc


def prep_inputs(inp):
    f32 = np.float32
    g = lambda k: np.asarray(inp[k], dtype=f32)
    rep = lambda v: np.ascontiguousarray(np.tile(v.reshape(1, -1), (128, 1)))
    sh = {}
    sh["w_in"] = np.ascontiguousarray(g("w_in")[0])
    sh["g1b"] = rep(g("norm1_g")[0])
    sh["g2b"] = rep(g("norm2_g")[0])
    sh["gfb"] = rep(g("final_g"))
    cw = g("conv_w")[0]
    sh["cwp"] = np.ascontiguousarray(cw.reshape(3, 12, 128).transpose(2, 1, 0)).reshape(128, 36)
    sh["cbp"] = np.ascontiguousarray(g("conv_b")[0].reshape(12, 128).T)
    sh["fbp"] = np.ascontiguousarray(g("f_bias")[0].reshape(4, 128).T)
    sh["mgp"] = np.ascontiguousarray(g("mix_g")[0].reshape(8, 128).T)
    sh["w_out"] = np.ascontiguousarray(g("w_out")[0])
    wr = np.concatenate([g("w_group")[0], g("w_router")[0].transpose(1, 0, 2).reshape(D, 32)], axis=1)
    sh["wr"] = np.ascontiguousarray(wr)
    sh["brb"] = rep(np.concatenate([g("b_group")[0], g("b_router")[0].reshape(32)]))
    sh["w_gate"] = np.ascontiguousarray(g("w_gate")[0])
    sh["w_up"] = np.ascontiguousarray(g("w_up")[0])
    sh["w_down"] = np.ascontiguousarray(g("w_down")[0])
    fwin = g("f_w_in")[0]
    w1 = np.zeros((66, 128), f32)
    w1[0:33, 0:64] = fwin
    w1[33:66, 64:128] = fwin
    sh["fwin2"] = w1
    fm = g("f_w_mid")[0]
    wm = np.zeros((128, 2, 128), f32)
    for l in range(2):
        wm[0:64, l, 0:64] = fm[l]
        wm[64:128, l, 64:128] = fm[l]
    sh["fwmid2"] = wm.reshape(128, 256)
    fq = g("f_freq")[0].T
    sh["fq"] = np.ascontiguousarray(np.concatenate([fq, fq], 0))
    fb = np.stack([g("f_b_in")[0], g("f_b_mid")[0][0], g("f_b_mid")[0][1]], 1)
    sh["fbb"] = np.ascontiguousarray(np.concatenate([fb, fb], 0))
    fo = g("f_w_out")[0]
    sh["fwout2"] = np.ascontiguousarray(np.concatenate([fo, fo], 0))
    return sh


_CACHE = {}


def kernel(**inputs):
    if "nc" not in _CACHE:
        _CACHE["nc"] = build_nc()
        _CACHE["consts"] = make_consts()
    nc = _CACHE["nc"]
    shared = prep_inputs(inputs)
    shared.update(_CACHE["consts"])
    x = np.asarray(inputs["x"], dtype=np.float32)
    in_maps = []
    for c in range(8):
        m = dict(shared)
        m["x"] = np.ascontiguousarray(x[c])
        in_maps.append(m)
    res = run_bass_kernel_spmd(nc, in_maps, core_ids=list(range(8)))
    out = np.stack([np.asarray(res.results[c]["out"], dtype=np.float32) for c in range(8)], axis=0)
    return out
```

```python
import numpy as np
import ml_dtypes
from contextlib import ExitStack
import concourse.bass as bass
import concourse.mybir as mybir
from concourse.bass_utils import run_bass_kernel_spmd

F32 = mybir.dt.float32
BF16 = mybir.dt.bfloat16
I32 = mybir.dt.int32
AF = mybir.ActivationFunctionType
ALU = mybir.AluOpType
AX = mybir.AxisListType
bf = ml_dtypes.bfloat16

L = 4096
NF = 8192
D = 1024
CAP = 320
NS = 32 * CAP
NROWS = NS + 128
EPS = 1e-6
TWO_PI = float(2 * np.pi)


class Emit:
    def __init__(self, nc, stack, n_dma_sems=48):
        self.nc = nc
        self.eng = {"pe": nc.tensor, "act": nc.scalar, "dve": nc.vector, "pool": nc.gpsimd, "sp": nc.sync}
        self.sem = {}
        self.cnt = {}
        for k in self.eng:
            self.sem[k] = stack.enter_context(nc.semaphore("s_" + k))
            self.cnt[k] = 0
        self.dma_sems = [stack.enter_context(nc.semaphore("d%d" % i)) for i in range(n_dma_sems)]
        self.dma_cnt = [0] * n_dma_sems
        n_sw = 16
        self.dma_pool = {"hw": list(range(n_sw, n_dma_sems)), "sw": list(range(n_sw))}
        self.dma_rr = {"hw": 0, "sw": 0}
        self.seen = {k: {} for k in self.eng}
        self.lastw = {}
        self.reads = {}
        self.n_wait = 0
        self.n_ins = 0

    def _wait(self, e, ev):
        sem, val, src = ev
        sid = id(sem)
        if self.seen[e].get(sid, 0) >= val:
            return
        self.seen[e][sid] = val
        self.eng[e].wait_ge(sem, val)
        self.n_wait += 1

    def _deps(self, e, reads, writes):
        evs = []
        for r in reads:
            w = self.lastw.get(r)
            if w is not None and not (w[2] == e and e == "pe"):
                evs.append(w)
        same_ok = e in ("pe",)
        for wkey in writes:
            w = self.lastw.get(wkey)
            if w is not None and (w[2] != e or not same_ok):
                evs.append(w)
            for ev in self.reads.get(wkey, {}).values():
                if ev[2] != e or not same_ok:
                    evs.append(ev)
        return evs

    def _commit(self, ev, reads, writes):
        for r in reads:
            self.reads.setdefault(r, {})[(ev[2], id(ev[0]))] = ev
        for w in writes:
            self.lastw[w] = ev
            self.reads[w] = {}

    def op(self, e, fn, reads=(), writes=()):
        for ev in self._deps(e, reads, writes):
            self._wait(e, ev)
        ins = fn()
        self.cnt[e] += 1
        ins.then_inc(self.sem[e], 1)
        ev = (self.sem[e], self.cnt[e], e)
        self._commit(ev, reads, writes)
        self.n_ins += 1
        return ev

    def dma(self, q, fn, reads=(), writes=()):
        for ev in self._deps(q, reads, writes):
            self._wait(q, ev)
        kind = "sw" if q == "pool" else "hw"
        lst = self.dma_pool[kind]
        i = lst[self.dma_rr[kind]]
        self.dma_rr[kind] = (self.dma_rr[kind] + 1) % len(lst)
        sem = self.dma_sems[i]
        if self.dma_cnt[i] > 0:
            self._wait(q, (sem, 16 * self.dma_cnt[i], "dma"))
        ins = fn()
        self.dma_cnt[i] += 1
        ins.then_inc(sem, 16)
        ev = (sem, 16 * self.dma_cnt[i], "dma%d" % i)
        self._commit(ev, reads, writes)
        self.n_ins += 1
        return ev

    def barrier(self):
        for e in self.eng:
            for e2 in self.eng:
                if e2 != e and self.cnt[e2] > 0:
                    self._wait(e, (self.sem[e2], self.cnt[e2], e2))
            for i, s in enumerate(self.dma_sems):
                if self.dma_cnt[i] > 0:
                    self._wait(e, (s, 16 * self.dma_cnt[i], "dma"))
        self.lastw = {}
        self.reads = {}


def _kron4(M):
    return np.kron(M, np.eye(4))


def make_consts():
    c = {}
    p = np.arange(256)[:, None].astype(np.float64)
    k1 = np.arange(128)[None, :].astype(np.float64)
    fa = np.zeros((2, 32, 2, 128, 128))
    gt = np.zeros((32, 2, 128, 128))
    for j in range(32):
        th = 2 * np.pi * (p * (k1 + 0.5) / 256.0 + j * (k1 + 0.5) / NF)
        for pt in range(2):
            sgn = 1.0 if pt == 0 else -1.0
            fa[pt, j, 0] = sgn * np.cos(th[pt * 128:(pt + 1) * 128])
            fa[pt, j, 1] = -sgn * np.sin(th[pt * 128:(pt + 1) * 128])
        gt[j, 0] = (2.0 / NF) * np.cos(th[:128]).T
        gt[j, 1] = -(2.0 / NF) * np.sin(th[:128]).T
    c["fa"] = np.ascontiguousarray(fa.transpose(3, 0, 1, 2, 4)).reshape(128, 2 * 32 * 2 * 128).astype(bf)
    c["gt"] = np.ascontiguousarray(gt.transpose(2, 0, 1, 3)).reshape(128, 32 * 2 * 128).astype(bf)
    jj = np.arange(32)[:, None].astype(np.float64)
    kk = np.arange(32)[None, :].astype(np.float64)
    ph = 2 * np.pi * jj * kk / 32.0
    Tc = _kron4(np.cos(ph))
    Ts = _kron4(np.sin(ph))
    c["tct"] = np.stack([Tc, Ts, -Ts], 1).reshape(128, 3 * 128).astype(bf)
    c["tcf"] = np.stack([Tc / 512.0, Ts / 512.0], 1).reshape(128, 2 * 128).astype(bf)
    pa = np.arange(128)[:, None].astype(np.float64)
    ka = np.arange(128)[None, :].astype(np.float64)
    et = np.zeros((32, 3, 128, 128))
    for j in range(32):
        th = 2 * np.pi * (pa * ka / 128.0 + j * ka / 4096.0)
        et[j, 0] = np.cos(th)
        et[j, 1] = np.sin(th)
        et[j, 2] = -np.sin(th)
    c["et"] = np.ascontiguousarray(et.transpose(2, 0, 1, 3)).reshape(128, 32 * 3 * 128).astype(bf)
    cc = np.arange(64)[:, None].astype(np.float64)
    c2 = np.arange(64)[None, :].astype(np.float64)
    C64 = np.cos(2 * np.pi * cc * c2 / 64.0)
    S64 = np.sin(2 * np.pi * cc * c2 / 64.0)
    c["bdt"] = np.stack([np.kron(np.eye(2), C64), np.kron(np.eye(2), -S64)], 1).reshape(128, 256).astype(bf)
    c["ident"] = np.eye(128).astype(bf)
    c["blk"] = (np.kron(np.eye(2), np.ones((64, 64))) / 64.0).astype(bf)
    c["tri"] = np.triu(np.ones((128, 128)), 1).astype(bf)
    c["ones"] = np.ones((128, 128)).astype(bf)
    c["onesf"] = np.ones((128, 128), np.float32)
    c["ecb"] = np.tile((np.arange(32) * CAP).astype(np.float32)[None, :], (128, 1))
    c["trb"] = np.tile((NS + np.arange(128)).astype(np.float32)[:, None], (1, 32))
    n = np.arange(NF)
    m = np.where(n < L, n, NF - n).astype(np.float64)
    m[L] = 0
    t = (m / (L - 1)).astype(np.float32)
    w = (2.0 * np.pi / L) * m
    f = np.linspace(1e-4, 15, 16)[None, :]
    emb = np.concatenate([t[:, None], np.cos(f * w[:, None]), -np.sin(f * w[:, None])], -1).astype(np.float32)
    c["emb2"] = np.concatenate([emb[:L].T, emb[L:].T], 0).astype(np.float32)
    tn = -t.astype(np.float32)
    tn[L] = -1e4
    c["tneg"] = np.ascontiguousarray(tn.reshape(2, 128, 32).transpose(1, 0, 2)).reshape(128, 64)
    max_decay = np.log(1e-2) / 0.3
    min_decay = np.log(1e-2) / 1.5
    deltas = np.abs(np.linspace(min_decay, max_decay, 512)).astype(np.float32)
    c["deltab"] = np.tile(deltas[None, :], (128, 1))
    return c


CONST_SPECS = [
    ("fa", [128, 16384], BF16), ("gt", [128, 8192], BF16), ("tct", [128, 384], BF16), ("tcf", [128, 256], BF16),
    ("et", [128, 12288], BF16), ("bdt", [128, 256], BF16), ("ident", [128, 128], BF16), ("blk", [128, 128], BF16),
    ("tri", [128, 128], BF16), ("ones", [128, 128], BF16), ("onesf", [128, 128], F32), ("ecb", [128, 32], F32),
    ("trb", [128, 32], F32), ("emb2", [66, 4096], F32), ("tneg", [128, 64], F32), ("deltab", [128, 512], F32),
]
IN_SPECS = [
    ("x", [L, D], F32), ("w_in", [D, 2048], F32), ("g1b", [128, D], F32), ("g2b", [128, D], F32), ("gfb", [128, D], F32),
    ("cwp", [128, 36], F32), ("cbp", [128, 12], F32), ("fbp", [128, 4], F32), ("mgp", [128, 8], F32),
    ("w_out", [D, D], F32), ("wr", [D, 36], F32), ("brb", [128, 36], F32),
    ("w_gate", [32, D, 512], F32), ("w_up", [32, D, 512], F32), ("w_down", [32, 512, D], F32),
    ("fwin2", [66, 128], F32), ("fwmid2", [128, 256], F32), ("fq", [128, 3], F32), ("fbb", [128, 3], F32),
    ("fwout2", [128, 1024], F32),
]


def build_nc(stop_after=None, debug=False):
    nc = bass.Bass("TRN2", target_bir_lowering=False)
    T = {}
    for name, shape, dt in IN_SPECS + CONST_SPECS:
        T[name] = nc.dram_tensor(name, shape, dt, kind="ExternalInput").ap()
    out_d = nc.dram_tensor("out", [L, D], F32, kind="ExternalOutput").ap()
    skind = "ExternalOutput" if debug else "Internal"
    Pbuf = nc.dram_tensor("Pbuf", [1536, L], BF16, kind=skind).ap()
    wbuf = [nc.dram_tensor("wbuf%d" % i, [L, 512], BF16, kind=skind).ap() for i in range(2)]
    Hbuf = nc.dram_tensor("Hbuf", [4, 128, 8192], BF16, kind=skind).ap()
    yTbuf = nc.dram_tensor("yTbuf", [D, L], BF16, kind=skind).ap()
    x1buf = nc.dram_tensor("x1buf", [L, D], F32, kind=skind).ap()
    xg = nc.dram_tensor("xg", [NROWS, D], BF16, kind=skind).ap()
    Ybuf = nc.dram_tensor("Ybuf", [NROWS, D], F32, kind=skind).ap()
    dbgL = nc.dram_tensor("dbgL", [128, 32 * 40], F32, kind=skind).ap()

    with ExitStack() as st:
        em = Emit(nc, st)
        V, A, G, PE = nc.vector, nc.scalar, nc.gpsimd, nc.tensor
        ENG = {"dve": V, "act": A, "pool": G}

        def sbuf(stack, name, shape, dt):
            return stack.enter_context(nc.sbuf_tensor("sb_" + name, shape, dt))

        PS = [st.enter_context(nc.psum_tensor("ps%d" % i, [128, 512], F32)) for i in range(8)]
        ps_rr = [0]

        def ps_next():
            i = ps_rr[0]
            ps_rr[0] = (i + 1) % 8
            return PS[i], "ps%d" % i

        def mm(out, lhsT, rhs, start, stop, reads, pk):
            return em.op("pe", lambda: PE.matmul(out, lhsT=lhsT, rhs=rhs, start=start, stop=stop), reads=reads, writes=[pk])

        def tr(out, in_, reads, pk, kdim=128):
            idn = ident[:] if kdim == 128 else ident[0:kdim, 0:kdim]
            return em.op("pe", lambda: PE.transpose(out, in_, idn), reads=list(reads) + ["ident"], writes=[pk])

        def cp(e, out, in_, reads, writes):
            if e == "act":
                return em.op("act", lambda: A.copy(out=out, in_=in_), reads=reads, writes=writes)
            return em.op(e, lambda: ENG[e].tensor_copy(out=out, in_=in_), reads=reads, writes=writes)

        def tt(e, out, in0, in1, op, reads, writes):
            return em.op(e, lambda: ENG[e].tensor_tensor(out=out, in0=in0, in1=in1, op=op), reads=reads, writes=writes)

        def ts(e, out, in0, s1, s2, op0, op1, reads, writes):
            if op1 is None:
                return em.op(e, lambda: ENG[e].tensor_scalar(out=out, in0=in0, scalar1=s1, scalar2=None, op0=op0), reads=reads, writes=writes)
            return em.op(e, lambda: ENG[e].tensor_scalar(out=out, in0=in0, scalar1=s1, scalar2=s2, op0=op0, op1=op1), reads=reads, writes=writes)

        def stt(e, out, in0, scalar, in1, op0, op1, reads, writes):
            return em.op(e, lambda: ENG[e].scalar_tensor_tensor(out=out, in0=in0, scalar=scalar, in1=in1, op0=op0, op1=op1), reads=reads, writes=writes)

        def act(out, in_, func, reads, writes, **kw):
            return em.op("act", lambda: A.activation(out=out, in_=in_, func=func, **kw), reads=reads, writes=writes)

        def dma(q, out, in_, reads, writes):
            e = {"sp": nc.sync, "act": A, "pool": G}[q]
            return em.dma(q, lambda: e.dma_start(out=out, in_=in_), reads=reads, writes=writes)

        final_events = []

        ident = sbuf(st, "ident", [128, 128], BF16)
        blk = sbuf(st, "blk", [128, 128], BF16)
        tct = sbuf(st, "tct", [128, 3, 128], BF16)
        epst = sbuf(st, "epst", [128, 1], F32)
        rinvP = sbuf(st, "rinvP", [128, 4], F32)
        dma("sp", ident[:], T["ident"], [], ["ident"])
        dma("sp", blk[:], T["blk"], [], ["blk"])
        dma("sp", tct[:].rearrange("p a b -> p (a b)"), T["tct"], [], ["tct"])
        em.op("dve", lambda: V.memset(epst[:], EPS), writes=["epst"])

        ztp = sbuf(st, "ztp", [128, 2, D], BF16)
        em.op("pool", lambda: G.memset(ztp[:], 0.0), writes=["ztp"])
        zf_chunks = [(r0, min(256, NROWS - r0)) for r0 in range(0, NROWS, 256)]

        def zero_fill_some(n):
            for _ in range(n):
                if zf_chunks:
                    r0, nr = zf_chunks.pop(0)
                    dma("sp", xg[r0:r0 + nr, :].rearrange("(s p) f -> p s f", p=128), ztp[:, 0:nr // 128, :], ["ztp"], [("xg", r0)])

        def fft_fwd(src, npt, fa_t, bufA, bufB, srck, sink):
            for j0 in range(0, 32, 4):
                for cs in range(2):
                    ps, pk = ps_next()
                    for jj in range(4):
                        j = j0 + jj
                        for pt in range(npt):
                            mm(ps[:, jj * 128:(jj + 1) * 128], fa_t[:, pt, j, cs, :], src[:, pt * 32 + j, :],
                               pt == 0, pt == npt - 1, (srck if isinstance(srck, list) else [srck]) + ["fa"], pk)
                    cp("act" if cs == 0 else "dve",
                       bufA[:].rearrange("p a g (j c) -> p a g j c", c=4)[:, cs, :, j0:j0 + 4, :].rearrange("p g j c -> p j g c"),
                       ps[:].rearrange("p (j g c) -> p j g c", j=4, g=32), [pk], [("bufA", cs, j0)])
            for cs in range(2):
                for g0 in range(0, 32, 8):
                    ps, pk = ps_next()
                    psb = ps[:].bitcast(BF16)
                    for gg in range(8):
                        g = g0 + gg
                        tr(psb[:, gg * 128:(gg + 1) * 128], bufA[:, cs, g, :],
                           [("bufA", cs, j0) for j0 in range(0, 32, 4)], pk)
                    cp("act" if cs == 0 else "dve", bufB[:, cs, g0:g0 + 8, :], psb.rearrange("p (a b) -> p a b", b=128),
                       [pk], [("bufB", cs, g0)])
            for ch in range(8):
                psR, kR = ps_next()
                psI, kI = ps_next()
                bBf = bufB[:].rearrange("p a b c -> p a (b c)")
                br = bBf[:, 0, ch * 512:(ch + 1) * 512]
                bi = bBf[:, 1, ch * 512:(ch + 1) * 512]
                rk = [("bufB", 0, (4 * ch) // 8 * 8), ("bufB", 1, (4 * ch) // 8 * 8), "tct"]
                mm(psR[:], tct[:, 0, :], br, True, False, rk, kR)
                mm(psR[:], tct[:, 1, :], bi, False, True, rk, kR)
                mm(psI[:], tct[:, 0, :], bi, True, False, rk, kI)
                mm(psI[:], tct[:, 2, :], br, False, True, rk, kI)
                sink(ch, psR, kR, psI, kI)

        with ExitStack() as ph:
            fa_t = sbuf(ph, "fa_t", [128, 2, 32, 2, 128], BF16)
            w1t = sbuf(ph, "w1t", [66, 128], F32)
            wmt = sbuf(ph, "wmt", [128, 2, 128], F32)
            fq = sbuf(ph, "fq", [128, 3], F32)
            fbb = sbuf(ph, "fbb", [128, 3], F32)
            fq2 = sbuf(ph, "fq2", [128, 3], F32)
            fqb2 = sbuf(ph, "fqb2", [128, 3], F32)
            fwo = sbuf(ph, "fwo", [128, 1024], BF16)
            onesf = sbuf(ph, "onesf", [128, 128], F32)
            tneg = sbuf(ph, "tneg", [128, 64], F32)
            deltab = sbuf(ph, "deltab", [128, 512], F32)
            hid = sbuf(ph, "hid", [128, L], BF16)
            dma("sp", fa_t[:].rearrange("p a b c d -> p (a b c d)"), T["fa"], [], ["fa"])
            dma("sp", w1t[:], T["fwin2"], [], ["w1t"])
            dma("sp", wmt[:].rearrange("p a b -> p (a b)"), T["fwmid2"], [], ["wmt"])
            dma("sp", fq[:], T["fq"], [], ["fq"])
            dma("sp", fbb[:], T["fbb"], [], ["fbb"])
            dma("pool", fwo[:], T["fwout2"], [], ["fwo"])
            dma("sp", onesf[:], T["onesf"], [], ["onesf"])
            dma("sp", tneg[:], T["tneg"], [], ["tneg"])
            dma("sp", deltab[:], T["deltab"], [], ["deltab"])
            ts("dve", fq2[:], fq[:], float(1.0 / 3.0), None, ALU.mult, None, ["fq"], ["fq2"])
            tt("dve", fqb2[:], fq2[:], fbb[:], ALU.mult, ["fq2", "fbb"], ["fqb2"])
            with ExitStack() as ph2:
                emb = sbuf(ph2, "emb", [66, L], F32)
                hA = sbuf(ph2, "hA", [128, L], F32)
                hB = sbuf(ph2, "hB", [128, L], F32)
                for ch in range(8):
                    dma("sp", emb[:, ch * 512:(ch + 1) * 512], T["emb2"][:, ch * 512:(ch + 1) * 512], [], [("emb", ch)])
                ub = [sbuf(ph2, "ub%d" % i, [128, 512], F32) for i in range(8)]
                rb = [sbuf(ph2, "rb%d" % i, [128, 512], F32) for i in range(8)]
                srcs = [(emb, "emb", w1t[:]), (hA, "hA", wmt[:, 0, :]), (hB, "hB", wmt[:, 1, :])]
                dsts = [(hA, "hA"), (hB, "hB"), (hid, "hid")]
                for l in range(3):
                    s_t, s_k, lhsT = srcs[l]
                    d_t, d_k = dsts[l]
                    pss = {}
                    for ch in range(8):
                        ps, pk = ps_next()
                        pss[ch] = (ps, pk)
                        mm(ps[:], lhsT, s_t[:, ch * 512:(ch + 1) * 512], True, True, [(s_k, ch), "w1t", "wmt"], pk)
                    for ch in range(8):
                        ps, pk = pss[ch]
                        act(ub[ch][:], ps[:], AF.Sin, [pk, "fq2", "fqb2"], [("ub", ch)], scale=fq2[:, l:l + 1], bias=fqb2[:, l:l + 1])
                    for ch in range(8):
                        tt("dve", rb[ch][:], ub[ch][:], ub[ch][:], ALU.mult, [("ub", ch)], [("rb", ch)])
                    for ch in range(8):
                        ts("dve", rb[ch][:], rb[ch][:], -4.0, 3.0, ALU.mult, ALU.add, [("rb", ch)], [("rb", ch)])
                    for ch in range(8):
                        tt("dve", d_t[:, ch * 512:(ch + 1) * 512], rb[ch][:], ub[ch][:], ALU.mult, [("rb", ch), ("ub", ch)], [(d_k, ch)])
                em.barrier()
            with ExitStack() as ph2:
                decs = [sbuf(ph2, "dec%d" % i, [128, 64, 128], BF16) for i in range(2)]
                kbs = [sbuf(ph2, "kb%d" % i, [128, 64, 128], BF16) for i in range(2)]
                acc = sbuf(ph2, "acc", [128, 128], F32)
                bufA = sbuf(ph2, "bufA", [128, 2, 32, 128], BF16)
                bufB = sbuf(ph2, "bufB", [128, 2, 32, 128], BF16)
                Hsb = sbuf(ph2, "Hsb", [128, 2, L], BF16)

                def dec_gen(hc):
                    for col in range(64):
                        act(decs[hc % 2][:, col, :], deltab[:, hc * 128:(hc + 1) * 128], AF.Exp, ["deltab", "tneg"], [("dec", hc % 2, col // 4)],
                            scale=tneg[:, col:col + 1])

                def out_layer(hc):
                    dec = decs[hc % 2]
                    kb = kbs[hc % 2]
                    for c0 in range(0, 64, 4):
                        ps, pk = ps_next()
                        for cc in range(4):
                            col = c0 + cc
                            pt, j = col // 32, col % 32
                            mm(ps[:, cc * 128:(cc + 1) * 128], hid[64 * pt:64 * pt + 64, j::32],
                               fwo[64 * pt:64 * pt + 64, pt * 512 + hc * 128:pt * 512 + (hc + 1) * 128], True, True,
                               [("hid", ch) for ch in range(8)] + ["fwo"], pk)
                        tt("dve", kb[:, c0:c0 + 4, :], ps[:].rearrange("p (a b) -> p a b", b=128), dec[:, c0:c0 + 4, :], ALU.mult,
                           [pk, ("dec", hc % 2, c0 // 4)], [("kb", hc % 2, c0 // 4)])

                def sinkH(ch, psR, kR, psI, kI):
                    cp("act", Hsb[:, 0, ch * 512:(ch + 1) * 512], psR[:], [kR], [("Hsb", ch)])
                    cp("dve", Hsb[:, 1, ch * 512:(ch + 1) * 512], psI[:], [kI], [("Hsb", ch)])

                dec_gen(0)
                out_layer(0)
                for hc in range(4):
                    kb = kbs[hc % 2]
                    kbk = [("kb", hc % 2, i) for i in range(16)]
                    if hc + 1 < 4:
                        dec_gen(hc + 1)
                        out_layer(hc + 1)
                    fft_fwd(kb, 2, fa_t, bufA, bufB, kbk, sinkH)
                    dma("sp", Hbuf[hc], Hsb[:].rearrange("p a b -> p (a b)"), [("Hsb", ch) for ch in range(8)], [("Hbuf", hc)])
                    em.op("dve", lambda: V.tensor_reduce(out=acc[:], in_=kb[:].rearrange("p n c -> p c n"), axis=AX.X, op=ALU.add,
                                                         apply_absolute_value=True), reads=kbk, writes=["acc"])
                    ps, pk = ps_next()
                    mm(ps[:, 0:1], acc[:], onesf[:, 0:1], True, True, ["onesf", "acc"], pk)
                    em.op("dve", lambda: V.reciprocal(out=rinvP[:, hc:hc + 1], in_=ps[:, 0:1]), reads=[pk], writes=[("rinvP", hc)])
                em.barrier()
        if stop_after == "F0":
            em.barrier()
            return nc

        with ExitStack() as ph:
            Wb = sbuf(ph, "Wb", [128, 8, 2048], BF16)
            Wf = sbuf(ph, "Wf", [128, 2, 8, 512], BF16)
            g1b = sbuf(ph, "g1b", [128, D], F32)
            for kc in range(8):
                for h2_ in range(2):
                    dma("pool", Wb[:, kc, h2_ * 1024:(h2_ + 1) * 1024], T["w_in"][kc * 128:(kc + 1) * 128, h2_ * 1024:(h2_ + 1) * 1024],
                        [], [("Wb", kc, h2_)])
            dma("sp", g1b[:], T["g1b"], [], ["g1b"])
            if True:
                bdt = sbuf(ph, "bdt", [128, 2, 128], BF16)
                WfT = sbuf(ph, "WfT", [128, 4, D], BF16)
                dma("sp", bdt[:].rearrange("p a b -> p (a b)"), T["bdt"], [], ["bdt"])
                for n4 in range(4):
                    ps, pk = ps_next()
                    psb = ps[:].bitcast(BF16)
                    for kc in range(8):
                        tr(psb[:, kc * 128:(kc + 1) * 128], Wb[:, kc, 1536 + n4 * 128:1536 + (n4 + 1) * 128], [("Wb", kc, 0), ("Wb", kc, 1)], pk)
                    cp("act", WfT[:, n4, :], psb, [pk], [("WfT", n4)])
                for part in range(2):
                    for kc in range(8):
                        ps, pk = ps_next()
                        for n4 in range(4):
                            mm(ps[:, n4 * 128:(n4 + 1) * 128], WfT[:, n4, kc * 128:(kc + 1) * 128], bdt[:, part, :], True, True,
                               [("WfT", n4), "bdt"], pk)
                        cp("dve", Wf[:, part, kc, :], ps[:], [pk], [("Wf", part, kc)])
            xt = [sbuf(ph, "xt%d" % i, [128, D], F32) for i in range(4)]
            junk = sbuf(ph, "junk", [128, D], BF16)
            ss = sbuf(ph, "ss", [128, 32], F32)
            rs = sbuf(ph, "rs", [128, 32], F32)
            hb = [sbuf(ph, "hb%d" % i, [128, D], BF16) for i in range(2)]
            hTc = [sbuf(ph, "hTc%d" % i, [128, 8, 512], BF16) for i in range(2)]
            Pst = [sbuf(ph, "Pst%d" % i, [128, 12, 512], BF16) for i in range(2)]
            wst = [sbuf(ph, "wst%d" % i, [128, 2, 512], BF16) for i in range(2)]
            em.op("dve", lambda: V.memset(ss[:], 0.0), writes=["ss"])
            ev_cnt = [0]

            def a_load(j2):
                dma("sp", xt[j2 % 4][:], T["x"][j2 * 128:(j2 + 1) * 128, :], [], [("xt", j2 % 4)])
                zero_fill_some(2)

            def a_norm(tc, i):
                hb_ = tc % 2
                j2 = 4 * tc + i
                b = j2 % 2
                if j2 + 3 < 32:
                    a_load(j2 + 3)
                xb = j2 % 4
                act(junk[:], xt[xb][:], AF.Square, [("xt", xb), "ss"], ["junk", ("ss", j2)], accum_out=ss[:, j2:j2 + 1])
                act(rs[:, j2:j2 + 1], ss[:, j2:j2 + 1], AF.Sqrt, [("ss", j2), "epst"], [("rs", j2)], scale=1.0 / D, bias=epst[:])
                em.op("dve", lambda: V.reciprocal(out=rs[:, j2:j2 + 1], in_=rs[:, j2:j2 + 1]), reads=[("rs", j2)], writes=[("rs", j2)])
                stt("dve", hb[b][:], xt[xb][:], rs[:, j2:j2 + 1], g1b[:], ALU.mult, ALU.mult, [("xt", xb), ("rs", j2), "g1b"], [("hb", b)])
                ps, pk = ps_next()
                psb = ps[:].bitcast(BF16)
                for kc in range(8):
                    tr(psb[:, kc * 128:(kc + 1) * 128], hb[b][:, kc * 128:(kc + 1) * 128], [("hb", b)], pk)
                cp("act", hTc[hb_][:, :, i * 128:(i + 1) * 128], psb.rearrange("p (a b) -> p a b", b=128), [pk], [("hTc", hb_, i)])

            def a_mm(tc, part):
                hb_ = tc % 2
                hk = [("hTc", hb_, i) for i in range(4)]
                for cch in range(3 * part, 3 * part + 3):
                    ps, pk = ps_next()
                    for kc in range(8):
                        mm(ps[:], Wb[:, kc, cch * 128:(cch + 1) * 128], hTc[hb_][:, kc, :], kc == 0, kc == 7, hk + [("Wb", kc, 0), ("Wb", kc, 1)], pk)
                    ev_cnt[0] += 1
                    cp("act" if ev_cnt[0] % 2 else "dve", Pst[hb_][:, cch, :], ps[:], [pk], [("Pst", hb_, cch)])
                if part == 3:
                    dma("pool", Pbuf[:, tc * 512:(tc + 1) * 512].rearrange("(c p) t -> p c t", p=128), Pst[hb_][:],
                        [("Pst", hb_, c_) for c_ in range(12)], [("Pbuf", tc)])
                i = part
                j2 = 4 * tc + i
                wb_ = j2 % 2
                for fpart in range(2):
                    ps, pk = ps_next()
                    for kc in range(8):
                        mm(ps[:], hTc[hb_][:, kc, i * 128:(i + 1) * 128], Wf[:, fpart, kc, :], kc == 0, kc == 7,
                           [("hTc", hb_, i), ("Wf", fpart, kc)], pk)
                    ev_cnt[0] += 1
                    cp("act" if ev_cnt[0] % 2 else "dve", wst[wb_][:, fpart, :], ps[:], [pk], [("wst", wb_, fpart)])
                    dma("pool", wbuf[fpart][j2 * 128:(j2 + 1) * 128, :], wst[wb_][:, fpart, :], [("wst", wb_, fpart)], [("wbuf", fpart, j2)])

            for j2_ in range(3):
                a_load(j2_)
            for i in range(4):
                a_norm(0, i)
            for tc in range(8):
                for part in range(4):
                    if tc + 1 < 8:
                        a_norm(tc + 1, part)
                    a_mm(tc, part)
            em.barrier()
        if stop_after == "A":
            return nc

        def head_norm_wave(rs_, mg_ap, row0, rsqs, rstdb):
            for q in range(4):
                act(rsqs[q][:], rs_[q][0], AF.Square, [rs_[q][1]], [("rsq", q)])
            for q in range(4):
                for h_ in range(2):
                    ps, pk = ps_next()
                    mm(ps[:], blk[:], rsqs[q][:, h_ * 512:(h_ + 1) * 512], True, True, [("rsq", q), "blk"], pk)
                    act(rstdb[q][:, h_ * 512:(h_ + 1) * 512], ps[:], AF.Ln, [pk, "epst"], [("rstd_t", q, h_)], bias=epst[:], scale=1.0)
            for q in range(4):
                act(rstdb[q][:], rstdb[q][:], AF.Exp, [("rstd_t", q, 0), ("rstd_t", q, 1)], [("rstd_t", q, 0), ("rstd_t", q, 1)], scale=-0.5)
            for q in range(4):
                stt("dve", rsqs[q][:], rs_[q][0], mg_ap, rstdb[q][:], ALU.mult, ALU.mult,
                    [rs_[q][1], ("rstd_t", q, 0), ("rstd_t", q, 1), "mgp"], [("rsq", q)])
                dma("sp", yTbuf[row0:row0 + 128, q * 1024:(q + 1) * 1024], rsqs[q][:], [("rsq", q)], [("yTbuf", row0, q)])

        with ExitStack() as ph:
            fa_t = sbuf(ph, "fa_tB", [128, 32, 2, 128], BF16)
            gt_t = sbuf(ph, "gt_t", [128, 32, 2, 128], BF16)
            cwp = sbuf(ph, "cwp", [128, 12, 3], F32)
            cbp = sbuf(ph, "cbp", [128, 12], F32)
            fbp = sbuf(ph, "fbp", [128, 4], F32)
            mgp = sbuf(ph, "mgp", [128, 8], F32)
            dma("sp", fa_t[:].rearrange("p b c d -> p (b c d)"), T["fa"][:, 0:8192], [], ["fa"])
            dma("sp", gt_t[:].rearrange("p b c d -> p (b c d)"), T["gt"], [], ["gt"])
            dma("sp", cwp[:].rearrange("p a b -> p (a b)"), T["cwp"], [], ["cwp"])
            dma("sp", cbp[:], T["cbp"], [], ["cbp"])
            dma("sp", fbp[:], T["fbp"], [], ["fbp"])
            dma("sp", mgp[:], T["mgp"], [], ["mgp"])
            Pt = [sbuf(ph, "Pt%d" % s, [128, L + 2], BF16) for s in range(3)]
            Hsb = sbuf(ph, "HsbB", [128, 2, L], BF16)
            ux0s = [sbuf(ph, "ux0_%d" % i, [128, L], BF16) for i in range(2)]
            zTs = [sbuf(ph, "zT_%d" % i, [128, L], BF16) for i in range(2)]
            tA = sbuf(ph, "tA", [128, 1024], F32)
            tB = sbuf(ph, "tB", [128, 1024], F32)
            tC = sbuf(ph, "tC", [128, 1024], F32)
            zP1 = sbuf(ph, "zP1", [128, 32, 128], BF16)
            bufA = sbuf(ph, "bufAB", [128, 2, 32, 128], BF16)
            bufB = sbuf(ph, "bufBB", [128, 2, 32, 128], BF16)
            yc = sbuf(ph, "yc", [128, L], BF16)
            rsqs = [sbuf(ph, "rsq%d" % i, [128, 1024], BF16) for i in range(4)]
            rstdb = [sbuf(ph, "rstdb%d" % i, [128, 1024], BF16) for i in range(4)]
            tD = sbuf(ph, "tD", [128, 1024], F32)
            mts = [[sbuf(ph, "mt%d_%d" % (i, k), [128, 512], F32) for k in range(4)] for i in range(2)]
            for s in range(3):
                em.op("dve", lambda s=s: V.memset(Pt[s][:, 0:1], 0.0), writes=[("Ppad", s)])
                em.op("dve", lambda s=s: V.memset(Pt[s][:, L + 1:L + 2], 0.0), writes=[("Ppad", s)])
            fa5 = fa_t[:].rearrange("p (a b) c d -> p a b c d", a=1)
            def load_P(hc):
                for s in range(3):
                    dma("sp", Pt[s][:, 1:L + 1], Pbuf[(s * 4 + hc) * 128:(s * 4 + hc + 1) * 128, :], [], [("Pt", s)])

            def load_H(hc):
                dma("sp", Hsb[:].rearrange("p a b -> p (a b)"), Hbuf[hc], [], ["HsbB"])

            def conv(hc, qs=(0, 1, 2, 3)):
                ux0 = ux0s[hc % 2]
                zT = zTs[hc % 2]
                par = hc % 2
                for q in qs:
                    t0 = q * 1024
                    tmps = [(tA, "tA"), (tB, "tB"), (tC, "tC")]
                    cc = hc
                    pk_ = [("Pt", 0), ("Ppad", 0), "cwp", "cbp"]
                    act(tA[:], Pt[0][:, 1 + t0:1 + t0 + 1024], AF.Identity, pk_, ["tA"], scale=cwp[:, cc, 1:2], bias=cbp[:, cc:cc + 1])
                    act(tD[:], Pt[0][:, t0:t0 + 1024], AF.Identity, pk_, ["tD"], scale=cwp[:, cc, 0:1])
                    tt("pool", tA[:], tA[:], tD[:], ALU.add, ["tA", "tD"], ["tA"])
                    act(tD[:], Pt[0][:, 2 + t0:2 + t0 + 1024], AF.Identity, pk_, ["tD"], scale=cwp[:, cc, 2:3])
                    tt("pool", ux0[:, t0:t0 + 1024], tA[:], tD[:], ALU.add, ["tA", "tD"], [("ux0", par, q)])
                    for s in (1, 2):
                        cc = s * 4 + hc
                        pk_ = [("Pt", s), ("Ppad", s), "cwp", "cbp"]
                        tmp, tk = tmps[s]
                        if s == 2:
                            act(tmp[:], Pt[s][:, 1 + t0:1 + t0 + 1024], AF.Identity, pk_, [tk], scale=cwp[:, cc, 1:2], bias=cbp[:, cc:cc + 1])
                        else:
                            ts("dve", tmp[:], Pt[s][:, 1 + t0:1 + t0 + 1024], cwp[:, cc, 1:2], cbp[:, cc:cc + 1], ALU.mult, ALU.add, pk_, [tk])
                        stt("dve", tmp[:], Pt[s][:, t0:t0 + 1024], cwp[:, cc, 0:1], tmp[:], ALU.mult, ALU.add, pk_ + [tk], [tk])
                        stt("dve", tmp[:], Pt[s][:, 2 + t0:2 + t0 + 1024], cwp[:, cc, 2:3], tmp[:], ALU.mult, ALU.add, pk_ + [tk], [tk])
                    tt("dve", zT[:, t0:t0 + 1024], tB[:], tC[:], ALU.mult, ["tB", "tC"], [("zT", par, q)])

            def fwd(hc):
                zT = zTs[hc % 2]
                par = hc % 2
                zk = [("zT", par, q) for q in range(4)]
                for j0 in range(0, 32, 8):
                    ps, pk = ps_next()
                    psb = ps[:].bitcast(BF16)
                    for jj in range(8):
                        tr(psb[:, jj * 128:(jj + 1) * 128], zT[:, j0 + jj::32], zk, pk)
                    cp("act", zP1[:, j0:j0 + 8, :], psb.rearrange("p (a b) -> p a b", b=128), [pk], ["zP1"])

                fft_fwd_keys_A = [("bufA", cs, j0) for cs in range(2) for j0 in range(0, 32, 4)]

                def sinkY_guard(ch, psR, kR, psI, kI):
                    sl = slice(ch * 512, (ch + 1) * 512)
                    ya = bufA[:].rearrange("p a b c -> p a (b c)")
                    m = mts[ch % 2]
                    mk = [("mt", ch % 2, k) for k in range(4)]
                    tt("dve", m[0][:], psR[:], Hsb[:, 0, sl], ALU.mult, [kR, "HsbB"], [mk[0]])
                    tt("dve", m[1][:], psI[:], Hsb[:, 1, sl], ALU.mult, [kI, "HsbB"], [mk[1]])
                    tt("dve", m[2][:], psR[:], Hsb[:, 1, sl], ALU.mult, [kR, "HsbB"], [mk[2]])
                    tt("dve", m[3][:], psI[:], Hsb[:, 0, sl], ALU.mult, [kI, "HsbB"], [mk[3]])
                    tt("pool", ya[:, 0, sl], m[0][:], m[1][:], ALU.subtract, [mk[0], mk[1]], [("Y", ch)] + fft_fwd_keys_A)
                    tt("pool", ya[:, 1, sl], m[2][:], m[3][:], ALU.add, [mk[2], mk[3]], [("Y", ch)])

                fft_fwd(zP1, 1, fa5, bufA, bufB, "zP1", sinkY_guard)

            def inv(hc, hooks):
                ux0 = ux0s[hc % 2]
                zT = zTs[hc % 2]
                par = hc % 2
                zk = [("zT", par, q) for q in range(4)]
                ya = bufA[:].rearrange("p a b c -> p a (b c)")
                bufB_keys = [("bufB", cs, g0) for cs in range(2) for g0 in range(0, 32, 8)]
                for ch in range(8):
                    sl = slice(ch * 512, (ch + 1) * 512)
                    psR, kR = ps_next()
                    psI, kI = ps_next()
                    rk = [("Y", ch), "tct"]
                    mm(psR[:], tct[:, 0, :], ya[:, 0, sl], True, False, rk, kR)
                    mm(psR[:], tct[:, 2, :], ya[:, 1, sl], False, True, rk, kR)
                    mm(psI[:], tct[:, 1, :], ya[:, 0, sl], True, False, rk, kI)
                    mm(psI[:], tct[:, 0, :], ya[:, 1, sl], False, True, rk, kI)
                    wk = [("Cb", ch)] + (bufB_keys if ch == 0 else [])
                    cp("act", bufB[:, 0, 4 * ch:4 * ch + 4, :], psR[:].rearrange("p (a b) -> p a b", b=128), [kR], wk)
                    cp("dve", bufB[:, 1, 4 * ch:4 * ch + 4, :], psI[:].rearrange("p (a b) -> p a b", b=128), [kI], [("Cb", ch)])
                hooks[0]()
                first = True
                for cs in range(2):
                    for g0 in range(0, 32, 8):
                        ps, pk = ps_next()
                        psb = ps[:].bitcast(BF16)
                        for gg in range(8):
                            g = g0 + gg
                            tr(psb[:, gg * 128:(gg + 1) * 128], bufB[:, cs, g, :], [("Cb", g // 4)], pk)
                        wk = [("Ct", cs, g0)] + ([("Y", ch) for ch in range(8)] if first else [])
                        first = False
                        cp("act" if cs == 0 else "dve", bufA[:, cs, :, 4 * g0:4 * g0 + 32].rearrange("p j (g c) -> p g j c", c=4),
                           psb.rearrange("p (g j c) -> p g j c", g=8, j=32), [pk], wk)
                ctk = [("Ct", cs, g0) for cs in range(2) for g0 in range(0, 32, 8)]
                hooks[1]()
                yc3 = yc[:].rearrange("c (p j) -> c p j", j=32)
                for j0 in range(0, 32, 4):
                    ps, pk = ps_next()
                    for jj in range(4):
                        j = j0 + jj
                        mm(ps[:, jj * 128:(jj + 1) * 128], bufA[:, 0, j, :], gt_t[:, j, 0, :], True, False, ctk + ["gt"], pk)
                        mm(ps[:, jj * 128:(jj + 1) * 128], bufA[:, 1, j, :], gt_t[:, j, 1, :], False, True, ctk + ["gt"], pk)
                    if (j0 // 4) % 2 == 0:
                        act(yc3[:, :, j0:j0 + 4].rearrange("c p j -> c j p"), ps[:].rearrange("c (j p) -> c j p", p=128), AF.Identity,
                            [pk, ("rinvP", hc)], [("yc", j0)], scale=rinvP[:, hc:hc + 1])
                    else:
                        ts("dve", yc3[:, :, j0:j0 + 4].rearrange("c p j -> c j p"), ps[:].rearrange("c (j p) -> c j p", p=128),
                           rinvP[:, hc:hc + 1], None, ALU.mult, None, [pk, ("rinvP", hc)], [("yc", j0)])
                yck = [("yc", j0) for j0 in range(0, 32, 4)]
                hooks[2]()
                tq = [(tA, "tA"), (tB, "tB"), (tC, "tC"), (tD, "tD")]
                for q in range(4):
                    t0 = q * 1024
                    stt("dve", tq[q][0][:], zT[:, t0:t0 + 1024], fbp[:, hc:hc + 1], yc[:, t0:t0 + 1024], ALU.mult, ALU.add,
                        zk + yck + ["fbp"], [tq[q][1]])
                for q in range(4):
                    t0 = q * 1024
                    tt("pool", tq[q][0][:], tq[q][0][:], ux0[:, t0:t0 + 1024], ALU.mult, [tq[q][1], ("ux0", par, q)], [tq[q][1]])
                head_norm_wave([(tq[q][0][:], tq[q][1]) for q in range(4)], mgp[:, hc:hc + 1], hc * 128, rsqs, rstdb)

            load_P(0)
            load_H(0)
            conv(0)
            load_P(1)
            nop = lambda: None
            for hc in range(4):
                fwd(hc)
                if hc + 1 < 4:
                    load_H(hc + 1)

                    def h0(hc=hc):
                        conv(hc + 1, (0,))

                    def h1(hc=hc):
                        conv(hc + 1, (1, 2))

                    def h2(hc=hc):
                        conv(hc + 1, (3,))
                        if hc + 2 < 4:
                            load_P(hc + 2)

                    inv(hc, [h0, h1, h2])
                else:
                    inv(hc, [nop, nop, nop])
            em.barrier()
        if stop_after == "B":
            return nc

        with ExitStack() as ph:
            et_t = sbuf(ph, "et_t", [128, 32, 3, 128], BF16)
            tcf = sbuf(ph, "tcf", [128, 2, 128], BF16)
            mgp = sbuf(ph, "mgpC", [128, 8], F32)
            dma("sp", et_t[:].rearrange("p b c d -> p (b c d)"), T["et"], [], ["et"])
            dma("sp", tcf[:].rearrange("p a b -> p (a b)"), T["tcf"], [], ["tcf"])
            dma("sp", mgp[:], T["mgp"], [], ["mgp"])
            wris = [sbuf(ph, "wri%d" % i, [128, 2, 32, 128], BF16) for i in range(2)]
            bufA = sbuf(ph, "bufAC", [128, 2, 32, 128], BF16)
            bufB = sbuf(ph, "bufBC", [128, 2, 32, 128], BF16)
            yf = sbuf(ph, "yf", [128, 32, 128], BF16)
            rTs = [sbuf(ph, "rT%d" % i, [128, 1024], F32) for i in range(4)]
            rsqs = [sbuf(ph, "rsqC%d" % i, [128, 1024], BF16) for i in range(4)]
            rstdb = [sbuf(ph, "rstdbC%d" % i, [128, 1024], BF16) for i in range(4)]
            def c_load(fc):
                for part in range(2):
                    dma("sp", wris[fc % 2][:, part, :, :], wbuf[part][:, fc * 128:(fc + 1) * 128].rearrange("(p j) c -> p j c", j=32), [],
                        [("wri", fc % 2, part)])

            c_load(0)
            for fc in range(4):
                wri = wris[fc % 2]
                if fc + 1 < 4:
                    c_load(fc + 1)
                for j0 in range(0, 32, 4):
                    for cs in range(2):
                        ps, pk = ps_next()
                        for jj in range(4):
                            j = j0 + jj
                            if cs == 0:
                                mm(ps[:, jj * 128:(jj + 1) * 128], et_t[:, j, 0, :], wri[:, 0, j, :], True, False, [("wri", fc % 2, 0), ("wri", fc % 2, 1), "et"], pk)
                                mm(ps[:, jj * 128:(jj + 1) * 128], et_t[:, j, 1, :], wri[:, 1, j, :], False, True, [("wri", fc % 2, 0), ("wri", fc % 2, 1), "et"], pk)
                            else:
                                mm(ps[:, jj * 128:(jj + 1) * 128], et_t[:, j, 0, :], wri[:, 1, j, :], True, False, [("wri", fc % 2, 0), ("wri", fc % 2, 1), "et"], pk)
                                mm(ps[:, jj * 128:(jj + 1) * 128], et_t[:, j, 2, :], wri[:, 0, j, :], False, True, [("wri", fc % 2, 0), ("wri", fc % 2, 1), "et"], pk)
                        cp("act" if cs == 0 else "dve",
                           bufA[:].rearrange("p a g (j c) -> p a g j c", c=4)[:, cs, :, j0:j0 + 4, :].rearrange("p g j c -> p j g c"),
                           ps[:].rearrange("p (j g c) -> p j g c", j=4, g=32), [pk], [("bufA", cs, j0)])
                ak = [("bufA", cs, j0) for cs in range(2) for j0 in range(0, 32, 4)]
                for cs in range(2):
                    for g0 in range(0, 32, 8):
                        ps, pk = ps_next()
                        psb = ps[:].bitcast(BF16)
                        for gg in range(8):
                            g = g0 + gg
                            tr(psb[:, gg * 128:(gg + 1) * 128], bufA[:, cs, g, :], ak, pk)
                        cp("act" if cs == 0 else "dve", bufB[:, cs, g0:g0 + 8, :], psb.rearrange("p (a b) -> p a b", b=128), [pk], [("bufB", cs, g0)])
                for g0 in range(0, 32, 4):
                    ps, pk = ps_next()
                    for gg in range(4):
                        g = g0 + gg
                        rk = [("bufB", 0, g // 8 * 8), ("bufB", 1, g // 8 * 8), "tcf"]
                        mm(ps[:, gg * 128:(gg + 1) * 128], bufB[:, 0, g, :], tcf[:, 0, :], True, False, rk, pk)
                        mm(ps[:, gg * 128:(gg + 1) * 128], bufB[:, 1, g, :], tcf[:, 1, :], False, True, rk, pk)
                    cp("act" if (g0 // 4) % 2 else "dve", yf[:, :, 4 * g0:4 * g0 + 16].rearrange("p k (g c) -> p g k c", c=4),
                       ps[:].rearrange("p (g k c) -> p g k c", g=4, k=32), [pk], [("yf", g0)])
                yfk = [("yf", g0) for g0 in range(0, 32, 4)]
                for q in range(4):
                    ps, pk = ps_next()
                    psb = ps[:].bitcast(BF16)
                    for kk_ in range(8):
                        kb_ = q * 8 + kk_
                        tr(psb[:, kk_ * 128:(kk_ + 1) * 128], yf[:, kb_, :], yfk, pk)
                    cp("dve" if q % 2 else "act", rTs[q][:], psb, [pk], [("rT", q)])
                head_norm_wave([(rTs[q][:], ("rT", q)) for q in range(4)], mgp[:, 4 + fc:5 + fc], 512 + fc * 128, rsqs, rstdb)
            em.barrier()
        if stop_after == "C":
            return nc

        ph_moe = st.enter_context(ExitStack())
        w1 = sbuf(ph_moe, "w1", [128, 32], F32)
        w2 = sbuf(ph_moe, "w2", [128, 32], F32)
        ds_i = sbuf(ph_moe, "ds_i", [128, 2, 32], I32)
        dg_i = sbuf(ph_moe, "dg_i", [128, 2, 32], I32)
        ph_tok = ExitStack()
        h2tok = sbuf(ph_tok, "h2tok", [128, 32, D], BF16)
        Lg = sbuf(ph_tok, "Lg", [128, 32, 36], F32)
        with ExitStack() as ph:
            Wo = sbuf(ph, "Wo", [128, 8, D], BF16)
            Wr = sbuf(ph, "Wr", [128, 8, 36], BF16)
            g2b = sbuf(ph, "g2b", [128, D], F32)
            brb = sbuf(ph, "brb", [128, 36], F32)
            for kc in range(8):
                dma("pool", Wo[:, kc, :], T["w_out"][kc * 128:(kc + 1) * 128, :], [], [("Wo", kc)])
            dma("pool", Wr[:], T["wr"].rearrange("(k p) n -> p k n", p=128), [], ["Wr"])
            dma("sp", g2b[:], T["g2b"], [], ["g2b"])
            dma("sp", brb[:], T["brb"], [], ["brb"])
            yTb = [sbuf(ph, "yTb%d" % i, [128, 8, 512], BF16) for i in range(2)]
            xt = [sbuf(ph, "xtD%d" % i, [128, D], F32) for i in range(3)]
            x1t = [sbuf(ph, "x1t%d" % i, [128, D], F32) for i in range(3)]
            junk = sbuf(ph, "junkD", [128, D], BF16)
            ss = sbuf(ph, "ssD", [128, 32], F32)
            rs = sbuf(ph, "rsD", [128, 32], F32)
            h2T = [sbuf(ph, "h2T%d" % i, [128, 8, 128], BF16) for i in range(2)]
            em.op("dve", lambda: V.memset(ss[:], 0.0), writes=["ssD"])
            wo_ps = {}

            def d_front(j2):
                tc, i = j2 // 4, j2 % 4
                yb = tc % 2
                b = j2 % 3
                if i == 0:
                    dma("sp", yTb[yb][:], yTbuf[:, tc * 512:(tc + 1) * 512].rearrange("(c p) t -> p c t", p=128), [], [("yTb", yb)])
                dma("sp", xt[b][:], T["x"][j2 * 128:(j2 + 1) * 128, :], [], [("xtD", b)])
                for half in range(2):
                    ps, pk = ps_next()
                    for cc in range(8):
                        mm(ps[:], yTb[yb][:, cc, i * 128:(i + 1) * 128], Wo[:, cc, half * 512:(half + 1) * 512], cc == 0, cc == 7,
                           [("yTb", yb), ("Wo", cc)], pk)
                    wo_ps[(j2, half)] = (ps, pk)

            def d_back(j2):
                b = j2 % 3
                for half in range(2):
                    ps, pk = wo_ps.pop((j2, half))
                    tt("dve", x1t[b][:, half * 512:(half + 1) * 512], xt[b][:, half * 512:(half + 1) * 512], ps[:], ALU.add,
                       [("xtD", b), pk], [("x1t", b, half)])
                xk = [("x1t", b, 0), ("x1t", b, 1)]
                dma("sp", x1buf[j2 * 128:(j2 + 1) * 128, :], x1t[b][:], xk, [("x1buf", j2)])
                act(junk[:], x1t[b][:], AF.Square, xk + ["ssD"], ["junkD", ("ssD", j2)], accum_out=ss[:, j2:j2 + 1])
                act(rs[:, j2:j2 + 1], ss[:, j2:j2 + 1], AF.Sqrt, [("ssD", j2), "epst"], [("rsD", j2)], scale=1.0 / D, bias=epst[:])
                em.op("dve", lambda: V.reciprocal(out=rs[:, j2:j2 + 1], in_=rs[:, j2:j2 + 1]), reads=[("rsD", j2)], writes=[("rsD", j2)])
                stt("dve", h2tok[:, j2, :], x1t[b][:], rs[:, j2:j2 + 1], g2b[:], ALU.mult, ALU.mult, xk + [("rsD", j2), "g2b"], [("h2tok", j2)])
                ps, pk = ps_next()
                psb = ps[:].bitcast(BF16)
                for kc in range(8):
                    tr(psb[:, kc * 128:(kc + 1) * 128], h2tok[:, j2, kc * 128:(kc + 1) * 128], [("h2tok", j2)], pk)
                cp("act", h2T[j2 % 2][:], psb.rearrange("p (a b) -> p a b", b=128), [pk], [("h2T", j2 % 2)])
                ps, pk = ps_next()
                for kc in range(8):
                    mm(ps[:, 0:36], h2T[j2 % 2][:, kc, :], Wr[:, kc, :], kc == 0, kc == 7, [("h2T", j2 % 2), "Wr"], pk)
                tt("dve", Lg[:, j2, :], ps[:, 0:36], brb[:], ALU.add, [pk, "brb"], [("Lg", j2)])

            d_front(0)
            for j2 in range(32):
                if j2 + 1 < 32:
                    d_front(j2 + 1)
                d_back(j2)
            em.barrier()
        if debug:
            dma("sp", dbgL[:, 0:32 * 36], Lg[:].rearrange("p a b -> p (a b)"), [("Lg", j2) for j2 in range(32)], ["dbgL"])
        if stop_after == "D":
            em.barrier()
            return nc

        with ExitStack() as ph:
            tri = sbuf(ph, "tri", [128, 128], BF16)
            ones = sbuf(ph, "ones", [128, 128], BF16)
            ecb = sbuf(ph, "ecb", [128, 32], F32)
            trb = sbuf(ph, "trb", [128, 32], F32)
            dma("sp", tri[:], T["tri"], [], ["tri"])
            dma("sp", ones[:], T["ones"], [], ["ones"])
            dma("sp", ecb[:], T["ecb"], [], ["ecb"])
            dma("sp", trb[:], T["trb"], [], ["trb"])
            S = lambda name, shape, dt=F32: sbuf(ph, name, shape, dt)
            gmax = S("gmax", [128, 32]); goh = S("goh", [128, 32, 4]); gd = S("gd", [128, 32, 4]); gsum = S("gsum", [128, 32])
            pg = S("pg", [128, 32]); sel4 = S("sel4", [128, 32, 4, 8]); esel = S("esel", [128, 32, 8]); m1 = S("m1", [128, 32])
            oh1 = S("oh1", [128, 32, 8]); e2 = S("e2", [128, 32, 8]); m2 = S("m2", [128, 32]); oh2 = S("oh2", [128, 32, 8])
            dd = S("dd", [128, 32]); A1 = S("A1", [128, 32, 4, 8]); A2 = S("A2", [128, 32, 4, 8]); Mb = S("Mb", [128, 1024], BF16)
            pin = S("pin", [128, 32, 32]); cnt = S("cnt", [128, 32, 32]); base = S("base", [128, 32, 32]); slot = S("slot", [128, 32, 32])
            tmp3 = S("tmp3", [128, 32, 32]); dk = S("dk", [128, 2, 32]); pk_ = S("pk_", [128, 2, 32]); ok = S("ok", [128, 2, 32])
            dsf = S("dsf", [128, 2, 32]); dgf = S("dgf", [128, 2, 32])
            allL = ["LgAll"]
            K = "route"
            lg_keys = [("Lg", j2) for j2 in range(32)]
            gl = Lg[:, :, 0:4]
            em.op("dve", lambda: V.tensor_reduce(out=gmax[:], in_=gl, axis=AX.X, op=ALU.max), reads=lg_keys, writes=["gmax"])
            tt("dve", goh[:], gl, gmax[:].unsqueeze(2).to_broadcast([128, 32, 4]), ALU.is_equal, lg_keys + ["gmax"], ["goh"])
            tt("dve", gd[:], gl, gmax[:].unsqueeze(2).to_broadcast([128, 32, 4]), ALU.subtract, lg_keys + ["gmax"], ["gd"])
            act(gd[:], gd[:], AF.Exp, ["gd"], ["gd"])
            em.op("dve", lambda: V.tensor_reduce(out=gsum[:], in_=gd[:], axis=AX.X, op=ALU.add), reads=["gd"], writes=["gsum"])
            em.op("dve", lambda: V.reciprocal(out=pg[:], in_=gsum[:]), reads=["gsum"], writes=["pg"])
            el4 = Lg[:, :, 4:36].rearrange("p a (g i) -> p a g i", i=8)
            tt("dve", sel4[:], el4, goh[:].unsqueeze(3).to_broadcast([128, 32, 4, 8]), ALU.mult, lg_keys + ["goh"], ["sel4"])
            tt("dve", esel[:], sel4[:, :, 0, :], sel4[:, :, 1, :], ALU.add, ["sel4"], ["esel"])
            tt("dve", esel[:], esel[:], sel4[:, :, 2, :], ALU.add, ["sel4", "esel"], ["esel"])
            tt("dve", esel[:], esel[:], sel4[:, :, 3, :], ALU.add, ["sel4", "esel"], ["esel"])
            em.op("dve", lambda: V.tensor_reduce(out=m1[:], in_=esel[:], axis=AX.X, op=ALU.max), reads=["esel"], writes=["m1"])
            tt("dve", oh1[:], esel[:], m1[:].unsqueeze(2).to_broadcast([128, 32, 8]), ALU.is_equal, ["esel", "m1"], ["oh1"])
            stt("dve", e2[:], oh1[:], -1e30, esel[:], ALU.mult, ALU.add, ["oh1", "esel"], ["e2"])
            em.op("dve", lambda: V.tensor_reduce(out=m2[:], in_=e2[:], axis=AX.X, op=ALU.max), reads=["e2"], writes=["m2"])
            tt("dve", oh2[:], e2[:], m2[:].unsqueeze(2).to_broadcast([128, 32, 8]), ALU.is_equal, ["e2", "m2"], ["oh2"])
            tt("dve", dd[:], m2[:], m1[:], ALU.subtract, ["m1", "m2"], ["dd"])
            act(dd[:], dd[:], AF.Exp, ["dd"], ["dd"])
            ts("dve", w1[:], dd[:], 1.0, None, ALU.add, None, ["dd"], ["w1"])
            em.op("dve", lambda: V.reciprocal(out=w1[:], in_=w1[:]), reads=["w1"], writes=["w1"])
            tt("dve", w2[:], dd[:], w1[:], ALU.mult, ["dd", "w1"], ["w2"])
            tt("dve", w1[:], w1[:], pg[:], ALU.mult, ["w1", "pg"], ["w1"])
            tt("dve", w2[:], w2[:], pg[:], ALU.mult, ["w2", "pg"], ["w2"])
            gb = goh[:].unsqueeze(3).to_broadcast([128, 32, 4, 8])
            tt("dve", A1[:], gb, oh1[:].unsqueeze(2).to_broadcast([128, 32, 4, 8]), ALU.mult, ["goh", "oh1"], ["A1"])
            tt("dve", A2[:], gb, oh2[:].unsqueeze(2).to_broadcast([128, 32, 4, 8]), ALU.mult, ["goh", "oh2"], ["A2"])
            A1f = A1[:].rearrange("p a g i -> p a (g i)")
            A2f = A2[:].rearrange("p a g i -> p a (g i)")
            tt("dve", Mb[:].rearrange("p (a e) -> p a e", e=32), A1f, A2f, ALU.add, ["A1", "A2"], ["Mb"])
            for h_ in range(2):
                ps, pk = ps_next()
                mm(ps[:], tri[:], Mb[:, h_ * 512:(h_ + 1) * 512], True, True, ["tri", "Mb"], pk)
                cp("act", pin[:, h_ * 16:(h_ + 1) * 16, :], ps[:].rearrange("p (a e) -> p a e", e=32), [pk], ["pin"])
                ps, pk = ps_next()
                mm(ps[:], ones[:], Mb[:, h_ * 512:(h_ + 1) * 512], True, True, ["ones", "Mb"], pk)
                cp("act", cnt[:, h_ * 16:(h_ + 1) * 16, :], ps[:].rearrange("p (a e) -> p a e", e=32), [pk], ["cnt"])
            em.op("dve", lambda: V.memset(base[:, 0, :], 0.0), writes=["base"])
            for j2 in range(1, 32):
                tt("dve", base[:, j2, :], base[:, j2 - 1, :], cnt[:, j2 - 1, :], ALU.add, ["base", "cnt"], ["base"])
            tt("dve", slot[:], pin[:], base[:], ALU.add, ["pin", "base"], ["slot"])
            for k, Af in enumerate([A1f, A2f]):
                tt("dve", tmp3[:], Af, slot[:], ALU.mult, ["A1", "A2", "slot"], ["tmp3"])
                em.op("dve", lambda k=k: V.tensor_reduce(out=pk_[:, k, :], in_=tmp3[:], axis=AX.X, op=ALU.add), reads=["tmp3"], writes=["pk_"])
                tt("dve", tmp3[:], Af, ecb[:].unsqueeze(1).to_broadcast([128, 32, 32]), ALU.mult, ["A1", "A2", "ecb"], ["tmp3"])
                em.op("dve", lambda k=k: V.tensor_reduce(out=dk[:, k, :], in_=tmp3[:], axis=AX.X, op=ALU.add), reads=["tmp3"], writes=["dk"])
            tt("dve", dk[:], dk[:], pk_[:], ALU.add, ["dk", "pk_"], ["dk"])
            ts("dve", ok[:], pk_[:], float(CAP), None, ALU.is_lt, None, ["pk_"], ["ok"])
            tt("dve", dgf[:], dk[:], ok[:], ALU.mult, ["dk", "ok"], ["dgf"])
            tt("dve", dsf[:], dk[:], trb[:].unsqueeze(1).to_broadcast([128, 2, 32]), ALU.subtract, ["dk", "trb"], ["dsf"])
            tt("dve", dsf[:], dsf[:], ok[:], ALU.mult, ["dsf", "ok"], ["dsf"])
            tt("dve", dsf[:], dsf[:], trb[:].unsqueeze(1).to_broadcast([128, 2, 32]), ALU.add, ["dsf", "trb"], ["dsf"])
            tt("dve", w1[:], w1[:], ok[:, 0, :], ALU.mult, ["w1", "ok"], ["w1"])
            tt("dve", w2[:], w2[:], ok[:, 1, :], ALU.mult, ["w2", "ok"], ["w2"])
            cp("dve", ds_i[:], dsf[:], ["dsf"], ["ds_i"])
            cp("dve", dg_i[:], dgf[:], ["dgf"], ["dg_i"])
            if debug:
                dma("sp", dbgL[:, 32 * 36:32 * 36 + 64], dsf[:].rearrange("p a b -> p (a b)"), ["dsf"], ["dbgL2"])
                dma("sp", dbgL[:, 32 * 36 + 64:32 * 36 + 96], w1[:], ["w1"], ["dbgL3"])
                dma("sp", dbgL[:, 32 * 36 + 96:32 * 36 + 128], w2[:], ["w2"], ["dbgL4"])
            for j2 in range(32):
                for k in range(2):
                    em.dma("pool", lambda j2=j2, k=k: G.indirect_dma_start(
                        out=xg, out_offset=bass.IndirectOffsetOnAxis(ap=ds_i[:, k, j2:j2 + 1], axis=0),
                        in_=h2tok[:, j2, :], in_offset=None), reads=["ds_i", ("h2tok", j2)],
                        writes=[("xgs", j2, k)])
            em.barrier()
        ph_tok.close()
        if stop_after == "E":
            return nc

        with ExitStack() as ph:
            NB = 2
            Wg = [sbuf(ph, "Wg%d" % i, [128, 8, 512], BF16) for i in range(NB)]
            Wu = [sbuf(ph, "Wu%d" % i, [128, 8, 512], BF16) for i in range(NB)]
            Wd = [sbuf(ph, "Wd%d" % i, [128, 4, D], BF16) for i in range(NB)]
            xgt = [sbuf(ph, "xgt%d" % i, [128, 3, D], BF16) for i in range(2)]
            xgT = [sbuf(ph, "xgT%d" % i, [128, 8, CAP], BF16) for i in range(2)]
            sg = [sbuf(ph, "sg%d" % i, [128, CAP], F32) for i in range(2)]
            hT = [sbuf(ph, "hT%d" % i, [128, 4, CAP], BF16) for i in range(2)]
            yt = [sbuf(ph, "yt%d" % i, [128, D], F32) for i in range(4)]
            yt_rr = 0
            NST = (CAP + 127) // 128
            NSTG = 5
            stg = [sbuf(ph, "stg%d" % i, [128, 4096], F32) for i in range(NSTG)]
            stg_rr = [0]

            def load_expert(e):
                b = e % NB
                b2 = e % 2
                items = [
                    (T["w_gate"][e].rearrange("(p k) n -> p k n", k=8), Wg[b], ("Wg", b), 8, 512, "act"),
                    (T["w_up"][e].rearrange("(p k) n -> p k n", k=8), Wu[b], ("Wu", b), 8, 512, "dve"),
                    (T["w_down"][e].rearrange("(k p) n -> p k n", p=128), Wd[b], ("Wd", b), 4, 1024, "pool"),
                ]
                for src, dst, key, nk, nn, ce in items:
                    i = stg_rr[0] % NSTG
                    stg_rr[0] += 1
                    sv = stg[i][:].rearrange("p (k n) -> p k n", k=nk)
                    dma("sp", sv, src, [], [("stg", i)])
                    hk_ = nk // 2
                    for h_ in range(2):
                        wk = [(key[0], key[1], 0), (key[0], key[1], 4 if key[0] != "Wd" else 2)] if h_ == 0 else []
                        ce_ = ce if ce != "pool" else ("dve" if h_ == 0 else "act")
                        cp(ce_, dst[:, h_ * hk_:(h_ + 1) * hk_, :], sv[:, h_ * hk_:(h_ + 1) * hk_, :], [("stg", i)],
                           [(key[0], key[1], "h%d" % h_)] + wk)
                for s_ in range((CAP + 127) // 128):
                    w_ = min(128, CAP - s_ * 128)
                    dma("pool", xgt[b2][0:w_, s_, :], xg[e * CAP + s_ * 128:e * CAP + s_ * 128 + w_, :], [], [("xgt", b2, s_)])

            def f_transposes(e):
                b2 = e % 2
                for s in range(3):
                    if s * 128 >= CAP:
                        break
                    w_ = min(128, CAP - s * 128)
                    ps, pk = ps_next()
                    psb = ps[:].bitcast(BF16)
                    for kc in range(8):
                        tr(psb[:, kc * 128:kc * 128 + w_], xgt[b2][0:w_, s, kc::8], [("xgt", b2, s)], pk, kdim=w_)
                    cp("act" if s % 2 else "dve", xgT[b2][:, :, s * 128:s * 128 + w_],
                       psb.rearrange("p (a b) -> p a b", b=128)[:, :, 0:w_], [pk], [("xgT", b2, s)])

            load_expert(0)
            f_transposes(0)
            for e in range(32):
                b = e % NB
                b2 = e % 2
                if e + 1 < 32:
                    load_expert(e + 1)
                xk = [("xgT", b2, s) for s in range(NST)]
                for mc in range(4):
                    psG, kG = ps_next()
                    psU, kU = ps_next()
                    for kc in range(8):
                        mm(psG[:, 0:CAP], Wg[b][:, kc, mc * 128:(mc + 1) * 128], xgT[b2][:, kc, :], kc == 0, kc == 7, xk + [("Wg", b, "h0"), ("Wg", b, "h1"), ("Wg", b, 0), ("Wg", b, 4)], kG)
                    for kc in range(8):
                        mm(psU[:, 0:CAP], Wu[b][:, kc, mc * 128:(mc + 1) * 128], xgT[b2][:, kc, :], kc == 0, kc == 7, xk + [("Wu", b, "h0"), ("Wu", b, "h1"), ("Wu", b, 0), ("Wu", b, 4)], kU)
                    sb_ = mc % 2
                    act(sg[sb_][:], psG[:, 0:CAP], AF.Silu, [kG], [("sg", sb_)])
                    tt("dve", hT[b2][:, mc, :], sg[sb_][:], psU[:, 0:CAP], ALU.mult, [("sg", sb_), kU], [("hT", b2, mc)])
                if e + 1 < 32:
                    f_transposes(e + 1)
                hk = [("hT", b2, mc) for mc in range(4)]
                for s in range(NST):
                    w_ = min(128, CAP - s * 128)
                    yb = yt_rr % 4
                    yt_rr += 1
                    for half in range(2):
                        ps, pk = ps_next()
                        for mc in range(4):
                            mm(ps[0:w_, :], hT[b2][:, mc, s * 128:s * 128 + w_], Wd[b][:, mc, half * 512:(half + 1) * 512], mc == 0, mc == 3,
                               hk + [("Wd", b, "h0"), ("Wd", b, "h1"), ("Wd", b, 0), ("Wd", b, 2)], pk)
                        cp("act" if half else "dve", yt[yb][0:w_, half * 512:(half + 1) * 512], ps[0:w_, :], [pk], [("yt", yb, half)])
                    dma("pool", Ybuf[e * CAP + s * 128:e * CAP + s * 128 + w_, :], yt[yb][0:w_, :], [("yt", yb, 0), ("yt", yb, 1)], [("Ybuf", e, s)])
            em.barrier()
        if stop_after == "F":
            return nc

        with ExitStack() as ph:
            gfb = sbuf(ph, "gfb", [128, D], F32)
            dma("sp", gfb[:], T["gfb"], [], ["gfb"])
            NG = 4
            Y1 = [sbuf(ph, "Y1_%d" % i, [128, D], F32) for i in range(NG)]
            Y2 = [sbuf(ph, "Y2_%d" % i, [128, D], F32) for i in range(NG)]
            xt = [sbuf(ph, "xtG%d" % i, [128, D], F32) for i in range(NG)]
            junk = sbuf(ph, "junkG", [128, D], BF16)
            ss = sbuf(ph, "ssG", [128, 32], F32)
            rs = sbuf(ph, "rsG", [128, 32], F32)
            em.op("dve", lambda: V.memset(ss[:], 0.0), writes=["ssG"])

            def g_load(j2):
                b = j2 % NG
                em.dma("pool", lambda: G.indirect_dma_start(out=Y1[b][:], out_offset=None, in_=Ybuf,
                                                            in_offset=bass.IndirectOffsetOnAxis(ap=dg_i[:, 0, j2:j2 + 1], axis=0)),
                       reads=["dg_i"], writes=[("Y1", b)])
                em.dma("pool", lambda: G.indirect_dma_start(out=Y2[b][:], out_offset=None, in_=Ybuf,
                                                            in_offset=bass.IndirectOffsetOnAxis(ap=dg_i[:, 1, j2:j2 + 1], axis=0)),
                       reads=["dg_i"], writes=[("Y2", b)])
                dma("sp", xt[b][:], x1buf[j2 * 128:(j2 + 1) * 128, :], [], [("xtG", b)])

            for j2 in range(min(3, 32)):
                g_load(j2)
            for j2 in range(32):
                b = j2 % NG
                if j2 + 3 < 32:
                    g_load(j2 + 3)
                stt("dve", xt[b][:], Y1[b][:], w1[:, j2:j2 + 1], xt[b][:], ALU.mult, ALU.add, [("Y1", b), ("xtG", b), "w1"], [("xtG", b)])
                stt("dve", xt[b][:], Y2[b][:], w2[:, j2:j2 + 1], xt[b][:], ALU.mult, ALU.add, [("Y2", b), ("xtG", b), "w2"], [("xtG", b)])
                act(junk[:], xt[b][:], AF.Square, [("xtG", b), "ssG"], ["junkG", ("ssG", j2)], accum_out=ss[:, j2:j2 + 1])
                act(rs[:, j2:j2 + 1], ss[:, j2:j2 + 1], AF.Sqrt, [("ssG", j2), "epst"], [("rsG", j2)], scale=1.0 / D, bias=epst[:])
                em.op("dve", lambda: V.reciprocal(out=rs[:, j2:j2 + 1], in_=rs[:, j2:j2 + 1]), reads=[("rsG", j2)], writes=[("rsG", j2)])
                stt("dve", Y1[b][:], xt[b][:], rs[:, j2:j2 + 1], gfb[:], ALU.mult, ALU.mult, [("xtG", b), ("rsG", j2), "gfb"], [("Y1", b)])
                final_events.append(dma("sp", out_d[j2 * 128:(j2 + 1) * 128, :], Y1[b][:], [("Y1", b)], [("out", j2)]))
            em.barrier()
    return nc


def prep_inputs(inp):
    f32 = np.float32
    g = lambda k: np.asarray(inp[k], dtype=f32)
    rep = lambda v: np.ascontiguousarray(np.tile(v.reshape(1, -1), (128, 1)))
    sh = {}
    sh["w_in"] = np.ascontiguousarray(g("w_in")[0])
    sh["g1b"] = rep(g("norm1_g")[0])
    sh["g2b"] = rep(g("norm2_g")[0])
    sh["gfb"] = rep(g("final_g"))
    cw = g("conv_w")[0]
    sh["cwp"] = np.ascontiguousarray(cw.reshape(3, 12, 128).transpose(2, 1, 0)).reshape(128, 36)
    sh["cbp"] = np.ascontiguousarray(g("conv_b")[0].reshape(12, 128).T)
    sh["fbp"] = np.ascontiguousarray(g("f_bias")[0].reshape(4, 128).T)
    sh["mgp"] = np.ascontiguousarray(g("mix_g")[0].reshape(8, 128).T)
    sh["w_out"] = np.ascontiguousarray(g("w_out")[0])
    wr = np.concatenate([g("w_group")[0], g("w_router")[0].transpose(1, 0, 2).reshape(D, 32)], axis=1)
    sh["wr"] = np.ascontiguousarray(wr)
    sh["brb"] = rep(np.concatenate([g("b_group")[0], g("b_router")[0].reshape(32)]))
    sh["w_gate"] = np.ascontiguousarray(g("w_gate")[0])
    sh["w_up"] = np.ascontiguousarray(g("w_up")[0])
    sh["w_down"] = np.ascontiguousarray(g("w_down")[0])
    fwin = g("f_w_in")[0]
    w1 = np.zeros((66, 128), f32)
    w1[0:33, 0:64] = fwin
    w1[33:66, 64:128] = fwin
    sh["fwin2"] = w1
    fm = g("f_w_mid")[0]
    wm = np.zeros((128, 2, 128), f32)
    for l in range(2):
        wm[0:64, l, 0:64] = fm[l]
        wm[64:128, l, 64:128] = fm[l]
    sh["fwmid2"] = wm.reshape(128, 256)
    fq = g("f_freq")[0].T
    sh["fq"] = np.ascontiguousarray(np.concatenate([fq, fq], 0))
    fb = np.stack([g("f_b_in")[0], g("f_b_mid")[0][0], g("f_b_mid")[0][1]], 1)
    sh["fbb"] = np.ascontiguousarray(np.concatenate([fb, fb], 0))
    fo = g("f_w_out")[0]
    sh["fwout2"] = np.ascontiguousarray(np.concatenate([fo, fo], 0))
    return sh


_CACHE = {}


def kernel(**inputs):
    if "nc" not in _CACHE:
        _CACHE["nc"] = build_nc()
        _CACHE["consts"] = make_consts()
    nc = _CACHE["nc"]
    shared = prep_inputs(inputs)
    shared.update(_CACHE["consts"])
    x = np.asarray(inputs["x"], dtype=np.float32)
    in_maps = []
    for c in range(8):
        m = dict(shared)
        m["x"] = np.ascontiguousarray(x[c])
        in_maps.append(m)
    res = run_bass_kernel_spmd(nc, in_maps, core_ids=list(range(8)))
    out = np.stack([np.asarray(res.results[c]["out"], dtype=np.float32) for c in range(8)], axis=0)
    return out
```

```python
import numpy as np
import ml_dtypes
from contextlib import ExitStack
import concourse.bass as bass
import concourse.mybir as mybir
from concourse.bass_utils import run_bass_kernel_spmd

F32 = mybir.dt.float32
BF16 = mybir.dt.bfloat16
I32 = mybir.dt.int32
AF = mybir.ActivationFunctionType
ALU = mybir.AluOpType
AX = mybir.AxisListType
bf = ml_dtypes.bfloat16

L = 4096
NF = 8192
D = 1024
CAP = 320
NS = 32 * CAP
NROWS = NS + 128
EPS = 1e-6
TWO_PI = float(2 * np.pi)


class Emit:
    def __init__(self, nc, stack, n_dma_sems=48):
        self.nc = nc
        self.eng = {"pe": nc.tensor, "act": nc.scalar, "dve": nc.vector, "pool": nc.gpsimd, "sp": nc.sync}
        self.sem = {}
        self.cnt = {}
        for k in self.eng:
            self.sem[k] = stack.enter_context(nc.semaphore("s_" + k))
            self.cnt[k] = 0
        self.dma_sems = [stack.enter_context(nc.semaphore("d%d" % i)) for i in range(n_dma_sems)]
        self.dma_cnt = [0] * n_dma_sems
        n_sw = 16
        self.dma_pool = {"hw": list(range(n_sw, n_dma_sems)), "sw": list(range(n_sw))}
        self.dma_rr = {"hw": 0, "sw": 0}
        self.seen = {k: {} for k in self.eng}
        self.lastw = {}
        self.reads = {}
        self.n_wait = 0
        self.n_ins = 0

    def _wait(self, e, ev):
        sem, val, src = ev
        sid = id(sem)
        if self.seen[e].get(sid, 0) >= val:
            return
        self.seen[e][sid] = val
        self.eng[e].wait_ge(sem, val)
        self.n_wait += 1

    def _deps(self, e, reads, writes):
        evs = []
        for r in reads:
            w = self.lastw.get(r)
            if w is not None and not (w[2] == e and e == "pe"):
                evs.append(w)
        same_ok = e in ("pe",)
        for wkey in writes:
            w = self.lastw.get(wkey)
            if w is not None and (w[2] != e or not same_ok):
                evs.append(w)
            for ev in self.reads.get(wkey, {}).values():
                if ev[2] != e or not same_ok:
                    evs.append(ev)
        return evs

    def _commit(self, ev, reads, writes):
        for r in reads:
            self.reads.setdefault(r, {})[(ev[2], id(ev[0]))] = ev
        for w in writes:
            self.lastw[w] = ev
            self.reads[w] = {}

    def op(self, e, fn, reads=(), writes=()):
        for ev in self._deps(e, reads, writes):
            self._wait(e, ev)
        ins = fn()
        self.cnt[e] += 1
        ins.then_inc(self.sem[e], 1)
        ev = (self.sem[e], self.cnt[e], e)
        self._commit(ev, reads, writes)
        self.n_ins += 1
        return ev

    def dma(self, q, fn, reads=(), writes=()):
        for ev in self._deps(q, reads, writes):
            self._wait(q, ev)
        kind = "sw" if q == "pool" else "hw"
        lst = self.dma_pool[kind]
        i = lst[self.dma_rr[kind]]
        self.dma_rr[kind] = (self.dma_rr[kind] + 1) % len(lst)
        sem = self.dma_sems[i]
        if self.dma_cnt[i] > 0:
            self._wait(q, (sem, 16 * self.dma_cnt[i], "dma"))
        ins = fn()
        self.dma_cnt[i] += 1
        ins.then_inc(sem, 16)
        ev = (sem, 16 * self.dma_cnt[i], "dma%d" % i)
        self._commit(ev, reads, writes)
        self.n_ins += 1
        return ev

    def barrier(self):
        for e in self.eng:
            for e2 in self.eng:
                if self.cnt[e2] > 0 and (e2 != e or e in ("act", "dve", "pool")):
                    self._wait(e, (self.sem[e2], self.cnt[e2], e2))
            for i, s in enumerate(self.dma_sems):
                if self.dma_cnt[i] > 0:
                    self._wait(e, (s, 16 * self.dma_cnt[i], "dma"))
        self.lastw = {}
        self.reads = {}


def _kron4(M):
    return np.kron(M, np.eye(4))


def make_consts():
    c = {}
    p = np.arange(256)[:, None].astype(np.float64)
    k1 = np.arange(128)[None, :].astype(np.float64)
    fa = np.zeros((2, 32, 2, 128, 128))
    gt = np.zeros((32, 2, 128, 128))
    for j in range(32):
        th = 2 * np.pi * (p * (k1 + 0.5) / 256.0 + j * (k1 + 0.5) / NF)
        for pt in range(2):
            sgn = 1.0 if pt == 0 else -1.0
            fa[pt, j, 0] = sgn * np.cos(th[pt * 128:(pt + 1) * 128])
            fa[pt, j, 1] = -sgn * np.sin(th[pt * 128:(pt + 1) * 128])
        gt[j, 0] = (2.0 / NF) * np.cos(th[:128]).T
        gt[j, 1] = -(2.0 / NF) * np.sin(th[:128]).T
    c["fa"] = np.ascontiguousarray(fa.transpose(3, 0, 1, 2, 4)).reshape(128, 2 * 32 * 2 * 128).astype(bf)
    c["gt"] = np.ascontiguousarray(gt.transpose(2, 0, 1, 3)).reshape(128, 32 * 2 * 128).astype(bf)
    jj = np.arange(32)[:, None].astype(np.float64)
    kk = np.arange(32)[None, :].astype(np.float64)
    ph = 2 * np.pi * jj * kk / 32.0
    Tc = _kron4(np.cos(ph))
    Ts = _kron4(np.sin(ph))
    c["tct"] = np.stack([Tc, Ts, -Ts], 1).reshape(128, 3 * 128).astype(bf)
    c["tcf"] = np.stack([Tc / 512.0, Ts / 512.0], 1).reshape(128, 2 * 128).astype(bf)
    pa = np.arange(128)[:, None].astype(np.float64)
    ka = np.arange(128)[None, :].astype(np.float64)
    et = np.zeros((32, 3, 128, 128))
    for j in range(32):
        th = 2 * np.pi * (pa * ka / 128.0 + j * ka / 4096.0)
        et[j, 0] = np.cos(th)
        et[j, 1] = np.sin(th)
        et[j, 2] = -np.sin(th)
    c["et"] = np.ascontiguousarray(et.transpose(2, 0, 1, 3)).reshape(128, 32 * 3 * 128).astype(bf)
    cc = np.arange(64)[:, None].astype(np.float64)
    c2 = np.arange(64)[None, :].astype(np.float64)
    C64 = np.cos(2 * np.pi * cc * c2 / 64.0)
    S64 = np.sin(2 * np.pi * cc * c2 / 64.0)
    c["bdt"] = np.stack([np.kron(np.eye(2), C64), np.kron(np.eye(2), -S64)], 1).reshape(128, 256).astype(bf)
    c["ident"] = np.eye(128).astype(bf)
    c["blk"] = (np.kron(np.eye(2), np.ones((64, 64))) / 64.0).astype(bf)
    c["tri"] = np.triu(np.ones((128, 128)), 1).astype(bf)
    c["ones"] = np.ones((128, 128)).astype(bf)
    c["onesf"] = np.ones((128, 128), np.float32)
    c["ecb"] = np.tile((np.arange(32) * CAP).astype(np.float32)[None, :], (128, 1))
    c["trb"] = np.tile((NS + np.arange(128)).astype(np.float32)[:, None], (1, 32))
    n = np.arange(NF)
    m = np.where(n < L, n, NF - n).astype(np.float64)
    m[L] = 0
    t = (m / (L - 1)).astype(np.float32)
    w = (2.0 * np.pi / L) * m
    f = np.linspace(1e-4, 15, 16)[None, :]
    emb = np.concatenate([t[:, None], np.cos(f * w[:, None]), -np.sin(f * w[:, None])], -1).astype(np.float32)
    c["emb2"] = np.concatenate([emb[:L].T, emb[L:].T], 0).astype(np.float32)
    tn = -t.astype(np.float32)
    tn[L] = -1e4
    c["tneg"] = np.ascontiguousarray(tn.reshape(2, 128, 32).transpose(1, 0, 2)).reshape(128, 64)
    max_decay = np.log(1e-2) / 0.3
    min_decay = np.log(1e-2) / 1.5
    deltas = np.abs(np.linspace(min_decay, max_decay, 512)).astype(np.float32)
    c["deltab"] = np.tile(deltas[None, :], (128, 1))
    return c


CONST_SPECS = [
    ("fa", [128, 16384], BF16), ("gt", [128, 8192], BF16), ("tct", [128, 384], BF16), ("tcf", [128, 256], BF16),
    ("et", [128, 12288], BF16), ("bdt", [128, 256], BF16), ("ident", [128, 128], BF16), ("blk", [128, 128], BF16),
    ("tri", [128, 128], BF16), ("ones", [128, 128], BF16), ("onesf", [128, 128], F32), ("ecb", [128, 32], F32),
    ("trb", [128, 32], F32), ("emb2", [66, 4096], F32), ("tneg", [128, 64], F32), ("deltab", [128, 512], F32),
]
IN_SPECS = [
    ("x", [L, D], F32), ("w_in", [D, 2048], F32), ("g1b", [128, D], F32), ("g2b", [128, D], F32), ("gfb", [128, D], F32),
    ("cwp", [128, 36], F32), ("cbp", [128, 12], F32), ("fbp", [128, 4], F32), ("mgp", [128, 8], F32),
    ("w_out", [D, D], F32), ("wr", [D, 36], F32), ("brb", [128, 36], F32),
    ("w_gate", [32, D, 512], F32), ("w_up", [32, D, 512], F32), ("w_down", [32, 512, D], F32),
    ("fwin2", [66, 128], F32), ("fwmid2", [128, 256], F32), ("fq", [128, 3], F32), ("fbb", [128, 3], F32),
    ("fwout2", [128, 1024], F32),
]


def build_nc(stop_after=None, debug=False):
    nc = bass.Bass("TRN2", target_bir_lowering=False)
    T = {}
    for name, shape, dt in IN_SPECS + CONST_SPECS:
        T[name] = nc.dram_tensor(name, shape, dt, kind="ExternalInput").ap()
    out_d = nc.dram_tensor("out", [L, D], F32, kind="ExternalOutput").ap()
    skind = "ExternalOutput" if debug else "Internal"
    Pbuf = nc.dram_tensor("Pbuf", [1536, L], BF16, kind=skind).ap()
    wbuf = [nc.dram_tensor("wbuf%d" % i, [L, 512], BF16, kind=skind).ap() for i in range(2)]
    Hbuf = nc.dram_tensor("Hbuf", [4, 128, 8192], BF16, kind=skind).ap()
    yTbuf = nc.dram_tensor("yTbuf", [D, L], BF16, kind=skind).ap()
    x1buf = nc.dram_tensor("x1buf", [L, D], F32, kind=skind).ap()
    xg = nc.dram_tensor("xg", [NROWS, D], BF16, kind=skind).ap()
    Ybuf = nc.dram_tensor("Ybuf", [NROWS, D], F32, kind=skind).ap()
    dbgL = nc.dram_tensor("dbgL", [128, 32 * 40], F32, kind=skind).ap()

    with ExitStack() as st:
        em = Emit(nc, st)
        V, A, G, PE = nc.vector, nc.scalar, nc.gpsimd, nc.tensor
        ENG = {"dve": V, "act": A, "pool": G}

        def sbuf(stack, name, shape, dt):
            return stack.enter_context(nc.sbuf_tensor("sb_" + name, shape, dt))

        PS = [st.enter_context(nc.psum_tensor("ps%d" % i, [128, 512], F32)) for i in range(8)]
        ps_rr = [0]

        def ps_next():
            i = ps_rr[0]
            ps_rr[0] = (i + 1) % 8
            return PS[i], "ps%d" % i

        def mm(out, lhsT, rhs, start, stop, reads, pk):
            return em.op("pe", lambda: PE.matmul(out, lhsT=lhsT, rhs=rhs, start=start, stop=stop), reads=reads, writes=[pk])

        def tr(out, in_, reads, pk, kdim=128):
            idn = ident[:] if kdim == 128 else ident[0:kdim, 0:kdim]
            return em.op("pe", lambda: PE.transpose(out, in_, idn), reads=list(reads) + ["ident"], writes=[pk])

        def cp(e, out, in_, reads, writes):
            if e == "act":
                return em.op("act", lambda: A.copy(out=out, in_=in_), reads=reads, writes=writes)
            return em.op(e, lambda: ENG[e].tensor_copy(out=out, in_=in_), reads=reads, writes=writes)

        def tt(e, out, in0, in1, op, reads, writes):
            return em.op(e, lambda: ENG[e].tensor_tensor(out=out, in0=in0, in1=in1, op=op), reads=reads, writes=writes)

        def ts(e, out, in0, s1, s2, op0, op1, reads, writes):
            if op1 is None:
                return em.op(e, lambda: ENG[e].tensor_scalar(out=out, in0=in0, scalar1=s1, scalar2=None, op0=op0), reads=reads, writes=writes)
            return em.op(e, lambda: ENG[e].tensor_scalar(out=out, in0=in0, scalar1=s1, scalar2=s2, op0=op0, op1=op1), reads=reads, writes=writes)

        def stt(e, out, in0, scalar, in1, op0, op1, reads, writes):
            return em.op(e, lambda: ENG[e].scalar_tensor_tensor(out=out, in0=in0, scalar=scalar, in1=in1, op0=op0, op1=op1), reads=reads, writes=writes)

        def act(out, in_, func, reads, writes, **kw):
            return em.op("act", lambda: A.activation(out=out, in_=in_, func=func, **kw), reads=reads, writes=writes)

        def dma(q, out, in_, reads, writes):
            e = {"sp": nc.sync, "act": A, "pool": G}[q]
            return em.dma(q, lambda: e.dma_start(out=out, in_=in_), reads=reads, writes=writes)

        final_events = []

        ident = sbuf(st, "ident", [128, 128], BF16)
        blk = sbuf(st, "blk", [128, 128], BF16)
        tct = sbuf(st, "tct", [128, 3, 128], BF16)
        epst = sbuf(st, "epst", [128, 1], F32)
        rinvP = sbuf(st, "rinvP", [128, 4], F32)
        dma("sp", ident[:], T["ident"], [], ["ident"])
        dma("sp", blk[:], T["blk"], [], ["blk"])
        dma("sp", tct[:].rearrange("p a b -> p (a b)"), T["tct"], [], ["tct"])
        em.op("dve", lambda: V.memset(epst[:], EPS), writes=["epst"])

        ztp = sbuf(st, "ztp", [128, 2, D], BF16)
        em.op("pool", lambda: G.memset(ztp[:], 0.0), writes=["ztp"])
        zf_chunks = [(r0, min(256, NROWS - r0)) for r0 in range(0, NROWS, 256)]

        def zero_fill_some(n):
            for _ in range(n):
                if zf_chunks:
                    r0, nr = zf_chunks.pop(0)
                    dma("sp", xg[r0:r0 + nr, :].rearrange("(s p) f -> p s f", p=128), ztp[:, 0:nr // 128, :], ["ztp"], [("xg", r0)])

        def fft_fwd(src, npt, fa_t, bufA, bufB, srck, sink):
            for j0 in range(0, 32, 4):
                for cs in range(2):
                    ps, pk = ps_next()
                    for jj in range(4):
                        j = j0 + jj
                        for pt in range(npt):
                            mm(ps[:, jj * 128:(jj + 1) * 128], fa_t[:, pt, j, cs, :], src[:, pt * 32 + j, :],
                               pt == 0, pt == npt - 1, (srck if isinstance(srck, list) else [srck]) + ["fa"], pk)
                    cp("act" if cs == 0 else "dve",
                       bufA[:].rearrange("p a g (j c) -> p a g j c", c=4)[:, cs, :, j0:j0 + 4, :].rearrange("p g j c -> p j g c"),
                       ps[:].rearrange("p (j g c) -> p j g c", j=4, g=32), [pk], [("bufA", cs, j0)])
            for cs in range(2):
                for g0 in range(0, 32, 8):
                    ps, pk = ps_next()
                    psb = ps[:].bitcast(BF16)
                    for gg in range(8):
                        g = g0 + gg
                        tr(psb[:, gg * 128:(gg + 1) * 128], bufA[:, cs, g, :],
                           [("bufA", cs, j0) for j0 in range(0, 32, 4)], pk)
                    cp("act" if cs == 0 else "dve", bufB[:, cs, g0:g0 + 8, :], psb.rearrange("p (a b) -> p a b", b=128),
                       [pk], [("bufB", cs, g0)])
            for ch in range(8):
                psR, kR = ps_next()
                psI, kI = ps_next()
                bBf = bufB[:].rearrange("p a b c -> p a (b c)")
                br = bBf[:, 0, ch * 512:(ch + 1) * 512]
                bi = bBf[:, 1, ch * 512:(ch + 1) * 512]
                rk = [("bufB", 0, (4 * ch) // 8 * 8), ("bufB", 1, (4 * ch) // 8 * 8), "tct"]
                mm(psR[:], tct[:, 0, :], br, True, False, rk, kR)
                mm(psR[:], tct[:, 1, :], bi, False, True, rk, kR)
                mm(psI[:], tct[:, 0, :], bi, True, False, rk, kI)
                mm(psI[:], tct[:, 2, :], br, False, True, rk, kI)
                sink(ch, psR, kR, psI, kI)

        with ExitStack() as ph:
            fa_t = sbuf(ph, "fa_t", [128, 2, 32, 2, 128], BF16)
            w1t = sbuf(ph, "w1t", [66, 128], F32)
            wmt = sbuf(ph, "wmt", [128, 2, 128], F32)
            fq = sbuf(ph, "fq", [128, 3], F32)
            fbb = sbuf(ph, "fbb", [128, 3], F32)
            fq2 = sbuf(ph, "fq2", [128, 3], F32)
            fqb2 = sbuf(ph, "fqb2", [128, 3], F32)
            fwo = sbuf(ph, "fwo", [128, 1024], BF16)
            onesf = sbuf(ph, "onesf", [128, 128], F32)
            tneg = sbuf(ph, "tneg", [128, 64], F32)
            deltab = sbuf(ph, "deltab", [128, 512], F32)
            hid = sbuf(ph, "hid", [128, L], BF16)
            dma("sp", fa_t[:].rearrange("p a b c d -> p (a b c d)"), T["fa"], [], ["fa"])
            dma("sp", w1t[:], T["fwin2"], [], ["w1t"])
            dma("sp", wmt[:].rearrange("p a b -> p (a b)"), T["fwmid2"], [], ["wmt"])
            dma("sp", fq[:], T["fq"], [], ["fq"])
            dma("sp", fbb[:], T["fbb"], [], ["fbb"])
            dma("pool", fwo[:], T["fwout2"], [], ["fwo"])
            dma("sp", onesf[:], T["onesf"], [], ["onesf"])
            dma("sp", tneg[:], T["tneg"], [], ["tneg"])
            dma("sp", deltab[:], T["deltab"], [], ["deltab"])
            ts("dve", fq2[:], fq[:], float(1.0 / 3.0), None, ALU.mult, None, ["fq"], ["fq2"])
            tt("dve", fqb2[:], fq2[:], fbb[:], ALU.mult, ["fq2", "fbb"], ["fqb2"])
            with ExitStack() as ph2:
                emb = sbuf(ph2, "emb", [66, L], F32)
                hA = sbuf(ph2, "hA", [128, L], F32)
                hB = sbuf(ph2, "hB", [128, L], F32)
                for ch in range(8):
                    dma("sp", emb[:, ch * 512:(ch + 1) * 512], T["emb2"][:, ch * 512:(ch + 1) * 512], [], [("emb", ch)])
                ub = [sbuf(ph2, "ub%d" % i, [128, 512], F32) for i in range(8)]
                rb = [sbuf(ph2, "rb%d" % i, [128, 512], F32) for i in range(8)]
                srcs = [(emb, "emb", w1t[:]), (hA, "hA", wmt[:, 0, :]), (hB, "hB", wmt[:, 1, :])]
                dsts = [(hA, "hA"), (hB, "hB"), (hid, "hid")]
                for l in range(3):
                    s_t, s_k, lhsT = srcs[l]
                    d_t, d_k = dsts[l]
                    pss = {}
                    for ch in range(8):
                        ps, pk = ps_next()
                        pss[ch] = (ps, pk)
                        mm(ps[:], lhsT, s_t[:, ch * 512:(ch + 1) * 512], True, True, [(s_k, ch), "w1t", "wmt"], pk)
                    for ch in range(8):
                        ps, pk = pss[ch]
                        act(ub[ch][:], ps[:], AF.Sin, [pk, "fq2", "fqb2"], [("ub", ch)], scale=fq2[:, l:l + 1], bias=fqb2[:, l:l + 1])
                    for ch in range(8):
                        tt("dve", rb[ch][:], ub[ch][:], ub[ch][:], ALU.mult, [("ub", ch)], [("rb", ch)])
                    for ch in range(8):
                        ts("dve", rb[ch][:], rb[ch][:], -4.0, 3.0, ALU.mult, ALU.add, [("rb", ch)], [("rb", ch)])
                    for ch in range(8):
                        tt("dve", d_t[:, ch * 512:(ch + 1) * 512], rb[ch][:], ub[ch][:], ALU.mult, [("rb", ch), ("ub", ch)], [(d_k, ch)])
                em.barrier()
            with ExitStack() as ph2:
                decs = [sbuf(ph2, "dec%d" % i, [128, 64, 128], BF16) for i in range(2)]
                kbs = [sbuf(ph2, "kb%d" % i, [128, 64, 128], BF16) for i in range(2)]
                acc = sbuf(ph2, "acc", [128, 128], F32)
                bufA = sbuf(ph2, "bufA", [128, 2, 32, 128], BF16)
                bufB = sbuf(ph2, "bufB", [128, 2, 32, 128], BF16)
                Hsb = sbuf(ph2, "Hsb", [128, 2, L], BF16)

                def dec_gen(hc):
                    for col in range(64):
                        act(decs[hc % 2][:, col, :], deltab[:, hc * 128:(hc + 1) * 128], AF.Exp, ["deltab", "tneg"], [("dec", hc % 2, col // 4)],
                            scale=tneg[:, col:col + 1])

                def out_layer(hc):
                    dec = decs[hc % 2]
                    kb = kbs[hc % 2]
                    for c0 in range(0, 64, 4):
                        ps, pk = ps_next()
                        for cc in range(4):
                            col = c0 + cc
                            pt, j = col // 32, col % 32
                            mm(ps[:, cc * 128:(cc + 1) * 128], hid[64 * pt:64 * pt + 64, j::32],
                               fwo[64 * pt:64 * pt + 64, pt * 512 + hc * 128:pt * 512 + (hc + 1) * 128], True, True,
                               [("hid", ch) for ch in range(8)] + ["fwo"], pk)
                        tt("dve", kb[:, c0:c0 + 4, :], ps[:].rearrange("p (a b) -> p a b", b=128), dec[:, c0:c0 + 4, :], ALU.mult,
                           [pk, ("dec", hc % 2, c0 // 4)], [("kb", hc % 2, c0 // 4)])

                def sinkH(ch, psR, kR, psI, kI):
                    cp("act", Hsb[:, 0, ch * 512:(ch + 1) * 512], psR[:], [kR], [("Hsb", ch)])
                    cp("dve", Hsb[:, 1, ch * 512:(ch + 1) * 512], psI[:], [kI], [("Hsb", ch)])

                dec_gen(0)
                out_layer(0)
                for hc in range(4):
                    kb = kbs[hc % 2]
                    kbk = [("kb", hc % 2, i) for i in range(16)]
                    if hc + 1 < 4:
                        dec_gen(hc + 1)
                        out_layer(hc + 1)
                    fft_fwd(kb, 2, fa_t, bufA, bufB, kbk, sinkH)
                    dma("sp", Hbuf[hc], Hsb[:].rearrange("p a b -> p (a b)"), [("Hsb", ch) for ch in range(8)], [("Hbuf", hc)])
                    em.op("dve", lambda: V.tensor_reduce(out=acc[:], in_=kb[:].rearrange("p n c -> p c n"), axis=AX.X, op=ALU.add,
                                                         apply_absolute_value=True), reads=kbk, writes=["acc"])
                    ps, pk = ps_next()
                    mm(ps[:, 0:1], acc[:], onesf[:, 0:1], True, True, ["onesf", "acc"], pk)
                    em.op("dve", lambda: V.reciprocal(out=rinvP[:, hc:hc + 1], in_=ps[:, 0:1]), reads=[pk], writes=[("rinvP", hc)])
                em.barrier()
        if stop_after == "F0":
            em.barrier()
            return nc

        with ExitStack() as ph:
            Wb = sbuf(ph, "Wb", [128, 8, 2048], BF16)
            Wf = sbuf(ph, "Wf", [128, 2, 8, 512], BF16)
            g1b = sbuf(ph, "g1b", [128, D], F32)
            for kc in range(8):
                for h2_ in range(2):
                    dma("pool", Wb[:, kc, h2_ * 1024:(h2_ + 1) * 1024], T["w_in"][kc * 128:(kc + 1) * 128, h2_ * 1024:(h2_ + 1) * 1024],
                        [], [("Wb", kc, h2_)])
            dma("sp", g1b[:], T["g1b"], [], ["g1b"])
            if True:
                bdt = sbuf(ph, "bdt", [128, 2, 128], BF16)
                WfT = sbuf(ph, "WfT", [128, 4, D], BF16)
                dma("sp", bdt[:].rearrange("p a b -> p (a b)"), T["bdt"], [], ["bdt"])
                for n4 in range(4):
                    ps, pk = ps_next()
                    psb = ps[:].bitcast(BF16)
                    for kc in range(8):
                        tr(psb[:, kc * 128:(kc + 1) * 128], Wb[:, kc, 1536 + n4 * 128:1536 + (n4 + 1) * 128], [("Wb", kc, 0), ("Wb", kc, 1)], pk)
                    cp("act", WfT[:, n4, :], psb, [pk], [("WfT", n4)])
                for part in range(2):
                    for kc in range(8):
                        ps, pk = ps_next()
                        for n4 in range(4):
                            mm(ps[:, n4 * 128:(n4 + 1) * 128], WfT[:, n4, kc * 128:(kc + 1) * 128], bdt[:, part, :], True, True,
                               [("WfT", n4), "bdt"], pk)
                        cp("dve", Wf[:, part, kc, :], ps[:], [pk], [("Wf", part, kc)])
            xt = [sbuf(ph, "xt%d" % i, [128, D], F32) for i in range(4)]
            junk = sbuf(ph, "junk", [128, D], BF16)
            ss = sbuf(ph, "ss", [128, 32], F32)
            rs = sbuf(ph, "rs", [128, 32], F32)
            hb = [sbuf(ph, "hb%d" % i, [128, D], BF16) for i in range(2)]
            hTc = [sbuf(ph, "hTc%d" % i, [128, 8, 512], BF16) for i in range(2)]
            Pst = [sbuf(ph, "Pst%d" % i, [128, 12, 512], BF16) for i in range(2)]
            wst = [sbuf(ph, "wst%d" % i, [128, 2, 512], BF16) for i in range(2)]
            em.op("dve", lambda: V.memset(ss[:], 0.0), writes=["ss"])
            ev_cnt = [0]

            def a_load(j2):
                dma("sp", xt[j2 % 4][:], T["x"][j2 * 128:(j2 + 1) * 128, :], [], [("xt", j2 % 4)])
                zero_fill_some(2)

            def a_norm(tc, i):
                hb_ = tc % 2
                j2 = 4 * tc + i
                b = j2 % 2
                if j2 + 3 < 32:
                    a_load(j2 + 3)
                xb = j2 % 4
                act(junk[:], xt[xb][:], AF.Square, [("xt", xb), "ss"], ["junk", ("ss", j2)], accum_out=ss[:, j2:j2 + 1])
                act(rs[:, j2:j2 + 1], ss[:, j2:j2 + 1], AF.Sqrt, [("ss", j2), "epst"], [("rs", j2)], scale=1.0 / D, bias=epst[:])
                em.op("dve", lambda: V.reciprocal(out=rs[:, j2:j2 + 1], in_=rs[:, j2:j2 + 1]), reads=[("rs", j2)], writes=[("rs", j2)])
                stt("dve", hb[b][:], xt[xb][:], rs[:, j2:j2 + 1], g1b[:], ALU.mult, ALU.mult, [("xt", xb), ("rs", j2), "g1b"], [("hb", b)])
                ps, pk = ps_next()
                psb = ps[:].bitcast(BF16)
                for kc in range(8):
                    tr(psb[:, kc * 128:(kc + 1) * 128], hb[b][:, kc * 128:(kc + 1) * 128], [("hb", b)], pk)
                cp("act", hTc[hb_][:, :, i * 128:(i + 1) * 128], psb.rearrange("p (a b) -> p a b", b=128), [pk], [("hTc", hb_, i)])

            def a_mm(tc, part):
                hb_ = tc % 2
                hk = [("hTc", hb_, i) for i in range(4)]
                for cch in range(3 * part, 3 * part + 3):
                    ps, pk = ps_next()
                    for kc in range(8):
                        mm(ps[:], Wb[:, kc, cch * 128:(cch + 1) * 128], hTc[hb_][:, kc, :], kc == 0, kc == 7, hk + [("Wb", kc, 0), ("Wb", kc, 1)], pk)
                    ev_cnt[0] += 1
                    cp("act" if ev_cnt[0] % 2 else "dve", Pst[hb_][:, cch, :], ps[:], [pk], [("Pst", hb_, cch)])
                if part == 3:
                    dma("pool", Pbuf[:, tc * 512:(tc + 1) * 512].rearrange("(c p) t -> p c t", p=128), Pst[hb_][:],
                        [("Pst", hb_, c_) for c_ in range(12)], [("Pbuf", tc)])
                i = part
                j2 = 4 * tc + i
                wb_ = j2 % 2
                for fpart in range(2):
                    ps, pk = ps_next()
                    for kc in range(8):
                        mm(ps[:], hTc[hb_][:, kc, i * 128:(i + 1) * 128], Wf[:, fpart, kc, :], kc == 0, kc == 7,
                           [("hTc", hb_, i), ("Wf", fpart, kc)], pk)
                    ev_cnt[0] += 1
                    cp("act" if ev_cnt[0] % 2 else "dve", wst[wb_][:, fpart, :], ps[:], [pk], [("wst", wb_, fpart)])
                    dma("pool", wbuf[fpart][j2 * 128:(j2 + 1) * 128, :], wst[wb_][:, fpart, :], [("wst", wb_, fpart)], [("wbuf", fpart, j2)])

            for j2_ in range(3):
                a_load(j2_)
            for i in range(4):
                a_norm(0, i)
            for tc in range(8):
                for part in range(4):
                    if tc + 1 < 8:
                        a_norm(tc + 1, part)
                    a_mm(tc, part)
            em.barrier()
        if stop_after == "A":
            return nc

        def head_norm_wave(rs_, mg_ap, row0, rsqs, rstdb):
            for q in range(4):
                act(rsqs[q][:], rs_[q][0], AF.Square, [rs_[q][1]], [("rsq", q)])
            for q in range(4):
                for h_ in range(2):
                    ps, pk = ps_next()
                    mm(ps[:], blk[:], rsqs[q][:, h_ * 512:(h_ + 1) * 512], True, True, [("rsq", q), "blk"], pk)
                    act(rstdb[q][:, h_ * 512:(h_ + 1) * 512], ps[:], AF.Ln, [pk, "epst"], [("rstd_t", q, h_)], bias=epst[:], scale=1.0)
            for q in range(4):
                act(rstdb[q][:], rstdb[q][:], AF.Exp, [("rstd_t", q, 0), ("rstd_t", q, 1)], [("rstd_t", q, 0), ("rstd_t", q, 1)], scale=-0.5)
            for q in range(4):
                stt("dve", rsqs[q][:], rs_[q][0], mg_ap, rstdb[q][:], ALU.mult, ALU.mult,
                    [rs_[q][1], ("rstd_t", q, 0), ("rstd_t", q, 1), "mgp"], [("rsq", q)])
                dma("sp", yTbuf[row0:row0 + 128, q * 1024:(q + 1) * 1024], rsqs[q][:], [("rsq", q)], [("yTbuf", row0, q)])

        with ExitStack() as ph:
            fa_t = sbuf(ph, "fa_tB", [128, 32, 2, 128], BF16)
            gt_t = sbuf(ph, "gt_t", [128, 32, 2, 128], BF16)
            cwp = sbuf(ph, "cwp", [128, 12, 3], F32)
            cbp = sbuf(ph, "cbp", [128, 12], F32)
            fbp = sbuf(ph, "fbp", [128, 4], F32)
            mgp = sbuf(ph, "mgp", [128, 8], F32)
            dma("sp", fa_t[:].rearrange("p b c d -> p (b c d)"), T["fa"][:, 0:8192], [], ["fa"])
            dma("sp", gt_t[:].rearrange("p b c d -> p (b c d)"), T["gt"], [], ["gt"])
            dma("sp", cwp[:].rearrange("p a b -> p (a b)"), T["cwp"], [], ["cwp"])
            dma("sp", cbp[:], T["cbp"], [], ["cbp"])
            dma("sp", fbp[:], T["fbp"], [], ["fbp"])
            dma("sp", mgp[:], T["mgp"], [], ["mgp"])
            Pt = [sbuf(ph, "Pt%d" % s, [128, L + 2], BF16) for s in range(3)]
            Hsb = sbuf(ph, "HsbB", [128, 2, L], BF16)
            ux0s = [sbuf(ph, "ux0_%d" % i, [128, L], BF16) for i in range(2)]
            zTs = [sbuf(ph, "zT_%d" % i, [128, L], BF16) for i in range(2)]
            tA = sbuf(ph, "tA", [128, 1024], F32)
            tB = sbuf(ph, "tB", [128, 1024], F32)
            tC = sbuf(ph, "tC", [128, 1024], F32)
            zP1 = sbuf(ph, "zP1", [128, 32, 128], BF16)
            bufA = sbuf(ph, "bufAB", [128, 2, 32, 128], BF16)
            bufB = sbuf(ph, "bufBB", [128, 2, 32, 128], BF16)
            yc = sbuf(ph, "yc", [128, L], BF16)
            rsqs = [sbuf(ph, "rsq%d" % i, [128, 1024], BF16) for i in range(4)]
            rstdb = [sbuf(ph, "rstdb%d" % i, [128, 1024], BF16) for i in range(4)]
            tD = sbuf(ph, "tD", [128, 1024], F32)
            mts = [[sbuf(ph, "mt%d_%d" % (i, k), [128, 512], F32) for k in range(4)] for i in range(2)]
            for s in range(3):
                em.op("dve", lambda s=s: V.memset(Pt[s][:, 0:1], 0.0), writes=[("Ppad", s)])
                em.op("dve", lambda s=s: V.memset(Pt[s][:, L + 1:L + 2], 0.0), writes=[("Ppad", s)])
            fa5 = fa_t[:].rearrange("p (a b) c d -> p a b c d", a=1)
            def load_P(hc):
                for s in range(3):
                    dma("sp", Pt[s][:, 1:L + 1], Pbuf[(s * 4 + hc) * 128:(s * 4 + hc + 1) * 128, :], [], [("Pt", s)])

            def load_H(hc):
                dma("sp", Hsb[:].rearrange("p a b -> p (a b)"), Hbuf[hc], [], ["HsbB"])

            def conv(hc, qs=(0, 1, 2, 3)):
                ux0 = ux0s[hc % 2]
                zT = zTs[hc % 2]
                par = hc % 2
                for q in qs:
                    t0 = q * 1024
                    tmps = [(tA, "tA"), (tB, "tB"), (tC, "tC")]
                    cc = hc
                    pk_ = [("Pt", 0), ("Ppad", 0), "cwp", "cbp"]
                    act(tA[:], Pt[0][:, 1 + t0:1 + t0 + 1024], AF.Identity, pk_, ["tA"], scale=cwp[:, cc, 1:2], bias=cbp[:, cc:cc + 1])
                    act(tD[:], Pt[0][:, t0:t0 + 1024], AF.Identity, pk_, ["tD"], scale=cwp[:, cc, 0:1])
                    tt("pool", tA[:], tA[:], tD[:], ALU.add, ["tA", "tD"], ["tA"])
                    act(tD[:], Pt[0][:, 2 + t0:2 + t0 + 1024], AF.Identity, pk_, ["tD"], scale=cwp[:, cc, 2:3])
                    tt("pool", ux0[:, t0:t0 + 1024], tA[:], tD[:], ALU.add, ["tA", "tD"], [("ux0", par, q)])
                    for s in (1, 2):
                        cc = s * 4 + hc
                        pk_ = [("Pt", s), ("Ppad", s), "cwp", "cbp"]
                        tmp, tk = tmps[s]
                        if s == 2:
                            act(tmp[:], Pt[s][:, 1 + t0:1 + t0 + 1024], AF.Identity, pk_, [tk], scale=cwp[:, cc, 1:2], bias=cbp[:, cc:cc + 1])
                        else:
                            ts("dve", tmp[:], Pt[s][:, 1 + t0:1 + t0 + 1024], cwp[:, cc, 1:2], cbp[:, cc:cc + 1], ALU.mult, ALU.add, pk_, [tk])
                        stt("dve", tmp[:], Pt[s][:, t0:t0 + 1024], cwp[:, cc, 0:1], tmp[:], ALU.mult, ALU.add, pk_ + [tk], [tk])
                        stt("dve", tmp[:], Pt[s][:, 2 + t0:2 + t0 + 1024], cwp[:, cc, 2:3], tmp[:], ALU.mult, ALU.add, pk_ + [tk], [tk])
                    tt("dve", zT[:, t0:t0 + 1024], tB[:], tC[:], ALU.mult, ["tB", "tC"], [("zT", par, q)])

            def fwd(hc):
                zT = zTs[hc % 2]
                par = hc % 2
                zk = [("zT", par, q) for q in range(4)]
                for j0 in range(0, 32, 8):
                    ps, pk = ps_next()
                    psb = ps[:].bitcast(BF16)
                    for jj in range(8):
                        tr(psb[:, jj * 128:(jj + 1) * 128], zT[:, j0 + jj::32], zk, pk)
                    cp("act", zP1[:, j0:j0 + 8, :], psb.rearrange("p (a b) -> p a b", b=128), [pk], ["zP1"])

                fft_fwd_keys_A = [("bufA", cs, j0) for cs in range(2) for j0 in range(0, 32, 4)]

                def sinkY_guard(ch, psR, kR, psI, kI):
                    sl = slice(ch * 512, (ch + 1) * 512)
                    ya = bufA[:].rearrange("p a b c -> p a (b c)")
                    m = mts[ch % 2]
                    mk = [("mt", ch % 2, k) for k in range(4)]
                    tt("dve", m[0][:], psR[:], Hsb[:, 0, sl], ALU.mult, [kR, "HsbB"], [mk[0]])
                    tt("dve", m[1][:], psI[:], Hsb[:, 1, sl], ALU.mult, [kI, "HsbB"], [mk[1]])
                    tt("dve", m[2][:], psR[:], Hsb[:, 1, sl], ALU.mult, [kR, "HsbB"], [mk[2]])
                    tt("dve", m[3][:], psI[:], Hsb[:, 0, sl], ALU.mult, [kI, "HsbB"], [mk[3]])
                    tt("pool", ya[:, 0, sl], m[0][:], m[1][:], ALU.subtract, [mk[0], mk[1]], [("Y", ch)] + fft_fwd_keys_A)
                    tt("pool", ya[:, 1, sl], m[2][:], m[3][:], ALU.add, [mk[2], mk[3]], [("Y", ch)])

                fft_fwd(zP1, 1, fa5, bufA, bufB, "zP1", sinkY_guard)

            def inv(hc, hooks):
                ux0 = ux0s[hc % 2]
                zT = zTs[hc % 2]
                par = hc % 2
                zk = [("zT", par, q) for q in range(4)]
                ya = bufA[:].rearrange("p a b c -> p a (b c)")
                bufB_keys = [("bufB", cs, g0) for cs in range(2) for g0 in range(0, 32, 8)]
                for ch in range(8):
                    sl = slice(ch * 512, (ch + 1) * 512)
                    psR, kR = ps_next()
                    psI, kI = ps_next()
                    rk = [("Y", ch), "tct"]
                    mm(psR[:], tct[:, 0, :], ya[:, 0, sl], True, False, rk, kR)
                    mm(psR[:], tct[:, 2, :], ya[:, 1, sl], False, True, rk, kR)
                    mm(psI[:], tct[:, 1, :], ya[:, 0, sl], True, False, rk, kI)
                    mm(psI[:], tct[:, 0, :], ya[:, 1, sl], False, True, rk, kI)
                    wk = [("Cb", ch)] + (bufB_keys if ch == 0 else [])
                    cp("act", bufB[:, 0, 4 * ch:4 * ch + 4, :], psR[:].rearrange("p (a b) -> p a b", b=128), [kR], wk)
                    cp("dve", bufB[:, 1, 4 * ch:4 * ch + 4, :], psI[:].rearrange("p (a b) -> p a b", b=128), [kI], [("Cb", ch)])
                hooks[0]()
                first = True
                for cs in range(2):
                    for g0 in range(0, 32, 8):
                        ps, pk = ps_next()
                        psb = ps[:].bitcast(BF16)
                        for gg in range(8):
                            g = g0 + gg
                            tr(psb[:, gg * 128:(gg + 1) * 128], bufB[:, cs, g, :], [("Cb", g // 4)], pk)
                        wk = [("Ct", cs, g0)] + ([("Y", ch) for ch in range(8)] if first else [])
                        first = False
                        cp("act" if cs == 0 else "dve", bufA[:, cs, :, 4 * g0:4 * g0 + 32].rearrange("p j (g c) -> p g j c", c=4),
                           psb.rearrange("p (g j c) -> p g j c", g=8, j=32), [pk], wk)
                ctk = [("Ct", cs, g0) for cs in range(2) for g0 in range(0, 32, 8)]
                hooks[1]()
                yc3 = yc[:].rearrange("c (p j) -> c p j", j=32)
                for j0 in range(0, 32, 4):
                    ps, pk = ps_next()
                    for jj in range(4):
                        j = j0 + jj
                        mm(ps[:, jj * 128:(jj + 1) * 128], bufA[:, 0, j, :], gt_t[:, j, 0, :], True, False, ctk + ["gt"], pk)
                        mm(ps[:, jj * 128:(jj + 1) * 128], bufA[:, 1, j, :], gt_t[:, j, 1, :], False, True, ctk + ["gt"], pk)
                    if (j0 // 4) % 2 == 0:
                        act(yc3[:, :, j0:j0 + 4].rearrange("c p j -> c j p"), ps[:].rearrange("c (j p) -> c j p", p=128), AF.Identity,
                            [pk, ("rinvP", hc)], [("yc", j0)], scale=rinvP[:, hc:hc + 1])
                    else:
                        ts("dve", yc3[:, :, j0:j0 + 4].rearrange("c p j -> c j p"), ps[:].rearrange("c (j p) -> c j p", p=128),
                           rinvP[:, hc:hc + 1], None, ALU.mult, None, [pk, ("rinvP", hc)], [("yc", j0)])
                yck = [("yc", j0) for j0 in range(0, 32, 4)]
                hooks[2]()
                tq = [(tA, "tA"), (tB, "tB"), (tC, "tC"), (tD, "tD")]
                for q in range(4):
                    t0 = q * 1024
                    stt("dve", tq[q][0][:], zT[:, t0:t0 + 1024], fbp[:, hc:hc + 1], yc[:, t0:t0 + 1024], ALU.mult, ALU.add,
                        zk + yck + ["fbp"], [tq[q][1]])
                for q in range(4):
                    t0 = q * 1024
                    tt("pool", tq[q][0][:], tq[q][0][:], ux0[:, t0:t0 + 1024], ALU.mult, [tq[q][1], ("ux0", par, q)], [tq[q][1]])
                head_norm_wave([(tq[q][0][:], tq[q][1]) for q in range(4)], mgp[:, hc:hc + 1], hc * 128, rsqs, rstdb)

            load_P(0)
            load_H(0)
            conv(0)
            load_P(1)
            nop = lambda: None
            for hc in range(4):
                fwd(hc)
                if hc + 1 < 4:
                    load_H(hc + 1)

                    def h0(hc=hc):
                        conv(hc + 1, (0,))

                    def h1(hc=hc):
                        conv(hc + 1, (1, 2))

                    def h2(hc=hc):
                        conv(hc + 1, (3,))
                        if hc + 2 < 4:
                            load_P(hc + 2)

                    inv(hc, [h0, h1, h2])
                else:
                    inv(hc, [nop, nop, nop])
            em.barrier()
        if stop_after == "B":
            return nc

        with ExitStack() as ph:
            et_t = sbuf(ph, "et_t", [128, 32, 3, 128], BF16)
            tcf = sbuf(ph, "tcf", [128, 2, 128], BF16)
            mgp = sbuf(ph, "mgpC", [128, 8], F32)
            dma("sp", et_t[:].rearrange("p b c d -> p (b c d)"), T["et"], [], ["et"])
            dma("sp", tcf[:].rearrange("p a b -> p (a b)"), T["tcf"], [], ["tcf"])
            dma("sp", mgp[:], T["mgp"], [], ["mgp"])
            wris = [sbuf(ph, "wri%d" % i, [128, 2, 32, 128], BF16) for i in range(2)]
            bufA = sbuf(ph, "bufAC", [128, 2, 32, 128], BF16)
            bufB = sbuf(ph, "bufBC", [128, 2, 32, 128], BF16)
            yf = sbuf(ph, "yf", [128, 32, 128], BF16)
            rTs = [sbuf(ph, "rT%d" % i, [128, 1024], F32) for i in range(4)]
            rsqs = [sbuf(ph, "rsqC%d" % i, [128, 1024], BF16) for i in range(4)]
            rstdb = [sbuf(ph, "rstdbC%d" % i, [128, 1024], BF16) for i in range(4)]
            def c_load(fc):
                for part in range(2):
                    dma("sp", wris[fc % 2][:, part, :, :], wbuf[part][:, fc * 128:(fc + 1) * 128].rearrange("(p j) c -> p j c", j=32), [],
                        [("wri", fc % 2, part)])

            c_load(0)
            for fc in range(4):
                wri = wris[fc % 2]
                if fc + 1 < 4:
                    c_load(fc + 1)
                for j0 in range(0, 32, 4):
                    for cs in range(2):
                        ps, pk = ps_next()
                        for jj in range(4):
                            j = j0 + jj
                            if cs == 0:
                                mm(ps[:, jj * 128:(jj + 1) * 128], et_t[:, j, 0, :], wri[:, 0, j, :], True, False, [("wri", fc % 2, 0), ("wri", fc % 2, 1), "et"], pk)
                                mm(ps[:, jj * 128:(jj + 1) * 128], et_t[:, j, 1, :], wri[:, 1, j, :], False, True, [("wri", fc % 2, 0), ("wri", fc % 2, 1), "et"], pk)
                            else:
                                mm(ps[:, jj * 128:(jj + 1) * 128], et_t[:, j, 0, :], wri[:, 1, j, :], True, False, [("wri", fc % 2, 0), ("wri", fc % 2, 1), "et"], pk)
                                mm(ps[:, jj * 128:(jj + 1) * 128], et_t[:, j, 2, :], wri[:, 0, j, :], False, True, [("wri", fc % 2, 0), ("wri", fc % 2, 1), "et"], pk)
                        cp("act" if cs == 0 else "dve",
                           bufA[:].rearrange("p a g (j c) -> p a g j c", c=4)[:, cs, :, j0:j0 + 4, :].rearrange("p g j c -> p j g c"),
                           ps[:].rearrange("p (j g c) -> p j g c", j=4, g=32), [pk], [("bufA", cs, j0)])
                ak = [("bufA", cs, j0) for cs in range(2) for j0 in range(0, 32, 4)]
                for cs in range(2):
                    for g0 in range(0, 32, 8):
                        ps, pk = ps_next()
                        psb = ps[:].bitcast(BF16)
                        for gg in range(8):
                            g = g0 + gg
                            tr(psb[:, gg * 128:(gg + 1) * 128], bufA[:, cs, g, :], ak, pk)
                        cp("act" if cs == 0 else "dve", bufB[:, cs, g0:g0 + 8, :], psb.rearrange("p (a b) -> p a b", b=128), [pk], [("bufB", cs, g0)])
                for g0 in range(0, 32, 4):
                    ps, pk = ps_next()
                    for gg in range(4):
                        g = g0 + gg
                        rk = [("bufB", 0, g // 8 * 8), ("bufB", 1, g // 8 * 8), "tcf"]
                        mm(ps[:, gg * 128:(gg + 1) * 128], bufB[:, 0, g, :], tcf[:, 0, :], True, False, rk, pk)
                        mm(ps[:, gg * 128:(gg + 1) * 128], bufB[:, 1, g, :], tcf[:, 1, :], False, True, rk, pk)
                    cp("act" if (g0 // 4) % 2 else "dve", yf[:, :, 4 * g0:4 * g0 + 16].rearrange("p k (g c) -> p g k c", c=4),
                       ps[:].rearrange("p (g k c) -> p g k c", g=4, k=32), [pk], [("yf", g0)])
                yfk = [("yf", g0) for g0 in range(0, 32, 4)]
                for q in range(4):
                    ps, pk = ps_next()
                    psb = ps[:].bitcast(BF16)
                    for kk_ in range(8):
                        kb_ = q * 8 + kk_
                        tr(psb[:, kk_ * 128:(kk_ + 1) * 128], yf[:, kb_, :], yfk, pk)
                    cp("dve" if q % 2 else "act", rTs[q][:], psb, [pk], [("rT", q)])
                head_norm_wave([(rTs[q][:], ("rT", q)) for q in range(4)], mgp[:, 4 + fc:5 + fc], 512 + fc * 128, rsqs, rstdb)
            em.barrier()
        if stop_after == "C":
            return nc

        ph_moe = st.enter_context(ExitStack())
        w1 = sbuf(ph_moe, "w1", [128, 32], F32)
        w2 = sbuf(ph_moe, "w2", [128, 32], F32)
        ds_i = sbuf(ph_moe, "ds_i", [128, 2, 32], I32)
        dg_i = sbuf(ph_moe, "dg_i", [128, 2, 32], I32)
        ph_tok = ExitStack()
        h2tok = sbuf(ph_tok, "h2tok", [128, 32, D], BF16)
        Lg = sbuf(ph_tok, "Lg", [128, 32, 36], F32)
        with ExitStack() as ph:
            Wo = sbuf(ph, "Wo", [128, 8, D], BF16)
            Wr = sbuf(ph, "Wr", [128, 8, 36], BF16)
            g2b = sbuf(ph, "g2b", [128, D], F32)
            brb = sbuf(ph, "brb", [128, 36], F32)
            for kc in range(8):
                dma("pool", Wo[:, kc, :], T["w_out"][kc * 128:(kc + 1) * 128, :], [], [("Wo", kc)])
            dma("pool", Wr[:], T["wr"].rearrange("(k p) n -> p k n", p=128), [], ["Wr"])
            dma("sp", g2b[:], T["g2b"], [], ["g2b"])
            dma("sp", brb[:], T["brb"], [], ["brb"])
            yTb = [sbuf(ph, "yTb%d" % i, [128, 8, 512], BF16) for i in range(2)]
            xt = [sbuf(ph, "xtD%d" % i, [128, D], F32) for i in range(3)]
            x1t = [sbuf(ph, "x1t%d" % i, [128, D], F32) for i in range(3)]
            junk = sbuf(ph, "junkD", [128, D], BF16)
            ss = sbuf(ph, "ssD", [128, 32], F32)
            rs = sbuf(ph, "rsD", [128, 32], F32)
            h2T = [sbuf(ph, "h2T%d" % i, [128, 8, 128], BF16) for i in range(2)]
            em.op("dve", lambda: V.memset(ss[:], 0.0), writes=["ssD"])
            wo_ps = {}

            def d_front(j2):
                tc, i = j2 // 4, j2 % 4
                yb = tc % 2
                b = j2 % 3
                if i == 0:
                    dma("sp", yTb[yb][:], yTbuf[:, tc * 512:(tc + 1) * 512].rearrange("(c p) t -> p c t", p=128), [], [("yTb", yb)])
                dma("sp", xt[b][:], T["x"][j2 * 128:(j2 + 1) * 128, :], [], [("xtD", b)])
                for half in range(2):
                    ps, pk = ps_next()
                    for cc in range(8):
                        mm(ps[:], yTb[yb][:, cc, i * 128:(i + 1) * 128], Wo[:, cc, half * 512:(half + 1) * 512], cc == 0, cc == 7,
                           [("yTb", yb), ("Wo", cc)], pk)
                    wo_ps[(j2, half)] = (ps, pk)

            def d_back(j2):
                b = j2 % 3
                for half in range(2):
                    ps, pk = wo_ps.pop((j2, half))
                    tt("dve", x1t[b][:, half * 512:(half + 1) * 512], xt[b][:, half * 512:(half + 1) * 512], ps[:], ALU.add,
                       [("xtD", b), pk], [("x1t", b, half)])
                xk = [("x1t", b, 0), ("x1t", b, 1)]
                dma("sp", x1buf[j2 * 128:(j2 + 1) * 128, :], x1t[b][:], xk, [("x1buf", j2)])
                act(junk[:], x1t[b][:], AF.Square, xk + ["ssD"], ["junkD", ("ssD", j2)], accum_out=ss[:, j2:j2 + 1])
                act(rs[:, j2:j2 + 1], ss[:, j2:j2 + 1], AF.Sqrt, [("ssD", j2), "epst"], [("rsD", j2)], scale=1.0 / D, bias=epst[:])
                em.op("dve", lambda: V.reciprocal(out=rs[:, j2:j2 + 1], in_=rs[:, j2:j2 + 1]), reads=[("rsD", j2)], writes=[("rsD", j2)])
                stt("dve", h2tok[:, j2, :], x1t[b][:], rs[:, j2:j2 + 1], g2b[:], ALU.mult, ALU.mult, xk + [("rsD", j2), "g2b"], [("h2tok", j2)])
                ps, pk = ps_next()
                psb = ps[:].bitcast(BF16)
                for kc in range(8):
                    tr(psb[:, kc * 128:(kc + 1) * 128], h2tok[:, j2, kc * 128:(kc + 1) * 128], [("h2tok", j2)], pk)
                cp("act", h2T[j2 % 2][:], psb.rearrange("p (a b) -> p a b", b=128), [pk], [("h2T", j2 % 2)])
                ps, pk = ps_next()
                for kc in range(8):
                    mm(ps[:, 0:36], h2T[j2 % 2][:, kc, :], Wr[:, kc, :], kc == 0, kc == 7, [("h2T", j2 % 2), "Wr"], pk)
                tt("dve", Lg[:, j2, :], ps[:, 0:36], brb[:], ALU.add, [pk, "brb"], [("Lg", j2)])

            d_front(0)
            for j2 in range(32):
                if j2 + 1 < 32:
                    d_front(j2 + 1)
                d_back(j2)
            em.barrier()
        if debug:
            dma("sp", dbgL[:, 0:32 * 36], Lg[:].rearrange("p a b -> p (a b)"), [("Lg", j2) for j2 in range(32)], ["dbgL"])
        if stop_after == "D":
            em.barrier()
            return nc

        with ExitStack() as ph:
            tri = sbuf(ph, "tri", [128, 128], BF16)
            ones = sbuf(ph, "ones", [128, 128], BF16)
            ecb = sbuf(ph, "ecb", [128, 32], F32)
            trb = sbuf(ph, "trb", [128, 32], F32)
            dma("sp", tri[:], T["tri"], [], ["tri"])
            dma("sp", ones[:], T["ones"], [], ["ones"])
            dma("sp", ecb[:], T["ecb"], [], ["ecb"])
            dma("sp", trb[:], T["trb"], [], ["trb"])
            S = lambda name, shape, dt=F32: sbuf(ph, name, shape, dt)
            gmax = S("gmax", [128, 32]); goh = S("goh", [128, 32, 4]); gd = S("gd", [128, 32, 4]); gsum = S("gsum", [128, 32])
            pg = S("pg", [128, 32]); sel4 = S("sel4", [128, 32, 4, 8]); esel = S("esel", [128, 32, 8]); m1 = S("m1", [128, 32])
            oh1 = S("oh1", [128, 32, 8]); e2 = S("e2", [128, 32, 8]); m2 = S("m2", [128, 32]); oh2 = S("oh2", [128, 32, 8])
            dd = S("dd", [128, 32]); A1 = S("A1", [128, 32, 4, 8]); A2 = S("A2", [128, 32, 4, 8]); Mb = S("Mb", [128, 1024], BF16)
            pin = S("pin", [128, 32, 32]); cnt = S("cnt", [128, 32, 32]); base = S("base", [128, 32, 32]); slot = S("slot", [128, 32, 32])
            tmp3 = S("tmp3", [128, 32, 32]); dk = S("dk", [128, 2, 32]); pk_ = S("pk_", [128, 2, 32]); ok = S("ok", [128, 2, 32])
            dsf = S("dsf", [128, 2, 32]); dgf = S("dgf", [128, 2, 32])
            allL = ["LgAll"]
            K = "route"
            lg_keys = [("Lg", j2) for j2 in range(32)]
            gl = Lg[:, :, 0:4]
            em.op("dve", lambda: V.tensor_reduce(out=gmax[:], in_=gl, axis=AX.X, op=ALU.max), reads=lg_keys, writes=["gmax"])
            tt("dve", goh[:], gl, gmax[:].unsqueeze(2).to_broadcast([128, 32, 4]), ALU.is_equal, lg_keys + ["gmax"], ["goh"])
            tt("dve", gd[:], gl, gmax[:].unsqueeze(2).to_broadcast([128, 32, 4]), ALU.subtract, lg_keys + ["gmax"], ["gd"])
            act(gd[:], gd[:], AF.Exp, ["gd"], ["gd"])
            em.op("dve", lambda: V.tensor_reduce(out=gsum[:], in_=gd[:], axis=AX.X, op=ALU.add), reads=["gd"], writes=["gsum"])
            em.op("dve", lambda: V.reciprocal(out=pg[:], in_=gsum[:]), reads=["gsum"], writes=["pg"])
            el4 = Lg[:, :, 4:36].rearrange("p a (g i) -> p a g i", i=8)
            tt("dve", sel4[:], el4, goh[:].unsqueeze(3).to_broadcast([128, 32, 4, 8]), ALU.mult, lg_keys + ["goh"], ["sel4"])
            tt("dve", esel[:], sel4[:, :, 0, :], sel4[:, :, 1, :], ALU.add, ["sel4"], ["esel"])
            tt("dve", esel[:], esel[:], sel4[:, :, 2, :], ALU.add, ["sel4", "esel"], ["esel"])
            tt("dve", esel[:], esel[:], sel4[:, :, 3, :], ALU.add, ["sel4", "esel"], ["esel"])
            em.op("dve", lambda: V.tensor_reduce(out=m1[:], in_=esel[:], axis=AX.X, op=ALU.max), reads=["esel"], writes=["m1"])
            tt("dve", oh1[:], esel[:], m1[:].unsqueeze(2).to_broadcast([128, 32, 8]), ALU.is_equal, ["esel", "m1"], ["oh1"])
            stt("dve", e2[:], oh1[:], -1e30, esel[:], ALU.mult, ALU.add, ["oh1", "esel"], ["e2"])
            em.op("dve", lambda: V.tensor_reduce(out=m2[:], in_=e2[:], axis=AX.X, op=ALU.max), reads=["e2"], writes=["m2"])
            tt("dve", oh2[:], e2[:], m2[:].unsqueeze(2).to_broadcast([128, 32, 8]), ALU.is_equal, ["e2", "m2"], ["oh2"])
            tt("dve", dd[:], m2[:], m1[:], ALU.subtract, ["m1", "m2"], ["dd"])
            act(dd[:], dd[:], AF.Exp, ["dd"], ["dd"])
            ts("dve", w1[:], dd[:], 1.0, None, ALU.add, None, ["dd"], ["w1"])
            em.op("dve", lambda: V.reciprocal(out=w1[:], in_=w1[:]), reads=["w1"], writes=["w1"])
            tt("dve", w2[:], dd[:], w1[:], ALU.mult, ["dd", "w1"], ["w2"])
            tt("dve", w1[:], w1[:], pg[:], ALU.mult, ["w1", "pg"], ["w1"])
            tt("dve", w2[:], w2[:], pg[:], ALU.mult, ["w2", "pg"], ["w2"])
            gb = goh[:].unsqueeze(3).to_broadcast([128, 32, 4, 8])
            tt("dve", A1[:], gb, oh1[:].unsqueeze(2).to_broadcast([128, 32, 4, 8]), ALU.mult, ["goh", "oh1"], ["A1"])
            tt("dve", A2[:], gb, oh2[:].unsqueeze(2).to_broadcast([128, 32, 4, 8]), ALU.mult, ["goh", "oh2"], ["A2"])
            A1f = A1[:].rearrange("p a g i -> p a (g i)")
            A2f = A2[:].rearrange("p a g i -> p a (g i)")
            tt("dve", Mb[:].rearrange("p (a e) -> p a e", e=32), A1f, A2f, ALU.add, ["A1", "A2"], ["Mb"])
            for h_ in range(2):
                ps, pk = ps_next()
                mm(ps[:], tri[:], Mb[:, h_ * 512:(h_ + 1) * 512], True, True, ["tri", "Mb"], pk)
                cp("act", pin[:, h_ * 16:(h_ + 1) * 16, :], ps[:].rearrange("p (a e) -> p a e", e=32), [pk], ["pin"])
                ps, pk = ps_next()
                mm(ps[:], ones[:], Mb[:, h_ * 512:(h_ + 1) * 512], True, True, ["ones", "Mb"], pk)
                cp("act", cnt[:, h_ * 16:(h_ + 1) * 16, :], ps[:].rearrange("p (a e) -> p a e", e=32), [pk], ["cnt"])
            em.op("dve", lambda: V.memset(base[:, 0, :], 0.0), writes=["base"])
            for j2 in range(1, 32):
                tt("dve", base[:, j2, :], base[:, j2 - 1, :], cnt[:, j2 - 1, :], ALU.add, ["base", "cnt"], ["base"])
            tt("dve", slot[:], pin[:], base[:], ALU.add, ["pin", "base"], ["slot"])
            for k, Af in enumerate([A1f, A2f]):
                tt("dve", tmp3[:], Af, slot[:], ALU.mult, ["A1", "A2", "slot"], ["tmp3"])
                em.op("dve", lambda k=k: V.tensor_reduce(out=pk_[:, k, :], in_=tmp3[:], axis=AX.X, op=ALU.add), reads=["tmp3"], writes=["pk_"])
                tt("dve", tmp3[:], Af, ecb[:].unsqueeze(1).to_broadcast([128, 32, 32]), ALU.mult, ["A1", "A2", "ecb"], ["tmp3"])
                em.op("dve", lambda k=k: V.tensor_reduce(out=dk[:, k, :], in_=tmp3[:], axis=AX.X, op=ALU.add), reads=["tmp3"], writes=["dk"])
            tt("dve", dk[:], dk[:], pk_[:], ALU.add, ["dk", "pk_"], ["dk"])
            ts("dve", ok[:], pk_[:], float(CAP), None, ALU.is_lt, None, ["pk_"], ["ok"])
            tt("dve", dgf[:], dk[:], ok[:], ALU.mult, ["dk", "ok"], ["dgf"])
            tt("dve", dsf[:], dk[:], trb[:].unsqueeze(1).to_broadcast([128, 2, 32]), ALU.subtract, ["dk", "trb"], ["dsf"])
            tt("dve", dsf[:], dsf[:], ok[:], ALU.mult, ["dsf", "ok"], ["dsf"])
            tt("dve", dsf[:], dsf[:], trb[:].unsqueeze(1).to_broadcast([128, 2, 32]), ALU.add, ["dsf", "trb"], ["dsf"])
            tt("dve", w1[:], w1[:], ok[:, 0, :], ALU.mult, ["w1", "ok"], ["w1"])
            tt("dve", w2[:], w2[:], ok[:, 1, :], ALU.mult, ["w2", "ok"], ["w2"])
            cp("dve", ds_i[:], dsf[:], ["dsf"], ["ds_i"])
            cp("dve", dg_i[:], dgf[:], ["dgf"], ["dg_i"])
            if debug:
                dma("sp", dbgL[:, 32 * 36:32 * 36 + 64], dsf[:].rearrange("p a b -> p (a b)"), ["dsf"], ["dbgL2"])
                dma("sp", dbgL[:, 32 * 36 + 64:32 * 36 + 96], w1[:], ["w1"], ["dbgL3"])
                dma("sp", dbgL[:, 32 * 36 + 96:32 * 36 + 128], w2[:], ["w2"], ["dbgL4"])
            for j2 in range(32):
                for k in range(2):
                    em.dma("pool", lambda j2=j2, k=k: G.indirect_dma_start(
                        out=xg, out_offset=bass.IndirectOffsetOnAxis(ap=ds_i[:, k, j2:j2 + 1], axis=0),
                        in_=h2tok[:, j2, :], in_offset=None), reads=["ds_i", ("h2tok", j2)],
                        writes=[("xgs", j2, k)])
            em.barrier()
        ph_tok.close()
        if stop_after == "E":
            return nc

        with ExitStack() as ph:
            NB = 2
            Wg = [sbuf(ph, "Wg%d" % i, [128, 8, 512], BF16) for i in range(NB)]
            Wu = [sbuf(ph, "Wu%d" % i, [128, 8, 512], BF16) for i in range(NB)]
            Wd = [sbuf(ph, "Wd%d" % i, [128, 4, D], BF16) for i in range(NB)]
            xgt = [sbuf(ph, "xgt%d" % i, [128, 3, D], BF16) for i in range(2)]
            xgT = [sbuf(ph, "xgT%d" % i, [128, 8, CAP], BF16) for i in range(2)]
            sg = [sbuf(ph, "sg%d" % i, [128, CAP], F32) for i in range(2)]
            hT = [sbuf(ph, "hT%d" % i, [128, 4, CAP], BF16) for i in range(2)]
            yt = [sbuf(ph, "yt%d" % i, [128, D], F32) for i in range(4)]
            yt_rr = 0
            NST = (CAP + 127) // 128
            NSTG = 5
            stg = [sbuf(ph, "stg%d" % i, [128, 4096], F32) for i in range(NSTG)]
            stg_rr = [0]

            def load_expert(e):
                b = e % NB
                b2 = e % 2
                items = [
                    (T["w_gate"][e].rearrange("(p k) n -> p k n", k=8), Wg[b], ("Wg", b), 8, 512, "act"),
                    (T["w_up"][e].rearrange("(p k) n -> p k n", k=8), Wu[b], ("Wu", b), 8, 512, "dve"),
                    (T["w_down"][e].rearrange("(k p) n -> p k n", p=128), Wd[b], ("Wd", b), 4, 1024, "pool"),
                ]
                for src, dst, key, nk, nn, ce in items:
                    i = stg_rr[0] % NSTG
                    stg_rr[0] += 1
                    sv = stg[i][:].rearrange("p (k n) -> p k n", k=nk)
                    dma("sp", sv, src, [], [("stg", i)])
                    hk_ = nk // 2
                    for h_ in range(2):
                        wk = [(key[0], key[1], 0), (key[0], key[1], 4 if key[0] != "Wd" else 2)] if h_ == 0 else []
                        ce_ = ce if ce != "pool" else ("dve" if h_ == 0 else "act")
                        cp(ce_, dst[:, h_ * hk_:(h_ + 1) * hk_, :], sv[:, h_ * hk_:(h_ + 1) * hk_, :], [("stg", i)],
                           [(key[0], key[1], "h%d" % h_)] + wk)
                for s_ in range((CAP + 127) // 128):
                    w_ = min(128, CAP - s_ * 128)
                    dma("pool", xgt[b2][0:w_, s_, :], xg[e * CAP + s_ * 128:e * CAP + s_ * 128 + w_, :], [], [("xgt", b2, s_)])

            def f_transposes(e):
                b2 = e % 2
                for s in range(3):
                    if s * 128 >= CAP:
                        break
                    w_ = min(128, CAP - s * 128)
                    ps, pk = ps_next()
                    psb = ps[:].bitcast(BF16)
                    for kc in range(8):
                        tr(psb[:, kc * 128:kc * 128 + w_], xgt[b2][0:w_, s, kc::8], [("xgt", b2, s)], pk, kdim=w_)
                    cp("act" if s % 2 else "dve", xgT[b2][:, :, s * 128:s * 128 + w_],
                       psb.rearrange("p (a b) -> p a b", b=128)[:, :, 0:w_], [pk], [("xgT", b2, s)])

            load_expert(0)
            f_transposes(0)
            for e in range(32):
                b = e % NB
                b2 = e % 2
                if e + 1 < 32:
                    load_expert(e + 1)
                xk = [("xgT", b2, s) for s in range(NST)]
                for mc in range(4):
                    psG, kG = ps_next()
                    psU, kU = ps_next()
                    for kc in range(8):
                        mm(psG[:, 0:CAP], Wg[b][:, kc, mc * 128:(mc + 1) * 128], xgT[b2][:, kc, :], kc == 0, kc == 7, xk + [("Wg", b, "h0"), ("Wg", b, "h1"), ("Wg", b, 0), ("Wg", b, 4)], kG)
                    for kc in range(8):
                        mm(psU[:, 0:CAP], Wu[b][:, kc, mc * 128:(mc + 1) * 128], xgT[b2][:, kc, :], kc == 0, kc == 7, xk + [("Wu", b, "h0"), ("Wu", b, "h1"), ("Wu", b, 0), ("Wu", b, 4)], kU)
                    sb_ = mc % 2
                    act(sg[sb_][:], psG[:, 0:CAP], AF.Silu, [kG], [("sg", sb_)])
                    tt("dve", hT[b2][:, mc, :], sg[sb_][:], psU[:, 0:CAP], ALU.mult, [("sg", sb_), kU], [("hT", b2, mc)])
                if e + 1 < 32:
                    f_transposes(e + 1)
                hk = [("hT", b2, mc) for mc in range(4)]
                for s in range(NST):
                    w_ = min(128, CAP - s * 128)
                    yb = yt_rr % 4
                    yt_rr += 1
                    for half in range(2):
                        ps, pk = ps_next()
                        for mc in range(4):
                            mm(ps[0:w_, :], hT[b2][:, mc, s * 128:s * 128 + w_], Wd[b][:, mc, half * 512:(half + 1) * 512], mc == 0, mc == 3,
                               hk + [("Wd", b, "h0"), ("Wd", b, "h1"), ("Wd", b, 0), ("Wd", b, 2)], pk)
                        cp("act" if half else "dve", yt[yb][0:w_, half * 512:(half + 1) * 512], ps[0:w_, :], [pk], [("yt", yb, half)])
                    dma("pool", Ybuf[e * CAP + s * 128:e * CAP + s * 128 + w_, :], yt[yb][0:w_, :], [("yt", yb, 0), ("yt", yb, 1)], [("Ybuf", e, s)])
            em.barrier()
        if stop_after == "F":
            return nc

        with ExitStack() as ph:
            gfb = sbuf(ph, "gfb", [128, D], F32)
            dma("sp", gfb[:], T["gfb"], [], ["gfb"])
            NG = 4
            Y1 = [sbuf(ph, "Y1_%d" % i, [128, D], F32) for i in range(NG)]
            Y2 = [sbuf(ph, "Y2_%d" % i, [128, D], F32) for i in range(NG)]
            xt = [sbuf(ph, "xtG%d" % i, [128, D], F32) for i in range(NG)]
            junk = sbuf(ph, "junkG", [128, D], BF16)
            ss = sbuf(ph, "ssG", [128, 32], F32)
            rs = sbuf(ph, "rsG", [128, 32], F32)
            em.op("dve", lambda: V.memset(ss[:], 0.0), writes=["ssG"])

            def g_load(j2):
                b = j2 % NG
                em.dma("pool", lambda: G.indirect_dma_start(out=Y1[b][:], out_offset=None, in_=Ybuf,
                                                            in_offset=bass.IndirectOffsetOnAxis(ap=dg_i[:, 0, j2:j2 + 1], axis=0)),
                       reads=["dg_i"], writes=[("Y1", b)])
                em.dma("pool", lambda: G.indirect_dma_start(out=Y2[b][:], out_offset=None, in_=Ybuf,
                                                            in_offset=bass.IndirectOffsetOnAxis(ap=dg_i[:, 1, j2:j2 + 1], axis=0)),
                       reads=["dg_i"], writes=[("Y2", b)])
                dma("sp", xt[b][:], x1buf[j2 * 128:(j2 + 1) * 128, :], [], [("xtG", b)])

            for j2 in range(min(3, 32)):
                g_load(j2)
            for j2 in range(32):
                b = j2 % NG
                if j2 + 3 < 32:
                    g_load(j2 + 3)
                stt("dve", xt[b][:], Y1[b][:], w1[:, j2:j2 + 1], xt[b][:], ALU.mult, ALU.add, [("Y1", b), ("xtG", b), "w1"], [("xtG", b)])
                stt("dve", xt[b][:], Y2[b][:], w2[:, j2:j2 + 1], xt[b][:], ALU.mult, ALU.add, [("Y2", b), ("xtG", b), "w2"], [("xtG", b)])
                act(junk[:], xt[b][:], AF.Square, [("xtG", b), "ssG"], ["junkG", ("ssG", j2)], accum_out=ss[:, j2:j2 + 1])
                act(rs[:, j2:j2 + 1], ss[:, j2:j2 + 1], AF.Sqrt, [("ssG", j2), "epst"], [("rsG", j2)], scale=1.0 / D, bias=epst[:])
                em.op("dve", lambda: V.reciprocal(out=rs[:, j2:j2 + 1], in_=rs[:, j2:j2 + 1]), reads=[("rsG", j2)], writes=[("rsG", j2)])
                stt("dve", Y1[b][:], xt[b][:], rs[:, j2:j2 + 1], gfb[:], ALU.mult, ALU.mult, [("xtG", b), ("rsG", j2), "gfb"], [("Y1", b)])
                final_events.append(dma("sp", out_d[j2 * 128:(j2 + 1) * 128, :], Y1[b][:], [("Y1", b)], [("out", j2)]))
            em.barrier()
    return nc


def prep_inputs(inp):
    f32 = np.float32
    g = lambda k: np.asarray(inp[k], dtype=f32)
    rep = lambda v: np.ascontiguousarray(np.tile(v.reshape(1, -1), (128, 1)))
    sh = {}
    sh["w_in"] = np.ascontiguousarray(g("w_in")[0])
    sh["g1b"] = rep(g("norm1_g")[0])
    sh["g2b"] = rep(g("norm2_g")[0])
    sh["gfb"] = rep(g("final_g"))
    cw = g("conv_w")[0]
    sh["cwp"] = np.ascontiguousarray(cw.reshape(3, 12, 128).transpose(2, 1, 0)).reshape(128, 36)
    sh["cbp"] = np.ascontiguousarray(g("conv_b")[0].reshape(12, 128).T)
    sh["fbp"] = np.ascontiguousarray(g("f_bias")[0].reshape(4, 128).T)
    sh["mgp"] = np.ascontiguousarray(g("mix_g")[0].reshape(8, 128).T)
    sh["w_out"] = np.ascontiguousarray(g("w_out")[0])
    wr = np.concatenate([g("w_group")[0], g("w_router")[0].transpose(1, 0, 2).reshape(D, 32)], axis=1)
    sh["wr"] = np.ascontiguousarray(wr)
    sh["brb"] = rep(np.concatenate([g("b_group")[0], g("b_router")[0].reshape(32)]))
    sh["w_gate"] = np.ascontiguousarray(g("w_gate")[0])
    sh["w_up"] = np.ascontiguousarray(g("w_up")[0])
    sh["w_down"] = np.ascontiguousarray(g("w_down")[0])
    fwin = g("f_w_in")[0]
    w1 = np.zeros((66, 128), f32)
    w1[0:33, 0:64] = fwin
    w1[33:66, 64:128] = fwin
    sh["fwin2"] = w1
    fm = g("f_w_mid")[0]
    wm = np.zeros((128, 2, 128), f32)
    for l in range(2):
        wm[0:64, l, 0:64] = fm[l]
        wm[64:128, l, 64:128] = fm[l]
    sh["fwmid2"] = wm.reshape(128, 256)
    fq = g("f_freq")[0].T
    sh["fq"] = np.ascontiguousarray(np.concatenate([fq, fq], 0))
    fb = np.stack([g("f_b_in")[0], g("f_b_mid")[0][0], g("f_b_mid")[0][1]], 1)
    sh["fbb"] = np.ascontiguousarray(np.concatenate([fb, fb], 0))
    fo = g("f_w_out")[0]
    sh["fwout2"] = np.ascontiguousarray(np.concatenate([fo, fo], 0))
    return sh


_CACHE = {}


def kernel(**inputs):
    if "nc" not in _CACHE:
        _CACHE["nc"] = build_nc()
        _CACHE["consts"] = make_consts()
    nc = _CACHE["nc"]
    shared = prep_inputs(inputs)
    shared.update(_CACHE["consts"])
    x = np.asarray(inputs["x"], dtype=np.float32)
    in_maps = []
    for c in range(8):
        m = dict(shared)
        m["x"] = np.ascontiguousarray(x[c])
        in_maps.append(m)
    res = run_bass_kernel_spmd(nc, in_maps, core_ids=list(range(8)))
    out = np.stack([np.asarray(res.results[c]["out"], dtype=np.float32) for c in range(8)], axis=0)
    return out
```
